# Optimizing a Trainium2 kernel written in Bass

```python
import numpy as np
import jax
import jax.numpy as jnp
from jax import lax

D_MODEL = 1024
BATCH = 2
SEQ = 8192
DEPTH = 2

HEAD_DIM = 64
NSA_HEADS = D_MODEL // (2 * HEAD_DIM)
NSA_KV_GROUPS = max(1, NSA_HEADS // 4)
NSA_HPG = NSA_HEADS // NSA_KV_GROUPS
NSA_CMP_LEN = 32
NSA_CMP_STRIDE = 16
NSA_CMP_HID = 128
NSA_SEL_BLOCK = 64
NSA_SEL_TOP = 16
NSA_WINDOW = 512
NSA_Q_CHUNK = 64
MOBA_HEADS = D_MODEL // (2 * HEAD_DIM)
MOBA_BLOCK = 256
MOBA_TOP = 3
MOBA_Q_CHUNK = 32
N_EXPERTS = 32
TOP_K = 4
D_FF = 1024
SWIGLU_LIMIT = 7.0
SWIGLU_ALPHA = 1.702
MOE_BLOCK = 128
EPS = 1e-6
NEG = -1e30
FORCE_SCORE = 1e4

NSA_W = NSA_HEADS * HEAD_DIM
NSA_KV_W = NSA_KV_GROUPS * HEAD_DIM
MOBA_W = MOBA_HEADS * HEAD_DIM
MIX_W = NSA_W + MOBA_W
IN_WIDTHS = (NSA_W, NSA_KV_W, NSA_KV_W, NSA_KV_W, NSA_KV_W, NSA_KV_W, NSA_KV_W,
             3 * NSA_HEADS, MOBA_W, MOBA_W, MOBA_W)
IN_W = sum(IN_WIDTHS)

kernel_name = 'hybrid_nsa_moba_moe_block'


def rms_norm(x, g):
    xf = x.astype(jnp.float32)
    y = xf * lax.rsqrt(jnp.mean(xf * xf, axis=-1, keepdims=True) + EPS)
    return (y * g.astype(jnp.float32)).astype(x.dtype)


def alibi_slopes(n):
    return jnp.asarray(2.0 ** (-8.0 * np.arange(1, n + 1) / n), dtype=jnp.float32)


def masked_softmax(s, mask):
    s = jnp.where(mask, s.astype(jnp.float32), NEG)
    p = jax.nn.softmax(s, axis=-1)
    return jnp.where(mask, p, 0.0)


def _heads(t, n_heads):
    b, s, _ = t.shape
    return t.reshape(b, s, n_heads, HEAD_DIM).transpose(0, 2, 1, 3)


_gather_blocks = jax.vmap(jax.vmap(lambda blocks, idx: blocks[idx]))


def _compress(kv, pos, w1, w2):
    s = kv.shape[2]
    n_cmp = (s - NSA_CMP_LEN) // NSA_CMP_STRIDE + 1
    idx = NSA_CMP_STRIDE * np.arange(n_cmp)[:, None] + np.arange(NSA_CMP_LEN)[None, :]
    blocks = kv[:, :, idx] + pos
    flat = blocks.reshape(kv.shape[0], kv.shape[1], n_cmp, NSA_CMP_LEN * HEAD_DIM)
    return jax.nn.gelu(flat @ w1) @ w2


def nsa_mixer(q, k_cmp, v_cmp, k_slc, v_slc, k_win, v_win, gates, k_gain, cmp_pos, cmp_w1, cmp_w2):
    B, G, Hg, S, dh = q.shape
    scale = dh ** -0.5
    sl = alibi_slopes(G * Hg).reshape(1, G, Hg, 1, 1)
    kc = rms_norm(_compress(k_cmp, cmp_pos[0], cmp_w1[0], cmp_w2[0]), k_gain)
    vc = _compress(v_cmp, cmp_pos[1], cmp_w1[1], cmp_w2[1])
    n_cmp = kc.shape[2]
    cmp_start = NSA_CMP_STRIDE * np.arange(n_cmp)
    cmp_end = jnp.asarray(cmp_start + NSA_CMP_LEN - 1, dtype=jnp.int32)
    cmp_center = jnp.asarray(cmp_start + 0.5 * (NSA_CMP_LEN - 1), dtype=jnp.float32)
    n_sel = S // NSA_SEL_BLOCK
    sel_start = NSA_SEL_BLOCK * np.arange(n_sel)
    incid = jnp.asarray((cmp_start[:, None] <= sel_start[None, :] + NSA_SEL_BLOCK - 1)
                        & (cmp_start[:, None] + NSA_CMP_LEN - 1 >= sel_start[None, :]), dtype=jnp.float32)
    n_top = min(NSA_SEL_TOP, n_sel)
    ks = rms_norm(k_slc, k_gain).reshape(B, G, n_sel, NSA_SEL_BLOCK, dh)
    vs = v_slc.reshape(B, G, n_sel, NSA_SEL_BLOCK, dh)
    pad = ((0, 0), (0, 0), (NSA_WINDOW, 0), (0, 0))
    kw = jnp.pad(rms_norm(k_win, k_gain), pad)
    vw = jnp.pad(v_win, pad)
    blk_ids = jnp.arange(n_sel)[None, :]
    in_blk = jnp.arange(NSA_SEL_BLOCK)
    win_off = jnp.arange(NSA_WINDOW + NSA_Q_CHUNK)
    C = NSA_Q_CHUNK
    m_sel = n_top * NSA_SEL_BLOCK

    def chunk(q0):
        qc = lax.dynamic_slice_in_dim(q, q0, C, axis=3)
        gc = lax.dynamic_slice_in_dim(gates, q0, C, axis=3)
        t = q0 + jnp.arange(C)
        tf = t.astype(jnp.float32)
        s = jnp.einsum('bghcd,bgnd->bghcn', qc, kc, preferred_element_type=jnp.float32) * scale
        s = s - sl * (tf[:, None] - cmp_center[None, :])
        p_cmp = masked_softmax(s, cmp_end[None, :] <= t[:, None])
        o_cmp = jnp.einsum('bghcn,bgnd->bghcd', p_cmp.astype(vc.dtype), vc)
        imp = jnp.einsum('bghcn,nj->bgcj', p_cmp, incid)
        cur = (t // NSA_SEL_BLOCK)[:, None]
        forced = (blk_ids == 0) | (blk_ids == cur) | (blk_ids == cur - 1)
        imp = jnp.where(forced, FORCE_SCORE, imp)
        imp = jnp.where(blk_ids <= cur, imp, NEG)
        _, sel = lax.top_k(imp, n_top)
        ksel = _gather_blocks(ks, sel).reshape(B, G, C, m_sel, dh)
        vsel = _gather_blocks(vs, sel).reshape(B, G, C, m_sel, dh)
        pos = (sel[..., None] * NSA_SEL_BLOCK + in_blk).reshape(B, G, C, m_sel)
        dist = (t[None, None, :, None] - pos)[:, :, None]
        s = jnp.einsum('bghcd,bgcmd->bghcm', qc, ksel, preferred_element_type=jnp.float32) * scale
        s = s - sl * dist.astype(jnp.float32)
        p = masked_softmax(s, dist >= 0)
        o_sel = jnp.einsum('bghcm,bgcmd->bghcd', p.astype(vsel.dtype), vsel)
        kwc = lax.dynamic_slice_in_dim(kw, q0, NSA_WINDOW + C, axis=2)
        vwc = lax.dynamic_slice_in_dim(vw, q0, NSA_WINDOW + C, axis=2)
        pos_w = q0 - NSA_WINDOW + win_off
        dist_w = t[:, None] - pos_w[None, :]
        mask_w = (dist_w >= 0) & (dist_w < NSA_WINDOW) & (pos_w >= 0)[None, :]
        s = jnp.einsum('bghcd,bgkd->bghck', qc, kwc, preferred_element_type=jnp.float32) * scale
        s = s - sl * dist_w.astype(jnp.float32)
        p = masked_softmax(s, mask_w)
        o_win = jnp.einsum('bghck,bgkd->bghcd', p.astype(vwc.dtype), vwc)
        return gc[..., 0:1] * o_cmp + gc[..., 1:2] * o_sel + gc[..., 2:3] * o_win

    out = lax.map(chunk, jnp.arange(0, S, C))
    return out.transpose(1, 0, 4, 2, 3, 5).reshape(B, S, G * Hg * dh)


def moba_mixer(q, k, v):
    B, H, S, dh = q.shape
    scale = dh ** -0.5
    sl = alibi_slopes(H).reshape(1, H, 1, 1)
    n_blk = -(-S // MOBA_BLOCK)
    pad = ((0, 0), (0, 0), (0, n_blk * MOBA_BLOCK - S), (0, 0))
    kp = jnp.pad(k, pad)
    vp = jnp.pad(v, pad)
    kb = kp.reshape(B, H, n_blk, MOBA_BLOCK, dh)
    vb = vp.reshape(B, H, n_blk, MOBA_BLOCK, dh)
    kmean = jnp.mean(kb.astype(jnp.float32), axis=3)
    n_top = min(MOBA_TOP, n_blk - 1)
    in_blk = jnp.arange(MOBA_BLOCK)
    blk_ids = jnp.arange(n_blk)
    C = MOBA_Q_CHUNK
    m_g = n_top * MOBA_BLOCK

    def chunk(q0):
        qc = lax.dynamic_slice_in_dim(q, q0, C, axis=2)
        t = q0 + jnp.arange(C)
        cb = q0 // MOBA_BLOCK
        kcur = lax.dynamic_slice_in_dim(kp, cb * MOBA_BLOCK, MOBA_BLOCK, axis=2)
        vcur = lax.dynamic_slice_in_dim(vp, cb * MOBA_BLOCK, MOBA_BLOCK, axis=2)
        dist_c = t[:, None] - (cb * MOBA_BLOCK + in_blk)[None, :]
        s_c = jnp.einsum('bhcd,bhkd->bhck', qc, kcur, preferred_element_type=jnp.float32) * scale
        s_c = s_c - sl * dist_c.astype(jnp.float32)
        mask_c = jnp.broadcast_to(dist_c >= 0, s_c.shape)
        if n_top > 0:
            gate = jnp.einsum('bhcd,bhnd->bhcn', qc, kmean, preferred_element_type=jnp.float32)
            gate = jnp.where(blk_ids < cb, gate, NEG)
            _, sel = lax.top_k(gate, n_top)
            kg = _gather_blocks(kb, sel).reshape(B, H, C, m_g, dh)
            vg = _gather_blocks(vb, sel).reshape(B, H, C, m_g, dh)
            pos_g = (sel[..., None] * MOBA_BLOCK + in_blk).reshape(B, H, C, m_g)
            dist_g = t[None, None, :, None] - pos_g
            s_g = jnp.einsum('bhcd,bhcmd->bhcm', qc, kg, preferred_element_type=jnp.float32) * scale
            s_g = s_g - sl * dist_g.astype(jnp.float32)
            mask_g = jnp.repeat(sel < cb, MOBA_BLOCK, axis=-1)
            p = masked_softmax(jnp.concatenate([s_g, s_c], axis=-1),
                               jnp.concatenate([mask_g, mask_c], axis=-1))
            o = (jnp.einsum('bhcm,bhcmd->bhcd', p[..., :m_g].astype(vg.dtype), vg)
                 + jnp.einsum('bhck,bhkd->bhcd', p[..., m_g:].astype(vcur.dtype), vcur))
        else:
            p = masked_softmax(s_c, mask_c)
            o = jnp.einsum('bhck,bhkd->bhcd', p.astype(vcur.dtype), vcur)
        return o

    out = lax.map(chunk, jnp.arange(0, S, C))
    return out.transpose(1, 0, 3, 2, 4).reshape(B, S, H * dh)


def hybrid_mixer(h, w_in, w_out, nsa_q_gain, nsa_k_gain, cmp_pos, cmp_w1, cmp_w2, moba_q_gain, moba_k_gain):
    B, S, _ = h.shape
    z = h @ w_in
    points = [int(v) for v in np.cumsum(IN_WIDTHS)[:-1]]
    q_n, kc, vc, ks, vs, kw, vw, g_n, q_m, k_m, v_m = jnp.split(z, points, axis=-1)
    qn = rms_norm(_heads(q_n, NSA_HEADS), nsa_q_gain).reshape(B, NSA_KV_GROUPS, NSA_HPG, S, HEAD_DIM)
    gates = jax.nn.sigmoid(g_n.reshape(B, S, NSA_KV_GROUPS, NSA_HPG, 3)).transpose(0, 2, 3, 1, 4)
    o_nsa = nsa_mixer(qn, _heads(kc, NSA_KV_GROUPS), _heads(vc, NSA_KV_GROUPS),
                      _heads(ks, NSA_KV_GROUPS), _heads(vs, NSA_KV_GROUPS),
                      _heads(kw, NSA_KV_GROUPS), _heads(vw, NSA_KV_GROUPS),
                      gates, nsa_k_gain, cmp_pos, cmp_w1, cmp_w2)
    o_moba = moba_mixer(rms_norm(_heads(q_m, MOBA_HEADS), moba_q_gain),
                        rms_norm(_heads(k_m, MOBA_HEADS), moba_k_gain),
                        _heads(v_m, MOBA_HEADS))
    return jnp.concatenate([o_nsa, o_moba], axis=-1) @ w_out


def sparse_moe(h, router_w, router_b, w1, b1, w2, b2):
    B, S, D = h.shape
    T = B * S
    hf = h.reshape(T, D)
    logits = (hf @ router_w + router_b).astype(jnp.float32)
    top_val, top_idx = lax.top_k(logits, TOP_K)
    gate = jax.nn.softmax(top_val, axis=-1)
    A = T * TOP_K
    e_flat = top_idx.reshape(A)
    tok_flat = jnp.arange(A, dtype=jnp.int32) // TOP_K
    w_flat = gate.reshape(A).astype(h.dtype)
    order = jnp.argsort(e_flat)
    e_s, tok_s, w_s = e_flat[order], tok_flat[order], w_flat[order]
    counts = jnp.bincount(e_flat, length=N_EXPERTS)
    padded = (counts + MOE_BLOCK - 1) // MOE_BLOCK * MOE_BLOCK
    start = jnp.cumsum(counts) - counts
    pend = jnp.cumsum(padded)
    pstart = pend - padded
    dest = pstart[e_s] + jnp.arange(A) - start[e_s]
    R = A + N_EXPERTS * MOE_BLOCK
    n_blocks = R // MOE_BLOCK
    row_tok = jnp.zeros((R,), jnp.int32).at[dest].set(tok_s)
    row_w = jnp.zeros((R,), h.dtype).at[dest].set(w_s)
    blk_e = jnp.minimum(jnp.searchsorted(pend, jnp.arange(n_blocks) * MOE_BLOCK, side='right'), N_EXPERTS - 1)

    def expert_block(args):
        e, toks, wts = args
        u = hf[toks] @ w1[e] + b1[e]
        g_, lin = u[:, :D_FF], u[:, D_FF:]
        g_ = jnp.minimum(g_, SWIGLU_LIMIT)
        lin = jnp.clip(lin, -SWIGLU_LIMIT, SWIGLU_LIMIT)
        y = ((lin + 1.0) * g_ * jax.nn.sigmoid(SWIGLU_ALPHA * g_)) @ w2[e] + b2[e]
        return y * wts[:, None]

    y = lax.map(expert_block, (blk_e, row_tok.reshape(n_blocks, MOE_BLOCK), row_w.reshape(n_blocks, MOE_BLOCK)))
    out = jax.ops.segment_sum(y.reshape(R, D), row_tok, num_segments=T)
    return out.reshape(B, S, D)


def setup_inputs(seed: int = 0) -> dict:
    key = jax.random.key(seed)
    ks = jax.random.split(key, 21)
    n = jax.random.normal
    f32 = jnp.float32
    D = D_MODEL
    return {
        'x': n(ks[0], (BATCH, SEQ, D), f32),
        'c': n(ks[1], (BATCH, D), f32),
        'ada_w': n(ks[2], (DEPTH, D, 6 * D), f32) * (0.5 * D ** -0.5),
        'ada_b': n(ks[3], (DEPTH, 6 * D), f32) * 0.01,
        'ln1_g': 1.0 + 0.01 * n(ks[4], (DEPTH, D), f32),
        'ln2_g': 1.0 + 0.01 * n(ks[5], (DEPTH, D), f32),
        'w_in': n(ks[6], (DEPTH, D, IN_W), f32) * D ** -0.5,
        'nsa_q_gain': 1.0 + 0.01 * n(ks[7], (DEPTH, HEAD_DIM), f32),
        'nsa_k_gain': 1.0 + 0.01 * n(ks[8], (DEPTH, HEAD_DIM), f32),
        'nsa_cmp_pos': 0.1 * n(ks[9], (DEPTH, 2, NSA_CMP_LEN, HEAD_DIM), f32),
        'nsa_cmp_w1': n(ks[10], (DEPTH, 2, NSA_CMP_LEN * HEAD_DIM, NSA_CMP_HID), f32) * (NSA_CMP_LEN * HEAD_DIM) ** -0.5,
        'nsa_cmp_w2': n(ks[11], (DEPTH, 2, NSA_CMP_HID, HEAD_DIM), f32) * NSA_CMP_HID ** -0.5,
        'moba_q_gain': 1.0 + 0.01 * n(ks[12], (DEPTH, HEAD_DIM), f32),
        'moba_k_gain': 1.0 + 0.01 * n(ks[13], (DEPTH, HEAD_DIM), f32),
        'w_out': n(ks[14], (DEPTH, MIX_W, D), f32) * MIX_W ** -0.5,
        'router_w': n(ks[15], (DEPTH, D, N_EXPERTS), f32) * D ** -0.5,
        'router_b': 0.01 * n(ks[16], (DEPTH, N_EXPERTS), f32),
        'exp_w1': n(ks[17], (DEPTH, N_EXPERTS, D, 2 * D_FF), f32) * D ** -0.5,
        'exp_b1': 0.01 * n(ks[18], (DEPTH, N_EXPERTS, 2 * D_FF), f32),
        'exp_w2': n(ks[19], (DEPTH, N_EXPERTS, D_FF, D), f32) * D_FF ** -0.5,
        'exp_b2': 0.01 * n(ks[20], (DEPTH, N_EXPERTS, D), f32),
    }


def reference(x, c, ada_w, ada_b, ln1_g, ln2_g, w_in, nsa_q_gain, nsa_k_gain, nsa_cmp_pos, nsa_cmp_w1,
              nsa_cmp_w2, moba_q_gain, moba_k_gain, w_out, router_w, router_b, exp_w1, exp_b1, exp_w2, exp_b2):
    cond = jax.nn.silu(c)
    for l in range(DEPTH):
        mod = (cond @ ada_w[l] + ada_b[l])[:, None, :]
        sh1, sc1, g1, sh2, sc2, g2 = jnp.split(mod, 6, axis=-1)
        h = rms_norm(x, ln1_g[l]) * (1.0 + sc1) + sh1
        x = x + g1 * hybrid_mixer(h, w_in[l], w_out[l], nsa_q_gain[l], nsa_k_gain[l], nsa_cmp_pos[l],
                                  nsa_cmp_w1[l], nsa_cmp_w2[l], moba_q_gain[l], moba_k_gain[l])
        h = rms_norm(x, ln2_g[l]) * (1.0 + sc2) + sh2
        x = x + g2 * sparse_moe(h, router_w[l], router_b[l], exp_w1[l], exp_b1[l], exp_w2[l], exp_b2[l])
    return x
```

```python
from concourse.bass_utils import run_bass_kernel_spmd
D = 1024
EPS = 1e-6

import contextlib
import numpy as np
import concourse.bass as bass
import concourse.mybir as mybir

F32 = mybir.dt.float32
BF16 = mybir.dt.bfloat16
I32 = mybir.dt.int32
U32 = mybir.dt.uint32
AF = mybir.ActivationFunctionType
ALU = mybir.AluOpType
AX = mybir.AxisListType

DMA_K = 6
STRICT_SAME = True


class Prog:
    def __init__(self, nc, stack):
        self.nc = nc
        self.stack = stack
        self.ins = []
        self.eng = {"pe": nc.tensor, "act": nc.scalar, "dve": nc.vector, "pool": nc.gpsimd, "sp": nc.sync}
        self.nt = 0

    def sb(self, shape, dt, name=None):
        self.nt += 1
        return self.stack.enter_context(self.nc.sbuf_tensor(name or f"sb{self.nt}", list(shape), dt))

    def ps(self, shape, dt=F32, name=None):
        self.nt += 1
        return self.stack.enter_context(self.nc.psum_tensor(name or f"ps{self.nt}", list(shape), dt))

    def op(self, eng, fn, r=(), w=()):
        self.ins.append(dict(e=eng, fn=fn, r=tuple(r), w=tuple(w), dma=False))

    def dma(self, q, out, in_, r=(), w=(), **kw):
        def fn(e, out=out, in_=in_, kw=kw):
            return e.dma_start(out=out, in_=in_, **kw)
        self.ins.append(dict(e=q, fn=fn, r=tuple(r), w=tuple(w), dma=True))

    def emit(self):
        nc = self.nc
        ins = self.ins
        n = len(ins)
        last_w = {}
        readers = {}
        deps = [None] * n
        for i, I in enumerate(ins):
            d = set()
            for k in I["r"]:
                d.update(last_w.get(k, ()))
            for k in I["w"]:
                for j in last_w.get(k, ()):
                    if not (I["dma"] and ins[j]["dma"]):
                        d.add(j)
                rd = readers.get(k)
                if rd:
                    for v in rd[0].values():
                        d.add(v)
                    d.update(rd[1])
            d.discard(i)
            deps[i] = d
            for k in I["r"]:
                rd = readers.setdefault(k, ({}, []))
                if I["dma"]:
                    rd[1].append(i)
                else:
                    rd[0][I["e"]] = i
            for k in I["w"]:
                if I["dma"]:
                    last_w[k] = [j for j in last_w.get(k, ()) if ins[j]["dma"]] + [i]
                else:
                    last_w[k] = [i]
                readers[k] = ({}, [])
        signal = [False] * n
        fdeps = [None] * n
        for i, I in enumerate(ins):
            keep = []
            for j in deps[i]:
                J = ins[j]
                if J["dma"]:
                    keep.append(j)
                    continue
                if J["e"] == I["e"]:
                    if I["dma"]:
                        keep.append(j); signal[j] = True
                        continue
                    if STRICT_SAME and I["e"] in ("act", "dve", "pool"):
                        if any(k in J["w"] for k in I["r"]):
                            keep.append(j); signal[j] = True
                    continue
                keep.append(j); signal[j] = True
            fdeps[i] = keep
        engs = ["pe", "act", "dve", "pool", "sp"]
        sems = {e: self.stack.enter_context(nc.semaphore(f"s_{e}")) for e in engs}
        dsems = {e: [self.stack.enter_context(nc.semaphore(f"d_{e}{k}")) for k in range(DMA_K)]
                 for e in ("sp", "act", "pool")}
        cnt = {e: 0 for e in engs}
        dcnt = {e: 0 for e in dsems}
        tag = [None] * n
        for i, I in enumerate(ins):
            if I["dma"]:
                q = I["e"]
                idx = dcnt[q]; dcnt[q] += 1
                tag[i] = ("d", q, idx)
            elif signal[i]:
                cnt[I["e"]] += 1
                tag[i] = ("c", I["e"], cnt[I["e"]])
        waited = {e: {} for e in engs}
        nw = 0
        for i, I in enumerate(ins):
            e = I["e"]
            E = self.eng[e]
            wl = {}
            for j in fdeps[i]:
                t = tag[j]
                if t[0] == "d":
                    s = dsems[t[1]][t[2] % DMA_K]; v = 16 * (t[2] // DMA_K + 1)
                else:
                    s = sems[t[1]]; v = t[2]
                key = id(s)
                if key not in wl or wl[key][1] < v:
                    wl[key] = (s, v)
            if I["dma"]:
                t = tag[i]
                if t[2] >= DMA_K:
                    s = dsems[t[1]][t[2] % DMA_K]; v = 16 * (t[2] // DMA_K)
                    key = id(s)
                    if key not in wl or wl[key][1] < v:
                        wl[key] = (s, v)
            for key, (s, v) in wl.items():
                if waited[e].get(key, 0) >= v:
                    continue
                waited[e][key] = v
                E.wait_ge(s, v)
                nw += 1
            inst = I["fn"](E)
            t = tag[i]
            if t is not None:
                if t[0] == "d":
                    inst.then_inc(dsems[t[1]][t[2] % DMA_K], 16)
                else:
                    inst.then_inc(sems[t[1]], 1)
        E = self.eng["sp"]
        for q, c in dcnt.items():
            for k in range(DMA_K):
                m = (c - k + DMA_K - 1) // DMA_K if c > k else 0
                if m > 0 and waited["sp"].get(id(dsems[q][k]), 0) < 16 * m:
                    E.wait_ge(dsems[q][k], 16 * m)
        self.stats = dict(n=n, waits=nw, cnt=dict(cnt), dcnt=dict(dcnt))
        return self.stats

import contextlib
import numpy as np
import concourse.bass as bass
import concourse.mybir as mybir

INW = 2840
NT = 16
NORM_SEGS = [(0, 8), (768, 2), (1024, 2), (1304, 8), (1816, 8)]


def emit_mod(P, nc, adaw_dram, col0, ncols, condT, adabT, out_cols, tagp):
    nj = ncols // 128
    half = 1024
    pm = P.ps([128, 64], F32, name=f"pm_{tagp}")
    for hh in range(ncols // half):
        aw = P.sb([128, 8, half], F32, name=f"aw_{tagp}{hh}")
        for i in range(8):
            P.dma("sp" if i % 2 == 0 else "pool", aw[:, i, :], adaw_dram[i * 128:(i + 1) * 128, col0 + hh * half: col0 + (hh + 1) * half],
                  w=[f"aw_{tagp}{hh}_{i}"])
        for jj in range(half // 128):
            j = hh * (half // 128) + jj
            for i in range(8):
                P.op("pe", lambda e, aw=aw, i=i, jj=jj, j=j: e.matmul(pm[:, j:j + 1], lhsT=aw[:, i, jj * 128:(jj + 1) * 128],
                                                                rhs=condT[:, i:i + 1], start=(i == 0), stop=(i == 7)),
                     r=[f"aw_{tagp}{hh}_{i}", "condT"], w=[f"pm_{tagp}"])
    P.op("dve", lambda e: e.tensor_tensor(out=out_cols[:, 0:nj], in0=pm[:, 0:nj], in1=adabT, op=ALU.add),
         r=[f"pm_{tagp}", "adab"], w=[f"mod_{tagp}"])


class _Stop(Exception):
    pass


def build_A(stop=99):
    nc = bass.Bass("TRN2", target_bir_lowering=False)
    x = nc.dram_tensor("x", [NT * 128, D], F32, kind="ExternalInput").ap()
    cT = nc.dram_tensor("cT", [128, 8], F32, kind="ExternalInput").ap()
    adaw = nc.dram_tensor("adaw", [D, 2048], F32, kind="ExternalInput").ap()
    adabT = nc.dram_tensor("adabT", [128, 16], F32, kind="ExternalInput").ap()
    lngT = nc.dram_tensor("lngT", [128, 8], F32, kind="ExternalInput").ap()
    win = nc.dram_tensor("win", [D, INW], F32, kind="ExternalInput").ap()
    identd = nc.dram_tensor("identd", [128, 128], F32, kind="ExternalInput").ap()
    zo = nc.dram_tensor("z", [NT * 128, INW], F32, kind="ExternalOutput").ap()
    with contextlib.ExitStack() as st:
        P = Prog(nc, st)
        try:
            ident = P.sb([128, 128], BF16, name="ident")
            identf = P.sb([128, 128], F32, name="identf")
            condT = P.sb([128, 8], F32, name="condT")
            adab = P.sb([128, 16], F32, name="adab")
            lng = P.sb([128, 8], F32, name="lng")
            modc = P.sb([128, 16], F32, name="modc")
            Acol = P.sb([128, 8], F32, name="Acol")
            wb = P.sb([128, 8, INW], BF16, name="wb")
            P.dma("sp", identf[:], identd[:, :], w=["identf"])
            P.op("dve", lambda e: e.tensor_copy(out=ident[:], in_=identf[:]), r=["identf"], w=["ident"])
            P.dma("sp", condT[:], cT[:, :], w=["condT"])
            P.dma("sp", adab[:], adabT[:, :], w=["adab"])
            P.dma("sp", lng[:], lngT[:, :], w=["lng"])
            P.op("act", lambda e: e.activation(out=condT[:], in_=condT[:], func=AF.Silu), r=["condT"], w=["condT"])
            emit_mod(P, nc, adaw, 0, 2048, condT, adab[:, 0:16], modc, "a")
            P.op("dve", lambda e: e.scalar_tensor_tensor(out=Acol[:], in0=modc[:, 8:16], scalar=1.0, in1=lng[:], op0=ALU.add, op1=ALU.mult),
                 r=["mod_a", "lng"], w=["Acol"])
            if stop == 1:
                P.dma("sp", zo[0:128, 0:16], modc[:], r=["mod_a"])
                P.dma("sp", zo[0:128, 16:24], Acol[:], r=["Acol"])
                raise _Stop()
            for i in range(8):
                for cc in range(0, INW, 568):
                    P.dma("pool", wb[:, i, cc:cc + 568], win[i * 128:(i + 1) * 128, cc:cc + 568], w=[f"wb{i}"])
            xt = [P.sb([128, D], F32, name=f"xt{k}") for k in range(2)]
            xnb = [P.sb([128, D], BF16, name=f"xnb{k}") for k in range(2)]
            sq = P.sb([128, D], F32, name="sq")
            ss = [P.sb([128, 1], F32, name=f"ss{k}") for k in range(2)]
            hT = [P.sb([128, 8, 128], BF16, name=f"hT{k}") for k in range(2)]
            pT = [P.ps([128, D], BF16, name=f"pT{k}") for k in range(2)]
            pz = [P.ps([128, 512], F32, name=f"pz{k}") for k in range(3)]
            zsb = [P.sb([128, INW], F32, name=f"zsb{k}") for k in range(2)]
            ss2 = P.sb([128, 8], F32, name="ss2")
            groups = [(c0, min(512, INW - c0)) for c0 in range(0, INW, 512)]
            for t in range(NT):
                k = t % 2
                X, XN, SS, HT, PT, Z = xt[k], xnb[k], ss[k], hT[k], pT[k], zsb[k]
                P.dma("sp", X[:], x[t * 128:(t + 1) * 128, :], w=[f"xt{k}"])
                P.op("act", lambda e, X=X, SS=SS: e.activation(out=sq[:], in_=X[:], func=AF.Square, accum_out=SS[:, 0:1]),
                     r=[f"xt{k}"], w=["sq", f"ss{k}"])
                P.op("dve", lambda e, SS=SS: e.tensor_scalar(out=SS[:], in0=SS[:], scalar1=1.0 / D, scalar2=EPS, op0=ALU.mult, op1=ALU.add),
                     r=[f"ss{k}"], w=[f"ss{k}"])
                P.op("act", lambda e, SS=SS: e.activation(out=SS[:], in_=SS[:], func=AF.Sqrt), r=[f"ss{k}"], w=[f"ss{k}"])
                P.op("dve", lambda e, SS=SS: e.reciprocal(out=SS[:], in_=SS[:]), r=[f"ss{k}"], w=[f"ss{k}"])
                P.op("dve", lambda e, X=X, XN=XN, SS=SS: e.tensor_scalar(out=XN[:], in0=X[:], scalar1=SS[:, 0:1], scalar2=None, op0=ALU.mult),
                     r=[f"xt{k}", f"ss{k}"], w=[f"xnb{k}"])
                for c in range(8):
                    P.op("pe", lambda e, PT=PT, XN=XN, c=c: e.transpose(PT[:, c * 128:(c + 1) * 128], XN[:, c * 128:(c + 1) * 128], ident[:]),
                         r=[f"xnb{k}", "ident"], w=[f"pT{k}"])
                for c in range(8):
                    eng = "act"
                    if eng == "act":
                        P.op("act", lambda e, HT=HT, PT=PT, c=c: e.activation(out=HT[:, c, :], in_=PT[:, c * 128:(c + 1) * 128], func=AF.Identity,
                                                                         scale=Acol[:, c:c + 1], bias=modc[:, c:c + 1]),
                             r=[f"pT{k}", "Acol", "mod_a"], w=[f"hT{k}_{c}"])
                    else:
                        P.op("dve", lambda e, HT=HT, PT=PT, c=c: e.tensor_scalar(out=HT[:, c, :], in0=PT[:, c * 128:(c + 1) * 128],
                                                                            scalar1=Acol[:, c:c + 1], scalar2=modc[:, c:c + 1],
                                                                            op0=ALU.mult, op1=ALU.add),
                             r=[f"pT{k}", "Acol", "mod_a"], w=[f"hT{k}_{c}"])
                if stop == 2:
                    hf = P.sb([128, 8, 128], F32, name="hf")
                    P.op("dve", lambda e: e.tensor_copy(out=hf[:], in_=HT[:]), r=[f"hT{k}_{c}" for c in range(8)], w=["hf"])
                    P.dma("sp", zo[0:128, 0:1024], hf[:].rearrange("p c t -> p (c t)"), r=["hf"])
                    raise _Stop()
                for gi, (c0, cw) in enumerate(groups):
                    pzz = pz[gi % 3]
                    for c in range(8):
                        P.op("pe", lambda e, pzz=pzz, HT=HT, c=c, c0=c0, cw=cw: e.matmul(pzz[:, 0:cw], lhsT=HT[:, c, :], rhs=wb[:, c, c0:c0 + cw],
                                                                                   start=(c == 0), stop=(c == 7)),
                             r=[f"hT{k}_{c}", f"wb{c}"], w=[f"pz{gi % 3}"])
                    eng = "act" if gi % 2 == 0 else "dve"
                    if eng == "act":
                        P.op("act", lambda e, pzz=pzz, Z=Z, c0=c0, cw=cw: e.activation(out=Z[:, c0:c0 + cw], in_=pzz[:, 0:cw], func=AF.Copy),
                             r=[f"pz{gi % 3}"], w=[f"zsb{k}_{gi}"])
                    else:
                        P.op("dve", lambda e, pzz=pzz, Z=Z, c0=c0, cw=cw: e.tensor_copy(out=Z[:, c0:c0 + cw], in_=pzz[:, 0:cw]),
                             r=[f"pz{gi % 3}"], w=[f"zsb{k}_{gi}"])
                zkeys = [f"zsb{k}_{gi}" for gi in range(len(groups))]
                if stop == 3:
                    P.dma("sp", zo[t * 128:(t + 1) * 128, :], Z[:], r=zkeys)
                    raise _Stop()
                for (s0, nh) in NORM_SEGS:
                    V = Z[:, s0:s0 + nh * 64]
                    P.op("pool", lambda e, V=V, nh=nh: e.tensor_tensor(out=sq[:, 0:nh * 64], in0=V, in1=V, op=ALU.mult),
                         r=zkeys, w=["sq"])
                    P.op("dve", lambda e, nh=nh: e.tensor_reduce(out=ss2[:, 0:nh], in_=sq[:, 0:nh * 64].rearrange("p (h d) -> p h d", d=64),
                                                               axis=AX.X, op=ALU.add), r=["sq"], w=["ss2"])
                    P.op("dve", lambda e, nh=nh: e.tensor_scalar(out=ss2[:, 0:nh], in0=ss2[:, 0:nh], scalar1=1.0 / 64, scalar2=EPS,
                                                               op0=ALU.mult, op1=ALU.add), r=["ss2"], w=["ss2"])
                    P.op("act", lambda e, nh=nh: e.activation(out=ss2[:, 0:nh], in_=ss2[:, 0:nh], func=AF.Sqrt), r=["ss2"], w=["ss2"])
                    P.op("dve", lambda e, nh=nh: e.reciprocal(out=ss2[:, 0:nh], in_=ss2[:, 0:nh]), r=["ss2"], w=["ss2"])
                    P.op("dve", lambda e, V=V, nh=nh: e.tensor_tensor(out=V.rearrange("p (h d) -> p h d", d=64),
                                                                    in0=V.rearrange("p (h d) -> p h d", d=64),
                                                                    in1=ss2[:, 0:nh].unsqueeze(2).to_broadcast([128, nh, 64]), op=ALU.mult),
                         r=zkeys + ["ss2"], w=zkeys)
                P.op("act", lambda e, Z=Z: e.activation(out=Z[:, 1280:1304], in_=Z[:, 1280:1304], func=AF.Sigmoid), r=zkeys, w=zkeys)
                P.dma("sp", zo[t * 128:(t + 1) * 128, :], Z[:], r=zkeys)
        except _Stop:
            pass
        import os
        if os.environ.get("TRUNC"):
            N = int(os.environ["TRUNC"])
            for q, I in enumerate(P.ins[:N]):
                pass
            print("TRUNC at", N, "of", len(P.ins), P.ins[N - 1]["e"], P.ins[N - 1]["r"], P.ins[N - 1]["w"])
            P.ins = P.ins[:N]
            P.dma("sp", zo[0:128, 0:16], modc[:], r=["mod_a"])
        print("A", P.emit())
    return nc


def inputs_A(inp, l, core):
    b, r = core // 4, core % 4
    xl = inp["x_cur"][b, r * 2048:(r + 1) * 2048]
    return {
        "x": np.ascontiguousarray(xl),
        "cT": np.ascontiguousarray(inp["c"][b].reshape(8, 128).T),
        "adaw": np.ascontiguousarray(inp["ada_w"][l][:, 0:2048]),
        "adabT": np.ascontiguousarray(inp["ada_b"][l][0:2048].reshape(16, 128).T),
        "lngT": np.ascontiguousarray(inp["ln1_g"][l].reshape(8, 128).T),
        "win": np.ascontiguousarray(inp["w_in"][l]),
        "identd": np.eye(128, dtype=np.float32),
    }

import contextlib
import numpy as np
import concourse.bass as bass

S = 8192
NEGB = -30000.0
GELU_C = 1.5957691216057308


def build_B1(nslots=32):
    nc = bass.Bass("TRN2", target_bir_lowering=False)
    D_ = lambda n, s: nc.dram_tensor(n, s, F32, kind="ExternalInput").ap()
    qnT = D_("qnT", [64, 16384]); qaugc = D_("qaugc", [5, 16384]); gates = D_("gates", [128, 384])
    kcT = D_("kcT", [2, 64, 8224]); ksT = D_("ksT", [2, 64, S]); vsw = D_("vsw", [2, S, 64])
    kaugc = D_("kaugc", [4, S]); kcaugc = D_("kcaugc", [5, 512])
    gq = D_("gq", [64, 1]); gk = D_("gk", [64, 1])
    w1r = D_("w1r", [2, 64, 4096]); posT = D_("posT", [64, 64]); w2r = D_("w2r", [128, 128]); incid = D_("incid", [512, 128])
    enu = D_("enu", [128, S])
    masks = D_("masks", [6, 128, 128])
    cmt = D_("cmt", [32, 128, 512]); addt = D_("addt", [32, 128, 128])
    identd = D_("identd", [128, 128])
    o = nc.dram_tensor("o", [4096, 256], F32, kind="ExternalOutput").ap()
    with contextlib.ExitStack() as st:
        P = Prog(nc, st)
        ident = P.sb([128, 128], BF16, name="ident")
        M4 = P.sb([128, 6, 512], BF16, name="M4")
        Mt = P.sb([128, 6, 128], BF16, name="Mt")
        EN = P.sb([128, S], BF16, name="EN")
        KS = P.sb([68, S], BF16, name="KS"); KW = P.sb([68, S], BF16, name="KW")
        QN = P.sb([69, 16384], BF16, name="QN")
        VS = P.sb([128, 64, 65], BF16, name="VS"); VW = P.sb([128, 64, 65], BF16, name="VW")
        KC = P.sb([69, 512], BF16, name="KC"); VC = P.sb([128, 4, 193], BF16, name="VC")
        G = P.sb([128, 32, 12], F32, name="G")
        gqs = P.sb([64, 1], F32, name="gqs"); gks = P.sb([64, 1], F32, name="gks")
        stg = [P.sb([64, 2048], F32, name=f"stg{k}") for k in range(2)]
        KCT = P.sb([64, 8224], BF16, name="KCT")
        W1b = P.sb([64, 4096], BF16, name="W1b"); posb = P.sb([64, 64], BF16, name="posb"); W2b = P.sb([128, 128], BF16, name="W2b")
        pbias = P.sb([128, 2], F32, name="pbias")
        xh = P.sb([128, 512], F32, name="xh"); x2 = P.sb([128, 512], F32, name="x2"); sgm = P.sb([128, 512], F32, name="sgm")
        GE = P.sb([128, 512], BF16, name="GE")
        sqk = P.sb([128, 64], F32, name="sqk"); ssk = P.sb([128, 1], F32, name="ssk"); kcn = P.sb([128, 64], BF16, name="kcn")
        CMs = [P.sb([128, 512], BF16, name=f"CMs{k}") for k in range(2)]
        ADs = [P.sb([128, 128], F32, name=f"ADs{k}") for k in range(2)]
        PT = [P.sb([128, 512], BF16, name=f"PT{k}") for k in range(3)]
        SELT4 = P.sb([128, 512], BF16, name="SELT4")
        IMP = P.sb([128, 128], F32, name="IMP"); IMP2 = P.sb([128, 128], F32, name="IMP2"); selb = P.sb([128, 128], BF16, name="selb")
        m8a = P.sb([128, 8], F32, name="m8a"); m8b = P.sb([128, 8], F32, name="m8b")
        rd = P.sb([128, 4], F32, name="rd"); cf = P.sb([128, 4], F32, name="cf")
        OACC = [P.sb([128, 4, 64], F32, name=f"OACC{k}") for k in range(2)]
        ps_sc = [P.ps([128, 512], F32, name=f"ps_sc{k}") for k in range(2)]
        ps_o = [P.ps([128, 512], F32, name=f"ps_o{k}") for k in range(4)]
        ps_tr = P.ps([128, 1024], BF16, name="ps_tr")
        ps_m = P.ps([128, 512], F32, name="ps_m")
        P.dma("pool", ident[:], identd[:, :], w=["ident"])
        for m in range(6):
            P.dma("pool", Mt[:, m, :], masks[m, :, :], w=["Mt"])
        for m in range(6):
            for h in range(4):
                P.op("dve", lambda e, m=m, h=h: e.tensor_copy(out=M4[:, m, h * 128:(h + 1) * 128], in_=Mt[:, m, :]), r=["Mt"], w=["M4"])
        for c in range(4):
            cs = slice(c * 2048, (c + 1) * 2048)
            P.dma("pool", EN[:, cs], enu[:, cs], w=["EN"])
            P.dma("pool", KS[64:68, cs], kaugc[:, cs], w=["KSaug"])
            P.dma("pool", KW[64:68, cs], kaugc[:, cs], w=["KWaug"])
        P.dma("pool", KC[64:69, :], kcaugc[:, :], w=["KCaug"])
        P.dma("sp", gqs[:], gq[:, :], w=["gqs"]); P.dma("sp", gks[:], gk[:, :], w=["gks"])
        P.dma("sp", G[:].rearrange("p i c -> p (i c)"), gates[:, :], w=["G"])
        P.dma("pool", posb[:], posT[:, :], w=["posb"]); P.dma("pool", W2b[:], w2r[:, :], w=["W2b"])
        P.op("dve", lambda e: e.memset(VS[:, :, 64:65], 1.0), w=["VSone"])
        P.op("dve", lambda e: e.memset(VW[:, :, 64:65], 1.0), w=["VWone"])
        P.op("dve", lambda e: e.memset(VC[:, :, 64:65], 1.0), w=["VCone"])
        P.op("dve", lambda e: e.memset(GE[:], 0.0), w=["GE"])
        P.dma("pool", VC[:, :, 65:193], incid.rearrange("(nt p) j -> p nt j", p=128), w=["VCinc"])
        for kv, (KT, kk) in enumerate(((KS, "KS"), (KW, "KW"))):
            for c in range(4):
                sg = stg[c % 2]; cs = slice(c * 2048, (c + 1) * 2048)
                P.dma("sp", sg[:], ksT[kv, :, cs], w=[f"stg{c % 2}"])
                P.op("act", lambda e, sg=sg, KT=KT, cs=cs: e.activation(out=KT[0:64, cs], in_=sg[:], func=AF.Identity, scale=gks[:, 0:1]),
                     r=[f"stg{c % 2}", "gks"], w=[kk])
        for kv, (VT, vk) in enumerate(((VS, "VS"), (VW, "VW"))):
            for c in range(4):
                P.dma("pool", VT[:, c * 16:(c + 1) * 16, 0:64], vsw[kv, c * 2048:(c + 1) * 2048, :].rearrange("(kt p) d -> p kt d", p=128), w=[vk])
        for c in range(8):
            sg = stg[c % 2]; cs = slice(c * 2048, (c + 1) * 2048)
            P.dma("sp", sg[:], qnT[:, cs], w=[f"stg{c % 2}"])
            P.op("dve", lambda e, sg=sg, cs=cs: e.tensor_scalar(out=QN[0:64, cs], in0=sg[:], scalar1=gqs[:, 0:1], scalar2=None, op0=ALU.mult),
                 r=[f"stg{c % 2}", "gqs"], w=["QN"])
            P.dma("pool", QN[64:69, cs], qaugc[:, cs], w=["QNaug"])
        KCv = KCT[:].rearrange("p (n s) -> p n s", s=16)
        for kv in range(2):
            for c in range(4):
                P.dma("pool", KCT[:, c * 2056:(c + 1) * 2056], kcT[kv, :, c * 2056:(c + 1) * 2056], w=["KCT"])
            P.dma("pool", W1b[:], w1r[kv, :, :], w=["W1b"])
            for l in range(32):
                rhs = KCv[:, 0:511, l] if l < 16 else KCv[:, 1:512, l - 16]
                P.op("pe", lambda e, l=l, rhs=rhs: e.matmul(ps_m[:, 0:511], lhsT=W1b[:, l * 128:(l + 1) * 128], rhs=rhs, start=(l == 0), stop=(l == 31)),
                     r=["W1b", "KCT"], w=["ps_m"])
            for l in range(32):
                P.op("pe", lambda e, l=l, kv=kv: e.matmul(ps_sc[0][:, 0:1], lhsT=W1b[:, l * 128:(l + 1) * 128], rhs=posb[:, kv * 32 + l:kv * 32 + l + 1],
                                                      start=(l == 0), stop=(l == 31)), r=["W1b", "posb"], w=["ps_sc0"])
            P.op("dve", lambda e, kv=kv: e.tensor_copy(out=pbias[:, kv:kv + 1], in_=ps_sc[0][:, 0:1]), r=["ps_sc0"], w=["pbias"])
            P.op("act", lambda e, kv=kv: e.activation(out=xh[:, 0:511], in_=ps_m[:, 0:511], func=AF.Identity, bias=pbias[:, kv:kv + 1], scale=1.0),
                 r=["ps_m", "pbias"], w=["xh"])
            P.op("dve", lambda e: e.tensor_tensor(out=x2[:, 0:511], in0=xh[:, 0:511], in1=xh[:, 0:511], op=ALU.mult), r=["xh"], w=["x2"])
            P.op("dve", lambda e: e.tensor_scalar(out=x2[:, 0:511], in0=x2[:, 0:511], scalar1=0.044715, scalar2=1.0, op0=ALU.mult, op1=ALU.add), r=["x2"], w=["x2"])
            P.op("dve", lambda e: e.tensor_tensor(out=x2[:, 0:511], in0=x2[:, 0:511], in1=xh[:, 0:511], op=ALU.mult), r=["x2", "xh"], w=["x2"])
            P.op("act", lambda e: e.activation(out=sgm[:, 0:511], in_=x2[:, 0:511], func=AF.Sigmoid, scale=GELU_C), r=["x2"], w=["sgm"])
            P.op("dve", lambda e: e.tensor_tensor(out=GE[:, 0:511], in0=xh[:, 0:511], in1=sgm[:, 0:511], op=ALU.mult), r=["xh", "sgm", "GE"], w=["GE"])
            for nt in range(4):
                P.op("pe", lambda e, nt=nt, kv=kv: e.matmul(ps_sc[1][:, 0:64], lhsT=GE[:, nt * 128:(nt + 1) * 128], rhs=W2b[:, kv * 64:(kv + 1) * 64], start=True, stop=True),
                     r=["GE", "W2b"], w=["ps_sc1"])
                if kv == 0:
                    P.op("act", lambda e: e.activation(out=sqk[:], in_=ps_sc[1][:, 0:64], func=AF.Square, accum_out=ssk[:, 0:1]), r=["ps_sc1"], w=["sqk", "ssk"])
                    P.op("dve", lambda e: e.tensor_scalar(out=ssk[:], in0=ssk[:], scalar1=1.0 / 64, scalar2=1e-6, op0=ALU.mult, op1=ALU.add), r=["ssk"], w=["ssk"])
                    P.op("act", lambda e: e.activation(out=ssk[:], in_=ssk[:], func=AF.Sqrt), r=["ssk"], w=["ssk"])
                    P.op("dve", lambda e: e.reciprocal(out=ssk[:], in_=ssk[:]), r=["ssk"], w=["ssk"])
                    P.op("dve", lambda e: e.tensor_scalar(out=kcn[:], in0=ps_sc[1][:, 0:64], scalar1=ssk[:, 0:1], scalar2=None, op0=ALU.mult),
                         r=["ps_sc1", "ssk"], w=["kcn"])
                    P.op("pe", lambda e: e.transpose(ps_tr[0:64, 0:128], kcn[:, 0:64], ident[:]), r=["kcn", "ident"], w=["ps_tr"])
                    P.op("act", lambda e, nt=nt: e.activation(out=KC[0:64, nt * 128:(nt + 1) * 128], in_=ps_tr[0:64, 0:128], func=AF.Identity, scale=gks[:, 0:1]),
                         r=["ps_tr", "gks"], w=["KC"])
                else:
                    P.op("act", lambda e, nt=nt: e.activation(out=VC[:, nt, 0:64], in_=ps_sc[1][:, 0:64], func=AF.Copy), r=["ps_sc1"], w=["VC"])
        Gv = G[:].rearrange("p i (h b) -> p i h b", b=3)
        u = 0

        def attend(mms_fn, kts, VT, vkeys, ncol, okeys_first):
            nonlocal u
            for ki, kt in enumerate(kts):
                sc = ps_sc[u % 2]; sck = f"ps_sc{u % 2}"; pt = PT[u % 3]; ptk = f"PT{u % 3}"
                u += 1
                mms = mms_fn(kt, sc)
                for mi, (o_, l_, r_, rk) in enumerate(mms):
                    P.op("pe", lambda e, o_=o_, l_=l_, r_=r_, mi=mi, n=len(mms): e.matmul(o_, lhsT=l_, rhs=r_, start=(mi == 0), stop=(mi == n - 1)), r=rk, w=[sck])
                P.op("act", lambda e, pt=pt, sc=sc: e.activation(out=pt[:], in_=sc[:, 0:512], func=AF.Exp, scale=0.125), r=[sck], w=[ptk])
                for h in range(4):
                    P.op("pe", lambda e, pt=pt, h=h, kt=kt, ki=ki, n=len(kts): e.matmul(ps_o[h][:, 0:ncol], lhsT=pt[:, h * 128:(h + 1) * 128], rhs=VT[:, kt, 0:ncol],
                                                                                   start=(ki == 0), stop=(ki == n - 1)), r=[ptk] + vkeys, w=[f"ps_o{h}"])

        def finish(i, br, OA, oak, first):
            for h in range(4):
                P.op("dve", lambda e, h=h: e.tensor_scalar(out=rd[:, h:h + 1], in0=ps_o[h][:, 64:65], scalar1=1e-30, scalar2=None, op0=ALU.max), r=[f"ps_o{h}"], w=["rd"])
            P.op("dve", lambda e: e.reciprocal(out=rd[:], in_=rd[:]), r=["rd"], w=["rd"])
            P.op("dve", lambda e, i=i, br=br: e.tensor_tensor(out=cf[:], in0=rd[:], in1=Gv[:, i, :, br], op=ALU.mult), r=["rd", "G"], w=["cf"])
            for h in range(4):
                if first:
                    P.op("dve", lambda e, h=h, OA=OA: e.tensor_scalar(out=OA[:, h, :], in0=ps_o[h][:, 0:64], scalar1=cf[:, h:h + 1], scalar2=None, op0=ALU.mult),
                         r=[f"ps_o{h}", "cf"], w=[oak])
                else:
                    P.op("dve", lambda e, h=h, OA=OA: e.scalar_tensor_tensor(out=OA[:, h, :], in0=ps_o[h][:, 0:64], scalar=cf[:, h:h + 1], in1=OA[:, h, :],
                                                                           op0=ALU.mult, op1=ALU.add), r=[f"ps_o{h}", "cf", oak], w=[oak])

        for i in range(nslots):
            cols = slice(i * 512, (i + 1) * 512)
            CM = CMs[i % 2]; cmk = f"CMs{i % 2}"; AD = ADs[i % 2]; adk = f"ADs{i % 2}"
            OA = OACC[i % 2]; oak = f"OACC{i % 2}"
            P.dma("pool", CM[:], cmt[i, :, :], w=[cmk])
            P.dma("sp", AD[:], addt[i, :, :], w=[adk])
            def mm_cmp(nt, sc):
                mms = [(sc[:, 0:512], KC[0:69, nt * 128:(nt + 1) * 128], QN[0:69, cols], ["KC", "KCaug", "QN", "QNaug"])]
                for h in range(4):
                    mms.append((sc[:, h * 128:(h + 1) * 128], ident[:], CM[:, nt * 128:(nt + 1) * 128], ["ident", cmk]))
                return mms
            attend(mm_cmp, list(range(min(3, (256 * i + 224) // 2048) + 1)), VC, ["VC", "VCone", "VCinc"], 193, None)
            finish(i, 0, OA, oak, True)
            P.op("dve", lambda e: e.tensor_scalar(out=IMP[:], in0=ps_o[0][:, 65:193], scalar1=rd[:, 0:1], scalar2=None, op0=ALU.mult), r=["ps_o0", "rd"], w=["IMP"])
            for h in range(1, 4):
                P.op("dve", lambda e, h=h: e.scalar_tensor_tensor(out=IMP[:], in0=ps_o[h][:, 65:193], scalar=rd[:, h:h + 1], in1=IMP[:], op0=ALU.mult, op1=ALU.add),
                     r=[f"ps_o{h}", "rd", "IMP"], w=["IMP"])
            P.op("dve", lambda e, AD=AD: e.tensor_tensor(out=IMP[:], in0=IMP[:], in1=AD[:], op=ALU.add), r=["IMP", adk], w=["IMP"])
            P.op("dve", lambda e: e.max(out=m8a[:], in_=IMP[:]), r=["IMP"], w=["m8a"])
            P.op("dve", lambda e: e.match_replace(out=IMP2[:], in_to_replace=m8a[:], in_values=IMP[:], imm_value=-3.0e38), r=["IMP", "m8a"], w=["IMP2"])
            P.op("dve", lambda e: e.max(out=m8b[:], in_=IMP2[:]), r=["IMP2"], w=["m8b"])
            P.op("dve", lambda e: e.tensor_scalar(out=IMP2[:], in0=IMP[:], scalar1=m8b[:, 7:8], scalar2=None, op0=ALU.is_ge), r=["IMP", "m8b"], w=["IMP2"])
            P.op("dve", lambda e: e.tensor_scalar(out=selb[:], in0=IMP2[:], scalar1=-1.0, scalar2=-NEGB, op0=ALU.add, op1=ALU.mult), r=["IMP2"], w=["selb"])
            P.op("pe", lambda e: e.transpose(ps_tr[:, 0:128], selb[:, :], ident[:]), r=["selb", "ident"], w=["ps_tr"])
            for h in range(4):
                P.op("act", lambda e, h=h: e.activation(out=SELT4[:, h * 128:(h + 1) * 128], in_=ps_tr[:, 0:128], func=AF.Copy), r=["ps_tr"], w=["SELT4"])
            def mm_sel(kt, sc):
                mms = [(sc[:, 0:512], KS[0:68, kt * 128:(kt + 1) * 128], QN[0:68, cols], ["KS", "KSaug", "QN", "QNaug"]),
                       (sc[:, 0:512], EN[:, kt * 128:(kt + 1) * 128], SELT4[:, :], ["EN", "SELT4"])]
                if kt == 2 * i:
                    mms.append((sc[:, 0:512], ident[:], M4[:, 0, :], ["ident", "M4"]))
                if kt == 2 * i + 1:
                    mms.append((sc[:, 0:512], ident[:], M4[:, 1, :], ["ident", "M4"]))
                return mms
            attend(mm_sel, list(range(2 * i + 2)), VS, ["VS", "VSone"], 65, None)
            finish(i, 1, OA, oak, False)
            def mm_win(kt, sc):
                mms = [(sc[:, 0:512], KW[0:68, kt * 128:(kt + 1) * 128], QN[0:68, cols], ["KW", "KWaug", "QN", "QNaug"])]
                off = kt - 2 * i
                mi = {-4: 2, -3: 3, 0: 4, 1: 5}.get(off)
                if mi is not None:
                    mms.append((sc[:, 0:512], ident[:], M4[:, mi, :], ["ident", "M4"]))
                return mms
            attend(mm_win, [kt for kt in range(2 * i - 4, 2 * i + 2) if kt >= 0], VW, ["VW", "VWone"], 65, None)
            finish(i, 2, OA, oak, False)
            P.dma("sp", o[i * 128:(i + 1) * 128, :], OA[:].rearrange("p h d -> p (h d)"), r=[oak])
        print("B1", P.emit())
    return nc


def consts_B1(parity, slopes):
    k = np.arange(128)
    tri = np.where(k[None, :] >= k[:, None], 0.0, NEGB).astype(np.float32)
    tri2 = np.where(k[:, None] > k[None, :], 0.0, NEGB).astype(np.float32)
    opn = np.zeros((128, 128), np.float32); cls = np.full((128, 128), NEGB, np.float32)
    if parity == 0:
        masks = np.stack([tri, cls, tri2, opn, tri, cls])
    else:
        masks = np.stack([opn, tri, cls, tri2, opn, tri])
    n_ = np.arange(128)
    cmt = np.zeros((32, 128, 512), np.float32); addt = np.zeros((32, 128, 128), np.float32)
    j = np.arange(128)
    for i in range(32):
        qt = 2 * i + parity
        t = qt * 128 + np.arange(128)
        for nt in range(4):
            cend = 16 * (128 * nt + n_) + 31
            cmt[i, :, nt * 128:(nt + 1) * 128] = np.where(cend[:, None] <= t[None, :], 0.0, NEGB)
        cur = t // 64
        forced = (j[None, :] == 0) | (j[None, :] == cur[:, None]) | (j[None, :] == cur[:, None] - 1)
        addt[i] = np.where(j[None, :] <= cur[:, None], np.where(forced, 1e4, 0.0), -1e30)
    cmt[:, 127, 384:512] = NEGB
    enu = (np.arange(S)[None, :] // 64 == j[:, None]).astype(np.float32)
    kaug = np.stack([np.ones(S), np.full(S, 128.0), np.arange(S) % 128, (np.arange(S) // 128) * 128.0]).astype(np.float32)
    n512 = np.arange(512)
    kcaug = np.stack([np.ones(512), np.full(512, 128.0), 16.0 * (n512 % 128), 2048.0 * (n512 // 128), np.full(512, 15.5)]).astype(np.float32)
    qa = np.zeros((5, 32, 4, 128), np.float32)
    qp = np.arange(128.0)
    for i in range(32):
        qt = 2 * i + parity
        for h in range(4):
            Sx = 8.0 * slopes[h]
            qa[0, i, h] = -Sx * qp; qa[1, i, h] = -Sx * qt; qa[2:5, i, h] = Sx
    cs = 16 * np.arange(512); ss = 64 * np.arange(128)
    inc = ((cs[:, None] <= ss[None, :] + 63) & (cs[:, None] + 31 >= ss[None, :])).astype(np.float32)
    inc[511] = 0.0
    return dict(masks=masks, cmt=cmt, addt=addt, enu=enu, kaugc=kaug, kcaugc=kcaug, qaugc=qa.reshape(5, 16384), incid=inc,
                identd=np.eye(128, dtype=np.float32))


def inputs_B1(zb, g, parity, gq, gk, pos, w1, w2):
    slopes = 2.0 ** (-8.0 * np.arange(1, 9) / 8)[4 * g:4 * g + 4]
    d = consts_B1(parity, slopes)
    own = (np.arange(4096) // 128 * 2 + parity) * 128 + np.arange(4096) % 128
    q = zb[own][:, 0:512].reshape(32, 128, 8, 64)[:, :, 4 * g:4 * g + 4]
    d["qnT"] = np.ascontiguousarray(q.transpose(3, 0, 2, 1).reshape(64, 16384))
    d["gates"] = np.ascontiguousarray(zb[own][:, 1280 + 12 * g:1280 + 12 * (g + 1)].reshape(32, 128, 12).transpose(1, 0, 2).reshape(128, 384))
    kc = zb[:, 512 + 64 * g:512 + 64 * (g + 1)]; vc = zb[:, 640 + 64 * g:640 + 64 * (g + 1)]
    kcT = np.zeros((2, 64, 8224), np.float32); kcT[0, :, :S] = kc.T; kcT[1, :, :S] = vc.T
    d["kcT"] = kcT
    d["ksT"] = np.ascontiguousarray(np.stack([zb[:, 768 + 64 * g:768 + 64 * (g + 1)].T, zb[:, 1024 + 64 * g:1024 + 64 * (g + 1)].T]))
    d["vsw"] = np.ascontiguousarray(np.stack([zb[:, 896 + 64 * g:896 + 64 * (g + 1)], zb[:, 1152 + 64 * g:1152 + 64 * (g + 1)]]))
    d["gq"] = np.ascontiguousarray(gq.reshape(64, 1)); d["gk"] = np.ascontiguousarray(gk.reshape(64, 1))
    d["w1r"] = np.ascontiguousarray(w1.reshape(2, 32, 64, 128).transpose(0, 2, 1, 3).reshape(2, 64, 4096))
    d["posT"] = np.ascontiguousarray(pos.transpose(2, 0, 1).reshape(64, 64))
    d["w2r"] = np.ascontiguousarray(np.concatenate([w2[0], w2[1]], axis=1))
    return d

import contextlib
import numpy as np
import concourse.bass as bass

S = 8192
NEGB = -30000.0


def build_B2(nslots=32):
    nq = nslots * 128
    nc = bass.Bass("TRN2", target_bir_lowering=False)
    D_ = lambda n, s: nc.dram_tensor(n, s, F32, kind="ExternalInput").ap()
    qmT = D_("qmT", [4, 64, 4096]); kmT = D_("kmT", [4, 64, S]); vm = D_("vm", [4, S, 64])
    qaugc = D_("qaugc", [4, 4, 4096]); kaugc = D_("kaugc", [4, S])
    gq = D_("gq", [64, 1]); gk = D_("gk", [64, 1])
    emoba = D_("emoba", [32, S])
    maska = D_("maska", [128, 128]); maskb = D_("maskb", [128, 128])
    mneg = D_("mneg", [128, 32 * 32]); mvalid = D_("mvalid", [128, 32 * 32]); mcur = D_("mcur", [128, 32 * 32])
    identd = D_("identd", [128, 128])
    o = nc.dram_tensor("o", [4096, 256], F32, kind="ExternalOutput").ap()
    with contextlib.ExitStack() as st:
        P = Prog(nc, st)
        ident = P.sb([128, 128], BF16, name="ident")
        MA = P.sb([128, 128], BF16, name="MA"); MB = P.sb([128, 128], BF16, name="MB")
        EM = P.sb([32, S], BF16, name="EM")
        KA = P.sb([68, S], BF16, name="KA")
        QA = P.sb([68, 4096], BF16, name="QA")
        VA = P.sb([128, 64, 65], BF16, name="VA")
        MSB = P.sb([32, 4096], BF16, name="MSB")
        MNEG = P.sb([128, 1024], F32, name="MNEG"); MVAL = P.sb([128, 1024], F32, name="MVAL"); MCUR = P.sb([128, 1024], F32, name="MCUR")
        gqs = P.sb([64, 1], F32, name="gqs"); gks = P.sb([64, 1], F32, name="gks")
        stg = [P.sb([64, 2048], F32, name=f"stg{k}") for k in range(2)]
        kmf = P.sb([64, 32], F32, name="kmf")
        kmb = P.sb([64, 32], BF16, name="kmb")
        gm = P.sb([128, 32], F32, name="gm"); m8 = P.sb([128, 8], F32, name="m8")
        al = P.sb([128, 32], F32, name="al"); sbb = P.sb([128, 32], BF16, name="sbb")
        PT = [P.sb([128, 512], BF16, name=f"PT{k}") for k in range(3)]
        rden = P.sb([128, 4], F32, name="rden")
        osb = [P.sb([128, 4, 64], F32, name=f"osb{k}") for k in range(2)]
        ps_sc = [P.ps([128, 512], F32, name=f"ps_sc{k}") for k in range(2)]
        ps_o = [P.ps([128, 512], F32, name=f"ps_o{k}") for k in range(4)]
        ps_tr = P.ps([128, 1024], BF16, name="ps_tr")
        ps_g = P.ps([128, 512], F32, name="ps_g")
        P.dma("pool", ident[:], identd[:, :], w=["ident"])
        P.dma("pool", MA[:], maska[:, :], w=["MA"])
        P.dma("pool", MB[:], maskb[:, :], w=["MB"])
        for c in range(4):
            P.dma("pool", EM[:, c * 2048:(c + 1) * 2048], emoba[:, c * 2048:(c + 1) * 2048], w=["EM"])
            P.dma("pool", KA[64:68, c * 2048:(c + 1) * 2048], kaugc[:, c * 2048:(c + 1) * 2048], w=["KAaug"])
        P.dma("sp", MNEG[:], mneg[:, :], w=["MNEG"])
        P.dma("sp", MVAL[:], mvalid[:, :], w=["MVAL"])
        P.dma("sp", MCUR[:], mcur[:, :], w=["MCUR"])
        P.dma("sp", gqs[:], gq[:, :], w=["gqs"])
        P.dma("sp", gks[:], gk[:, :], w=["gks"])
        P.op("dve", lambda e: e.memset(VA[:, :, 64:65], 1.0), w=["VAone"])
        u = 0
        for h in range(4):
            for c in range(4):
                sg = stg[c % 2]
                P.dma("sp", sg[:], kmT[h, :, c * 2048:(c + 1) * 2048], w=[f"stg{c % 2}"])
                P.op("dve", lambda e, sg=sg: e.tensor_scalar(out=sg[:], in0=sg[:], scalar1=gks[:, 0:1], scalar2=None, op0=ALU.mult),
                     r=[f"stg{c % 2}", "gks"], w=[f"stg{c % 2}"])
                P.op("act", lambda e, sg=sg, c=c: e.activation(out=KA[0:64, c * 2048:(c + 1) * 2048], in_=sg[:], func=AF.Copy),
                     r=[f"stg{c % 2}"], w=["KA"])
                P.op("dve", lambda e, sg=sg, c=c: e.tensor_reduce(out=kmf[:, c * 8:(c + 1) * 8], in_=sg[:].rearrange("p (b k) -> p b k", k=256),
                                                                 axis=AX.X, op=ALU.add), r=[f"stg{c % 2}"], w=["kmf"])
            P.op("dve", lambda e: e.tensor_scalar(out=kmb[:], in0=kmf[:], scalar1=1.0 / 256, scalar2=None, op0=ALU.mult), r=["kmf"], w=["kmb"])
            for c in range((nq + 2047) // 2048):
                w_ = min(2048, nq - c * 2048)
                sg = stg[c % 2]
                P.dma("sp", sg[:, 0:w_], qmT[h, :, c * 2048:c * 2048 + w_], w=[f"stg{c % 2}"])
                P.op("dve", lambda e, sg=sg, c=c, w_=w_: e.tensor_scalar(out=QA[0:64, c * 2048:c * 2048 + w_], in0=sg[:, 0:w_], scalar1=gqs[:, 0:1],
                                                                       scalar2=None, op0=ALU.mult), r=[f"stg{c % 2}", "gqs"], w=["QA"])
            P.dma("pool", QA[64:68, 0:nq], qaugc[h, :, 0:nq], w=["QAaug"])
            for c in range(4):
                P.dma("pool", VA[:, c * 16:(c + 1) * 16, 0:64], vm[h, c * 2048:(c + 1) * 2048, :].rearrange("(kt p) d -> p kt d", p=128), w=["VA"])
            for i in range(nslots):
                P.op("pe", lambda e, i=i: e.matmul(ps_g[:, 0:32], lhsT=QA[0:64, i * 128:(i + 1) * 128], rhs=kmb[:, :], start=True, stop=True),
                     r=["QA", "kmb"], w=["ps_g"])
                P.op("dve", lambda e, i=i: e.tensor_tensor(out=gm[:], in0=ps_g[:, 0:32], in1=MNEG[:, i * 32:(i + 1) * 32], op=ALU.add),
                     r=["ps_g", "MNEG"], w=["gm"])
                P.op("dve", lambda e: e.max(out=m8[:], in_=gm[:]), r=["gm"], w=["m8"])
                P.op("dve", lambda e: e.tensor_scalar(out=al[:], in0=gm[:], scalar1=m8[:, 2:3], scalar2=None, op0=ALU.is_ge), r=["gm", "m8"], w=["al"])
                P.op("dve", lambda e, i=i: e.tensor_tensor(out=al[:], in0=al[:], in1=MVAL[:, i * 32:(i + 1) * 32], op=ALU.mult), r=["al", "MVAL"], w=["al"])
                P.op("dve", lambda e, i=i: e.tensor_tensor(out=al[:], in0=al[:], in1=MCUR[:, i * 32:(i + 1) * 32], op=ALU.add), r=["al", "MCUR"], w=["al"])
                P.op("dve", lambda e: e.tensor_scalar(out=sbb[:], in0=al[:], scalar1=-1.0, scalar2=-NEGB, op0=ALU.add, op1=ALU.mult), r=["al"], w=["sbb"])
                P.op("pe", lambda e: e.transpose(ps_tr[0:32, 0:128], sbb[:, 0:32], ident[:]), r=["sbb", "ident"], w=["ps_tr"])
                P.op("act", lambda e, i=i: e.activation(out=MSB[:, i * 128:(i + 1) * 128], in_=ps_tr[0:32, 0:128], func=AF.Copy), r=["ps_tr"], w=["MSB"])
            for gi in range(nslots // 4):
                i0 = 4 * gi
                cols = slice(i0 * 128, (i0 + 4) * 128)
                nkt = 2 * (i0 + 3) + 2
                for kt in range(nkt):
                    sc = ps_sc[u % 2]; sck = f"ps_sc{u % 2}"
                    pt = PT[u % 3]; ptk = f"PT{u % 3}"
                    mms = [(sc[:, 0:512], KA[0:68, kt * 128:(kt + 1) * 128], QA[0:68, cols], ["KA", "KAaug", "QA", "QAaug"]),
                           (sc[:, 0:512], EM[0:32, kt * 128:(kt + 1) * 128], MSB[0:32, cols], ["EM", "MSB"])]
                    for j in range(4):
                        if kt == 2 * (i0 + j):
                            mms.append((sc[:, j * 128:(j + 1) * 128], ident[:], MA[:], ["ident", "MA"]))
                        if kt == 2 * (i0 + j) + 1:
                            mms.append((sc[:, j * 128:(j + 1) * 128], ident[:], MB[:], ["ident", "MB"]))
                    for mi, (o_, l_, r_, rk) in enumerate(mms):
                        P.op("pe", lambda e, o_=o_, l_=l_, r_=r_, mi=mi, n=len(mms): e.matmul(o_, lhsT=l_, rhs=r_, start=(mi == 0), stop=(mi == n - 1)),
                             r=rk, w=[sck])
                    P.op("act", lambda e, pt=pt, sc=sc: e.activation(out=pt[:], in_=sc[:, 0:512], func=AF.Exp, scale=0.125), r=[sck], w=[ptk])
                    for j in range(4):
                        last = 2 * (i0 + j) + 1
                        if kt <= last:
                            P.op("pe", lambda e, pt=pt, j=j, kt=kt, last=last: e.matmul(ps_o[j][:, 0:65], lhsT=pt[:, j * 128:(j + 1) * 128],
                                                                                      rhs=VA[:, kt, 0:65], start=(kt == 0), stop=(kt == last)),
                                 r=[ptk, "VA", "VAone"], w=[f"ps_o{j}"])
                    u += 1
                ob = osb[gi % 2]; obk = f"osb{gi % 2}"
                for j in range(4):
                    P.op("dve", lambda e, j=j: e.tensor_scalar(out=rden[:, j:j + 1], in0=ps_o[j][:, 64:65], scalar1=1e-30, scalar2=None, op0=ALU.max),
                         r=[f"ps_o{j}"], w=["rden"])
                P.op("dve", lambda e: e.reciprocal(out=rden[:], in_=rden[:]), r=["rden"], w=["rden"])
                for j in range(4):
                    P.op("dve", lambda e, j=j, ob=ob: e.tensor_scalar(out=ob[:, j, :], in0=ps_o[j][:, 0:64], scalar1=rden[:, j:j + 1], scalar2=None, op0=ALU.mult),
                         r=[f"ps_o{j}", "rden"], w=[obk])
                for j in range(4):
                    P.dma("sp", o[(i0 + j) * 128:(i0 + j + 1) * 128, h * 64:(h + 1) * 64], ob[:, j, :], r=[obk])
        print("B2", P.emit())
    return nc


def consts_B2(parity):
    k = np.arange(128)
    tri = np.where(k[None, :] >= k[:, None], 0.0, NEGB).astype(np.float32)
    opn = np.zeros((128, 128), np.float32); cls = np.full((128, 128), NEGB, np.float32)
    maska, maskb = (tri, cls) if parity == 0 else (opn, tri)
    j = np.arange(32)
    mneg = np.zeros((128, 32, 32), np.float32); mval = np.zeros((128, 32, 32), np.float32); mcur = np.zeros((128, 32, 32), np.float32)
    for i in range(32):
        cb = i
        mneg[:, i, :] = np.where(j < cb, 0.0, -1e30)[None, :]
        mval[:, i, :] = (j < cb)[None, :]
        mcur[:, i, :] = (j == cb)[None, :]
    emoba = (np.arange(S)[None, :] // 256 == j[:, None]).astype(np.float32)
    kaug = np.stack([np.ones(S), np.full(S, 128.0), np.arange(S) % 128, (np.arange(S) // 128) * 128.0]).astype(np.float32)
    return dict(maska=maska, maskb=maskb, mneg=mneg.reshape(128, 1024), mvalid=mval.reshape(128, 1024), mcur=mcur.reshape(128, 1024),
                emoba=emoba, kaugc=kaug, identd=np.eye(128, dtype=np.float32))


def qaug_rows(slopes, parity, extra=0):
    i = np.arange(4096) // 128
    qp = (np.arange(4096) % 128).astype(np.float64)
    qt = 2 * i + parity
    out = []
    for s in slopes:
        Sx = 8.0 * s
        rows = [-Sx * qp, -Sx * qt, np.full(4096, Sx), np.full(4096, Sx)] + [np.full(4096, Sx)] * extra
        out.append(np.stack(rows))
    return np.stack(out).astype(np.float32)


def alibi(n):
    return 2.0 ** (-8.0 * np.arange(1, n + 1) / n)


def inputs_B2(zb, g, parity, gq, gk):
    own = (np.arange(4096) // 128 * 2 + parity) * 128 + np.arange(4096) % 128
    hs = [4 * g + a for a in range(4)]
    qm = zb[:, 1304:1816].reshape(S, 8, 64); km = zb[:, 1816:2328].reshape(S, 8, 64); vmm = zb[:, 2328:2840].reshape(S, 8, 64)
    d = consts_B2(parity)
    d.update(qmT=np.ascontiguousarray(qm[own][:, hs].transpose(1, 2, 0)),
             kmT=np.ascontiguousarray(km[:, hs].transpose(1, 2, 0)),
             vm=np.ascontiguousarray(vmm[:, hs].transpose(1, 0, 2)),
             qaugc=qaug_rows(alibi(8)[hs], parity),
             gq=np.ascontiguousarray(gq.reshape(64, 1)), gk=np.ascontiguousarray(gk.reshape(64, 1)))
    return d

import contextlib
import numpy as np
import concourse.bass as bass

NTC = 16
NH = 8
NE = 32


def build_C(ne=NE):
    nc = bass.Bass("TRN2", target_bir_lowering=False)
    D_ = lambda n, s: nc.dram_tensor(n, s, F32, kind="ExternalInput").ap()
    x = D_("x", [2048, D]); o = D_("o", [2048, D])
    cT = D_("cT", [128, 8]); adaw = D_("adaw", [D, 4096]); adab = D_("adab", [1, 4096])
    ln2 = D_("ln2", [128, D]); wout = D_("wout", [D, D])
    rw = D_("rw", [D, 32]); rb = D_("rb", [128, 32])
    w1 = D_("w1", [NE, D, 2048]); b1T = D_("b1T", [128, NE * 16]); w2 = D_("w2", [NE, D, D]); b2 = D_("b2", [NE, D])
    identd = D_("identd", [128, 128])
    y = nc.dram_tensor("y", [2048, D], F32, kind="ExternalOutput").ap()
    with contextlib.ExitStack() as st:
        P = Prog(nc, st)
        ident = P.sb([128, 128], BF16, name="ident")
        onesf = P.sb([128, 128], F32, name="onesf")
        condT = P.sb([128, 8], F32, name="condT")
        condrep = P.sb([128, 8, 128], F32, name="condrep")
        adabs = P.sb([1, 4096], F32, name="adabs")
        MODB = P.sb([128, 4096], F32, name="MODB")
        A2B = P.sb([128, D], F32, name="A2B")
        rwb = P.sb([128, 8, 32], BF16, name="rwb")
        rbs = P.sb([128, 32], F32, name="rbs")
        b1s = P.sb([128, NE * 16], F32, name="b1s")
        b2b = P.sb([32, D], BF16, name="b2b")
        h2T = P.sb([128, 8, 1024], BF16, name="h2T")
        acc = P.sb([128, NH, D], F32, name="acc")
        GATE = P.sb([128, NH, 32], F32, name="GATE")
        w1b = P.sb([128, 8, 2048], BF16, name="w1b")
        w2b = P.sb([128, 8, D], BF16, name="w2b")
        woutb = w2b
        AT = P.sb([128, 8, 512], BF16, name="AT")
        ps = [P.ps([128, 512], F32, name=f"ps{k}") for k in range(6)]
        ps_tr = [P.ps([128, 1024], BF16, name=f"ps_tr{k}") for k in range(2)]
        P.dma("pool", ident[:], identd[:, :], w=["ident"])
        P.op("pool", lambda e: e.memset(onesf[:], 1.0), w=["onesf"])
        P.dma("sp", condT[:], cT[:, :], w=["condT"])
        P.dma("sp", adabs[:], adab[:, :], w=["adabs"])
        P.dma("sp", A2B[:], ln2[:, :], w=["A2B"])
        P.dma("sp", rbs[:], rb[:, :], w=["rbs"])
        P.dma("sp", b1s[:], b1T[:, :], w=["b1s"])
        P.dma("pool", b2b[:], b2[:, :], w=["b2b"])
        for c in range(8):
            P.dma("pool", rwb[:, c, :], rw[c * 128:(c + 1) * 128, :], w=["rwb"])
        P.op("act", lambda e: e.activation(out=condT[:], in_=condT[:], func=AF.Silu), r=["condT"], w=["condT"])
        for c in range(8):
            P.op("act", lambda e, c=c: e.activation(out=condrep[:, c, :], in_=onesf[:], func=AF.Identity, scale=condT[:, c:c + 1]),
                 r=["condT", "onesf"], w=["condrep"])
        b1v = b1s[:].rearrange("p (e k) -> p e k", k=16)
        P.op("dve", lambda e: e.tensor_scalar(out=b1v[:, :, 8:16], in0=b1v[:, :, 8:16], scalar1=1.0, scalar2=None, op0=ALU.add), r=["b1s"], w=["b1s"])
        awt = [P.sb([128, 8, 512], F32, name=f"awt{k}") for k in range(1)]
        for n in range(8):
            aw = awt[0]; awk = "awt0"
            for i in range(8):
                P.dma("sp", aw[:, i, :], adaw[i * 128:(i + 1) * 128, n * 512:(n + 1) * 512], w=[awk])
            pm = ps[n % 2]; pmk = f"ps{n % 2}"
            for i in range(8):
                P.op("pe", lambda e, pm=pm, aw=aw, i=i: e.matmul(pm[:, 0:512], lhsT=condrep[:, i, :], rhs=aw[:, i, :], start=(i == 0), stop=False),
                     r=["condrep", awk], w=[pmk])
            P.op("pe", lambda e, pm=pm, n=n: e.matmul(pm[:, 0:512], lhsT=onesf[0:1, :], rhs=adabs[0:1, n * 512:(n + 1) * 512], start=False, stop=True),
                 r=["onesf", "adabs"], w=[pmk])
            P.op("act", lambda e, pm=pm, n=n: e.activation(out=MODB[:, n * 512:(n + 1) * 512], in_=pm[:, 0:512], func=AF.Copy), r=[pmk], w=["MODB"])
        P.op("dve", lambda e: e.scalar_tensor_tensor(out=A2B[:], in0=MODB[:, 2048:3072], scalar=1.0, in1=A2B[:], op0=ALU.add, op1=ALU.mult),
             r=["MODB", "A2B"], w=["A2B"])
        ot = [P.sb([128, D], BF16, name=f"ot{k}") for k in range(2)]
        oT = [P.sb([128, 8, 128], BF16, name=f"oT{k}") for k in range(2)]
        xt = [P.sb([128, D], F32, name=f"xt{k}") for k in range(2)]
        x1 = [P.sb([128, D], F32, name=f"x1{k}") for k in range(2)]
        hb = [P.sb([128, D], BF16, name=f"hb{k}") for k in range(2)]
        sq = P.sb([128, D], F32, name="sq")
        ssv = P.sb([128, 1], F32, name="ssv")
        lg = P.sb([128, 32], F32, name="lg"); m8 = P.sb([128, 8], F32, name="m8"); msk = P.sb([128, 32], F32, name="msk")
        nmx = P.sb([128, 1], F32, name="nmx"); ex = P.sb([128, 32], F32, name="ex"); den = P.sb([128, 1], F32, name="den")
        gb = P.sb([128, 32], BF16, name="gb"); gT = P.sb([32, 128], BF16, name="gT")
        gcl = P.sb([128, 512], F32, name="gcl"); sgl = P.sb([128, 512], F32, name="sgl"); lcl = P.sb([128, 512], F32, name="lcl")
        for half in range(2):
            for c in range(8):
                P.dma("pool", woutb[:, c, :], wout[c * 128:(c + 1) * 128, :], w=["w2b"])
            for tl in range(NH):
                t = half * NH + tl
                k = t % 2
                P.dma("pool", ot[k][:], o[t * 128:(t + 1) * 128, :], w=[f"ot{k}"])
                P.dma("sp", xt[k][:], x[t * 128:(t + 1) * 128, :], w=[f"xt{k}"])
                for c in range(8):
                    P.op("pe", lambda e, k=k, c=c: e.transpose(ps_tr[k][:, c * 128:(c + 1) * 128], ot[k][:, c * 128:(c + 1) * 128], ident[:]),
                         r=[f"ot{k}", "ident"], w=[f"ps_tr{k}"])
                P.op("act", lambda e, k=k: e.activation(out=oT[k][:].rearrange("p c t -> p (c t)"), in_=ps_tr[k][:, :], func=AF.Copy),
                     r=[f"ps_tr{k}"], w=[f"oT{k}"])
                for n in range(2):
                    pm = ps[2 + n]; pmk = f"ps{2 + n}"
                    for c in range(8):
                        P.op("pe", lambda e, pm=pm, k=k, c=c, n=n: e.matmul(pm[:, 0:512], lhsT=oT[k][:, c, :], rhs=woutb[:, c, n * 512:(n + 1) * 512],
                                                                        start=(c == 0), stop=(c == 7)), r=[f"oT{k}", "w2b"], w=[pmk])
                    P.op("dve", lambda e, pm=pm, k=k, n=n: e.tensor_tensor(out=x1[k][:, n * 512:(n + 1) * 512], in0=pm[:, 0:512], in1=MODB[:, n * 512:(n + 1) * 512],
                                                                          op=ALU.mult), r=[pmk, "MODB"], w=[f"x1{k}"])
                P.op("dve", lambda e, k=k: e.tensor_tensor(out=x1[k][:], in0=x1[k][:], in1=xt[k][:], op=ALU.add), r=[f"x1{k}", f"xt{k}"], w=[f"x1{k}"])
                P.dma("sp", y[t * 128:(t + 1) * 128, :], x1[k][:], r=[f"x1{k}"], w=[f"y{t}"])
                P.op("act", lambda e, k=k: e.activation(out=sq[:], in_=x1[k][:], func=AF.Square, accum_out=ssv[:, 0:1]), r=[f"x1{k}"], w=["sq", "ssv"])
                P.op("dve", lambda e: e.tensor_scalar(out=ssv[:], in0=ssv[:], scalar1=1.0 / D, scalar2=EPS, op0=ALU.mult, op1=ALU.add), r=["ssv"], w=["ssv"])
                P.op("act", lambda e: e.activation(out=ssv[:], in_=ssv[:], func=AF.Sqrt), r=["ssv"], w=["ssv"])
                P.op("dve", lambda e: e.reciprocal(out=ssv[:], in_=ssv[:]), r=["ssv"], w=["ssv"])
                P.op("dve", lambda e, k=k: e.scalar_tensor_tensor(out=sq[:], in0=x1[k][:], scalar=ssv[:, 0:1], in1=A2B[:], op0=ALU.mult, op1=ALU.mult),
                     r=[f"x1{k}", "ssv", "A2B"], w=["sq"])
                P.op("dve", lambda e, k=k: e.tensor_tensor(out=hb[k][:], in0=sq[:], in1=MODB[:, 1024:2048], op=ALU.add), r=["sq", "MODB"], w=[f"hb{k}"])
                for c in range(8):
                    P.op("pe", lambda e, k=k, c=c: e.transpose(ps_tr[k][:, c * 128:(c + 1) * 128], hb[k][:, c * 128:(c + 1) * 128], ident[:]),
                         r=[f"hb{k}", "ident"], w=[f"ps_tr{k}"])
                P.op("act", lambda e, k=k, t=t, tl=tl: e.activation(out=h2T[:, :, tl * 128:(tl + 1) * 128], in_=ps_tr[k][:, :].rearrange("p (c t) -> p c t", t=128),
                                                          func=AF.Copy), r=[f"ps_tr{k}"], w=["h2T"])
                pm = ps[4]; pmk = "ps4"
                for c in range(8):
                    P.op("pe", lambda e, pm=pm, c=c, t=t, tl=tl: e.matmul(pm[:, 0:32], lhsT=h2T[:, c, tl * 128:(tl + 1) * 128], rhs=rwb[:, c, :], start=(c == 0), stop=(c == 7)),
                         r=["h2T", "rwb"], w=[pmk])
                P.op("dve", lambda e, pm=pm: e.tensor_tensor(out=lg[:], in0=pm[:, 0:32], in1=rbs[:], op=ALU.add), r=[pmk, "rbs"], w=["lg"])
                P.op("dve", lambda e: e.max(out=m8[:], in_=lg[:]), r=["lg"], w=["m8"])
                P.op("dve", lambda e: e.tensor_scalar(out=msk[:], in0=lg[:], scalar1=m8[:, 3:4], scalar2=None, op0=ALU.is_ge), r=["lg", "m8"], w=["msk"])
                P.op("dve", lambda e: e.tensor_scalar(out=nmx[:], in0=m8[:, 0:1], scalar1=-1.0, scalar2=None, op0=ALU.mult), r=["m8"], w=["nmx"])
                P.op("act", lambda e: e.activation(out=ex[:], in_=lg[:], func=AF.Exp, bias=nmx[:, 0:1], scale=1.0), r=["lg", "nmx"], w=["ex"])
                P.op("dve", lambda e: e.tensor_tensor(out=ex[:], in0=ex[:], in1=msk[:], op=ALU.mult), r=["ex", "msk"], w=["ex"])
                P.op("dve", lambda e: e.tensor_reduce(out=den[:], in_=ex[:], axis=AX.X, op=ALU.add), r=["ex"], w=["den"])
                P.op("dve", lambda e: e.reciprocal(out=den[:], in_=den[:]), r=["den"], w=["den"])
                P.op("dve", lambda e, t=t, tl=tl: e.tensor_scalar(out=GATE[:, tl, :], in0=ex[:], scalar1=den[:, 0:1], scalar2=None, op0=ALU.mult), r=["ex", "den"], w=["GATE"])
                P.op("dve", lambda e, t=t, tl=tl: e.tensor_copy(out=gb[:], in_=GATE[:, tl, :]), r=["GATE"], w=["gb"])
                P.op("pe", lambda e, k=k: e.transpose(ps_tr[k][0:32, 0:128], gb[:, 0:32], ident[:]), r=["gb", "ident", "h2T"], w=[f"ps_tr{k}"])
                P.op("act", lambda e, k=k: e.activation(out=gT[:], in_=ps_tr[k][0:32, 0:128], func=AF.Copy), r=[f"ps_tr{k}"], w=["gT"])
                for n in range(2):
                    pm = ps[2 + n]; pmk = f"ps{2 + n}"
                    P.op("pe", lambda e, pm=pm, n=n: e.matmul(pm[:, 0:512], lhsT=gT[:, :], rhs=b2b[:, n * 512:(n + 1) * 512], start=True, stop=True),
                         r=["gT", "b2b"], w=[pmk])
                    P.op("act", lambda e, pm=pm, n=n, t=t, tl=tl: e.activation(out=acc[:, tl, n * 512:(n + 1) * 512], in_=pm[:, 0:512], func=AF.Copy), r=[pmk], w=[f"acc{tl}"])
            u = 0
            for ex_ in range(ne):
                for c in range(8):
                    P.dma("pool", w1b[:, c, :], w1[ex_, c * 128:(c + 1) * 128, :], w=["w1b"])
                for c in range(8):
                    P.dma("pool", w2b[:, c, :], w2[ex_, c * 128:(c + 1) * 128, :], w=["w2b"])
                for grp in range(2):
                    tk = slice(grp * 512, (grp + 1) * 512)
                    for kk in range(8):
                        pg = ps[u % 2]; pgk = f"ps{u % 2}"; pl = ps[2 + u % 2]; plk = f"ps{2 + u % 2}"
                        u += 1
                        for c in range(8):
                            P.op("pe", lambda e, pg=pg, c=c, kk=kk, tk=tk: e.matmul(pg[:, 0:512], lhsT=w1b[:, c, kk * 128:(kk + 1) * 128], rhs=h2T[:, c, tk],
                                                                                  start=(c == 0), stop=(c == 7)), r=["w1b", "h2T"], w=[pgk])
                        for c in range(8):
                            P.op("pe", lambda e, pl=pl, c=c, kk=kk, tk=tk: e.matmul(pl[:, 0:512], lhsT=w1b[:, c, 1024 + kk * 128:1024 + (kk + 1) * 128], rhs=h2T[:, c, tk],
                                                                                  start=(c == 0), stop=(c == 7)), r=["w1b", "h2T"], w=[plk])
                        bg = b1s[:, ex_ * 16 + kk:ex_ * 16 + kk + 1]; bl = b1s[:, ex_ * 16 + 8 + kk:ex_ * 16 + 8 + kk + 1]
                        P.op("dve", lambda e, pg=pg, bg=bg: e.tensor_scalar(out=gcl[:], in0=pg[:, 0:512], scalar1=bg, scalar2=7.0, op0=ALU.add, op1=ALU.min),
                             r=[pgk, "b1s"], w=["gcl"])
                        P.op("act", lambda e: e.activation(out=sgl[:], in_=gcl[:], func=AF.Sigmoid, scale=1.702), r=["gcl"], w=["sgl"])
                        P.op("dve", lambda e, pl=pl, bl=bl: e.tensor_scalar(out=lcl[:], in0=pl[:, 0:512], scalar1=bl, scalar2=-6.0, op0=ALU.add, op1=ALU.max),
                             r=[plk, "b1s"], w=["lcl"])
                        P.op("dve", lambda e: e.scalar_tensor_tensor(out=lcl[:], in0=lcl[:], scalar=8.0, in1=gcl[:], op0=ALU.min, op1=ALU.mult),
                             r=["lcl", "gcl"], w=["lcl"])
                        P.op("dve", lambda e, kk=kk: e.tensor_tensor(out=AT[:, kk, :], in0=lcl[:], in1=sgl[:], op=ALU.mult), r=["lcl", "sgl"], w=[f"AT{kk}"])
                    for tt in range(4):
                        tl = grp * 4 + tt; t = half * NH + tl
                        for n in range(2):
                            py = ps[4 + n]; pyk = f"ps{4 + n}"
                            for kk in range(8):
                                P.op("pe", lambda e, py=py, kk=kk, tt=tt, n=n: e.matmul(py[:, 0:512], lhsT=AT[:, kk, tt * 128:(tt + 1) * 128],
                                                                                      rhs=w2b[:, kk, n * 512:(n + 1) * 512], start=(kk == 0), stop=(kk == 7)),
                                     r=[f"AT{kk}", "w2b"], w=[pyk])
                            P.op("dve", lambda e, py=py, t=t, tl=tl, n=n, ex_=ex_: e.scalar_tensor_tensor(out=acc[:, tl, n * 512:(n + 1) * 512], in0=py[:, 0:512],
                                                                                                scalar=GATE[:, tl, ex_:ex_ + 1], in1=acc[:, tl, n * 512:(n + 1) * 512],
                                                                                                op0=ALU.mult, op1=ALU.add), r=[pyk, "GATE", f"acc{tl}"], w=[f"acc{tl}"])
            for tl in range(NH):
                t = half * NH + tl
                k = t % 2
                P.dma("sp", xt[k][:], y[t * 128:(t + 1) * 128, :], r=[f"y{t}"], w=[f"xt{k}"])
                P.op("dve", lambda e, t=t, tl=tl: e.tensor_tensor(out=acc[:, tl, :], in0=acc[:, tl, :], in1=MODB[:, 3072:4096], op=ALU.mult), r=[f"acc{tl}", "MODB"], w=[f"acc{tl}"])
                P.op("dve", lambda e, t=t, tl=tl, k=k: e.tensor_tensor(out=acc[:, tl, :], in0=acc[:, tl, :], in1=xt[k][:], op=ALU.add), r=[f"acc{tl}", f"xt{k}"], w=[f"acc{tl}"])
                P.dma("sp", y[t * 128:(t + 1) * 128, :], acc[:, tl, :], r=[f"acc{tl}"], w=[f"y{t}"])
        print("C", P.emit())
    return nc


def inputs_C(inp, l, core, xcur, ocat):
    b, r = core // 4, core % 4
    sl = slice(r * 2048, (r + 1) * 2048)
    return dict(
        x=np.ascontiguousarray(xcur[b, sl]), o=np.ascontiguousarray(ocat[b, sl]),
        cT=np.ascontiguousarray(inp["c"][b].reshape(8, 128).T),
        adaw=np.ascontiguousarray(inp["ada_w"][l][:, 2048:6144]),
        adab=np.ascontiguousarray(inp["ada_b"][l][2048:6144].reshape(1, 4096)),
        ln2=np.ascontiguousarray(np.broadcast_to(inp["ln2_g"][l][None, :], (128, D))),
        wout=np.ascontiguousarray(inp["w_out"][l]),
        rw=np.ascontiguousarray(inp["router_w"][l]),
        rb=np.ascontiguousarray(np.broadcast_to(inp["router_b"][l][None, :], (128, 32))),
        w1=np.ascontiguousarray(inp["exp_w1"][l]),
        b1T=np.ascontiguousarray(inp["exp_b1"][l].reshape(NE, 16, 128).transpose(2, 0, 1).reshape(128, NE * 16)),
        w2=np.ascontiguousarray(inp["exp_w2"][l]),
        b2=np.ascontiguousarray(inp["exp_b2"][l]),
        identd=np.eye(128, dtype=np.float32),
    )


_CACHE = {}


def _prog(name, fn):
    if name not in _CACHE:
        _CACHE[name] = fn()
    return _CACHE[name]


def kernel(**inputs):
    inp = {k: np.asarray(v) for k, v in inputs.items()}
    x_cur = np.ascontiguousarray(inp["x"], dtype=np.float32)
    cores = list(range(8))
    for l in range(2):
        inp["x_cur"] = x_cur
        resA = run_bass_kernel_spmd(_prog("A", build_A), [inputs_A(inp, l, c) for c in cores], core_ids=cores)
        z = np.stack([np.concatenate([resA.results[b * 4 + r]["z"] for r in range(4)], axis=0) for b in range(2)])
        ocat = np.zeros((2, 8192, 1024), np.float32)
        cfg = [(c // 4, (c // 2) % 2, c % 2) for c in cores]
        maps = [inputs_B1(z[b], g, p, inp["nsa_q_gain"][l], inp["nsa_k_gain"][l], inp["nsa_cmp_pos"][l],
                          inp["nsa_cmp_w1"][l], inp["nsa_cmp_w2"][l]) for (b, g, p) in cfg]
        resB1 = run_bass_kernel_spmd(_prog("B1", build_B1), maps, core_ids=cores)
        maps = [inputs_B2(z[b], g, p, inp["moba_q_gain"][l], inp["moba_k_gain"][l]) for (b, g, p) in cfg]
        resB2 = run_bass_kernel_spmd(_prog("B2", build_B2), maps, core_ids=cores)
        for c, (b, g, p) in enumerate(cfg):
            own = (np.arange(4096) // 128 * 2 + p) * 128 + np.arange(4096) % 128
            ocat[b, own, g * 256:(g + 1) * 256] = resB1.results[c]["o"]
            ocat[b, own, 512 + g * 256:512 + (g + 1) * 256] = resB2.results[c]["o"]
        resC = run_bass_kernel_spmd(_prog("C", build_C), [inputs_C(inp, l, c, x_cur, ocat) for c in cores], core_ids=cores)
        x_cur = np.stack([np.concatenate([resC.results[b * 4 + r]["y"] for r in range(4)], axis=0) for b in range(2)]).astype(np.float32)
    return x_cur
```

```python
from concourse.bass_utils import run_bass_kernel_spmd
D = 1024
EPS = 1e-6

import contextlib
import numpy as np
import concourse.bass as bass
import concourse.mybir as mybir

F32 = mybir.dt.float32
BF16 = mybir.dt.bfloat16
I32 = mybir.dt.int32
U32 = mybir.dt.uint32
AF = mybir.ActivationFunctionType
ALU = mybir.AluOpType
AX = mybir.AxisListType

DMA_K = 6
STRICT_SAME = True


class Prog:
    def __init__(self, nc, stack):
        self.nc = nc
        self.stack = stack
        self.ins = []
        self.eng = {"pe": nc.tensor, "act": nc.scalar, "dve": nc.vector, "pool": nc.gpsimd, "sp": nc.sync}
        self.nt = 0

    def sb(self, shape, dt, name=None):
        self.nt += 1
        return self.stack.enter_context(self.nc.sbuf_tensor(name or f"sb{self.nt}", list(shape), dt))

    def ps(self, shape, dt=F32, name=None):
        self.nt += 1
        return self.stack.enter_context(self.nc.psum_tensor(name or f"ps{self.nt}", list(shape), dt))

    def op(self, eng, fn, r=(), w=()):
        self.ins.append(dict(e=eng, fn=fn, r=tuple(r), w=tuple(w), dma=False))

    def dma(self, q, out, in_, r=(), w=(), **kw):
        def fn(e, out=out, in_=in_, kw=kw):
            return e.dma_start(out=out, in_=in_, **kw)
        self.ins.append(dict(e=q, fn=fn, r=tuple(r), w=tuple(w), dma=True))

    def emit(self):
        nc = self.nc
        ins = self.ins
        n = len(ins)
        last_w = {}
        readers = {}
        deps = [None] * n
        for i, I in enumerate(ins):
            d = set()
            for k in I["r"]:
                d.update(last_w.get(k, ()))
            for k in I["w"]:
                for j in last_w.get(k, ()):
                    if not (I["dma"] and ins[j]["dma"]):
                        d.add(j)
                rd = readers.get(k)
                if rd:
                    for v in rd[0].values():
                        d.add(v)
                    d.update(rd[1])
            d.discard(i)
            deps[i] = d
            for k in I["r"]:
                rd = readers.setdefault(k, ({}, []))
                if I["dma"]:
                    rd[1].append(i)
                else:
                    rd[0][I["e"]] = i
            for k in I["w"]:
                if I["dma"]:
                    last_w[k] = [j for j in last_w.get(k, ()) if ins[j]["dma"]] + [i]
                else:
                    last_w[k] = [i]
                readers[k] = ({}, [])
        signal = [False] * n
        fdeps = [None] * n
        for i, I in enumerate(ins):
            keep = []
            for j in deps[i]:
                J = ins[j]
                if J["dma"]:
                    keep.append(j)
                    continue
                if J["e"] == I["e"]:
                    if I["dma"]:
                        keep.append(j); signal[j] = True
                        continue
                    if STRICT_SAME and I["e"] in ("act", "dve", "pool"):
                        if any(k in J["w"] for k in I["r"]):
                            keep.append(j); signal[j] = True
                    continue
                keep.append(j); signal[j] = True
            fdeps[i] = keep
        engs = ["pe", "act", "dve", "pool", "sp"]
        sems = {e: self.stack.enter_context(nc.semaphore(f"s_{e}")) for e in engs}
        dsems = {e: [self.stack.enter_context(nc.semaphore(f"d_{e}{k}")) for k in range(DMA_K)]
                 for e in ("sp", "act", "pool")}
        cnt = {e: 0 for e in engs}
        dcnt = {e: 0 for e in dsems}
        tag = [None] * n
        for i, I in enumerate(ins):
            if I["dma"]:
                q = I["e"]
                idx = dcnt[q]; dcnt[q] += 1
                tag[i] = ("d", q, idx)
            elif signal[i]:
                cnt[I["e"]] += 1
                tag[i] = ("c", I["e"], cnt[I["e"]])
        waited = {e: {} for e in engs}
        nw = 0
        for i, I in enumerate(ins):
            e = I["e"]
            E = self.eng[e]
            wl = {}
            for j in fdeps[i]:
                t = tag[j]
                if t[0] == "d":
                    s = dsems[t[1]][t[2] % DMA_K]; v = 16 * (t[2] // DMA_K + 1)
                else:
                    s = sems[t[1]]; v = t[2]
                key = id(s)
                if key not in wl or wl[key][1] < v:
                    wl[key] = (s, v)
            if I["dma"]:
                t = tag[i]
                if t[2] >= DMA_K:
                    s = dsems[t[1]][t[2] % DMA_K]; v = 16 * (t[2] // DMA_K)
                    key = id(s)
                    if key not in wl or wl[key][1] < v:
                        wl[key] = (s, v)
            for key, (s, v) in wl.items():
                if waited[e].get(key, 0) >= v:
                    continue
                waited[e][key] = v
                E.wait_ge(s, v)
                nw += 1
            inst = I["fn"](E)
            t = tag[i]
            if t is not None:
                if t[0] == "d":
                    inst.then_inc(dsems[t[1]][t[2] % DMA_K], 16)
                else:
                    inst.then_inc(sems[t[1]], 1)
        E = self.eng["sp"]
        for q, c in dcnt.items():
            for k in range(DMA_K):
                m = (c - k + DMA_K - 1) // DMA_K if c > k else 0
                if m > 0 and waited["sp"].get(id(dsems[q][k]), 0) < 16 * m:
                    E.wait_ge(dsems[q][k], 16 * m)
        self.stats = dict(n=n, waits=nw, cnt=dict(cnt), dcnt=dict(dcnt))
        return self.stats

import contextlib
import numpy as np
import concourse.bass as bass
import concourse.mybir as mybir

INW = 2840
NT = 16
NORM_SEGS = [(0, 8), (768, 2), (1024, 2), (1304, 8), (1816, 8)]


def emit_mod(P, nc, adaw_dram, col0, ncols, condT, adabT, out_cols, tagp):
    nj = ncols // 128
    half = 1024
    pm = P.ps([128, 64], F32, name=f"pm_{tagp}")
    for hh in range(ncols // half):
        aw = P.sb([128, 8, half], F32, name=f"aw_{tagp}{hh}")
        for i in range(8):
            P.dma("sp" if i % 2 == 0 else "pool", aw[:, i, :], adaw_dram[i * 128:(i + 1) * 128, col0 + hh * half: col0 + (hh + 1) * half],
                  w=[f"aw_{tagp}{hh}_{i}"])
        for jj in range(half // 128):
            j = hh * (half // 128) + jj
            for i in range(8):
                P.op("pe", lambda e, aw=aw, i=i, jj=jj, j=j: e.matmul(pm[:, j:j + 1], lhsT=aw[:, i, jj * 128:(jj + 1) * 128],
                                                                rhs=condT[:, i:i + 1], start=(i == 0), stop=(i == 7)),
                     r=[f"aw_{tagp}{hh}_{i}", "condT"], w=[f"pm_{tagp}"])
    P.op("dve", lambda e: e.tensor_tensor(out=out_cols[:, 0:nj], in0=pm[:, 0:nj], in1=adabT, op=ALU.add),
         r=[f"pm_{tagp}", "adab"], w=[f"mod_{tagp}"])


class _Stop(Exception):
    pass


def build_A(stop=99):
    nc = bass.Bass("TRN2", target_bir_lowering=False)
    x = nc.dram_tensor("x", [NT * 128, D], F32, kind="ExternalInput").ap()
    cT = nc.dram_tensor("cT", [128, 8], F32, kind="ExternalInput").ap()
    adaw = nc.dram_tensor("adaw", [D, 2048], F32, kind="ExternalInput").ap()
    adabT = nc.dram_tensor("adabT", [128, 16], F32, kind="ExternalInput").ap()
    lngT = nc.dram_tensor("lngT", [128, 8], F32, kind="ExternalInput").ap()
    win = nc.dram_tensor("win", [D, INW], F32, kind="ExternalInput").ap()
    identd = nc.dram_tensor("identd", [128, 128], F32, kind="ExternalInput").ap()
    zo = nc.dram_tensor("z", [NT * 128, INW], F32, kind="ExternalOutput").ap()
    with contextlib.ExitStack() as st:
        P = Prog(nc, st)
        try:
            ident = P.sb([128, 128], BF16, name="ident")
            identf = P.sb([128, 128], F32, name="identf")
            condT = P.sb([128, 8], F32, name="condT")
            adab = P.sb([128, 16], F32, name="adab")
            lng = P.sb([128, 8], F32, name="lng")
            modc = P.sb([128, 16], F32, name="modc")
            Acol = P.sb([128, 8], F32, name="Acol")
            wb = P.sb([128, 8, INW], BF16, name="wb")
            P.dma("sp", identf[:], identd[:, :], w=["identf"])
            P.op("dve", lambda e: e.tensor_copy(out=ident[:], in_=identf[:]), r=["identf"], w=["ident"])
            P.dma("sp", condT[:], cT[:, :], w=["condT"])
            P.dma("sp", adab[:], adabT[:, :], w=["adab"])
            P.dma("sp", lng[:], lngT[:, :], w=["lng"])
            P.op("act", lambda e: e.activation(out=condT[:], in_=condT[:], func=AF.Silu), r=["condT"], w=["condT"])
            emit_mod(P, nc, adaw, 0, 2048, condT, adab[:, 0:16], modc, "a")
            P.op("dve", lambda e: e.scalar_tensor_tensor(out=Acol[:], in0=modc[:, 8:16], scalar=1.0, in1=lng[:], op0=ALU.add, op1=ALU.mult),
                 r=["mod_a", "lng"], w=["Acol"])
            if stop == 1:
                P.dma("sp", zo[0:128, 0:16], modc[:], r=["mod_a"])
                P.dma("sp", zo[0:128, 16:24], Acol[:], r=["Acol"])
                raise _Stop()
            for i in range(8):
                for cc in range(0, INW, 568):
                    P.dma("pool", wb[:, i, cc:cc + 568], win[i * 128:(i + 1) * 128, cc:cc + 568], w=[f"wb{i}"])
            xt = [P.sb([128, D], F32, name=f"xt{k}") for k in range(2)]
            xnb = [P.sb([128, D], BF16, name=f"xnb{k}") for k in range(2)]
            sq = P.sb([128, D], F32, name="sq")
            ss = [P.sb([128, 1], F32, name=f"ss{k}") for k in range(2)]
            hT = [P.sb([128, 8, 128], BF16, name=f"hT{k}") for k in range(2)]
            pT = [P.ps([128, D], BF16, name=f"pT{k}") for k in range(2)]
            pz = [P.ps([128, 512], F32, name=f"pz{k}") for k in range(3)]
            zsb = [P.sb([128, INW], F32, name=f"zsb{k}") for k in range(2)]
            ss2 = P.sb([128, 8], F32, name="ss2")
            groups = [(c0, min(512, INW - c0)) for c0 in range(0, INW, 512)]
            for t in range(NT):
                k = t % 2
                X, XN, SS, HT, PT, Z = xt[k], xnb[k], ss[k], hT[k], pT[k], zsb[k]
                P.dma("sp", X[:], x[t * 128:(t + 1) * 128, :], w=[f"xt{k}"])
                P.op("act", lambda e, X=X, SS=SS: e.activation(out=sq[:], in_=X[:], func=AF.Square, accum_out=SS[:, 0:1]),
                     r=[f"xt{k}"], w=["sq", f"ss{k}"])
                P.op("dve", lambda e, SS=SS: e.tensor_scalar(out=SS[:], in0=SS[:], scalar1=1.0 / D, scalar2=EPS, op0=ALU.mult, op1=ALU.add),
                     r=[f"ss{k}"], w=[f"ss{k}"])
                P.op("act", lambda e, SS=SS: e.activation(out=SS[:], in_=SS[:], func=AF.Sqrt), r=[f"ss{k}"], w=[f"ss{k}"])
                P.op("dve", lambda e, SS=SS: e.reciprocal(out=SS[:], in_=SS[:]), r=[f"ss{k}"], w=[f"ss{k}"])
                P.op("dve", lambda e, X=X, XN=XN, SS=SS: e.tensor_scalar(out=XN[:], in0=X[:], scalar1=SS[:, 0:1], scalar2=None, op0=ALU.mult),
                     r=[f"xt{k}", f"ss{k}"], w=[f"xnb{k}"])
                for c in range(8):
                    P.op("pe", lambda e, PT=PT, XN=XN, c=c: e.transpose(PT[:, c * 128:(c + 1) * 128], XN[:, c * 128:(c + 1) * 128], ident[:]),
                         r=[f"xnb{k}", "ident"], w=[f"pT{k}"])
                for c in range(8):
                    eng = "act"
                    if eng == "act":
                        P.op("act", lambda e, HT=HT, PT=PT, c=c: e.activation(out=HT[:, c, :], in_=PT[:, c * 128:(c + 1) * 128], func=AF.Identity,
                                                                         scale=Acol[:, c:c + 1], bias=modc[:, c:c + 1]),
                             r=[f"pT{k}", "Acol", "mod_a"], w=[f"hT{k}_{c}"])
                    else:
                        P.op("dve", lambda e, HT=HT, PT=PT, c=c: e.tensor_scalar(out=HT[:, c, :], in0=PT[:, c * 128:(c + 1) * 128],
                                                                            scalar1=Acol[:, c:c + 1], scalar2=modc[:, c:c + 1],
                                                                            op0=ALU.mult, op1=ALU.add),
                             r=[f"pT{k}", "Acol", "mod_a"], w=[f"hT{k}_{c}"])
                if stop == 2:
                    hf = P.sb([128, 8, 128], F32, name="hf")
                    P.op("dve", lambda e: e.tensor_copy(out=hf[:], in_=HT[:]), r=[f"hT{k}_{c}" for c in range(8)], w=["hf"])
                    P.dma("sp", zo[0:128, 0:1024], hf[:].rearrange("p c t -> p (c t)"), r=["hf"])
                    raise _Stop()
                for gi, (c0, cw) in enumerate(groups):
                    pzz = pz[gi % 3]
                    for c in range(8):
                        P.op("pe", lambda e, pzz=pzz, HT=HT, c=c, c0=c0, cw=cw: e.matmul(pzz[:, 0:cw], lhsT=HT[:, c, :], rhs=wb[:, c, c0:c0 + cw],
                                                                                   start=(c == 0), stop=(c == 7)),
                             r=[f"hT{k}_{c}", f"wb{c}"], w=[f"pz{gi % 3}"])
                    eng = "act" if gi % 2 == 0 else "dve"
                    if eng == "act":
                        P.op("act", lambda e, pzz=pzz, Z=Z, c0=c0, cw=cw: e.activation(out=Z[:, c0:c0 + cw], in_=pzz[:, 0:cw], func=AF.Copy),
                             r=[f"pz{gi % 3}"], w=[f"zsb{k}_{gi}"])
                    else:
                        P.op("dve", lambda e, pzz=pzz, Z=Z, c0=c0, cw=cw: e.tensor_copy(out=Z[:, c0:c0 + cw], in_=pzz[:, 0:cw]),
                             r=[f"pz{gi % 3}"], w=[f"zsb{k}_{gi}"])
                zkeys = [f"zsb{k}_{gi}" for gi in range(len(groups))]
                if stop == 3:
                    P.dma("sp", zo[t * 128:(t + 1) * 128, :], Z[:], r=zkeys)
                    raise _Stop()
                for (s0, nh) in NORM_SEGS:
                    V = Z[:, s0:s0 + nh * 64]
                    P.op("pool", lambda e, V=V, nh=nh: e.tensor_tensor(out=sq[:, 0:nh * 64], in0=V, in1=V, op=ALU.mult),
                         r=zkeys, w=["sq"])
                    P.op("dve", lambda e, nh=nh: e.tensor_reduce(out=ss2[:, 0:nh], in_=sq[:, 0:nh * 64].rearrange("p (h d) -> p h d", d=64),
                                                               axis=AX.X, op=ALU.add), r=["sq"], w=["ss2"])
                    P.op("dve", lambda e, nh=nh: e.tensor_scalar(out=ss2[:, 0:nh], in0=ss2[:, 0:nh], scalar1=1.0 / 64, scalar2=EPS,
                                                               op0=ALU.mult, op1=ALU.add), r=["ss2"], w=["ss2"])
                    P.op("act", lambda e, nh=nh: e.activation(out=ss2[:, 0:nh], in_=ss2[:, 0:nh], func=AF.Sqrt), r=["ss2"], w=["ss2"])
                    P.op("dve", lambda e, nh=nh: e.reciprocal(out=ss2[:, 0:nh], in_=ss2[:, 0:nh]), r=["ss2"], w=["ss2"])
                    P.op("dve", lambda e, V=V, nh=nh: e.tensor_tensor(out=V.rearrange("p (h d) -> p h d", d=64),
                                                                    in0=V.rearrange("p (h d) -> p h d", d=64),
                                                                    in1=ss2[:, 0:nh].unsqueeze(2).to_broadcast([128, nh, 64]), op=ALU.mult),
                         r=zkeys + ["ss2"], w=zkeys)
                P.op("act", lambda e, Z=Z: e.activation(out=Z[:, 1280:1304], in_=Z[:, 1280:1304], func=AF.Sigmoid), r=zkeys, w=zkeys)
                P.dma("sp", zo[t * 128:(t + 1) * 128, :], Z[:], r=zkeys)
        except _Stop:
            pass
        import os
        if os.environ.get("TRUNC"):
            N = int(os.environ["TRUNC"])
            for q, I in enumerate(P.ins[:N]):
                pass
            print("TRUNC at", N, "of", len(P.ins), P.ins[N - 1]["e"], P.ins[N - 1]["r"], P.ins[N - 1]["w"])
            P.ins = P.ins[:N]
            P.dma("sp", zo[0:128, 0:16], modc[:], r=["mod_a"])
        print("A", P.emit())
    return nc


def inputs_A(inp, l, core):
    b, r = core // 4, core % 4
    xl = inp["x_cur"][b, r * 2048:(r + 1) * 2048]
    return {
        "x": np.ascontiguousarray(xl),
        "cT": np.ascontiguousarray(inp["c"][b].reshape(8, 128).T),
        "adaw": np.ascontiguousarray(inp["ada_w"][l][:, 0:2048]),
        "adabT": np.ascontiguousarray(inp["ada_b"][l][0:2048].reshape(16, 128).T),
        "lngT": np.ascontiguousarray(inp["ln1_g"][l].reshape(8, 128).T),
        "win": np.ascontiguousarray(inp["w_in"][l]),
        "identd": np.eye(128, dtype=np.float32),
    }

import contextlib
import numpy as np
import concourse.bass as bass

S = 8192
NEGB = -30000.0
GELU_C = 1.5957691216057308


def build_B1(nslots=32):
    nc = bass.Bass("TRN2", target_bir_lowering=False)
    D_ = lambda n, s: nc.dram_tensor(n, s, F32, kind="ExternalInput").ap()
    qnT = D_("qnT", [64, 16384]); qaugc = D_("qaugc", [5, 16384]); gates = D_("gates", [128, 384])
    kcT = D_("kcT", [2, 64, 8224]); ksT = D_("ksT", [2, 64, S]); vsw = D_("vsw", [2, S, 64])
    kaugc = D_("kaugc", [4, S]); kcaugc = D_("kcaugc", [5, 512])
    gq = D_("gq", [64, 1]); gk = D_("gk", [64, 1])
    w1r = D_("w1r", [2, 64, 4096]); posT = D_("posT", [64, 64]); w2r = D_("w2r", [128, 128]); incid = D_("incid", [512, 128])
    enu = D_("enu", [128, S])
    masks = D_("masks", [6, 128, 128])
    cmt = D_("cmt", [32, 128, 512]); addt = D_("addt", [32, 128, 128])
    identd = D_("identd", [128, 128])
    o = nc.dram_tensor("o", [4096, 256], F32, kind="ExternalOutput").ap()
    with contextlib.ExitStack() as st:
        P = Prog(nc, st)
        ident = P.sb([128, 128], BF16, name="ident")
        M4 = P.sb([128, 6, 512], BF16, name="M4")
        Mt = P.sb([128, 6, 128], BF16, name="Mt")
        EN = P.sb([128, S], BF16, name="EN")
        KS = P.sb([68, S], BF16, name="KS"); KW = P.sb([68, S], BF16, name="KW")
        QN = P.sb([69, 16384], BF16, name="QN")
        VS = P.sb([128, 64, 65], BF16, name="VS"); VW = P.sb([128, 64, 65], BF16, name="VW")
        KC = P.sb([69, 512], BF16, name="KC"); VC = P.sb([128, 4, 193], BF16, name="VC")
        G = P.sb([128, 32, 12], F32, name="G")
        gqs = P.sb([64, 1], F32, name="gqs"); gks = P.sb([64, 1], F32, name="gks")
        stg = [P.sb([64, 2048], F32, name=f"stg{k}") for k in range(2)]
        KCT = P.sb([64, 8224], BF16, name="KCT")
        W1b = P.sb([64, 4096], BF16, name="W1b"); posb = P.sb([64, 64], BF16, name="posb"); W2b = P.sb([128, 128], BF16, name="W2b")
        pbias = P.sb([128, 2], F32, name="pbias")
        xh = P.sb([128, 512], F32, name="xh"); x2 = P.sb([128, 512], F32, name="x2"); sgm = P.sb([128, 512], F32, name="sgm")
        GE = P.sb([128, 512], BF16, name="GE")
        sqk = P.sb([128, 64], F32, name="sqk"); ssk = P.sb([128, 1], F32, name="ssk"); kcn = P.sb([128, 64], BF16, name="kcn")
        CMs = [P.sb([128, 512], BF16, name=f"CMs{k}") for k in range(2)]
        ADs = [P.sb([128, 128], F32, name=f"ADs{k}") for k in range(2)]
        PT = [P.sb([128, 512], BF16, name=f"PT{k}") for k in range(3)]
        SELT4 = P.sb([128, 512], BF16, name="SELT4")
        IMP = P.sb([128, 128], F32, name="IMP"); IMP2 = P.sb([128, 128], F32, name="IMP2"); selb = P.sb([128, 128], BF16, name="selb")
        m8a = P.sb([128, 8], F32, name="m8a"); m8b = P.sb([128, 8], F32, name="m8b")
        rd = P.sb([128, 4], F32, name="rd"); cf = P.sb([128, 4], F32, name="cf")
        OACC = [P.sb([128, 4, 64], F32, name=f"OACC{k}") for k in range(2)]
        ps_sc = [P.ps([128, 512], F32, name=f"ps_sc{k}") for k in range(2)]
        ps_o = [P.ps([128, 512], F32, name=f"ps_o{k}") for k in range(4)]
        ps_tr = P.ps([128, 1024], BF16, name="ps_tr")
        ps_m = P.ps([128, 512], F32, name="ps_m")
        P.dma("pool", ident[:], identd[:, :], w=["ident"])
        for m in range(6):
            P.dma("pool", Mt[:, m, :], masks[m, :, :], w=["Mt"])
        for m in range(6):
            for h in range(4):
                P.op("dve", lambda e, m=m, h=h: e.tensor_copy(out=M4[:, m, h * 128:(h + 1) * 128], in_=Mt[:, m, :]), r=["Mt"], w=["M4"])
        for c in range(4):
            cs = slice(c * 2048, (c + 1) * 2048)
            P.dma("pool", EN[:, cs], enu[:, cs], w=["EN"])
            P.dma("pool", KS[64:68, cs], kaugc[:, cs], w=["KSaug"])
            P.dma("pool", KW[64:68, cs], kaugc[:, cs], w=["KWaug"])
        P.dma("pool", KC[64:69, :], kcaugc[:, :], w=["KCaug"])
        P.dma("sp", gqs[:], gq[:, :], w=["gqs"]); P.dma("sp", gks[:], gk[:, :], w=["gks"])
        P.dma("sp", G[:].rearrange("p i c -> p (i c)"), gates[:, :], w=["G"])
        P.dma("pool", posb[:], posT[:, :], w=["posb"]); P.dma("pool", W2b[:], w2r[:, :], w=["W2b"])
        P.op("dve", lambda e: e.memset(VS[:, :, 64:65], 1.0), w=["VSone"])
        P.op("dve", lambda e: e.memset(VW[:, :, 64:65], 1.0), w=["VWone"])
        P.op("dve", lambda e: e.memset(VC[:, :, 64:65], 1.0), w=["VCone"])
        P.op("dve", lambda e: e.memset(GE[:], 0.0), w=["GE"])
        P.dma("pool", VC[:, :, 65:193], incid.rearrange("(nt p) j -> p nt j", p=128), w=["VCinc"])
        for kv, (KT, kk) in enumerate(((KS, "KS"), (KW, "KW"))):
            for c in range(4):
                sg = stg[c % 2]; cs = slice(c * 2048, (c + 1) * 2048)
                P.dma("sp", sg[:], ksT[kv, :, cs], w=[f"stg{c % 2}"])
                P.op("act", lambda e, sg=sg, KT=KT, cs=cs: e.activation(out=KT[0:64, cs], in_=sg[:], func=AF.Identity, scale=gks[:, 0:1]),
                     r=[f"stg{c % 2}", "gks"], w=[kk])
        for kv, (VT, vk) in enumerate(((VS, "VS"), (VW, "VW"))):
            for c in range(4):
                P.dma("pool", VT[:, c * 16:(c + 1) * 16, 0:64], vsw[kv, c * 2048:(c + 1) * 2048, :].rearrange("(kt p) d -> p kt d", p=128), w=[vk])
        for c in range(8):
            sg = stg[c % 2]; cs = slice(c * 2048, (c + 1) * 2048)
            P.dma("sp", sg[:], qnT[:, cs], w=[f"stg{c % 2}"])
            P.op("dve", lambda e, sg=sg, cs=cs: e.tensor_scalar(out=QN[0:64, cs], in0=sg[:], scalar1=gqs[:, 0:1], scalar2=None, op0=ALU.mult),
                 r=[f"stg{c % 2}", "gqs"], w=["QN"])
            P.dma("pool", QN[64:69, cs], qaugc[:, cs], w=["QNaug"])
        KCv = KCT[:].rearrange("p (n s) -> p n s", s=16)
        for kv in range(2):
            for c in range(4):
                P.dma("pool", KCT[:, c * 2056:(c + 1) * 2056], kcT[kv, :, c * 2056:(c + 1) * 2056], w=["KCT"])
            P.dma("pool", W1b[:], w1r[kv, :, :], w=["W1b"])
            for l in range(32):
                rhs = KCv[:, 0:511, l] if l < 16 else KCv[:, 1:512, l - 16]
                P.op("pe", lambda e, l=l, rhs=rhs: e.matmul(ps_m[:, 0:511], lhsT=W1b[:, l * 128:(l + 1) * 128], rhs=rhs, start=(l == 0), stop=(l == 31)),
                     r=["W1b", "KCT"], w=["ps_m"])
            for l in range(32):
                P.op("pe", lambda e, l=l, kv=kv: e.matmul(ps_sc[0][:, 0:1], lhsT=W1b[:, l * 128:(l + 1) * 128], rhs=posb[:, kv * 32 + l:kv * 32 + l + 1],
                                                      start=(l == 0), stop=(l == 31)), r=["W1b", "posb"], w=["ps_sc0"])
            P.op("dve", lambda e, kv=kv: e.tensor_copy(out=pbias[:, kv:kv + 1], in_=ps_sc[0][:, 0:1]), r=["ps_sc0"], w=["pbias"])
            P.op("act", lambda e, kv=kv: e.activation(out=xh[:, 0:511], in_=ps_m[:, 0:511], func=AF.Identity, bias=pbias[:, kv:kv + 1], scale=1.0),
                 r=["ps_m", "pbias"], w=["xh"])
            P.op("dve", lambda e: e.tensor_tensor(out=x2[:, 0:511], in0=xh[:, 0:511], in1=xh[:, 0:511], op=ALU.mult), r=["xh"], w=["x2"])
            P.op("dve", lambda e: e.tensor_scalar(out=x2[:, 0:511], in0=x2[:, 0:511], scalar1=0.044715, scalar2=1.0, op0=ALU.mult, op1=ALU.add), r=["x2"], w=["x2"])
            P.op("dve", lambda e: e.tensor_tensor(out=x2[:, 0:511], in0=x2[:, 0:511], in1=xh[:, 0:511], op=ALU.mult), r=["x2", "xh"], w=["x2"])
            P.op("act", lambda e: e.activation(out=sgm[:, 0:511], in_=x2[:, 0:511], func=AF.Sigmoid, scale=GELU_C), r=["x2"], w=["sgm"])
            P.op("dve", lambda e: e.tensor_tensor(out=GE[:, 0:511], in0=xh[:, 0:511], in1=sgm[:, 0:511], op=ALU.mult), r=["xh", "sgm", "GE"], w=["GE"])
            for nt in range(4):
                P.op("pe", lambda e, nt=nt, kv=kv: e.matmul(ps_sc[1][:, 0:64], lhsT=GE[:, nt * 128:(nt + 1) * 128], rhs=W2b[:, kv * 64:(kv + 1) * 64], start=True, stop=True),
                     r=["GE", "W2b"], w=["ps_sc1"])
                if kv == 0:
                    P.op("act", lambda e: e.activation(out=sqk[:], in_=ps_sc[1][:, 0:64], func=AF.Square, accum_out=ssk[:, 0:1]), r=["ps_sc1"], w=["sqk", "ssk"])
                    P.op("dve", lambda e: e.tensor_scalar(out=ssk[:], in0=ssk[:], scalar1=1.0 / 64, scalar2=1e-6, op0=ALU.mult, op1=ALU.add), r=["ssk"], w=["ssk"])
                    P.op("act", lambda e: e.activation(out=ssk[:], in_=ssk[:], func=AF.Sqrt), r=["ssk"], w=["ssk"])
                    P.op("dve", lambda e: e.reciprocal(out=ssk[:], in_=ssk[:]), r=["ssk"], w=["ssk"])
                    P.op("dve", lambda e: e.tensor_scalar(out=kcn[:], in0=ps_sc[1][:, 0:64], scalar1=ssk[:, 0:1], scalar2=None, op0=ALU.mult),
                         r=["ps_sc1", "ssk"], w=["kcn"])
                    P.op("pe", lambda e: e.transpose(ps_tr[0:64, 0:128], kcn[:, 0:64], ident[:]), r=["kcn", "ident"], w=["ps_tr"])
                    P.op("act", lambda e, nt=nt: e.activation(out=KC[0:64, nt * 128:(nt + 1) * 128], in_=ps_tr[0:64, 0:128], func=AF.Identity, scale=gks[:, 0:1]),
                         r=["ps_tr", "gks"], w=["KC"])
                else:
                    P.op("act", lambda e, nt=nt: e.activation(out=VC[:, nt, 0:64], in_=ps_sc[1][:, 0:64], func=AF.Copy), r=["ps_sc1"], w=["VC"])
        Gv = G[:].rearrange("p i (h b) -> p i h b", b=3)
        u = 0

        def attend(mms_fn, kts, VT, vkeys, ncol, okeys_first):
            nonlocal u
            for ki, kt in enumerate(kts):
                sc = ps_sc[u % 2]; sck = f"ps_sc{u % 2}"; pt = PT[u % 3]; ptk = f"PT{u % 3}"
                u += 1
                mms = mms_fn(kt, sc)
                for mi, (o_, l_, r_, rk) in enumerate(mms):
                    P.op("pe", lambda e, o_=o_, l_=l_, r_=r_, mi=mi, n=len(mms): e.matmul(o_, lhsT=l_, rhs=r_, start=(mi == 0), stop=(mi == n - 1)), r=rk, w=[sck])
                P.op("act", lambda e, pt=pt, sc=sc: e.activation(out=pt[:], in_=sc[:, 0:512], func=AF.Exp, scale=0.125), r=[sck], w=[ptk])
                for h in range(4):
                    P.op("pe", lambda e, pt=pt, h=h, kt=kt, ki=ki, n=len(kts): e.matmul(ps_o[h][:, 0:ncol], lhsT=pt[:, h * 128:(h + 1) * 128], rhs=VT[:, kt, 0:ncol],
                                                                                   start=(ki == 0), stop=(ki == n - 1)), r=[ptk] + vkeys, w=[f"ps_o{h}"])

        def finish(i, br, OA, oak, first):
            for h in range(4):
                P.op("dve", lambda e, h=h: e.tensor_scalar(out=rd[:, h:h + 1], in0=ps_o[h][:, 64:65], scalar1=1e-30, scalar2=None, op0=ALU.max), r=[f"ps_o{h}"], w=["rd"])
            P.op("dve", lambda e: e.reciprocal(out=rd[:], in_=rd[:]), r=["rd"], w=["rd"])
            P.op("dve", lambda e, i=i, br=br: e.tensor_tensor(out=cf[:], in0=rd[:], in1=Gv[:, i, :, br], op=ALU.mult), r=["rd", "G"], w=["cf"])
            for h in range(4):
                if first:
                    P.op("dve", lambda e, h=h, OA=OA: e.tensor_scalar(out=OA[:, h, :], in0=ps_o[h][:, 0:64], scalar1=cf[:, h:h + 1], scalar2=None, op0=ALU.mult),
                         r=[f"ps_o{h}", "cf"], w=[oak])
                else:
                    P.op("dve", lambda e, h=h, OA=OA: e.scalar_tensor_tensor(out=OA[:, h, :], in0=ps_o[h][:, 0:64], scalar=cf[:, h:h + 1], in1=OA[:, h, :],
                                                                           op0=ALU.mult, op1=ALU.add), r=[f"ps_o{h}", "cf", oak], w=[oak])

        for i in range(nslots):
            cols = slice(i * 512, (i + 1) * 512)
            CM = CMs[i % 2]; cmk = f"CMs{i % 2}"; AD = ADs[i % 2]; adk = f"ADs{i % 2}"
            OA = OACC[i % 2]; oak = f"OACC{i % 2}"
            P.dma("pool", CM[:], cmt[i, :, :], w=[cmk])
            P.dma("sp", AD[:], addt[i, :, :], w=[adk])
            def mm_cmp(nt, sc):
                mms = [(sc[:, 0:512], KC[0:69, nt * 128:(nt + 1) * 128], QN[0:69, cols], ["KC", "KCaug", "QN", "QNaug"])]
                for h in range(4):
                    mms.append((sc[:, h * 128:(h + 1) * 128], ident[:], CM[:, nt * 128:(nt + 1) * 128], ["ident", cmk]))
                return mms
            attend(mm_cmp, list(range(min(3, (256 * i + 224) // 2048) + 1)), VC, ["VC", "VCone", "VCinc"], 193, None)
            finish(i, 0, OA, oak, True)
            P.op("dve", lambda e: e.tensor_scalar(out=IMP[:], in0=ps_o[0][:, 65:193], scalar1=rd[:, 0:1], scalar2=None, op0=ALU.mult), r=["ps_o0", "rd"], w=["IMP"])
            for h in range(1, 4):
                P.op("dve", lambda e, h=h: e.scalar_tensor_tensor(out=IMP[:], in0=ps_o[h][:, 65:193], scalar=rd[:, h:h + 1], in1=IMP[:], op0=ALU.mult, op1=ALU.add),
                     r=[f"ps_o{h}", "rd", "IMP"], w=["IMP"])
            P.op("dve", lambda e, AD=AD: e.tensor_tensor(out=IMP[:], in0=IMP[:], in1=AD[:], op=ALU.add), r=["IMP", adk], w=["IMP"])
            P.op("dve", lambda e: e.max(out=m8a[:], in_=IMP[:]), r=["IMP"], w=["m8a"])
            P.op("dve", lambda e: e.match_replace(out=IMP2[:], in_to_replace=m8a[:], in_values=IMP[:], imm_value=-3.0e38), r=["IMP", "m8a"], w=["IMP2"])
            P.op("dve", lambda e: e.max(out=m8b[:], in_=IMP2[:]), r=["IMP2"], w=["m8b"])
            P.op("dve", lambda e: e.tensor_scalar(out=IMP2[:], in0=IMP[:], scalar1=m8b[:, 7:8], scalar2=None, op0=ALU.is_ge), r=["IMP", "m8b"], w=["IMP2"])
            P.op("dve", lambda e: e.tensor_scalar(out=selb[:], in0=IMP2[:], scalar1=-1.0, scalar2=-NEGB, op0=ALU.add, op1=ALU.mult), r=["IMP2"], w=["selb"])
            P.op("pe", lambda e: e.transpose(ps_tr[:, 0:128], selb[:, :], ident[:]), r=["selb", "ident"], w=["ps_tr"])
            for h in range(4):
                P.op("act", lambda e, h=h: e.activation(out=SELT4[:, h * 128:(h + 1) * 128], in_=ps_tr[:, 0:128], func=AF.Copy), r=["ps_tr"], w=["SELT4"])
            def mm_sel(kt, sc):
                mms = [(sc[:, 0:512], KS[0:68, kt * 128:(kt + 1) * 128], QN[0:68, cols], ["KS", "KSaug", "QN", "QNaug"]),
                       (sc[:, 0:512], EN[:, kt * 128:(kt + 1) * 128], SELT4[:, :], ["EN", "SELT4"])]
                if kt == 2 * i:
                    mms.append((sc[:, 0:512], ident[:], M4[:, 0, :], ["ident", "M4"]))
                if kt == 2 * i + 1:
                    mms.append((sc[:, 0:512], ident[:], M4[:, 1, :], ["ident", "M4"]))
                return mms
            attend(mm_sel, list(range(2 * i + 2)), VS, ["VS", "VSone"], 65, None)
            finish(i, 1, OA, oak, False)
            def mm_win(kt, sc):
                mms = [(sc[:, 0:512], KW[0:68, kt * 128:(kt + 1) * 128], QN[0:68, cols], ["KW", "KWaug", "QN", "QNaug"])]
                off = kt - 2 * i
                mi = {-4: 2, -3: 3, 0: 4, 1: 5}.get(off)
                if mi is not None:
                    mms.append((sc[:, 0:512], ident[:], M4[:, mi, :], ["ident", "M4"]))
                return mms
            attend(mm_win, [kt for kt in range(2 * i - 4, 2 * i + 2) if kt >= 0], VW, ["VW", "VWone"], 65, None)
            finish(i, 2, OA, oak, False)
            P.dma("sp", o[i * 128:(i + 1) * 128, :], OA[:].rearrange("p h d -> p (h d)"), r=[oak])
        print("B1", P.emit())
    return nc


def consts_B1(parity, slopes):
    k = np.arange(128)
    tri = np.where(k[None, :] >= k[:, None], 0.0, NEGB).astype(np.float32)
    tri2 = np.where(k[:, None] > k[None, :], 0.0, NEGB).astype(np.float32)
    opn = np.zeros((128, 128), np.float32); cls = np.full((128, 128), NEGB, np.float32)
    if parity == 0:
        masks = np.stack([tri, cls, tri2, opn, tri, cls])
    else:
        masks = np.stack([opn, tri, cls, tri2, opn, tri])
    n_ = np.arange(128)
    cmt = np.zeros((32, 128, 512), np.float32); addt = np.zeros((32, 128, 128), np.float32)
    j = np.arange(128)
    for i in range(32):
        qt = 2 * i + parity
        t = qt * 128 + np.arange(128)
        for nt in range(4):
            cend = 16 * (128 * nt + n_) + 31
            cmt[i, :, nt * 128:(nt + 1) * 128] = np.where(cend[:, None] <= t[None, :], 0.0, NEGB)
        cur = t // 64
        forced = (j[None, :] == 0) | (j[None, :] == cur[:, None]) | (j[None, :] == cur[:, None] - 1)
        addt[i] = np.where(j[None, :] <= cur[:, None], np.where(forced, 1e4, 0.0), -1e30)
    cmt[:, 127, 384:512] = NEGB
    enu = (np.arange(S)[None, :] // 64 == j[:, None]).astype(np.float32)
    kaug = np.stack([np.ones(S), np.full(S, 128.0), np.arange(S) % 128, (np.arange(S) // 128) * 128.0]).astype(np.float32)
    n512 = np.arange(512)
    kcaug = np.stack([np.ones(512), np.full(512, 128.0), 16.0 * (n512 % 128), 2048.0 * (n512 // 128), np.full(512, 15.5)]).astype(np.float32)
    qa = np.zeros((5, 32, 4, 128), np.float32)
    qp = np.arange(128.0)
    for i in range(32):
        qt = 2 * i + parity
        for h in range(4):
            Sx = 8.0 * slopes[h]
            qa[0, i, h] = -Sx * qp; qa[1, i, h] = -Sx * qt; qa[2:5, i, h] = Sx
    cs = 16 * np.arange(512); ss = 64 * np.arange(128)
    inc = ((cs[:, None] <= ss[None, :] + 63) & (cs[:, None] + 31 >= ss[None, :])).astype(np.float32)
    inc[511] = 0.0
    return dict(masks=masks, cmt=cmt, addt=addt, enu=enu, kaugc=kaug, kcaugc=kcaug, qaugc=qa.reshape(5, 16384), incid=inc,
                identd=np.eye(128, dtype=np.float32))


def inputs_B1(zb, g, parity, gq, gk, pos, w1, w2):
    slopes = 2.0 ** (-8.0 * np.arange(1, 9) / 8)[4 * g:4 * g + 4]
    d = consts_B1(parity, slopes)
    own = (np.arange(4096) // 128 * 2 + parity) * 128 + np.arange(4096) % 128
    q = zb[own][:, 0:512].reshape(32, 128, 8, 64)[:, :, 4 * g:4 * g + 4]
    d["qnT"] = np.ascontiguousarray(q.transpose(3, 0, 2, 1).reshape(64, 16384))
    d["gates"] = np.ascontiguousarray(zb[own][:, 1280 + 12 * g:1280 + 12 * (g + 1)].reshape(32, 128, 12).transpose(1, 0, 2).reshape(128, 384))
    kc = zb[:, 512 + 64 * g:512 + 64 * (g + 1)]; vc = zb[:, 640 + 64 * g:640 + 64 * (g + 1)]
    kcT = np.zeros((2, 64, 8224), np.float32); kcT[0, :, :S] = kc.T; kcT[1, :, :S] = vc.T
    d["kcT"] = kcT
    d["ksT"] = np.ascontiguousarray(np.stack([zb[:, 768 + 64 * g:768 + 64 * (g + 1)].T, zb[:, 1024 + 64 * g:1024 + 64 * (g + 1)].T]))
    d["vsw"] = np.ascontiguousarray(np.stack([zb[:, 896 + 64 * g:896 + 64 * (g + 1)], zb[:, 1152 + 64 * g:1152 + 64 * (g + 1)]]))
    d["gq"] = np.ascontiguousarray(gq.reshape(64, 1)); d["gk"] = np.ascontiguousarray(gk.reshape(64, 1))
    d["w1r"] = np.ascontiguousarray(w1.reshape(2, 32, 64, 128).transpose(0, 2, 1, 3).reshape(2, 64, 4096))
    d["posT"] = np.ascontiguousarray(pos.transpose(2, 0, 1).reshape(64, 64))
    d["w2r"] = np.ascontiguousarray(np.concatenate([w2[0], w2[1]], axis=1))
    return d

import contextlib
import numpy as np
import concourse.bass as bass

S = 8192
NEGB = -30000.0


def build_B2(nslots=32):
    nq = nslots * 128
    nc = bass.Bass("TRN2", target_bir_lowering=False)
    D_ = lambda n, s: nc.dram_tensor(n, s, F32, kind="ExternalInput").ap()
    qmT = D_("qmT", [4, 64, 4096]); kmT = D_("kmT", [4, 64, S]); vm = D_("vm", [4, S, 64])
    qaugc = D_("qaugc", [4, 4, 4096]); kaugc = D_("kaugc", [4, S])
    gq = D_("gq", [64, 1]); gk = D_("gk", [64, 1])
    emoba = D_("emoba", [32, S])
    maska = D_("maska", [128, 128]); maskb = D_("maskb", [128, 128])
    mneg = D_("mneg", [128, 32 * 32]); mvalid = D_("mvalid", [128, 32 * 32]); mcur = D_("mcur", [128, 32 * 32])
    identd = D_("identd", [128, 128])
    o = nc.dram_tensor("o", [4096, 256], F32, kind="ExternalOutput").ap()
    with contextlib.ExitStack() as st:
        P = Prog(nc, st)
        ident = P.sb([128, 128], BF16, name="ident")
        MA = P.sb([128, 128], BF16, name="MA"); MB = P.sb([128, 128], BF16, name="MB")
        EM = P.sb([32, S], BF16, name="EM")
        KA = P.sb([68, S], BF16, name="KA")
        QA = P.sb([68, 4096], BF16, name="QA")
        VA = P.sb([128, 64, 65], BF16, name="VA")
        MSB = P.sb([32, 4096], BF16, name="MSB")
        MNEG = P.sb([128, 1024], F32, name="MNEG"); MVAL = P.sb([128, 1024], F32, name="MVAL"); MCUR = P.sb([128, 1024], F32, name="MCUR")
        gqs = P.sb([64, 1], F32, name="gqs"); gks = P.sb([64, 1], F32, name="gks")
        stg = [P.sb([64, 2048], F32, name=f"stg{k}") for k in range(2)]
        kmf = P.sb([64, 32], F32, name="kmf")
        kmb = P.sb([64, 32], BF16, name="kmb")
        gm = P.sb([128, 32], F32, name="gm"); m8 = P.sb([128, 8], F32, name="m8")
        al = P.sb([128, 32], F32, name="al"); sbb = P.sb([128, 32], BF16, name="sbb")
        PT = [P.sb([128, 512], BF16, name=f"PT{k}") for k in range(3)]
        rden = P.sb([128, 4], F32, name="rden")
        osb = [P.sb([128, 4, 64], F32, name=f"osb{k}") for k in range(2)]
        ps_sc = [P.ps([128, 512], F32, name=f"ps_sc{k}") for k in range(2)]
        ps_o = [P.ps([128, 512], F32, name=f"ps_o{k}") for k in range(4)]
        ps_tr = P.ps([128, 1024], BF16, name="ps_tr")
        ps_g = P.ps([128, 512], F32, name="ps_g")
        P.dma("pool", ident[:], identd[:, :], w=["ident"])
        P.dma("pool", MA[:], maska[:, :], w=["MA"])
        P.dma("pool", MB[:], maskb[:, :], w=["MB"])
        for c in range(4):
            P.dma("pool", EM[:, c * 2048:(c + 1) * 2048], emoba[:, c * 2048:(c + 1) * 2048], w=["EM"])
            P.dma("pool", KA[64:68, c * 2048:(c + 1) * 2048], kaugc[:, c * 2048:(c + 1) * 2048], w=["KAaug"])
        P.dma("sp", MNEG[:], mneg[:, :], w=["MNEG"])
        P.dma("sp", MVAL[:], mvalid[:, :], w=["MVAL"])
        P.dma("sp", MCUR[:], mcur[:, :], w=["MCUR"])
        P.dma("sp", gqs[:], gq[:, :], w=["gqs"])
        P.dma("sp", gks[:], gk[:, :], w=["gks"])
        P.op("dve", lambda e: e.memset(VA[:, :, 64:65], 1.0), w=["VAone"])
        u = 0
        for h in range(4):
            for c in range(4):
                sg = stg[c % 2]
                P.dma("sp", sg[:], kmT[h, :, c * 2048:(c + 1) * 2048], w=[f"stg{c % 2}"])
                P.op("dve", lambda e, sg=sg: e.tensor_scalar(out=sg[:], in0=sg[:], scalar1=gks[:, 0:1], scalar2=None, op0=ALU.mult),
                     r=[f"stg{c % 2}", "gks"], w=[f"stg{c % 2}"])
                P.op("act", lambda e, sg=sg, c=c: e.activation(out=KA[0:64, c * 2048:(c + 1) * 2048], in_=sg[:], func=AF.Copy),
                     r=[f"stg{c % 2}"], w=["KA"])
                P.op("dve", lambda e, sg=sg, c=c: e.tensor_reduce(out=kmf[:, c * 8:(c + 1) * 8], in_=sg[:].rearrange("p (b k) -> p b k", k=256),
                                                                 axis=AX.X, op=ALU.add), r=[f"stg{c % 2}"], w=["kmf"])
            P.op("dve", lambda e: e.tensor_scalar(out=kmb[:], in0=kmf[:], scalar1=1.0 / 256, scalar2=None, op0=ALU.mult), r=["kmf"], w=["kmb"])
            for c in range((nq + 2047) // 2048):
                w_ = min(2048, nq - c * 2048)
                sg = stg[c % 2]
                P.dma("sp", sg[:, 0:w_], qmT[h, :, c * 2048:c * 2048 + w_], w=[f"stg{c % 2}"])
                P.op("dve", lambda e, sg=sg, c=c, w_=w_: e.tensor_scalar(out=QA[0:64, c * 2048:c * 2048 + w_], in0=sg[:, 0:w_], scalar1=gqs[:, 0:1],
                                                                       scalar2=None, op0=ALU.mult), r=[f"stg{c % 2}", "gqs"], w=["QA"])
            P.dma("pool", QA[64:68, 0:nq], qaugc[h, :, 0:nq], w=["QAaug"])
            for c in range(4):
                P.dma("pool", VA[:, c * 16:(c + 1) * 16, 0:64], vm[h, c * 2048:(c + 1) * 2048, :].rearrange("(kt p) d -> p kt d", p=128), w=["VA"])
            for i in range(nslots):
                P.op("pe", lambda e, i=i: e.matmul(ps_g[:, 0:32], lhsT=QA[0:64, i * 128:(i + 1) * 128], rhs=kmb[:, :], start=True, stop=True),
                     r=["QA", "kmb"], w=["ps_g"])
                P.op("dve", lambda e, i=i: e.tensor_tensor(out=gm[:], in0=ps_g[:, 0:32], in1=MNEG[:, i * 32:(i + 1) * 32], op=ALU.add),
                     r=["ps_g", "MNEG"], w=["gm"])
                P.op("dve", lambda e: e.max(out=m8[:], in_=gm[:]), r=["gm"], w=["m8"])
                P.op("dve", lambda e: e.tensor_scalar(out=al[:], in0=gm[:], scalar1=m8[:, 2:3], scalar2=None, op0=ALU.is_ge), r=["gm", "m8"], w=["al"])
                P.op("dve", lambda e, i=i: e.tensor_tensor(out=al[:], in0=al[:], in1=MVAL[:, i * 32:(i + 1) * 32], op=ALU.mult), r=["al", "MVAL"], w=["al"])
                P.op("dve", lambda e, i=i: e.tensor_tensor(out=al[:], in0=al[:], in1=MCUR[:, i * 32:(i + 1) * 32], op=ALU.add), r=["al", "MCUR"], w=["al"])
                P.op("dve", lambda e: e.tensor_scalar(out=sbb[:], in0=al[:], scalar1=-1.0, scalar2=-NEGB, op0=ALU.add, op1=ALU.mult), r=["al"], w=["sbb"])
                P.op("pe", lambda e: e.transpose(ps_tr[0:32, 0:128], sbb[:, 0:32], ident[:]), r=["sbb", "ident"], w=["ps_tr"])
                P.op("act", lambda e, i=i: e.activation(out=MSB[:, i * 128:(i + 1) * 128], in_=ps_tr[0:32, 0:128], func=AF.Copy), r=["ps_tr"], w=["MSB"])
            for gi in range(nslots // 4):
                i0 = 4 * gi
                cols = slice(i0 * 128, (i0 + 4) * 128)
                nkt = 2 * (i0 + 3) + 2
                for kt in range(nkt):
                    sc = ps_sc[u % 2]; sck = f"ps_sc{u % 2}"
                    pt = PT[u % 3]; ptk = f"PT{u % 3}"
                    mms = [(sc[:, 0:512], KA[0:68, kt * 128:(kt + 1) * 128], QA[0:68, cols], ["KA", "KAaug", "QA", "QAaug"]),
                           (sc[:, 0:512], EM[0:32, kt * 128:(kt + 1) * 128], MSB[0:32, cols], ["EM", "MSB"])]
                    for j in range(4):
                        if kt == 2 * (i0 + j):
                            mms.append((sc[:, j * 128:(j + 1) * 128], ident[:], MA[:], ["ident", "MA"]))
                        if kt == 2 * (i0 + j) + 1:
                            mms.append((sc[:, j * 128:(j + 1) * 128], ident[:], MB[:], ["ident", "MB"]))
                    for mi, (o_, l_, r_, rk) in enumerate(mms):
                        P.op("pe", lambda e, o_=o_, l_=l_, r_=r_, mi=mi, n=len(mms): e.matmul(o_, lhsT=l_, rhs=r_, start=(mi == 0), stop=(mi == n - 1)),
                             r=rk, w=[sck])
                    P.op("act", lambda e, pt=pt, sc=sc: e.activation(out=pt[:], in_=sc[:, 0:512], func=AF.Exp, scale=0.125), r=[sck], w=[ptk])
                    for j in range(4):
                        last = 2 * (i0 + j) + 1
                        if kt <= last:
                            P.op("pe", lambda e, pt=pt, j=j, kt=kt, last=last: e.matmul(ps_o[j][:, 0:65], lhsT=pt[:, j * 128:(j + 1) * 128],
                                                                                      rhs=VA[:, kt, 0:65], start=(kt == 0), stop=(kt == last)),
                                 r=[ptk, "VA", "VAone"], w=[f"ps_o{j}"])
                    u += 1
                ob = osb[gi % 2]; obk = f"osb{gi % 2}"
                for j in range(4):
                    P.op("dve", lambda e, j=j: e.tensor_scalar(out=rden[:, j:j + 1], in0=ps_o[j][:, 64:65], scalar1=1e-30, scalar2=None, op0=ALU.max),
                         r=[f"ps_o{j}"], w=["rden"])
                P.op("dve", lambda e: e.reciprocal(out=rden[:], in_=rden[:]), r=["rden"], w=["rden"])
                for j in range(4):
                    P.op("dve", lambda e, j=j, ob=ob: e.tensor_scalar(out=ob[:, j, :], in0=ps_o[j][:, 0:64], scalar1=rden[:, j:j + 1], scalar2=None, op0=ALU.mult),
                         r=[f"ps_o{j}", "rden"], w=[obk])
                for j in range(4):
                    P.dma("sp", o[(i0 + j) * 128:(i0 + j + 1) * 128, h * 64:(h + 1) * 64], ob[:, j, :], r=[obk])
        print("B2", P.emit())
    return nc


def consts_B2(parity):
    k = np.arange(128)
    tri = np.where(k[None, :] >= k[:, None], 0.0, NEGB).astype(np.float32)
    opn = np.zeros((128, 128), np.float32); cls = np.full((128, 128), NEGB, np.float32)
    maska, maskb = (tri, cls) if parity == 0 else (opn, tri)
    j = np.arange(32)
    mneg = np.zeros((128, 32, 32), np.float32); mval = np.zeros((128, 32, 32), np.float32); mcur = np.zeros((128, 32, 32), np.float32)
    for i in range(32):
        cb = i
        mneg[:, i, :] = np.where(j < cb, 0.0, -1e30)[None, :]
        mval[:, i, :] = (j < cb)[None, :]
        mcur[:, i, :] = (j == cb)[None, :]
    emoba = (np.arange(S)[None, :] // 256 == j[:, None]).astype(np.float32)
    kaug = np.stack([np.ones(S), np.full(S, 128.0), np.arange(S) % 128, (np.arange(S) // 128) * 128.0]).astype(np.float32)
    return dict(maska=maska, maskb=maskb, mneg=mneg.reshape(128, 1024), mvalid=mval.reshape(128, 1024), mcur=mcur.reshape(128, 1024),
                emoba=emoba, kaugc=kaug, identd=np.eye(128, dtype=np.float32))


def qaug_rows(slopes, parity, extra=0):
    i = np.arange(4096) // 128
    qp = (np.arange(4096) % 128).astype(np.float64)
    qt = 2 * i + parity
    out = []
    for s in slopes:
        Sx = 8.0 * s
        rows = [-Sx * qp, -Sx * qt, np.full(4096, Sx), np.full(4096, Sx)] + [np.full(4096, Sx)] * extra
        out.append(np.stack(rows))
    return np.stack(out).astype(np.float32)


def alibi(n):
    return 2.0 ** (-8.0 * np.arange(1, n + 1) / n)


def inputs_B2(zb, g, parity, gq, gk):
    own = (np.arange(4096) // 128 * 2 + parity) * 128 + np.arange(4096) % 128
    hs = [4 * g + a for a in range(4)]
    qm = zb[:, 1304:1816].reshape(S, 8, 64); km = zb[:, 1816:2328].reshape(S, 8, 64); vmm = zb[:, 2328:2840].reshape(S, 8, 64)
    d = consts_B2(parity)
    d.update(qmT=np.ascontiguousarray(qm[own][:, hs].transpose(1, 2, 0)),
             kmT=np.ascontiguousarray(km[:, hs].transpose(1, 2, 0)),
             vm=np.ascontiguousarray(vmm[:, hs].transpose(1, 0, 2)),
             qaugc=qaug_rows(alibi(8)[hs], parity),
             gq=np.ascontiguousarray(gq.reshape(64, 1)), gk=np.ascontiguousarray(gk.reshape(64, 1)))
    return d

import contextlib
import numpy as np
import concourse.bass as bass

NTC = 16
NH = 8
NE = 32


def build_C(ne=NE):
    nc = bass.Bass("TRN2", target_bir_lowering=False)
    D_ = lambda n, s: nc.dram_tensor(n, s, F32, kind="ExternalInput").ap()
    x = D_("x", [2048, D]); o = D_("o", [2048, D])
    cT = D_("cT", [128, 8]); adaw = D_("adaw", [D, 4096]); adab = D_("adab", [1, 4096])
    ln2 = D_("ln2", [128, D]); wout = D_("wout", [D, D])
    rw = D_("rw", [D, 32]); rb = D_("rb", [128, 32])
    w1 = D_("w1", [NE, D, 2048]); b1T = D_("b1T", [128, NE * 16]); w2 = D_("w2", [NE, D, D]); b2 = D_("b2", [NE, D])
    identd = D_("identd", [128, 128])
    y = nc.dram_tensor("y", [2048, D], F32, kind="ExternalOutput").ap()
    with contextlib.ExitStack() as st:
        P = Prog(nc, st)
        ident = P.sb([128, 128], BF16, name="ident")
        onesf = P.sb([128, 128], F32, name="onesf")
        condT = P.sb([128, 8], F32, name="condT")
        condrep = P.sb([128, 8, 128], F32, name="condrep")
        adabs = P.sb([1, 256], F32, name="adabs")
        MODB = P.sb([128, 4096], F32, name="MODB")
        A2B = P.sb([128, D], F32, name="A2B")
        rwb = P.sb([128, 8, 32], BF16, name="rwb")
        rbs = P.sb([128, 32], F32, name="rbs")
        b1s = P.sb([128, NE * 16], F32, name="b1s")
        b2b = P.sb([32, D], BF16, name="b2b")
        h2T = P.sb([128, 8, 1024], BF16, name="h2T")
        acc = P.sb([128, NH, D], F32, name="acc")
        GATE = P.sb([128, NH, 32], F32, name="GATE")
        w1ring = [P.sb([128, 8, 512], BF16, name=f"w1r{k}") for k in range(3)]
        w2buf = [P.sb([128, 8, D], BF16, name=f"w2b{k}") for k in range(2)]
        woutb = w2buf[0]
        AT = P.sb([128, 8, 1024], BF16, name="AT")
        rr = 0
        ps = [P.ps([128, 512], F32, name=f"ps{k}") for k in range(6)]
        ps_tr = [P.ps([128, 1024], BF16, name=f"ps_tr{k}") for k in range(2)]
        P.dma("pool", ident[:], identd[:, :], w=["ident"])
        P.op("pool", lambda e: e.memset(onesf[:], 1.0), w=["onesf"])
        P.dma("sp", condT[:], cT[:, :], w=["condT"])
        P.dma("sp", A2B[:], ln2[:, :], w=["A2B"])
        P.dma("sp", rbs[:], rb[:, :], w=["rbs"])
        P.dma("sp", b1s[:], b1T[:, :], w=["b1s"])
        P.dma("pool", b2b[:], b2[:, :], w=["b2b"])
        for c in range(8):
            P.dma("pool", rwb[:, c, :], rw[c * 128:(c + 1) * 128, :], w=["rwb"])
        P.op("act", lambda e: e.activation(out=condT[:], in_=condT[:], func=AF.Silu), r=["condT"], w=["condT"])
        for c in range(8):
            P.op("act", lambda e, c=c: e.activation(out=condrep[:, c, :], in_=onesf[:], func=AF.Identity, scale=condT[:, c:c + 1]),
                 r=["condT", "onesf"], w=["condrep"])
        b1v = b1s[:].rearrange("p (e k) -> p e k", k=16)
        P.op("dve", lambda e: e.tensor_scalar(out=b1v[:, :, 8:16], in0=b1v[:, :, 8:16], scalar1=1.0, scalar2=None, op0=ALU.add), r=["b1s"], w=["b1s"])
        awt = [P.sb([128, 8, 256], F32, name=f"awt{k}") for k in range(2)]
        for n in range(16):
            aw = awt[n % 2]; awk = f"awt{n % 2}"
            for i in range(8):
                P.dma("sp", aw[:, i, :], adaw[i * 128:(i + 1) * 128, n * 256:(n + 1) * 256], w=[awk])
            P.dma("sp", adabs[:], adab[:, n * 256:(n + 1) * 256], w=["adabs"])
            pm = ps[n % 2]; pmk = f"ps{n % 2}"
            for i in range(8):
                P.op("pe", lambda e, pm=pm, aw=aw, i=i: e.matmul(pm[:, 0:256], lhsT=condrep[:, i, :], rhs=aw[:, i, :], start=(i == 0), stop=False),
                     r=["condrep", awk], w=[pmk])
            P.op("pe", lambda e, pm=pm: e.matmul(pm[:, 0:256], lhsT=onesf[0:1, :], rhs=adabs[0:1, :], start=False, stop=True),
                 r=["onesf", "adabs"], w=[pmk])
            P.op("act", lambda e, pm=pm, n=n: e.activation(out=MODB[:, n * 256:(n + 1) * 256], in_=pm[:, 0:256], func=AF.Copy), r=[pmk], w=["MODB"])
        P.op("dve", lambda e: e.scalar_tensor_tensor(out=A2B[:], in0=MODB[:, 2048:3072], scalar=1.0, in1=A2B[:], op0=ALU.add, op1=ALU.mult),
             r=["MODB", "A2B"], w=["A2B"])
        ot = [P.sb([128, D], BF16, name=f"ot{k}") for k in range(2)]
        oT = [P.sb([128, 8, 128], BF16, name=f"oT{k}") for k in range(2)]
        xt = [P.sb([128, D], F32, name=f"xt{k}") for k in range(2)]
        x1 = [P.sb([128, D], F32, name=f"x1{k}") for k in range(2)]
        hb = [P.sb([128, D], BF16, name=f"hb{k}") for k in range(2)]
        sq = P.sb([128, D], F32, name="sq")
        ssv = P.sb([128, 1], F32, name="ssv")
        lg = P.sb([128, 32], F32, name="lg"); m8 = P.sb([128, 8], F32, name="m8"); msk = P.sb([128, 32], F32, name="msk")
        nmx = P.sb([128, 1], F32, name="nmx"); ex = P.sb([128, 32], F32, name="ex"); den = P.sb([128, 1], F32, name="den")
        gb = P.sb([128, 32], BF16, name="gb"); gT = P.sb([32, 128], BF16, name="gT")
        gcl = P.sb([128, 512], F32, name="gcl"); sgl = P.sb([128, 512], F32, name="sgl"); lcl = P.sb([128, 512], F32, name="lcl")
        for half in range(2):
            for c in range(8):
                P.dma("pool", woutb[:, c, :], wout[c * 128:(c + 1) * 128, :], w=["w2b0"])
            for tl in range(NH):
                t = half * NH + tl
                k = t % 2
                P.dma("pool", ot[k][:], o[t * 128:(t + 1) * 128, :], w=[f"ot{k}"])
                P.dma("sp", xt[k][:], x[t * 128:(t + 1) * 128, :], w=[f"xt{k}"])
                for c in range(8):
                    P.op("pe", lambda e, k=k, c=c: e.transpose(ps_tr[k][:, c * 128:(c + 1) * 128], ot[k][:, c * 128:(c + 1) * 128], ident[:]),
                         r=[f"ot{k}", "ident"], w=[f"ps_tr{k}"])
                P.op("act", lambda e, k=k: e.activation(out=oT[k][:].rearrange("p c t -> p (c t)"), in_=ps_tr[k][:, :], func=AF.Copy),
                     r=[f"ps_tr{k}"], w=[f"oT{k}"])
                for n in range(2):
                    pm = ps[2 + n]; pmk = f"ps{2 + n}"
                    for c in range(8):
                        P.op("pe", lambda e, pm=pm, k=k, c=c, n=n: e.matmul(pm[:, 0:512], lhsT=oT[k][:, c, :], rhs=woutb[:, c, n * 512:(n + 1) * 512],
                                                                        start=(c == 0), stop=(c == 7)), r=[f"oT{k}", "w2b0"], w=[pmk])
                    P.op("dve", lambda e, pm=pm, k=k, n=n: e.tensor_tensor(out=x1[k][:, n * 512:(n + 1) * 512], in0=pm[:, 0:512], in1=MODB[:, n * 512:(n + 1) * 512],
                                                                          op=ALU.mult), r=[pmk, "MODB"], w=[f"x1{k}"])
                P.op("dve", lambda e, k=k: e.tensor_tensor(out=x1[k][:], in0=x1[k][:], in1=xt[k][:], op=ALU.add), r=[f"x1{k}", f"xt{k}"], w=[f"x1{k}"])
                P.dma("sp", y[t * 128:(t + 1) * 128, :], x1[k][:], r=[f"x1{k}"], w=[f"y{t}"])
                P.op("act", lambda e, k=k: e.activation(out=sq[:], in_=x1[k][:], func=AF.Square, accum_out=ssv[:, 0:1]), r=[f"x1{k}"], w=["sq", "ssv"])
                P.op("dve", lambda e: e.tensor_scalar(out=ssv[:], in0=ssv[:], scalar1=1.0 / D, scalar2=EPS, op0=ALU.mult, op1=ALU.add), r=["ssv"], w=["ssv"])
                P.op("act", lambda e: e.activation(out=ssv[:], in_=ssv[:], func=AF.Sqrt), r=["ssv"], w=["ssv"])
                P.op("dve", lambda e: e.reciprocal(out=ssv[:], in_=ssv[:]), r=["ssv"], w=["ssv"])
                P.op("dve", lambda e, k=k: e.scalar_tensor_tensor(out=sq[:], in0=x1[k][:], scalar=ssv[:, 0:1], in1=A2B[:], op0=ALU.mult, op1=ALU.mult),
                     r=[f"x1{k}", "ssv", "A2B"], w=["sq"])
                P.op("dve", lambda e, k=k: e.tensor_tensor(out=hb[k][:], in0=sq[:], in1=MODB[:, 1024:2048], op=ALU.add), r=["sq", "MODB"], w=[f"hb{k}"])
                for c in range(8):
                    P.op("pe", lambda e, k=k, c=c: e.transpose(ps_tr[k][:, c * 128:(c + 1) * 128], hb[k][:, c * 128:(c + 1) * 128], ident[:]),
                         r=[f"hb{k}", "ident"], w=[f"ps_tr{k}"])
                P.op("act", lambda e, k=k, t=t, tl=tl: e.activation(out=h2T[:, :, tl * 128:(tl + 1) * 128], in_=ps_tr[k][:, :].rearrange("p (c t) -> p c t", t=128),
                                                          func=AF.Copy), r=[f"ps_tr{k}"], w=["h2T"])
                pm = ps[4]; pmk = "ps4"
                for c in range(8):
                    P.op("pe", lambda e, pm=pm, c=c, t=t, tl=tl: e.matmul(pm[:, 0:32], lhsT=h2T[:, c, tl * 128:(tl + 1) * 128], rhs=rwb[:, c, :], start=(c == 0), stop=(c == 7)),
                         r=["h2T", "rwb"], w=[pmk])
                P.op("dve", lambda e, pm=pm: e.tensor_tensor(out=lg[:], in0=pm[:, 0:32], in1=rbs[:], op=ALU.add), r=[pmk, "rbs"], w=["lg"])
                P.op("dve", lambda e: e.max(out=m8[:], in_=lg[:]), r=["lg"], w=["m8"])
                P.op("dve", lambda e: e.tensor_scalar(out=msk[:], in0=lg[:], scalar1=m8[:, 3:4], scalar2=None, op0=ALU.is_ge), r=["lg", "m8"], w=["msk"])
                P.op("dve", lambda e: e.tensor_scalar(out=nmx[:], in0=m8[:, 0:1], scalar1=-1.0, scalar2=None, op0=ALU.mult), r=["m8"], w=["nmx"])
                P.op("act", lambda e: e.activation(out=ex[:], in_=lg[:], func=AF.Exp, bias=nmx[:, 0:1], scale=1.0), r=["lg", "nmx"], w=["ex"])
                P.op("dve", lambda e: e.tensor_tensor(out=ex[:], in0=ex[:], in1=msk[:], op=ALU.mult), r=["ex", "msk"], w=["ex"])
                P.op("dve", lambda e: e.tensor_reduce(out=den[:], in_=ex[:], axis=AX.X, op=ALU.add), r=["ex"], w=["den"])
                P.op("dve", lambda e: e.reciprocal(out=den[:], in_=den[:]), r=["den"], w=["den"])
                P.op("dve", lambda e, t=t, tl=tl: e.tensor_scalar(out=GATE[:, tl, :], in0=ex[:], scalar1=den[:, 0:1], scalar2=None, op0=ALU.mult), r=["ex", "den"], w=["GATE"])
                P.op("dve", lambda e, t=t, tl=tl: e.tensor_copy(out=gb[:], in_=GATE[:, tl, :]), r=["GATE"], w=["gb"])
                P.op("pe", lambda e, k=k: e.transpose(ps_tr[k][0:32, 0:128], gb[:, 0:32], ident[:]), r=["gb", "ident", "h2T"], w=[f"ps_tr{k}"])
                P.op("act", lambda e, k=k: e.activation(out=gT[:], in_=ps_tr[k][0:32, 0:128], func=AF.Copy), r=[f"ps_tr{k}"], w=["gT"])
                for n in range(2):
                    pm = ps[2 + n]; pmk = f"ps{2 + n}"
                    P.op("pe", lambda e, pm=pm, n=n: e.matmul(pm[:, 0:512], lhsT=gT[:, :], rhs=b2b[:, n * 512:(n + 1) * 512], start=True, stop=True),
                         r=["gT", "b2b"], w=[pmk])
                    P.op("act", lambda e, pm=pm, n=n, t=t, tl=tl: e.activation(out=acc[:, tl, n * 512:(n + 1) * 512], in_=pm[:, 0:512], func=AF.Copy), r=[pmk], w=[f"acc{tl}"])
            u = 0
            for ex_ in range(ne):
                w2t = w2buf[ex_ % 2]; w2k = f"w2b{ex_ % 2}"
                for c in range(8):
                    P.dma("pool", w2t[:, c, :], w2[ex_, c * 128:(c + 1) * 128, :], w=[w2k])
                for kp in range(4):
                    ring = w1ring[rr % 3]; rk = f"w1r{rr % 3}"
                    rr += 1
                    P.dma("pool", ring[:, :, 0:256], w1[ex_, :, kp * 256:(kp + 1) * 256].rearrange("(c p) n -> p c n", p=128), w=[rk])
                    P.dma("pool", ring[:, :, 256:512], w1[ex_, :, 1024 + kp * 256:1024 + (kp + 1) * 256].rearrange("(c p) n -> p c n", p=128), w=[rk])
                    for j in range(2):
                        kk = kp * 2 + j
                        for grp in range(2):
                            tk = slice(grp * 512, (grp + 1) * 512)
                            pg = ps[u % 2]; pgk = f"ps{u % 2}"; pl = ps[2 + u % 2]; plk = f"ps{2 + u % 2}"
                            u += 1
                            for c in range(8):
                                P.op("pe", lambda e, pg=pg, c=c, j=j, tk=tk, ring=ring: e.matmul(pg[:, 0:512], lhsT=ring[:, c, j * 128:(j + 1) * 128], rhs=h2T[:, c, tk],
                                                                                             start=(c == 0), stop=(c == 7)), r=[rk, "h2T"], w=[pgk])
                            for c in range(8):
                                P.op("pe", lambda e, pl=pl, c=c, j=j, tk=tk, ring=ring: e.matmul(pl[:, 0:512], lhsT=ring[:, c, 256 + j * 128:256 + (j + 1) * 128], rhs=h2T[:, c, tk],
                                                                                             start=(c == 0), stop=(c == 7)), r=[rk, "h2T"], w=[plk])
                            bg = b1s[:, ex_ * 16 + kk:ex_ * 16 + kk + 1]; bl = b1s[:, ex_ * 16 + 8 + kk:ex_ * 16 + 8 + kk + 1]
                            P.op("dve", lambda e, pg=pg, bg=bg: e.tensor_scalar(out=gcl[:], in0=pg[:, 0:512], scalar1=bg, scalar2=7.0, op0=ALU.add, op1=ALU.min),
                                 r=[pgk, "b1s"], w=["gcl"])
                            P.op("act", lambda e: e.activation(out=sgl[:], in_=gcl[:], func=AF.Sigmoid, scale=1.702), r=["gcl"], w=["sgl"])
                            P.op("dve", lambda e, pl=pl, bl=bl: e.tensor_scalar(out=lcl[:], in0=pl[:, 0:512], scalar1=bl, scalar2=-6.0, op0=ALU.add, op1=ALU.max),
                                 r=[plk, "b1s"], w=["lcl"])
                            P.op("dve", lambda e: e.scalar_tensor_tensor(out=lcl[:], in0=lcl[:], scalar=8.0, in1=gcl[:], op0=ALU.min, op1=ALU.mult),
                                 r=["lcl", "gcl"], w=["lcl"])
                            P.op("dve", lambda e, kk=kk, tk=tk: e.tensor_tensor(out=AT[:, kk, tk], in0=lcl[:], in1=sgl[:], op=ALU.mult), r=["lcl", "sgl"], w=[f"AT{kk}_{grp}"])
                for tl in range(NH):
                    t = half * NH + tl
                    for n in range(2):
                        py = ps[4 + n]; pyk = f"ps{4 + n}"
                        for kk in range(8):
                            P.op("pe", lambda e, py=py, kk=kk, tl=tl, n=n, w2t=w2t: e.matmul(py[:, 0:512], lhsT=AT[:, kk, tl * 128:(tl + 1) * 128],
                                                                                         rhs=w2t[:, kk, n * 512:(n + 1) * 512], start=(kk == 0), stop=(kk == 7)),
                                 r=[f"AT{kk}_{tl // 4}", w2k], w=[pyk])
                        P.op("dve", lambda e, py=py, t=t, tl=tl, n=n, ex_=ex_: e.scalar_tensor_tensor(out=acc[:, tl, n * 512:(n + 1) * 512], in0=py[:, 0:512],
                                                                                            scalar=GATE[:, tl, ex_:ex_ + 1], in1=acc[:, tl, n * 512:(n + 1) * 512],
                                                                                            op0=ALU.mult, op1=ALU.add), r=[pyk, "GATE", f"acc{tl}"], w=[f"acc{tl}"])
            for tl in range(NH):
                t = half * NH + tl
                k = t % 2
                P.dma("sp", xt[k][:], y[t * 128:(t + 1) * 128, :], r=[f"y{t}"], w=[f"xt{k}"])
                P.op("dve", lambda e, t=t, tl=tl: e.tensor_tensor(out=acc[:, tl, :], in0=acc[:, tl, :], in1=MODB[:, 3072:4096], op=ALU.mult), r=[f"acc{tl}", "MODB"], w=[f"acc{tl}"])
                P.op("dve", lambda e, t=t, tl=tl, k=k: e.tensor_tensor(out=acc[:, tl, :], in0=acc[:, tl, :], in1=xt[k][:], op=ALU.add), r=[f"acc{tl}", f"xt{k}"], w=[f"acc{tl}"])
                P.dma("sp", y[t * 128:(t + 1) * 128, :], acc[:, tl, :], r=[f"acc{tl}"], w=[f"y{t}"])
        print("C", P.emit())
    return nc


def inputs_C(inp, l, core, xcur, ocat):
    b, r = core // 4, core % 4
    sl = slice(r * 2048, (r + 1) * 2048)
    return dict(
        x=np.ascontiguousarray(xcur[b, sl]), o=np.ascontiguousarray(ocat[b, sl]),
        cT=np.ascontiguousarray(inp["c"][b].reshape(8, 128).T),
        adaw=np.ascontiguousarray(inp["ada_w"][l][:, 2048:6144]),
        adab=np.ascontiguousarray(inp["ada_b"][l][2048:6144].reshape(1, 4096)),
        ln2=np.ascontiguousarray(np.broadcast_to(inp["ln2_g"][l][None, :], (128, D))),
        wout=np.ascontiguousarray(inp["w_out"][l]),
        rw=np.ascontiguousarray(inp["router_w"][l]),
        rb=np.ascontiguousarray(np.broadcast_to(inp["router_b"][l][None, :], (128, 32))),
        w1=np.ascontiguousarray(inp["exp_w1"][l]),
        b1T=np.ascontiguousarray(inp["exp_b1"][l].reshape(NE, 16, 128).transpose(2, 0, 1).reshape(128, NE * 16)),
        w2=np.ascontiguousarray(inp["exp_w2"][l]),
        b2=np.ascontiguousarray(inp["exp_b2"][l]),
        identd=np.eye(128, dtype=np.float32),
    )


_CACHE = {}


def _prog(name, fn):
    if name not in _CACHE:
        _CACHE[name] = fn()
    return _CACHE[name]


def kernel(**inputs):
    inp = {k: np.asarray(v) for k, v in inputs.items()}
    x_cur = np.ascontiguousarray(inp["x"], dtype=np.float32)
    cores = list(range(8))
    for l in range(2):
        inp["x_cur"] = x_cur
        resA = run_bass_kernel_spmd(_prog("A", build_A), [inputs_A(inp, l, c) for c in cores], core_ids=cores)
        z = np.stack([np.concatenate([resA.results[b * 4 + r]["z"] for r in range(4)], axis=0) for b in range(2)])
        ocat = np.zeros((2, 8192, 1024), np.float32)
        cfg = [(c // 4, (c // 2) % 2, c % 2) for c in cores]
        maps = [inputs_B1(z[b], g, p, inp["nsa_q_gain"][l], inp["nsa_k_gain"][l], inp["nsa_cmp_pos"][l],
                          inp["nsa_cmp_w1"][l], inp["nsa_cmp_w2"][l]) for (b, g, p) in cfg]
        resB1 = run_bass_kernel_spmd(_prog("B1", build_B1), maps, core_ids=cores)
        maps = [inputs_B2(z[b], g, p, inp["moba_q_gain"][l], inp["moba_k_gain"][l]) for (b, g, p) in cfg]
        resB2 = run_bass_kernel_spmd(_prog("B2", build_B2), maps, core_ids=cores)
        for c, (b, g, p) in enumerate(cfg):
            own = (np.arange(4096) // 128 * 2 + p) * 128 + np.arange(4096) % 128
            ocat[b, own, g * 256:(g + 1) * 256] = resB1.results[c]["o"]
            ocat[b, own, 512 + g * 256:512 + (g + 1) * 256] = resB2.results[c]["o"]
        resC = run_bass_kernel_spmd(_prog("C", build_C), [inputs_C(inp, l, c, x_cur, ocat) for c in cores], core_ids=cores)
        x_cur = np.stack([np.concatenate([resC.results[b * 4 + r]["y"] for r in range(4)], axis=0) for b in range(2)]).astype(np.float32)
    return x_cur
```

```python
from concourse.bass_utils import run_bass_kernel_spmd
D = 1024
EPS = 1e-6

import contextlib
import numpy as np
import concourse.bass as bass
import concourse.mybir as mybir

F32 = mybir.dt.float32
BF16 = mybir.dt.bfloat16
I32 = mybir.dt.int32
U32 = mybir.dt.uint32
AF = mybir.ActivationFunctionType
ALU = mybir.AluOpType
AX = mybir.AxisListType

DMA_K = 6
STRICT_SAME = True


class Prog:
    def __init__(self, nc, stack):
        self.nc = nc
        self.stack = stack
        self.ins = []
        self.eng = {"pe": nc.tensor, "act": nc.scalar, "dve": nc.vector, "pool": nc.gpsimd, "sp": nc.sync}
        self.nt = 0

    def sb(self, shape, dt, name=None):
        self.nt += 1
        return self.stack.enter_context(self.nc.sbuf_tensor(name or f"sb{self.nt}", list(shape), dt))

    def ps(self, shape, dt=F32, name=None):
        self.nt += 1
        return self.stack.enter_context(self.nc.psum_tensor(name or f"ps{self.nt}", list(shape), dt))

    def op(self, eng, fn, r=(), w=()):
        self.ins.append(dict(e=eng, fn=fn, r=tuple(r), w=tuple(w), dma=False))

    def dma(self, q, out, in_, r=(), w=(), **kw):
        def fn(e, out=out, in_=in_, kw=kw):
            return e.dma_start(out=out, in_=in_, **kw)
        self.ins.append(dict(e=q, fn=fn, r=tuple(r), w=tuple(w), dma=True))

    def emit(self):
        nc = self.nc
        ins = self.ins
        n = len(ins)
        last_w = {}
        readers = {}
        deps = [None] * n
        for i, I in enumerate(ins):
            d = set()
            for k in I["r"]:
                d.update(last_w.get(k, ()))
            for k in I["w"]:
                for j in last_w.get(k, ()):
                    if not (I["dma"] and ins[j]["dma"]):
                        d.add(j)
                rd = readers.get(k)
                if rd:
                    for v in rd[0].values():
                        d.add(v)
                    d.update(rd[1])
            d.discard(i)
            deps[i] = d
            for k in I["r"]:
                rd = readers.setdefault(k, ({}, []))
                if I["dma"]:
                    rd[1].append(i)
                else:
                    rd[0][I["e"]] = i
            for k in I["w"]:
                if I["dma"]:
                    last_w[k] = [j for j in last_w.get(k, ()) if ins[j]["dma"]] + [i]
                else:
                    last_w[k] = [i]
                readers[k] = ({}, [])
        signal = [False] * n
        fdeps = [None] * n
        for i, I in enumerate(ins):
            keep = []
            for j in deps[i]:
                J = ins[j]
                if J["dma"]:
                    keep.append(j)
                    continue
                if J["e"] == I["e"]:
                    if I["dma"]:
                        keep.append(j); signal[j] = True
                        continue
                    if STRICT_SAME and I["e"] in ("act", "dve", "pool"):
                        if any(k in J["w"] for k in I["r"]):
                            keep.append(j); signal[j] = True
                    continue
                keep.append(j); signal[j] = True
            fdeps[i] = keep
        engs = ["pe", "act", "dve", "pool", "sp"]
        sems = {e: self.stack.enter_context(nc.semaphore(f"s_{e}")) for e in engs}
        dsems = {e: [self.stack.enter_context(nc.semaphore(f"d_{e}{k}")) for k in range(DMA_K)]
                 for e in ("sp", "act", "pool")}
        cnt = {e: 0 for e in engs}
        dcnt = {e: 0 for e in dsems}
        tag = [None] * n
        for i, I in enumerate(ins):
            if I["dma"]:
                q = I["e"]
                idx = dcnt[q]; dcnt[q] += 1
                tag[i] = ("d", q, idx)
            elif signal[i]:
                cnt[I["e"]] += 1
                tag[i] = ("c", I["e"], cnt[I["e"]])
        waited = {e: {} for e in engs}
        nw = 0
        for i, I in enumerate(ins):
            e = I["e"]
            E = self.eng[e]
            wl = {}
            for j in fdeps[i]:
                t = tag[j]
                if t[0] == "d":
                    s = dsems[t[1]][t[2] % DMA_K]; v = 16 * (t[2] // DMA_K + 1)
                else:
                    s = sems[t[1]]; v = t[2]
                key = id(s)
                if key not in wl or wl[key][1] < v:
                    wl[key] = (s, v)
            if I["dma"]:
                t = tag[i]
                if t[2] >= DMA_K:
                    s = dsems[t[1]][t[2] % DMA_K]; v = 16 * (t[2] // DMA_K)
                    key = id(s)
                    if key not in wl or wl[key][1] < v:
                        wl[key] = (s, v)
            for key, (s, v) in wl.items():
                if waited[e].get(key, 0) >= v:
                    continue
                waited[e][key] = v
                E.wait_ge(s, v)
                nw += 1
            inst = I["fn"](E)
            t = tag[i]
            if t is not None:
                if t[0] == "d":
                    inst.then_inc(dsems[t[1]][t[2] % DMA_K], 16)
                else:
                    inst.then_inc(sems[t[1]], 1)
        E = self.eng["sp"]
        for q, c in dcnt.items():
            for k in range(DMA_K):
                m = (c - k + DMA_K - 1) // DMA_K if c > k else 0
                if m > 0 and waited["sp"].get(id(dsems[q][k]), 0) < 16 * m:
                    E.wait_ge(dsems[q][k], 16 * m)
        self.stats = dict(n=n, waits=nw, cnt=dict(cnt), dcnt=dict(dcnt))
        return self.stats

import contextlib
import numpy as np
import concourse.bass as bass
import concourse.mybir as mybir

INW = 2840
NT = 16
NORM_SEGS = [(0, 8), (768, 2), (1024, 2), (1304, 8), (1816, 8)]


def emit_mod(P, nc, adaw_dram, col0, ncols, condT, adabT, out_cols, tagp):
    nj = ncols // 128
    half = 1024
    pm = P.ps([128, 64], F32, name=f"pm_{tagp}")
    for hh in range(ncols // half):
        aw = P.sb([128, 8, half], F32, name=f"aw_{tagp}{hh}")
        for i in range(8):
            P.dma("sp" if i % 2 == 0 else "pool", aw[:, i, :], adaw_dram[i * 128:(i + 1) * 128, col0 + hh * half: col0 + (hh + 1) * half],
                  w=[f"aw_{tagp}{hh}_{i}"])
        for jj in range(half // 128):
            j = hh * (half // 128) + jj
            for i in range(8):
                P.op("pe", lambda e, aw=aw, i=i, jj=jj, j=j: e.matmul(pm[:, j:j + 1], lhsT=aw[:, i, jj * 128:(jj + 1) * 128],
                                                                rhs=condT[:, i:i + 1], start=(i == 0), stop=(i == 7)),
                     r=[f"aw_{tagp}{hh}_{i}", "condT"], w=[f"pm_{tagp}"])
    P.op("dve", lambda e: e.tensor_tensor(out=out_cols[:, 0:nj], in0=pm[:, 0:nj], in1=adabT, op=ALU.add),
         r=[f"pm_{tagp}", "adab"], w=[f"mod_{tagp}"])


class _Stop(Exception):
    pass


def build_A(stop=99):
    nc = bass.Bass("TRN2", target_bir_lowering=False)
    x = nc.dram_tensor("x", [NT * 128, D], F32, kind="ExternalInput").ap()
    cT = nc.dram_tensor("cT", [128, 8], F32, kind="ExternalInput").ap()
    adaw = nc.dram_tensor("adaw", [D, 2048], F32, kind="ExternalInput").ap()
    adabT = nc.dram_tensor("adabT", [128, 16], F32, kind="ExternalInput").ap()
    lngT = nc.dram_tensor("lngT", [128, 8], F32, kind="ExternalInput").ap()
    win = nc.dram_tensor("win", [D, INW], F32, kind="ExternalInput").ap()
    identd = nc.dram_tensor("identd", [128, 128], F32, kind="ExternalInput").ap()
    zo = nc.dram_tensor("z", [NT * 128, INW], F32, kind="ExternalOutput").ap()
    with contextlib.ExitStack() as st:
        P = Prog(nc, st)
        try:
            ident = P.sb([128, 128], BF16, name="ident")
            identf = P.sb([128, 128], F32, name="identf")
            condT = P.sb([128, 8], F32, name="condT")
            adab = P.sb([128, 16], F32, name="adab")
            lng = P.sb([128, 8], F32, name="lng")
            modc = P.sb([128, 16], F32, name="modc")
            Acol = P.sb([128, 8], F32, name="Acol")
            wb = P.sb([128, 8, INW], BF16, name="wb")
            P.dma("sp", identf[:], identd[:, :], w=["identf"])
            P.op("dve", lambda e: e.tensor_copy(out=ident[:], in_=identf[:]), r=["identf"], w=["ident"])
            P.dma("sp", condT[:], cT[:, :], w=["condT"])
            P.dma("sp", adab[:], adabT[:, :], w=["adab"])
            P.dma("sp", lng[:], lngT[:, :], w=["lng"])
            P.op("act", lambda e: e.activation(out=condT[:], in_=condT[:], func=AF.Silu), r=["condT"], w=["condT"])
            emit_mod(P, nc, adaw, 0, 2048, condT, adab[:, 0:16], modc, "a")
            P.op("dve", lambda e: e.scalar_tensor_tensor(out=Acol[:], in0=modc[:, 8:16], scalar=1.0, in1=lng[:], op0=ALU.add, op1=ALU.mult),
                 r=["mod_a", "lng"], w=["Acol"])
            if stop == 1:
                P.dma("sp", zo[0:128, 0:16], modc[:], r=["mod_a"])
                P.dma("sp", zo[0:128, 16:24], Acol[:], r=["Acol"])
                raise _Stop()
            for i in range(8):
                for cc in range(0, INW, 568):
                    P.dma("pool", wb[:, i, cc:cc + 568], win[i * 128:(i + 1) * 128, cc:cc + 568], w=[f"wb{i}"])
            xt = [P.sb([128, D], F32, name=f"xt{k}") for k in range(2)]
            xnb = [P.sb([128, D], BF16, name=f"xnb{k}") for k in range(2)]
            sq = P.sb([128, D], F32, name="sq")
            ss = [P.sb([128, 1], F32, name=f"ss{k}") for k in range(2)]
            hT = [P.sb([128, 8, 128], BF16, name=f"hT{k}") for k in range(2)]
            pT = [P.ps([128, D], BF16, name=f"pT{k}") for k in range(2)]
            pz = [P.ps([128, 512], F32, name=f"pz{k}") for k in range(3)]
            zsb = [P.sb([128, INW], F32, name=f"zsb{k}") for k in range(2)]
            ss2 = P.sb([128, 8], F32, name="ss2")
            groups = [(c0, min(512, INW - c0)) for c0 in range(0, INW, 512)]
            for t in range(NT):
                k = t % 2
                X, XN, SS, HT, PT, Z = xt[k], xnb[k], ss[k], hT[k], pT[k], zsb[k]
                P.dma("sp", X[:], x[t * 128:(t + 1) * 128, :], w=[f"xt{k}"])
                P.op("act", lambda e, X=X, SS=SS: e.activation(out=sq[:], in_=X[:], func=AF.Square, accum_out=SS[:, 0:1]),
                     r=[f"xt{k}"], w=["sq", f"ss{k}"])
                P.op("dve", lambda e, SS=SS: e.tensor_scalar(out=SS[:], in0=SS[:], scalar1=1.0 / D, scalar2=EPS, op0=ALU.mult, op1=ALU.add),
                     r=[f"ss{k}"], w=[f"ss{k}"])
                P.op("act", lambda e, SS=SS: e.activation(out=SS[:], in_=SS[:], func=AF.Sqrt), r=[f"ss{k}"], w=[f"ss{k}"])
                P.op("dve", lambda e, SS=SS: e.reciprocal(out=SS[:], in_=SS[:]), r=[f"ss{k}"], w=[f"ss{k}"])
                P.op("dve", lambda e, X=X, XN=XN, SS=SS: e.tensor_scalar(out=XN[:], in0=X[:], scalar1=SS[:, 0:1], scalar2=None, op0=ALU.mult),
                     r=[f"xt{k}", f"ss{k}"], w=[f"xnb{k}"])
                for c in range(8):
                    P.op("pe", lambda e, PT=PT, XN=XN, c=c: e.transpose(PT[:, c * 128:(c + 1) * 128], XN[:, c * 128:(c + 1) * 128], ident[:]),
                         r=[f"xnb{k}", "ident"], w=[f"pT{k}"])
                for c in range(8):
                    eng = "act"
                    if eng == "act":
                        P.op("act", lambda e, HT=HT, PT=PT, c=c: e.activation(out=HT[:, c, :], in_=PT[:, c * 128:(c + 1) * 128], func=AF.Identity,
                                                                         scale=Acol[:, c:c + 1], bias=modc[:, c:c + 1]),
                             r=[f"pT{k}", "Acol", "mod_a"], w=[f"hT{k}_{c}"])
                    else:
                        P.op("dve", lambda e, HT=HT, PT=PT, c=c: e.tensor_scalar(out=HT[:, c, :], in0=PT[:, c * 128:(c + 1) * 128],
                                                                            scalar1=Acol[:, c:c + 1], scalar2=modc[:, c:c + 1],
                                                                            op0=ALU.mult, op1=ALU.add),
                             r=[f"pT{k}", "Acol", "mod_a"], w=[f"hT{k}_{c}"])
                if stop == 2:
                    hf = P.sb([128, 8, 128], F32, name="hf")
                    P.op("dve", lambda e: e.tensor_copy(out=hf[:], in_=HT[:]), r=[f"hT{k}_{c}" for c in range(8)], w=["hf"])
                    P.dma("sp", zo[0:128, 0:1024], hf[:].rearrange("p c t -> p (c t)"), r=["hf"])
                    raise _Stop()
                for gi, (c0, cw) in enumerate(groups):
                    pzz = pz[gi % 3]
                    for c in range(8):
                        P.op("pe", lambda e, pzz=pzz, HT=HT, c=c, c0=c0, cw=cw: e.matmul(pzz[:, 0:cw], lhsT=HT[:, c, :], rhs=wb[:, c, c0:c0 + cw],
                                                                                   start=(c == 0), stop=(c == 7)),
                             r=[f"hT{k}_{c}", f"wb{c}"], w=[f"pz{gi % 3}"])
                    eng = "act" if gi % 2 == 0 else "dve"
                    if eng == "act":
                        P.op("act", lambda e, pzz=pzz, Z=Z, c0=c0, cw=cw: e.activation(out=Z[:, c0:c0 + cw], in_=pzz[:, 0:cw], func=AF.Copy),
                             r=[f"pz{gi % 3}"], w=[f"zsb{k}_{gi}"])
                    else:
                        P.op("dve", lambda e, pzz=pzz, Z=Z, c0=c0, cw=cw: e.tensor_copy(out=Z[:, c0:c0 + cw], in_=pzz[:, 0:cw]),
                             r=[f"pz{gi % 3}"], w=[f"zsb{k}_{gi}"])
                zkeys = [f"zsb{k}_{gi}" for gi in range(len(groups))]
                if stop == 3:
                    P.dma("sp", zo[t * 128:(t + 1) * 128, :], Z[:], r=zkeys)
                    raise _Stop()
                for (s0, nh) in NORM_SEGS:
                    V = Z[:, s0:s0 + nh * 64]
                    P.op("pool", lambda e, V=V, nh=nh: e.tensor_tensor(out=sq[:, 0:nh * 64], in0=V, in1=V, op=ALU.mult),
                         r=zkeys, w=["sq"])
                    P.op("dve", lambda e, nh=nh: e.tensor_reduce(out=ss2[:, 0:nh], in_=sq[:, 0:nh * 64].rearrange("p (h d) -> p h d", d=64),
                                                               axis=AX.X, op=ALU.add), r=["sq"], w=["ss2"])
                    P.op("dve", lambda e, nh=nh: e.tensor_scalar(out=ss2[:, 0:nh], in0=ss2[:, 0:nh], scalar1=1.0 / 64, scalar2=EPS,
                                                               op0=ALU.mult, op1=ALU.add), r=["ss2"], w=["ss2"])
                    P.op("act", lambda e, nh=nh: e.activation(out=ss2[:, 0:nh], in_=ss2[:, 0:nh], func=AF.Sqrt), r=["ss2"], w=["ss2"])
                    P.op("dve", lambda e, nh=nh: e.reciprocal(out=ss2[:, 0:nh], in_=ss2[:, 0:nh]), r=["ss2"], w=["ss2"])
                    P.op("dve", lambda e, V=V, nh=nh: e.tensor_tensor(out=V.rearrange("p (h d) -> p h d", d=64),
                                                                    in0=V.rearrange("p (h d) -> p h d", d=64),
                                                                    in1=ss2[:, 0:nh].unsqueeze(2).to_broadcast([128, nh, 64]), op=ALU.mult),
                         r=zkeys + ["ss2"], w=zkeys)
                P.op("act", lambda e, Z=Z: e.activation(out=Z[:, 1280:1304], in_=Z[:, 1280:1304], func=AF.Sigmoid), r=zkeys, w=zkeys)
                P.dma("sp", zo[t * 128:(t + 1) * 128, :], Z[:], r=zkeys)
        except _Stop:
            pass
        import os
        if os.environ.get("TRUNC"):
            N = int(os.environ["TRUNC"])
            for q, I in enumerate(P.ins[:N]):
                pass
            print("TRUNC at", N, "of", len(P.ins), P.ins[N - 1]["e"], P.ins[N - 1]["r"], P.ins[N - 1]["w"])
            P.ins = P.ins[:N]
            P.dma("sp", zo[0:128, 0:16], modc[:], r=["mod_a"])
        print("A", P.emit())
    return nc


def inputs_A(inp, l, core):
    b, r = core // 4, core % 4
    xl = inp["x_cur"][b, r * 2048:(r + 1) * 2048]
    return {
        "x": np.ascontiguousarray(xl),
        "cT": np.ascontiguousarray(inp["c"][b].reshape(8, 128).T),
        "adaw": np.ascontiguousarray(inp["ada_w"][l][:, 0:2048]),
        "adabT": np.ascontiguousarray(inp["ada_b"][l][0:2048].reshape(16, 128).T),
        "lngT": np.ascontiguousarray(inp["ln1_g"][l].reshape(8, 128).T),
        "win": np.ascontiguousarray(inp["w_in"][l]),
        "identd": np.eye(128, dtype=np.float32),
    }

import contextlib
import numpy as np
import concourse.bass as bass

S = 8192
NEGB = -30000.0
GELU_C = 1.5957691216057308


def build_B1(nslots=32):
    nc = bass.Bass("TRN2", target_bir_lowering=False)
    D_ = lambda n, s: nc.dram_tensor(n, s, F32, kind="ExternalInput").ap()
    qnT = D_("qnT", [64, 16384]); qaugc = D_("qaugc", [5, 16384]); gates = D_("gates", [128, 384])
    kcT = D_("kcT", [2, 64, 8224]); ksT = D_("ksT", [2, 64, S]); vsw = D_("vsw", [2, S, 64])
    kaugc = D_("kaugc", [4, S]); kcaugc = D_("kcaugc", [5, 512])
    gq = D_("gq", [64, 1]); gk = D_("gk", [64, 1])
    w1r = D_("w1r", [2, 64, 4096]); posT = D_("posT", [64, 64]); w2r = D_("w2r", [128, 128]); incid = D_("incid", [512, 128])
    enu = D_("enu", [128, S])
    masks = D_("masks", [6, 128, 128])
    cmt = D_("cmt", [32, 128, 512]); addt = D_("addt", [32, 128, 128])
    identd = D_("identd", [128, 128])
    o = nc.dram_tensor("o", [4096, 256], F32, kind="ExternalOutput").ap()
    with contextlib.ExitStack() as st:
        P = Prog(nc, st)
        ident = P.sb([128, 128], BF16, name="ident")
        M4 = P.sb([128, 6, 512], BF16, name="M4")
        Mt = P.sb([128, 6, 128], BF16, name="Mt")
        EN = P.sb([128, S], BF16, name="EN")
        KS = P.sb([68, S], BF16, name="KS"); KW = P.sb([68, S], BF16, name="KW")
        QN = P.sb([69, 16384], BF16, name="QN")
        VS = P.sb([128, 64, 65], BF16, name="VS"); VW = P.sb([128, 64, 65], BF16, name="VW")
        KC = P.sb([69, 512], BF16, name="KC"); VC = P.sb([128, 4, 193], BF16, name="VC")
        G = P.sb([128, 32, 12], F32, name="G")
        gqs = P.sb([64, 1], F32, name="gqs"); gks = P.sb([64, 1], F32, name="gks")
        stg = [P.sb([64, 2048], F32, name=f"stg{k}") for k in range(2)]
        KCT = P.sb([64, 8224], BF16, name="KCT")
        W1b = P.sb([64, 4096], BF16, name="W1b"); posb = P.sb([64, 64], BF16, name="posb"); W2b = P.sb([128, 128], BF16, name="W2b")
        pbias = P.sb([128, 2], F32, name="pbias")
        xh = P.sb([128, 512], F32, name="xh"); x2 = P.sb([128, 512], F32, name="x2"); sgm = P.sb([128, 512], F32, name="sgm")
        GE = P.sb([128, 512], BF16, name="GE")
        sqk = P.sb([128, 64], F32, name="sqk"); ssk = P.sb([128, 1], F32, name="ssk"); kcn = P.sb([128, 64], BF16, name="kcn")
        CMs = [P.sb([128, 512], BF16, name=f"CMs{k}") for k in range(2)]
        ADs = [P.sb([128, 128], F32, name=f"ADs{k}") for k in range(2)]
        PT = [P.sb([128, 512], BF16, name=f"PT{k}") for k in range(3)]
        SELT4 = P.sb([128, 512], BF16, name="SELT4")
        IMP = P.sb([128, 128], F32, name="IMP"); IMP2 = P.sb([128, 128], F32, name="IMP2"); selb = P.sb([128, 128], BF16, name="selb")
        m8a = P.sb([128, 8], F32, name="m8a"); m8b = P.sb([128, 8], F32, name="m8b")
        rd = P.sb([128, 4], F32, name="rd"); cf = P.sb([128, 4], F32, name="cf")
        OACC = [P.sb([128, 4, 64], F32, name=f"OACC{k}") for k in range(2)]
        ps_sc = [P.ps([128, 512], F32, name=f"ps_sc{k}") for k in range(2)]
        ps_o = [P.ps([128, 512], F32, name=f"ps_o{k}") for k in range(4)]
        ps_tr = P.ps([128, 1024], BF16, name="ps_tr")
        ps_m = P.ps([128, 512], F32, name="ps_m")
        P.dma("pool", ident[:], identd[:, :], w=["ident"])
        for m in range(6):
            P.dma("pool", Mt[:, m, :], masks[m, :, :], w=["Mt"])
        for m in range(6):
            for h in range(4):
                P.op("dve", lambda e, m=m, h=h: e.tensor_copy(out=M4[:, m, h * 128:(h + 1) * 128], in_=Mt[:, m, :]), r=["Mt"], w=["M4"])
        for c in range(4):
            cs = slice(c * 2048, (c + 1) * 2048)
            P.dma("pool", EN[:, cs], enu[:, cs], w=["EN"])
            P.dma("pool", KS[64:68, cs], kaugc[:, cs], w=["KSaug"])
            P.dma("pool", KW[64:68, cs], kaugc[:, cs], w=["KWaug"])
        P.dma("pool", KC[64:69, :], kcaugc[:, :], w=["KCaug"])
        P.dma("sp", gqs[:], gq[:, :], w=["gqs"]); P.dma("sp", gks[:], gk[:, :], w=["gks"])
        P.dma("sp", G[:].rearrange("p i c -> p (i c)"), gates[:, :], w=["G"])
        P.dma("pool", posb[:], posT[:, :], w=["posb"]); P.dma("pool", W2b[:], w2r[:, :], w=["W2b"])
        P.op("dve", lambda e: e.memset(VS[:, :, 64:65], 1.0), w=["VSone"])
        P.op("dve", lambda e: e.memset(VW[:, :, 64:65], 1.0), w=["VWone"])
        P.op("dve", lambda e: e.memset(VC[:, :, 64:65], 1.0), w=["VCone"])
        P.op("dve", lambda e: e.memset(GE[:], 0.0), w=["GE"])
        P.dma("pool", VC[:, :, 65:193], incid.rearrange("(nt p) j -> p nt j", p=128), w=["VCinc"])
        for kv, (KT, kk) in enumerate(((KS, "KS"), (KW, "KW"))):
            for c in range(4):
                sg = stg[c % 2]; cs = slice(c * 2048, (c + 1) * 2048)
                P.dma("sp", sg[:], ksT[kv, :, cs], w=[f"stg{c % 2}"])
                P.op("act", lambda e, sg=sg, KT=KT, cs=cs: e.activation(out=KT[0:64, cs], in_=sg[:], func=AF.Identity, scale=gks[:, 0:1]),
                     r=[f"stg{c % 2}", "gks"], w=[kk])
        for kv, (VT, vk) in enumerate(((VS, "VS"), (VW, "VW"))):
            for c in range(4):
                P.dma("pool", VT[:, c * 16:(c + 1) * 16, 0:64], vsw[kv, c * 2048:(c + 1) * 2048, :].rearrange("(kt p) d -> p kt d", p=128), w=[vk])
        for c in range(8):
            sg = stg[c % 2]; cs = slice(c * 2048, (c + 1) * 2048)
            P.dma("sp", sg[:], qnT[:, cs], w=[f"stg{c % 2}"])
            P.op("dve", lambda e, sg=sg, cs=cs: e.tensor_scalar(out=QN[0:64, cs], in0=sg[:], scalar1=gqs[:, 0:1], scalar2=None, op0=ALU.mult),
                 r=[f"stg{c % 2}", "gqs"], w=["QN"])
            P.dma("pool", QN[64:69, cs], qaugc[:, cs], w=["QNaug"])
        KCv = KCT[:].rearrange("p (n s) -> p n s", s=16)
        for kv in range(2):
            for c in range(4):
                P.dma("pool", KCT[:, c * 2056:(c + 1) * 2056], kcT[kv, :, c * 2056:(c + 1) * 2056], w=["KCT"])
            P.dma("pool", W1b[:], w1r[kv, :, :], w=["W1b"])
            for l in range(32):
                rhs = KCv[:, 0:511, l] if l < 16 else KCv[:, 1:512, l - 16]
                P.op("pe", lambda e, l=l, rhs=rhs: e.matmul(ps_m[:, 0:511], lhsT=W1b[:, l * 128:(l + 1) * 128], rhs=rhs, start=(l == 0), stop=(l == 31)),
                     r=["W1b", "KCT"], w=["ps_m"])
            for l in range(32):
                P.op("pe", lambda e, l=l, kv=kv: e.matmul(ps_sc[0][:, 0:1], lhsT=W1b[:, l * 128:(l + 1) * 128], rhs=posb[:, kv * 32 + l:kv * 32 + l + 1],
                                                      start=(l == 0), stop=(l == 31)), r=["W1b", "posb"], w=["ps_sc0"])
            P.op("dve", lambda e, kv=kv: e.tensor_copy(out=pbias[:, kv:kv + 1], in_=ps_sc[0][:, 0:1]), r=["ps_sc0"], w=["pbias"])
            P.op("act", lambda e, kv=kv: e.activation(out=xh[:, 0:511], in_=ps_m[:, 0:511], func=AF.Identity, bias=pbias[:, kv:kv + 1], scale=1.0),
                 r=["ps_m", "pbias"], w=["xh"])
            P.op("dve", lambda e: e.tensor_tensor(out=x2[:, 0:511], in0=xh[:, 0:511], in1=xh[:, 0:511], op=ALU.mult), r=["xh"], w=["x2"])
            P.op("dve", lambda e: e.tensor_scalar(out=x2[:, 0:511], in0=x2[:, 0:511], scalar1=0.044715, scalar2=1.0, op0=ALU.mult, op1=ALU.add), r=["x2"], w=["x2"])
            P.op("dve", lambda e: e.tensor_tensor(out=x2[:, 0:511], in0=x2[:, 0:511], in1=xh[:, 0:511], op=ALU.mult), r=["x2", "xh"], w=["x2"])
            P.op("act", lambda e: e.activation(out=sgm[:, 0:511], in_=x2[:, 0:511], func=AF.Sigmoid, scale=GELU_C), r=["x2"], w=["sgm"])
            P.op("dve", lambda e: e.tensor_tensor(out=GE[:, 0:511], in0=xh[:, 0:511], in1=sgm[:, 0:511], op=ALU.mult), r=["xh", "sgm", "GE"], w=["GE"])
            for nt in range(4):
                P.op("pe", lambda e, nt=nt, kv=kv: e.matmul(ps_sc[1][:, 0:64], lhsT=GE[:, nt * 128:(nt + 1) * 128], rhs=W2b[:, kv * 64:(kv + 1) * 64], start=True, stop=True),
                     r=["GE", "W2b"], w=["ps_sc1"])
                if kv == 0:
                    P.op("act", lambda e: e.activation(out=sqk[:], in_=ps_sc[1][:, 0:64], func=AF.Square, accum_out=ssk[:, 0:1]), r=["ps_sc1"], w=["sqk", "ssk"])
                    P.op("dve", lambda e: e.tensor_scalar(out=ssk[:], in0=ssk[:], scalar1=1.0 / 64, scalar2=1e-6, op0=ALU.mult, op1=ALU.add), r=["ssk"], w=["ssk"])
                    P.op("act", lambda e: e.activation(out=ssk[:], in_=ssk[:], func=AF.Sqrt), r=["ssk"], w=["ssk"])
                    P.op("dve", lambda e: e.reciprocal(out=ssk[:], in_=ssk[:]), r=["ssk"], w=["ssk"])
                    P.op("dve", lambda e: e.tensor_scalar(out=kcn[:], in0=ps_sc[1][:, 0:64], scalar1=ssk[:, 0:1], scalar2=None, op0=ALU.mult),
                         r=["ps_sc1", "ssk"], w=["kcn"])
                    P.op("pe", lambda e: e.transpose(ps_tr[0:64, 0:128], kcn[:, 0:64], ident[:]), r=["kcn", "ident"], w=["ps_tr"])
                    P.op("act", lambda e, nt=nt: e.activation(out=KC[0:64, nt * 128:(nt + 1) * 128], in_=ps_tr[0:64, 0:128], func=AF.Identity, scale=gks[:, 0:1]),
                         r=["ps_tr", "gks"], w=["KC"])
                else:
                    P.op("act", lambda e, nt=nt: e.activation(out=VC[:, nt, 0:64], in_=ps_sc[1][:, 0:64], func=AF.Copy), r=["ps_sc1"], w=["VC"])
        Gv = G[:].rearrange("p i (h b) -> p i h b", b=3)
        u = 0

        def attend(mms_fn, kts, VT, vkeys, ncol, okeys_first):
            nonlocal u
            n = len(kts)
            units = []
            for ki, kt in enumerate(kts):
                units.append((ki, kt, ps_sc[u % 2], f"ps_sc{u % 2}", PT[u % 3], f"PT{u % 3}"))
                u += 1

            def scores(U):
                ki, kt, sc, sck, pt, ptk = U
                mms = mms_fn(kt, sc)
                for mi, (o_, l_, r_, rk) in enumerate(mms):
                    P.op("pe", lambda e, o_=o_, l_=l_, r_=r_, mi=mi, nm=len(mms): e.matmul(o_, lhsT=l_, rhs=r_, start=(mi == 0), stop=(mi == nm - 1)), r=rk, w=[sck])

            def exp_pv(U):
                ki, kt, sc, sck, pt, ptk = U
                P.op("act", lambda e, pt=pt, sc=sc: e.activation(out=pt[:], in_=sc[:, 0:512], func=AF.Exp, scale=0.125), r=[sck], w=[ptk])
                for h in range(4):
                    P.op("pe", lambda e, pt=pt, h=h, kt=kt, ki=ki: e.matmul(ps_o[h][:, 0:ncol], lhsT=pt[:, h * 128:(h + 1) * 128], rhs=VT[:, kt, 0:ncol],
                                                                      start=(ki == 0), stop=(ki == n - 1)), r=[ptk] + vkeys, w=[f"ps_o{h}"])
            prev = None
            for U in units:
                scores(U)
                if prev is not None:
                    exp_pv(prev)
                prev = U
            exp_pv(prev)

        def finish(i, br, OA, oak, first):
            for h in range(4):
                P.op("dve", lambda e, h=h: e.tensor_scalar(out=rd[:, h:h + 1], in0=ps_o[h][:, 64:65], scalar1=1e-30, scalar2=None, op0=ALU.max), r=[f"ps_o{h}"], w=["rd"])
            P.op("dve", lambda e: e.reciprocal(out=rd[:], in_=rd[:]), r=["rd"], w=["rd"])
            P.op("dve", lambda e, i=i, br=br: e.tensor_tensor(out=cf[:], in0=rd[:], in1=Gv[:, i, :, br], op=ALU.mult), r=["rd", "G"], w=["cf"])
            for h in range(4):
                if first:
                    P.op("dve", lambda e, h=h, OA=OA: e.tensor_scalar(out=OA[:, h, :], in0=ps_o[h][:, 0:64], scalar1=cf[:, h:h + 1], scalar2=None, op0=ALU.mult),
                         r=[f"ps_o{h}", "cf"], w=[oak])
                else:
                    P.op("dve", lambda e, h=h, OA=OA: e.scalar_tensor_tensor(out=OA[:, h, :], in0=ps_o[h][:, 0:64], scalar=cf[:, h:h + 1], in1=OA[:, h, :],
                                                                           op0=ALU.mult, op1=ALU.add), r=[f"ps_o{h}", "cf", oak], w=[oak])

        for i in range(nslots):
            cols = slice(i * 512, (i + 1) * 512)
            CM = CMs[i % 2]; cmk = f"CMs{i % 2}"; AD = ADs[i % 2]; adk = f"ADs{i % 2}"
            OA = OACC[i % 2]; oak = f"OACC{i % 2}"
            P.dma("pool", CM[:], cmt[i, :, :], w=[cmk])
            P.dma("sp", AD[:], addt[i, :, :], w=[adk])
            def mm_cmp(nt, sc):
                mms = [(sc[:, 0:512], KC[0:69, nt * 128:(nt + 1) * 128], QN[0:69, cols], ["KC", "KCaug", "QN", "QNaug"])]
                for h in range(4):
                    mms.append((sc[:, h * 128:(h + 1) * 128], ident[:], CM[:, nt * 128:(nt + 1) * 128], ["ident", cmk]))
                return mms
            attend(mm_cmp, list(range(min(3, (256 * i + 224) // 2048) + 1)), VC, ["VC", "VCone", "VCinc"], 193, None)
            finish(i, 0, OA, oak, True)
            P.op("dve", lambda e: e.tensor_scalar(out=IMP[:], in0=ps_o[0][:, 65:193], scalar1=rd[:, 0:1], scalar2=None, op0=ALU.mult), r=["ps_o0", "rd"], w=["IMP"])
            for h in range(1, 4):
                P.op("dve", lambda e, h=h: e.scalar_tensor_tensor(out=IMP[:], in0=ps_o[h][:, 65:193], scalar=rd[:, h:h + 1], in1=IMP[:], op0=ALU.mult, op1=ALU.add),
                     r=[f"ps_o{h}", "rd", "IMP"], w=["IMP"])
            P.op("dve", lambda e, AD=AD: e.tensor_tensor(out=IMP[:], in0=IMP[:], in1=AD[:], op=ALU.add), r=["IMP", adk], w=["IMP"])
            P.op("dve", lambda e: e.max(out=m8a[:], in_=IMP[:]), r=["IMP"], w=["m8a"])
            P.op("dve", lambda e: e.match_replace(out=IMP2[:], in_to_replace=m8a[:], in_values=IMP[:], imm_value=-3.0e38), r=["IMP", "m8a"], w=["IMP2"])
            P.op("dve", lambda e: e.max(out=m8b[:], in_=IMP2[:]), r=["IMP2"], w=["m8b"])
            P.op("dve", lambda e: e.tensor_scalar(out=IMP2[:], in0=IMP[:], scalar1=m8b[:, 7:8], scalar2=None, op0=ALU.is_ge), r=["IMP", "m8b"], w=["IMP2"])
            P.op("dve", lambda e: e.tensor_scalar(out=selb[:], in0=IMP2[:], scalar1=-1.0, scalar2=-NEGB, op0=ALU.add, op1=ALU.mult), r=["IMP2"], w=["selb"])
            P.op("pe", lambda e: e.transpose(ps_tr[:, 0:128], selb[:, :], ident[:]), r=["selb", "ident"], w=["ps_tr"])
            for h in range(4):
                P.op("act", lambda e, h=h: e.activation(out=SELT4[:, h * 128:(h + 1) * 128], in_=ps_tr[:, 0:128], func=AF.Copy), r=["ps_tr"], w=["SELT4"])
            def mm_win(kt, sc):
                mms = [(sc[:, 0:512], KW[0:68, kt * 128:(kt + 1) * 128], QN[0:68, cols], ["KW", "KWaug", "QN", "QNaug"])]
                off = kt - 2 * i
                mi = {-4: 2, -3: 3, 0: 4, 1: 5}.get(off)
                if mi is not None:
                    mms.append((sc[:, 0:512], ident[:], M4[:, mi, :], ["ident", "M4"]))
                return mms
            attend(mm_win, [kt for kt in range(2 * i - 4, 2 * i + 2) if kt >= 0], VW, ["VW", "VWone"], 65, None)
            finish(i, 2, OA, oak, False)
            def mm_sel(kt, sc):
                mms = [(sc[:, 0:512], KS[0:68, kt * 128:(kt + 1) * 128], QN[0:68, cols], ["KS", "KSaug", "QN", "QNaug"]),
                       (sc[:, 0:512], EN[:, kt * 128:(kt + 1) * 128], SELT4[:, :], ["EN", "SELT4"])]
                if kt == 2 * i:
                    mms.append((sc[:, 0:512], ident[:], M4[:, 0, :], ["ident", "M4"]))
                if kt == 2 * i + 1:
                    mms.append((sc[:, 0:512], ident[:], M4[:, 1, :], ["ident", "M4"]))
                return mms
            attend(mm_sel, list(range(2 * i + 2)), VS, ["VS", "VSone"], 65, None)
            finish(i, 1, OA, oak, False)
            P.dma("sp", o[i * 128:(i + 1) * 128, :], OA[:].rearrange("p h d -> p (h d)"), r=[oak])
        print("B1", P.emit())
    return nc


def consts_B1(parity, slopes):
    k = np.arange(128)
    tri = np.where(k[None, :] >= k[:, None], 0.0, NEGB).astype(np.float32)
    tri2 = np.where(k[:, None] > k[None, :], 0.0, NEGB).astype(np.float32)
    opn = np.zeros((128, 128), np.float32); cls = np.full((128, 128), NEGB, np.float32)
    if parity == 0:
        masks = np.stack([tri, cls, tri2, opn, tri, cls])
    else:
        masks = np.stack([opn, tri, cls, tri2, opn, tri])
    n_ = np.arange(128)
    cmt = np.zeros((32, 128, 512), np.float32); addt = np.zeros((32, 128, 128), np.float32)
    j = np.arange(128)
    for i in range(32):
        qt = 2 * i + parity
        t = qt * 128 + np.arange(128)
        for nt in range(4):
            cend = 16 * (128 * nt + n_) + 31
            cmt[i, :, nt * 128:(nt + 1) * 128] = np.where(cend[:, None] <= t[None, :], 0.0, NEGB)
        cur = t // 64
        forced = (j[None, :] == 0) | (j[None, :] == cur[:, None]) | (j[None, :] == cur[:, None] - 1)
        addt[i] = np.where(j[None, :] <= cur[:, None], np.where(forced, 1e4, 0.0), -1e30)
    cmt[:, 127, 384:512] = NEGB
    enu = (np.arange(S)[None, :] // 64 == j[:, None]).astype(np.float32)
    kaug = np.stack([np.ones(S), np.full(S, 128.0), np.arange(S) % 128, (np.arange(S) // 128) * 128.0]).astype(np.float32)
    n512 = np.arange(512)
    kcaug = np.stack([np.ones(512), np.full(512, 128.0), 16.0 * (n512 % 128), 2048.0 * (n512 // 128), np.full(512, 15.5)]).astype(np.float32)
    qa = np.zeros((5, 32, 4, 128), np.float32)
    qp = np.arange(128.0)
    for i in range(32):
        qt = 2 * i + parity
        for h in range(4):
            Sx = 8.0 * slopes[h]
            qa[0, i, h] = -Sx * qp; qa[1, i, h] = -Sx * qt; qa[2:5, i, h] = Sx
    cs = 16 * np.arange(512); ss = 64 * np.arange(128)
    inc = ((cs[:, None] <= ss[None, :] + 63) & (cs[:, None] + 31 >= ss[None, :])).astype(np.float32)
    inc[511] = 0.0
    return dict(masks=masks, cmt=cmt, addt=addt, enu=enu, kaugc=kaug, kcaugc=kcaug, qaugc=qa.reshape(5, 16384), incid=inc,
                identd=np.eye(128, dtype=np.float32))


def inputs_B1(zb, g, parity, gq, gk, pos, w1, w2):
    slopes = 2.0 ** (-8.0 * np.arange(1, 9) / 8)[4 * g:4 * g + 4]
    d = consts_B1(parity, slopes)
    own = (np.arange(4096) // 128 * 2 + parity) * 128 + np.arange(4096) % 128
    q = zb[own][:, 0:512].reshape(32, 128, 8, 64)[:, :, 4 * g:4 * g + 4]
    d["qnT"] = np.ascontiguousarray(q.transpose(3, 0, 2, 1).reshape(64, 16384))
    d["gates"] = np.ascontiguousarray(zb[own][:, 1280 + 12 * g:1280 + 12 * (g + 1)].reshape(32, 128, 12).transpose(1, 0, 2).reshape(128, 384))
    kc = zb[:, 512 + 64 * g:512 + 64 * (g + 1)]; vc = zb[:, 640 + 64 * g:640 + 64 * (g + 1)]
    kcT = np.zeros((2, 64, 8224), np.float32); kcT[0, :, :S] = kc.T; kcT[1, :, :S] = vc.T
    d["kcT"] = kcT
    d["ksT"] = np.ascontiguousarray(np.stack([zb[:, 768 + 64 * g:768 + 64 * (g + 1)].T, zb[:, 1024 + 64 * g:1024 + 64 * (g + 1)].T]))
    d["vsw"] = np.ascontiguousarray(np.stack([zb[:, 896 + 64 * g:896 + 64 * (g + 1)], zb[:, 1152 + 64 * g:1152 + 64 * (g + 1)]]))
    d["gq"] = np.ascontiguousarray(gq.reshape(64, 1)); d["gk"] = np.ascontiguousarray(gk.reshape(64, 1))
    d["w1r"] = np.ascontiguousarray(w1.reshape(2, 32, 64, 128).transpose(0, 2, 1, 3).reshape(2, 64, 4096))
    d["posT"] = np.ascontiguousarray(pos.transpose(2, 0, 1).reshape(64, 64))
    d["w2r"] = np.ascontiguousarray(np.concatenate([w2[0], w2[1]], axis=1))
    return d

import contextlib
import numpy as np
import concourse.bass as bass

S = 8192
NEGB = -30000.0


def build_B2(nslots=32):
    nq = nslots * 128
    nc = bass.Bass("TRN2", target_bir_lowering=False)
    D_ = lambda n, s: nc.dram_tensor(n, s, F32, kind="ExternalInput").ap()
    qmT = D_("qmT", [4, 64, 4096]); kmT = D_("kmT", [4, 64, S]); vm = D_("vm", [4, S, 64])
    qaugc = D_("qaugc", [4, 4, 4096]); kaugc = D_("kaugc", [4, S])
    gq = D_("gq", [64, 1]); gk = D_("gk", [64, 1])
    emoba = D_("emoba", [32, S])
    maska = D_("maska", [128, 128]); maskb = D_("maskb", [128, 128])
    mneg = D_("mneg", [128, 32 * 32]); mvalid = D_("mvalid", [128, 32 * 32]); mcur = D_("mcur", [128, 32 * 32])
    identd = D_("identd", [128, 128])
    o = nc.dram_tensor("o", [4096, 256], F32, kind="ExternalOutput").ap()
    with contextlib.ExitStack() as st:
        P = Prog(nc, st)
        ident = P.sb([128, 128], BF16, name="ident")
        MA = P.sb([128, 128], BF16, name="MA"); MB = P.sb([128, 128], BF16, name="MB")
        EM = P.sb([32, S], BF16, name="EM")
        KA = P.sb([68, S], BF16, name="KA")
        QA = P.sb([68, 4096], BF16, name="QA")
        VA = P.sb([128, 64, 65], BF16, name="VA")
        MSB = P.sb([32, 4096], BF16, name="MSB")
        MNEG = P.sb([128, 1024], F32, name="MNEG"); MVAL = P.sb([128, 1024], F32, name="MVAL"); MCUR = P.sb([128, 1024], F32, name="MCUR")
        gqs = P.sb([64, 1], F32, name="gqs"); gks = P.sb([64, 1], F32, name="gks")
        stg = [P.sb([64, 2048], F32, name=f"stg{k}") for k in range(2)]
        kmf = P.sb([64, 32], F32, name="kmf")
        kmb = P.sb([64, 32], BF16, name="kmb")
        gm = P.sb([128, 32], F32, name="gm"); m8 = P.sb([128, 8], F32, name="m8")
        al = P.sb([128, 32], F32, name="al"); sbb = P.sb([128, 32], BF16, name="sbb")
        PT = [P.sb([128, 512], BF16, name=f"PT{k}") for k in range(3)]
        rden = P.sb([128, 4], F32, name="rden")
        osb = [P.sb([128, 4, 64], F32, name=f"osb{k}") for k in range(2)]
        ps_sc = [P.ps([128, 512], F32, name=f"ps_sc{k}") for k in range(2)]
        ps_o = [P.ps([128, 512], F32, name=f"ps_o{k}") for k in range(4)]
        ps_tr = P.ps([128, 1024], BF16, name="ps_tr")
        ps_g = P.ps([128, 512], F32, name="ps_g")
        P.dma("pool", ident[:], identd[:, :], w=["ident"])
        P.dma("pool", MA[:], maska[:, :], w=["MA"])
        P.dma("pool", MB[:], maskb[:, :], w=["MB"])
        for c in range(4):
            P.dma("pool", EM[:, c * 2048:(c + 1) * 2048], emoba[:, c * 2048:(c + 1) * 2048], w=["EM"])
            P.dma("pool", KA[64:68, c * 2048:(c + 1) * 2048], kaugc[:, c * 2048:(c + 1) * 2048], w=["KAaug"])
        P.dma("sp", MNEG[:], mneg[:, :], w=["MNEG"])
        P.dma("sp", MVAL[:], mvalid[:, :], w=["MVAL"])
        P.dma("sp", MCUR[:], mcur[:, :], w=["MCUR"])
        P.dma("sp", gqs[:], gq[:, :], w=["gqs"])
        P.dma("sp", gks[:], gk[:, :], w=["gks"])
        P.op("dve", lambda e: e.memset(VA[:, :, 64:65], 1.0), w=["VAone"])
        u = 0
        for h in range(4):
            for c in range(4):
                sg = stg[c % 2]
                P.dma("sp", sg[:], kmT[h, :, c * 2048:(c + 1) * 2048], w=[f"stg{c % 2}"])
                P.op("dve", lambda e, sg=sg: e.tensor_scalar(out=sg[:], in0=sg[:], scalar1=gks[:, 0:1], scalar2=None, op0=ALU.mult),
                     r=[f"stg{c % 2}", "gks"], w=[f"stg{c % 2}"])
                P.op("act", lambda e, sg=sg, c=c: e.activation(out=KA[0:64, c * 2048:(c + 1) * 2048], in_=sg[:], func=AF.Copy),
                     r=[f"stg{c % 2}"], w=["KA"])
                P.op("dve", lambda e, sg=sg, c=c: e.tensor_reduce(out=kmf[:, c * 8:(c + 1) * 8], in_=sg[:].rearrange("p (b k) -> p b k", k=256),
                                                                 axis=AX.X, op=ALU.add), r=[f"stg{c % 2}"], w=["kmf"])
            P.op("dve", lambda e: e.tensor_scalar(out=kmb[:], in0=kmf[:], scalar1=1.0 / 256, scalar2=None, op0=ALU.mult), r=["kmf"], w=["kmb"])
            for c in range((nq + 2047) // 2048):
                w_ = min(2048, nq - c * 2048)
                sg = stg[c % 2]
                P.dma("sp", sg[:, 0:w_], qmT[h, :, c * 2048:c * 2048 + w_], w=[f"stg{c % 2}"])
                P.op("dve", lambda e, sg=sg, c=c, w_=w_: e.tensor_scalar(out=QA[0:64, c * 2048:c * 2048 + w_], in0=sg[:, 0:w_], scalar1=gqs[:, 0:1],
                                                                       scalar2=None, op0=ALU.mult), r=[f"stg{c % 2}", "gqs"], w=["QA"])
            P.dma("pool", QA[64:68, 0:nq], qaugc[h, :, 0:nq], w=["QAaug"])
            for c in range(4):
                P.dma("pool", VA[:, c * 16:(c + 1) * 16, 0:64], vm[h, c * 2048:(c + 1) * 2048, :].rearrange("(kt p) d -> p kt d", p=128), w=["VA"])
            for i in range(nslots):
                P.op("pe", lambda e, i=i: e.matmul(ps_g[:, 0:32], lhsT=QA[0:64, i * 128:(i + 1) * 128], rhs=kmb[:, :], start=True, stop=True),
                     r=["QA", "kmb"], w=["ps_g"])
                P.op("dve", lambda e, i=i: e.tensor_tensor(out=gm[:], in0=ps_g[:, 0:32], in1=MNEG[:, i * 32:(i + 1) * 32], op=ALU.add),
                     r=["ps_g", "MNEG"], w=["gm"])
                P.op("dve", lambda e: e.max(out=m8[:], in_=gm[:]), r=["gm"], w=["m8"])
                P.op("dve", lambda e: e.tensor_scalar(out=al[:], in0=gm[:], scalar1=m8[:, 2:3], scalar2=None, op0=ALU.is_ge), r=["gm", "m8"], w=["al"])
                P.op("dve", lambda e, i=i: e.tensor_tensor(out=al[:], in0=al[:], in1=MVAL[:, i * 32:(i + 1) * 32], op=ALU.mult), r=["al", "MVAL"], w=["al"])
                P.op("dve", lambda e, i=i: e.tensor_tensor(out=al[:], in0=al[:], in1=MCUR[:, i * 32:(i + 1) * 32], op=ALU.add), r=["al", "MCUR"], w=["al"])
                P.op("dve", lambda e: e.tensor_scalar(out=sbb[:], in0=al[:], scalar1=-1.0, scalar2=-NEGB, op0=ALU.add, op1=ALU.mult), r=["al"], w=["sbb"])
                P.op("pe", lambda e: e.transpose(ps_tr[0:32, 0:128], sbb[:, 0:32], ident[:]), r=["sbb", "ident"], w=["ps_tr"])
                P.op("act", lambda e, i=i: e.activation(out=MSB[:, i * 128:(i + 1) * 128], in_=ps_tr[0:32, 0:128], func=AF.Copy), r=["ps_tr"], w=["MSB"])
            units = []
            for gi in range(nslots // 4):
                i0 = 4 * gi
                nkt = 2 * (i0 + 3) + 2
                for kt in range(nkt):
                    units.append((gi, i0, kt, nkt, u))
                    u += 1

            def scores(U):
                gi, i0, kt, nkt, uu = U
                cols = slice(i0 * 128, (i0 + 4) * 128)
                sc = ps_sc[uu % 2]; sck = f"ps_sc{uu % 2}"
                mms = [(sc[:, 0:512], KA[0:68, kt * 128:(kt + 1) * 128], QA[0:68, cols], ["KA", "KAaug", "QA", "QAaug"]),
                       (sc[:, 0:512], EM[0:32, kt * 128:(kt + 1) * 128], MSB[0:32, cols], ["EM", "MSB"])]
                for j in range(4):
                    if kt == 2 * (i0 + j):
                        mms.append((sc[:, j * 128:(j + 1) * 128], ident[:], MA[:], ["ident", "MA"]))
                    if kt == 2 * (i0 + j) + 1:
                        mms.append((sc[:, j * 128:(j + 1) * 128], ident[:], MB[:], ["ident", "MB"]))
                for mi, (o_, l_, r_, rk) in enumerate(mms):
                    P.op("pe", lambda e, o_=o_, l_=l_, r_=r_, mi=mi, n=len(mms): e.matmul(o_, lhsT=l_, rhs=r_, start=(mi == 0), stop=(mi == n - 1)),
                         r=rk, w=[sck])

            def exp_pv(U):
                gi, i0, kt, nkt, uu = U
                sc = ps_sc[uu % 2]; sck = f"ps_sc{uu % 2}"; pt = PT[uu % 3]; ptk = f"PT{uu % 3}"
                P.op("act", lambda e, pt=pt, sc=sc: e.activation(out=pt[:], in_=sc[:, 0:512], func=AF.Exp, scale=0.125), r=[sck], w=[ptk])
                for j in range(4):
                    last = 2 * (i0 + j) + 1
                    if kt <= last:
                        P.op("pe", lambda e, pt=pt, j=j, kt=kt, last=last: e.matmul(ps_o[j][:, 0:65], lhsT=pt[:, j * 128:(j + 1) * 128],
                                                                              rhs=VA[:, kt, 0:65], start=(kt == 0), stop=(kt == last)),
                             r=[ptk, "VA", "VAone"], w=[f"ps_o{j}"])
                if kt == nkt - 1:
                    ob = osb[gi % 2]; obk = f"osb{gi % 2}"
                    for j in range(4):
                        P.op("dve", lambda e, j=j: e.tensor_scalar(out=rden[:, j:j + 1], in0=ps_o[j][:, 64:65], scalar1=1e-30, scalar2=None, op0=ALU.max),
                             r=[f"ps_o{j}"], w=["rden"])
                    P.op("dve", lambda e: e.reciprocal(out=rden[:], in_=rden[:]), r=["rden"], w=["rden"])
                    for j in range(4):
                        P.op("dve", lambda e, j=j, ob=ob: e.tensor_scalar(out=ob[:, j, :], in0=ps_o[j][:, 0:64], scalar1=rden[:, j:j + 1], scalar2=None, op0=ALU.mult),
                             r=[f"ps_o{j}", "rden"], w=[obk])
                    for j in range(4):
                        P.dma("sp", o[(i0 + j) * 128:(i0 + j + 1) * 128, h * 64:(h + 1) * 64], ob[:, j, :], r=[obk])
            prev = None
            for U in units:
                scores(U)
                if prev is not None:
                    exp_pv(prev)
                prev = U
            exp_pv(prev)
        print("B2", P.emit())
    return nc


def consts_B2(parity):
    k = np.arange(128)
    tri = np.where(k[None, :] >= k[:, None], 0.0, NEGB).astype(np.float32)
    opn = np.zeros((128, 128), np.float32); cls = np.full((128, 128), NEGB, np.float32)
    maska, maskb = (tri, cls) if parity == 0 else (opn, tri)
    j = np.arange(32)
    mneg = np.zeros((128, 32, 32), np.float32); mval = np.zeros((128, 32, 32), np.float32); mcur = np.zeros((128, 32, 32), np.float32)
    for i in range(32):
        cb = i
        mneg[:, i, :] = np.where(j < cb, 0.0, -1e30)[None, :]
        mval[:, i, :] = (j < cb)[None, :]
        mcur[:, i, :] = (j == cb)[None, :]
    emoba = (np.arange(S)[None, :] // 256 == j[:, None]).astype(np.float32)
    kaug = np.stack([np.ones(S), np.full(S, 128.0), np.arange(S) % 128, (np.arange(S) // 128) * 128.0]).astype(np.float32)
    return dict(maska=maska, maskb=maskb, mneg=mneg.reshape(128, 1024), mvalid=mval.reshape(128, 1024), mcur=mcur.reshape(128, 1024),
                emoba=emoba, kaugc=kaug, identd=np.eye(128, dtype=np.float32))


def qaug_rows(slopes, parity, extra=0):
    i = np.arange(4096) // 128
    qp = (np.arange(4096) % 128).astype(np.float64)
    qt = 2 * i + parity
    out = []
    for s in slopes:
        Sx = 8.0 * s
        rows = [-Sx * qp, -Sx * qt, np.full(4096, Sx), np.full(4096, Sx)] + [np.full(4096, Sx)] * extra
        out.append(np.stack(rows))
    return np.stack(out).astype(np.float32)


def alibi(n):
    return 2.0 ** (-8.0 * np.arange(1, n + 1) / n)


def inputs_B2(zb, g, parity, gq, gk):
    own = (np.arange(4096) // 128 * 2 + parity) * 128 + np.arange(4096) % 128
    hs = [4 * g + a for a in range(4)]
    qm = zb[:, 1304:1816].reshape(S, 8, 64); km = zb[:, 1816:2328].reshape(S, 8, 64); vmm = zb[:, 2328:2840].reshape(S, 8, 64)
    d = consts_B2(parity)
    d.update(qmT=np.ascontiguousarray(qm[own][:, hs].transpose(1, 2, 0)),
             kmT=np.ascontiguousarray(km[:, hs].transpose(1, 2, 0)),
             vm=np.ascontiguousarray(vmm[:, hs].transpose(1, 0, 2)),
             qaugc=qaug_rows(alibi(8)[hs], parity),
             gq=np.ascontiguousarray(gq.reshape(64, 1)), gk=np.ascontiguousarray(gk.reshape(64, 1)))
    return d

import contextlib
import numpy as np
import concourse.bass as bass

NTC = 16
NH = 8
NE = 32


def build_C(ne=NE):
    nc = bass.Bass("TRN2", target_bir_lowering=False)
    D_ = lambda n, s: nc.dram_tensor(n, s, F32, kind="ExternalInput").ap()
    x = D_("x", [2048, D]); o = D_("o", [2048, D])
    cT = D_("cT", [128, 8]); adaw = D_("adaw", [D, 4096]); adab = D_("adab", [1, 4096])
    ln2 = D_("ln2", [128, D]); wout = D_("wout", [D, D])
    rw = D_("rw", [D, 32]); rb = D_("rb", [128, 32])
    w1 = D_("w1", [NE, D, 2048]); b1T = D_("b1T", [128, NE * 16]); w2 = D_("w2", [NE, D, D]); b2 = D_("b2", [NE, D])
    identd = D_("identd", [128, 128])
    y = nc.dram_tensor("y", [2048, D], F32, kind="ExternalOutput").ap()
    with contextlib.ExitStack() as st:
        P = Prog(nc, st)
        ident = P.sb([128, 128], BF16, name="ident")
        onesf = P.sb([128, 128], F32, name="onesf")
        condT = P.sb([128, 8], F32, name="condT")
        condrep = P.sb([128, 8, 128], F32, name="condrep")
        adabs = P.sb([1, 256], F32, name="adabs")
        MODB = P.sb([128, 4096], F32, name="MODB")
        A2B = P.sb([128, D], F32, name="A2B")
        rwb = P.sb([128, 8, 32], BF16, name="rwb")
        rbs = P.sb([128, 32], F32, name="rbs")
        b1s = P.sb([128, NE * 16], F32, name="b1s")
        b2b = P.sb([32, D], BF16, name="b2b")
        h2T = P.sb([128, 8, 1024], BF16, name="h2T")
        acc = P.sb([128, NH, D], F32, name="acc")
        GATE = P.sb([128, NH, 32], F32, name="GATE")
        w1ring = [P.sb([128, 8, 512], BF16, name=f"w1r{k}") for k in range(3)]
        w2buf = [P.sb([128, 8, D], BF16, name=f"w2b{k}") for k in range(2)]
        woutb = w2buf[0]
        AT = P.sb([128, 8, 1024], BF16, name="AT")
        rr = 0
        ps = [P.ps([128, 512], F32, name=f"ps{k}") for k in range(6)]
        ps_tr = [P.ps([128, 1024], BF16, name=f"ps_tr{k}") for k in range(2)]
        P.dma("pool", ident[:], identd[:, :], w=["ident"])
        P.op("pool", lambda e: e.memset(onesf[:], 1.0), w=["onesf"])
        P.dma("sp", condT[:], cT[:, :], w=["condT"])
        P.dma("sp", A2B[:], ln2[:, :], w=["A2B"])
        P.dma("sp", rbs[:], rb[:, :], w=["rbs"])
        P.dma("sp", b1s[:], b1T[:, :], w=["b1s"])
        P.dma("pool", b2b[:], b2[:, :], w=["b2b"])
        for c in range(8):
            P.dma("pool", rwb[:, c, :], rw[c * 128:(c + 1) * 128, :], w=["rwb"])
        P.op("act", lambda e: e.activation(out=condT[:], in_=condT[:], func=AF.Silu), r=["condT"], w=["condT"])
        for c in range(8):
            P.op("act", lambda e, c=c: e.activation(out=condrep[:, c, :], in_=onesf[:], func=AF.Identity, scale=condT[:, c:c + 1]),
                 r=["condT", "onesf"], w=["condrep"])
        b1v = b1s[:].rearrange("p (e k) -> p e k", k=16)
        P.op("dve", lambda e: e.tensor_scalar(out=b1v[:, :, 8:16], in0=b1v[:, :, 8:16], scalar1=1.0, scalar2=None, op0=ALU.add), r=["b1s"], w=["b1s"])
        awt = [P.sb([128, 8, 256], F32, name=f"awt{k}") for k in range(2)]
        for n in range(16):
            aw = awt[n % 2]; awk = f"awt{n % 2}"
            for i in range(8):
                P.dma("sp", aw[:, i, :], adaw[i * 128:(i + 1) * 128, n * 256:(n + 1) * 256], w=[awk])
            P.dma("sp", adabs[:], adab[:, n * 256:(n + 1) * 256], w=["adabs"])
            pm = ps[n % 2]; pmk = f"ps{n % 2}"
            for i in range(8):
                P.op("pe", lambda e, pm=pm, aw=aw, i=i: e.matmul(pm[:, 0:256], lhsT=condrep[:, i, :], rhs=aw[:, i, :], start=(i == 0), stop=False),
                     r=["condrep", awk], w=[pmk])
            P.op("pe", lambda e, pm=pm: e.matmul(pm[:, 0:256], lhsT=onesf[0:1, :], rhs=adabs[0:1, :], start=False, stop=True),
                 r=["onesf", "adabs"], w=[pmk])
            P.op("act", lambda e, pm=pm, n=n: e.activation(out=MODB[:, n * 256:(n + 1) * 256], in_=pm[:, 0:256], func=AF.Copy), r=[pmk], w=["MODB"])
        P.op("dve", lambda e: e.scalar_tensor_tensor(out=A2B[:], in0=MODB[:, 2048:3072], scalar=1.0, in1=A2B[:], op0=ALU.add, op1=ALU.mult),
             r=["MODB", "A2B"], w=["A2B"])
        ot = [P.sb([128, D], BF16, name=f"ot{k}") for k in range(2)]
        oT = [P.sb([128, 8, 128], BF16, name=f"oT{k}") for k in range(2)]
        xt = [P.sb([128, D], F32, name=f"xt{k}") for k in range(2)]
        x1 = [P.sb([128, D], F32, name=f"x1{k}") for k in range(2)]
        hb = [P.sb([128, D], BF16, name=f"hb{k}") for k in range(2)]
        sq = P.sb([128, D], F32, name="sq")
        ssv = P.sb([128, 1], F32, name="ssv")
        lg = P.sb([128, 32], F32, name="lg"); m8 = P.sb([128, 8], F32, name="m8"); msk = P.sb([128, 32], F32, name="msk")
        nmx = P.sb([128, 1], F32, name="nmx"); ex = P.sb([128, 32], F32, name="ex"); den = P.sb([128, 1], F32, name="den")
        gb = P.sb([128, 32], BF16, name="gb"); gT = P.sb([32, 128], BF16, name="gT")
        gcl = P.sb([128, 512], F32, name="gcl"); sgl = P.sb([128, 512], F32, name="sgl"); lcl = P.sb([128, 512], F32, name="lcl")
        for half in range(2):
            for c in range(8):
                P.dma("pool", woutb[:, c, :], wout[c * 128:(c + 1) * 128, :], w=["w2b0"])
            for tl in range(NH):
                t = half * NH + tl
                k = t % 2
                P.dma("pool", ot[k][:], o[t * 128:(t + 1) * 128, :], w=[f"ot{k}"])
                P.dma("sp", xt[k][:], x[t * 128:(t + 1) * 128, :], w=[f"xt{k}"])
                for c in range(8):
                    P.op("pe", lambda e, k=k, c=c: e.transpose(ps_tr[k][:, c * 128:(c + 1) * 128], ot[k][:, c * 128:(c + 1) * 128], ident[:]),
                         r=[f"ot{k}", "ident"], w=[f"ps_tr{k}"])
                P.op("act", lambda e, k=k: e.activation(out=oT[k][:].rearrange("p c t -> p (c t)"), in_=ps_tr[k][:, :], func=AF.Copy),
                     r=[f"ps_tr{k}"], w=[f"oT{k}"])
                for n in range(2):
                    pm = ps[2 + n]; pmk = f"ps{2 + n}"
                    for c in range(8):
                        P.op("pe", lambda e, pm=pm, k=k, c=c, n=n: e.matmul(pm[:, 0:512], lhsT=oT[k][:, c, :], rhs=woutb[:, c, n * 512:(n + 1) * 512],
                                                                        start=(c == 0), stop=(c == 7)), r=[f"oT{k}", "w2b0"], w=[pmk])
                    P.op("dve", lambda e, pm=pm, k=k, n=n: e.tensor_tensor(out=x1[k][:, n * 512:(n + 1) * 512], in0=pm[:, 0:512], in1=MODB[:, n * 512:(n + 1) * 512],
                                                                          op=ALU.mult), r=[pmk, "MODB"], w=[f"x1{k}"])
                P.op("dve", lambda e, k=k: e.tensor_tensor(out=x1[k][:], in0=x1[k][:], in1=xt[k][:], op=ALU.add), r=[f"x1{k}", f"xt{k}"], w=[f"x1{k}"])
                P.dma("sp", y[t * 128:(t + 1) * 128, :], x1[k][:], r=[f"x1{k}"], w=[f"y{t}"])
                P.op("act", lambda e, k=k: e.activation(out=sq[:], in_=x1[k][:], func=AF.Square, accum_out=ssv[:, 0:1]), r=[f"x1{k}"], w=["sq", "ssv"])
                P.op("dve", lambda e: e.tensor_scalar(out=ssv[:], in0=ssv[:], scalar1=1.0 / D, scalar2=EPS, op0=ALU.mult, op1=ALU.add), r=["ssv"], w=["ssv"])
                P.op("act", lambda e: e.activation(out=ssv[:], in_=ssv[:], func=AF.Sqrt), r=["ssv"], w=["ssv"])
                P.op("dve", lambda e: e.reciprocal(out=ssv[:], in_=ssv[:]), r=["ssv"], w=["ssv"])
                P.op("dve", lambda e, k=k: e.scalar_tensor_tensor(out=sq[:], in0=x1[k][:], scalar=ssv[:, 0:1], in1=A2B[:], op0=ALU.mult, op1=ALU.mult),
                     r=[f"x1{k}", "ssv", "A2B"], w=["sq"])
                P.op("dve", lambda e, k=k: e.tensor_tensor(out=hb[k][:], in0=sq[:], in1=MODB[:, 1024:2048], op=ALU.add), r=["sq", "MODB"], w=[f"hb{k}"])
                for c in range(8):
                    P.op("pe", lambda e, k=k, c=c: e.transpose(ps_tr[k][:, c * 128:(c + 1) * 128], hb[k][:, c * 128:(c + 1) * 128], ident[:]),
                         r=[f"hb{k}", "ident"], w=[f"ps_tr{k}"])
                P.op("act", lambda e, k=k, t=t, tl=tl: e.activation(out=h2T[:, :, tl * 128:(tl + 1) * 128], in_=ps_tr[k][:, :].rearrange("p (c t) -> p c t", t=128),
                                                          func=AF.Copy), r=[f"ps_tr{k}"], w=["h2T"])
                pm = ps[4]; pmk = "ps4"
                for c in range(8):
                    P.op("pe", lambda e, pm=pm, c=c, t=t, tl=tl: e.matmul(pm[:, 0:32], lhsT=h2T[:, c, tl * 128:(tl + 1) * 128], rhs=rwb[:, c, :], start=(c == 0), stop=(c == 7)),
                         r=["h2T", "rwb"], w=[pmk])
                P.op("dve", lambda e, pm=pm: e.tensor_tensor(out=lg[:], in0=pm[:, 0:32], in1=rbs[:], op=ALU.add), r=[pmk, "rbs"], w=["lg"])
                P.op("dve", lambda e: e.max(out=m8[:], in_=lg[:]), r=["lg"], w=["m8"])
                P.op("dve", lambda e: e.tensor_scalar(out=msk[:], in0=lg[:], scalar1=m8[:, 3:4], scalar2=None, op0=ALU.is_ge), r=["lg", "m8"], w=["msk"])
                P.op("dve", lambda e: e.tensor_scalar(out=nmx[:], in0=m8[:, 0:1], scalar1=-1.0, scalar2=None, op0=ALU.mult), r=["m8"], w=["nmx"])
                P.op("act", lambda e: e.activation(out=ex[:], in_=lg[:], func=AF.Exp, bias=nmx[:, 0:1], scale=1.0), r=["lg", "nmx"], w=["ex"])
                P.op("dve", lambda e: e.tensor_tensor(out=ex[:], in0=ex[:], in1=msk[:], op=ALU.mult), r=["ex", "msk"], w=["ex"])
                P.op("dve", lambda e: e.tensor_reduce(out=den[:], in_=ex[:], axis=AX.X, op=ALU.add), r=["ex"], w=["den"])
                P.op("dve", lambda e: e.reciprocal(out=den[:], in_=den[:]), r=["den"], w=["den"])
                P.op("dve", lambda e, t=t, tl=tl: e.tensor_scalar(out=GATE[:, tl, :], in0=ex[:], scalar1=den[:, 0:1], scalar2=None, op0=ALU.mult), r=["ex", "den"], w=["GATE"])
                P.op("dve", lambda e, t=t, tl=tl: e.tensor_copy(out=gb[:], in_=GATE[:, tl, :]), r=["GATE"], w=["gb"])
                P.op("pe", lambda e, k=k: e.transpose(ps_tr[k][0:32, 0:128], gb[:, 0:32], ident[:]), r=["gb", "ident", "h2T"], w=[f"ps_tr{k}"])
                P.op("act", lambda e, k=k: e.activation(out=gT[:], in_=ps_tr[k][0:32, 0:128], func=AF.Copy), r=[f"ps_tr{k}"], w=["gT"])
                for n in range(2):
                    pm = ps[2 + n]; pmk = f"ps{2 + n}"
                    P.op("pe", lambda e, pm=pm, n=n: e.matmul(pm[:, 0:512], lhsT=gT[:, :], rhs=b2b[:, n * 512:(n + 1) * 512], start=True, stop=True),
                         r=["gT", "b2b"], w=[pmk])
                    P.op("act", lambda e, pm=pm, n=n, t=t, tl=tl: e.activation(out=acc[:, tl, n * 512:(n + 1) * 512], in_=pm[:, 0:512], func=AF.Copy), r=[pmk], w=[f"acc{tl}"])
            u = 0
            for ex_ in range(ne):
                w2t = w2buf[ex_ % 2]; w2k = f"w2b{ex_ % 2}"
                for c in range(8):
                    P.dma("pool", w2t[:, c, :], w2[ex_, c * 128:(c + 1) * 128, :], w=[w2k])
                for kp in range(4):
                    ring = w1ring[rr % 3]; rk = f"w1r{rr % 3}"
                    rr += 1
                    P.dma("pool", ring[:, :, 0:256], w1[ex_, :, kp * 256:(kp + 1) * 256].rearrange("(c p) n -> p c n", p=128), w=[rk])
                    P.dma("pool", ring[:, :, 256:512], w1[ex_, :, 1024 + kp * 256:1024 + (kp + 1) * 256].rearrange("(c p) n -> p c n", p=128), w=[rk])
                    for j in range(2):
                        kk = kp * 2 + j
                        for grp in range(2):
                            tk = slice(grp * 512, (grp + 1) * 512)
                            pg = ps[u % 2]; pgk = f"ps{u % 2}"; pl = ps[2 + u % 2]; plk = f"ps{2 + u % 2}"
                            u += 1
                            for c in range(8):
                                P.op("pe", lambda e, pg=pg, c=c, j=j, tk=tk, ring=ring: e.matmul(pg[:, 0:512], lhsT=ring[:, c, j * 128:(j + 1) * 128], rhs=h2T[:, c, tk],
                                                                                             start=(c == 0), stop=(c == 7)), r=[rk, "h2T"], w=[pgk])
                            for c in range(8):
                                P.op("pe", lambda e, pl=pl, c=c, j=j, tk=tk, ring=ring: e.matmul(pl[:, 0:512], lhsT=ring[:, c, 256 + j * 128:256 + (j + 1) * 128], rhs=h2T[:, c, tk],
                                                                                             start=(c == 0), stop=(c == 7)), r=[rk, "h2T"], w=[plk])
                            bg = b1s[:, ex_ * 16 + kk:ex_ * 16 + kk + 1]; bl = b1s[:, ex_ * 16 + 8 + kk:ex_ * 16 + 8 + kk + 1]
                            P.op("dve", lambda e, pg=pg, bg=bg: e.tensor_scalar(out=gcl[:], in0=pg[:, 0:512], scalar1=bg, scalar2=7.0, op0=ALU.add, op1=ALU.min),
                                 r=[pgk, "b1s"], w=["gcl"])
                            P.op("act", lambda e: e.activation(out=sgl[:], in_=gcl[:], func=AF.Sigmoid, scale=1.702), r=["gcl"], w=["sgl"])
                            P.op("dve", lambda e, pl=pl, bl=bl: e.tensor_scalar(out=lcl[:], in0=pl[:, 0:512], scalar1=bl, scalar2=-6.0, op0=ALU.add, op1=ALU.max),
                                 r=[plk, "b1s"], w=["lcl"])
                            P.op("dve", lambda e: e.scalar_tensor_tensor(out=lcl[:], in0=lcl[:], scalar=8.0, in1=gcl[:], op0=ALU.min, op1=ALU.mult),
                                 r=["lcl", "gcl"], w=["lcl"])
                            P.op("dve", lambda e, kk=kk, tk=tk: e.tensor_tensor(out=AT[:, kk, tk], in0=lcl[:], in1=sgl[:], op=ALU.mult), r=["lcl", "sgl"], w=[f"AT{kk}_{grp}"])
                for tl in range(NH):
                    t = half * NH + tl
                    for n in range(2):
                        py = ps[4 + n]; pyk = f"ps{4 + n}"
                        for kk in range(8):
                            P.op("pe", lambda e, py=py, kk=kk, tl=tl, n=n, w2t=w2t: e.matmul(py[:, 0:512], lhsT=AT[:, kk, tl * 128:(tl + 1) * 128],
                                                                                         rhs=w2t[:, kk, n * 512:(n + 1) * 512], start=(kk == 0), stop=(kk == 7)),
                                 r=[f"AT{kk}_{tl // 4}", w2k], w=[pyk])
                        P.op("dve", lambda e, py=py, t=t, tl=tl, n=n, ex_=ex_: e.scalar_tensor_tensor(out=acc[:, tl, n * 512:(n + 1) * 512], in0=py[:, 0:512],
                                                                                            scalar=GATE[:, tl, ex_:ex_ + 1], in1=acc[:, tl, n * 512:(n + 1) * 512],
                                                                                            op0=ALU.mult, op1=ALU.add), r=[pyk, "GATE", f"acc{tl}"], w=[f"acc{tl}"])
            for tl in range(NH):
                t = half * NH + tl
                k = t % 2
                P.dma("sp", xt[k][:], y[t * 128:(t + 1) * 128, :], r=[f"y{t}"], w=[f"xt{k}"])
                P.op("dve", lambda e, t=t, tl=tl: e.tensor_tensor(out=acc[:, tl, :], in0=acc[:, tl, :], in1=MODB[:, 3072:4096], op=ALU.mult), r=[f"acc{tl}", "MODB"], w=[f"acc{tl}"])
                P.op("dve", lambda e, t=t, tl=tl, k=k: e.tensor_tensor(out=acc[:, tl, :], in0=acc[:, tl, :], in1=xt[k][:], op=ALU.add), r=[f"acc{tl}", f"xt{k}"], w=[f"acc{tl}"])
                P.dma("sp", y[t * 128:(t + 1) * 128, :], acc[:, tl, :], r=[f"acc{tl}"], w=[f"y{t}"])
        print("C", P.emit())
    return nc


def inputs_C(inp, l, core, xcur, ocat):
    b, r = core // 4, core % 4
    sl = slice(r * 2048, (r + 1) * 2048)
    return dict(
        x=np.ascontiguousarray(xcur[b, sl]), o=np.ascontiguousarray(ocat[b, sl]),
        cT=np.ascontiguousarray(inp["c"][b].reshape(8, 128).T),
        adaw=np.ascontiguousarray(inp["ada_w"][l][:, 2048:6144]),
        adab=np.ascontiguousarray(inp["ada_b"][l][2048:6144].reshape(1, 4096)),
        ln2=np.ascontiguousarray(np.broadcast_to(inp["ln2_g"][l][None, :], (128, D))),
        wout=np.ascontiguousarray(inp["w_out"][l]),
        rw=np.ascontiguousarray(inp["router_w"][l]),
        rb=np.ascontiguousarray(np.broadcast_to(inp["router_b"][l][None, :], (128, 32))),
        w1=np.ascontiguousarray(inp["exp_w1"][l]),
        b1T=np.ascontiguousarray(inp["exp_b1"][l].reshape(NE, 16, 128).transpose(2, 0, 1).reshape(128, NE * 16)),
        w2=np.ascontiguousarray(inp["exp_w2"][l]),
        b2=np.ascontiguousarray(inp["exp_b2"][l]),
        identd=np.eye(128, dtype=np.float32),
    )


_CACHE = {}


def _prog(name, fn):
    if name not in _CACHE:
        _CACHE[name] = fn()
    return _CACHE[name]


def kernel(**inputs):
    inp = {k: np.asarray(v) for k, v in inputs.items()}
    x_cur = np.ascontiguousarray(inp["x"], dtype=np.float32)
    cores = list(range(8))
    for l in range(2):
        inp["x_cur"] = x_cur
        resA = run_bass_kernel_spmd(_prog("A", build_A), [inputs_A(inp, l, c) for c in cores], core_ids=cores)
        z = np.stack([np.concatenate([resA.results[b * 4 + r]["z"] for r in range(4)], axis=0) for b in range(2)])
        ocat = np.zeros((2, 8192, 1024), np.float32)
        cfg = [(c // 4, (c // 2) % 2, c % 2) for c in cores]
        maps = [inputs_B1(z[b], g, p, inp["nsa_q_gain"][l], inp["nsa_k_gain"][l], inp["nsa_cmp_pos"][l],
                          inp["nsa_cmp_w1"][l], inp["nsa_cmp_w2"][l]) for (b, g, p) in cfg]
        resB1 = run_bass_kernel_spmd(_prog("B1", build_B1), maps, core_ids=cores)
        maps = [inputs_B2(z[b], g, p, inp["moba_q_gain"][l], inp["moba_k_gain"][l]) for (b, g, p) in cfg]
        resB2 = run_bass_kernel_spmd(_prog("B2", build_B2), maps, core_ids=cores)
        for c, (b, g, p) in enumerate(cfg):
            own = (np.arange(4096) // 128 * 2 + p) * 128 + np.arange(4096) % 128
            ocat[b, own, g * 256:(g + 1) * 256] = resB1.results[c]["o"]
            ocat[b, own, 512 + g * 256:512 + (g + 1) * 256] = resB2.results[c]["o"]
        resC = run_bass_kernel_spmd(_prog("C", build_C), [inputs_C(inp, l, c, x_cur, ocat) for c in cores], core_ids=cores)
        x_cur = np.stack([np.concatenate([resC.results[b * 4 + r]["y"] for r in range(4)], axis=0) for b in range(2)]).astype(np.float32)
    return x_cur
```

```python
from concourse.bass_utils import run_bass_kernel_spmd
D = 1024
EPS = 1e-6

import contextlib
import numpy as np
import concourse.bass as bass
import concourse.mybir as mybir

F32 = mybir.dt.float32
BF16 = mybir.dt.bfloat16
I32 = mybir.dt.int32
U32 = mybir.dt.uint32
AF = mybir.ActivationFunctionType
ALU = mybir.AluOpType
AX = mybir.AxisListType

DMA_K = 6
STRICT_SAME = True


class Prog:
    def __init__(self, nc, stack):
        self.nc = nc
        self.stack = stack
        self.ins = []
        self.eng = {"pe": nc.tensor, "act": nc.scalar, "dve": nc.vector, "pool": nc.gpsimd, "sp": nc.sync}
        self.nt = 0

    def sb(self, shape, dt, name=None):
        self.nt += 1
        return self.stack.enter_context(self.nc.sbuf_tensor(name or f"sb{self.nt}", list(shape), dt))

    def ps(self, shape, dt=F32, name=None):
        self.nt += 1
        return self.stack.enter_context(self.nc.psum_tensor(name or f"ps{self.nt}", list(shape), dt))

    def op(self, eng, fn, r=(), w=()):
        self.ins.append(dict(e=eng, fn=fn, r=tuple(r), w=tuple(w), dma=False))

    def dma(self, q, out, in_, r=(), w=(), **kw):
        def fn(e, out=out, in_=in_, kw=kw):
            return e.dma_start(out=out, in_=in_, **kw)
        self.ins.append(dict(e=q, fn=fn, r=tuple(r), w=tuple(w), dma=True))

    def emit(self):
        nc = self.nc
        ins = self.ins
        n = len(ins)
        last_w = {}
        readers = {}
        deps = [None] * n
        for i, I in enumerate(ins):
            d = set()
            for k in I["r"]:
                d.update(last_w.get(k, ()))
            for k in I["w"]:
                for j in last_w.get(k, ()):
                    if not (I["dma"] and ins[j]["dma"]):
                        d.add(j)
                rd = readers.get(k)
                if rd:
                    for v in rd[0].values():
                        d.add(v)
                    d.update(rd[1])
            d.discard(i)
            deps[i] = d
            for k in I["r"]:
                rd = readers.setdefault(k, ({}, []))
                if I["dma"]:
                    rd[1].append(i)
                else:
                    rd[0][I["e"]] = i
            for k in I["w"]:
                if I["dma"]:
                    last_w[k] = [j for j in last_w.get(k, ()) if ins[j]["dma"]] + [i]
                else:
                    last_w[k] = [i]
                readers[k] = ({}, [])
        signal = [False] * n
        fdeps = [None] * n
        for i, I in enumerate(ins):
            keep = []
            for j in deps[i]:
                J = ins[j]
                if J["dma"]:
                    keep.append(j)
                    continue
                if J["e"] == I["e"]:
                    if I["dma"]:
                        keep.append(j); signal[j] = True
                        continue
                    if STRICT_SAME and I["e"] in ("act", "dve", "pool"):
                        if any(k in J["w"] for k in I["r"]):
                            keep.append(j); signal[j] = True
                    continue
                keep.append(j); signal[j] = True
            fdeps[i] = keep
        engs = ["pe", "act", "dve", "pool", "sp"]
        sems = {e: self.stack.enter_context(nc.semaphore(f"s_{e}")) for e in engs}
        dsems = {e: [self.stack.enter_context(nc.semaphore(f"d_{e}{k}")) for k in range(DMA_K)]
                 for e in ("sp", "act", "pool")}
        cnt = {e: 0 for e in engs}
        dcnt = {e: 0 for e in dsems}
        tag = [None] * n
        for i, I in enumerate(ins):
            if I["dma"]:
                q = I["e"]
                idx = dcnt[q]; dcnt[q] += 1
                tag[i] = ("d", q, idx)
            elif signal[i]:
                cnt[I["e"]] += 1
                tag[i] = ("c", I["e"], cnt[I["e"]])
        waited = {e: {} for e in engs}
        nw = 0
        for i, I in enumerate(ins):
            e = I["e"]
            E = self.eng[e]
            wl = {}
            for j in fdeps[i]:
                t = tag[j]
                if t[0] == "d":
                    s = dsems[t[1]][t[2] % DMA_K]; v = 16 * (t[2] // DMA_K + 1)
                else:
                    s = sems[t[1]]; v = t[2]
                key = id(s)
                if key not in wl or wl[key][1] < v:
                    wl[key] = (s, v)
            if I["dma"]:
                t = tag[i]
                if t[2] >= DMA_K:
                    s = dsems[t[1]][t[2] % DMA_K]; v = 16 * (t[2] // DMA_K)
                    key = id(s)
                    if key not in wl or wl[key][1] < v:
                        wl[key] = (s, v)
            for key, (s, v) in wl.items():
                if waited[e].get(key, 0) >= v:
                    continue
                waited[e][key] = v
                E.wait_ge(s, v)
                nw += 1
            inst = I["fn"](E)
            t = tag[i]
            if t is not None:
                if t[0] == "d":
                    inst.then_inc(dsems[t[1]][t[2] % DMA_K], 16)
                else:
                    inst.then_inc(sems[t[1]], 1)
        E = self.eng["sp"]
        for q, c in dcnt.items():
            for k in range(DMA_K):
                m = (c - k + DMA_K - 1) // DMA_K if c > k else 0
                if m > 0 and waited["sp"].get(id(dsems[q][k]), 0) < 16 * m:
                    E.wait_ge(dsems[q][k], 16 * m)
        self.stats = dict(n=n, waits=nw, cnt=dict(cnt), dcnt=dict(dcnt))
        return self.stats

import contextlib
import numpy as np
import concourse.bass as bass
import concourse.mybir as mybir

INW = 2840
NT = 16
NORM_SEGS = [(0, 8), (768, 2), (1024, 2), (1304, 8), (1816, 8)]


def emit_mod(P, nc, adaw_dram, col0, ncols, condT, adabT, out_cols, tagp):
    nj = ncols // 128
    half = 1024
    pm = P.ps([128, 64], F32, name=f"pm_{tagp}")
    for hh in range(ncols // half):
        aw = P.sb([128, 8, half], F32, name=f"aw_{tagp}{hh}")
        for i in range(8):
            P.dma("sp" if i % 2 == 0 else "pool", aw[:, i, :], adaw_dram[i * 128:(i + 1) * 128, col0 + hh * half: col0 + (hh + 1) * half],
                  w=[f"aw_{tagp}{hh}_{i}"])
        for jj in range(half // 128):
            j = hh * (half // 128) + jj
            for i in range(8):
                P.op("pe", lambda e, aw=aw, i=i, jj=jj, j=j: e.matmul(pm[:, j:j + 1], lhsT=aw[:, i, jj * 128:(jj + 1) * 128],
                                                                rhs=condT[:, i:i + 1], start=(i == 0), stop=(i == 7)),
                     r=[f"aw_{tagp}{hh}_{i}", "condT"], w=[f"pm_{tagp}"])
    P.op("dve", lambda e: e.tensor_tensor(out=out_cols[:, 0:nj], in0=pm[:, 0:nj], in1=adabT, op=ALU.add),
         r=[f"pm_{tagp}", "adab"], w=[f"mod_{tagp}"])


class _Stop(Exception):
    pass


def build_A(stop=99):
    nc = bass.Bass("TRN2", target_bir_lowering=False)
    x = nc.dram_tensor("x", [NT * 128, D], F32, kind="ExternalInput").ap()
    cT = nc.dram_tensor("cT", [128, 8], F32, kind="ExternalInput").ap()
    adaw = nc.dram_tensor("adaw", [D, 2048], F32, kind="ExternalInput").ap()
    adabT = nc.dram_tensor("adabT", [128, 16], F32, kind="ExternalInput").ap()
    lngT = nc.dram_tensor("lngT", [128, 8], F32, kind="ExternalInput").ap()
    win = nc.dram_tensor("win", [D, INW], F32, kind="ExternalInput").ap()
    identd = nc.dram_tensor("identd", [128, 128], F32, kind="ExternalInput").ap()
    zo = nc.dram_tensor("z", [NT * 128, INW], F32, kind="ExternalOutput").ap()
    with contextlib.ExitStack() as st:
        P = Prog(nc, st)
        try:
            ident = P.sb([128, 128], BF16, name="ident")
            identf = P.sb([128, 128], F32, name="identf")
            condT = P.sb([128, 8], F32, name="condT")
            adab = P.sb([128, 16], F32, name="adab")
            lng = P.sb([128, 8], F32, name="lng")
            modc = P.sb([128, 16], F32, name="modc")
            Acol = P.sb([128, 8], F32, name="Acol")
            wb = P.sb([128, 8, INW], BF16, name="wb")
            P.dma("sp", identf[:], identd[:, :], w=["identf"])
            P.op("dve", lambda e: e.tensor_copy(out=ident[:], in_=identf[:]), r=["identf"], w=["ident"])
            P.dma("sp", condT[:], cT[:, :], w=["condT"])
            P.dma("sp", adab[:], adabT[:, :], w=["adab"])
            P.dma("sp", lng[:], lngT[:, :], w=["lng"])
            P.op("act", lambda e: e.activation(out=condT[:], in_=condT[:], func=AF.Silu), r=["condT"], w=["condT"])
            emit_mod(P, nc, adaw, 0, 2048, condT, adab[:, 0:16], modc, "a")
            P.op("dve", lambda e: e.scalar_tensor_tensor(out=Acol[:], in0=modc[:, 8:16], scalar=1.0, in1=lng[:], op0=ALU.add, op1=ALU.mult),
                 r=["mod_a", "lng"], w=["Acol"])
            if stop == 1:
                P.dma("sp", zo[0:128, 0:16], modc[:], r=["mod_a"])
                P.dma("sp", zo[0:128, 16:24], Acol[:], r=["Acol"])
                raise _Stop()
            for i in range(8):
                for cc in range(0, INW, 568):
                    P.dma("pool", wb[:, i, cc:cc + 568], win[i * 128:(i + 1) * 128, cc:cc + 568], w=[f"wb{i}"])
            xt = [P.sb([128, D], F32, name=f"xt{k}") for k in range(2)]
            xnb = [P.sb([128, D], BF16, name=f"xnb{k}") for k in range(2)]
            sq = P.sb([128, D], F32, name="sq")
            ss = [P.sb([128, 1], F32, name=f"ss{k}") for k in range(2)]
            hT = [P.sb([128, 8, 128], BF16, name=f"hT{k}") for k in range(2)]
            pT = [P.ps([128, D], BF16, name=f"pT{k}") for k in range(2)]
            pz = [P.ps([128, 512], F32, name=f"pz{k}") for k in range(3)]
            zsb = [P.sb([128, INW], F32, name=f"zsb{k}") for k in range(2)]
            ss2 = P.sb([128, 8], F32, name="ss2")
            groups = [(c0, min(512, INW - c0)) for c0 in range(0, INW, 512)]
            sqf = P.sb([128, D], F32, name="sqf")
            zkeys_of = lambda k: [f"zsb{k}_{gi}" for gi in range(len(groups))]

            def front(t):
                k = t % 2
                X, XN, SS, HT, PT = xt[k], xnb[k], ss[k], hT[k], pT[k]
                P.dma("sp", X[:], x[t * 128:(t + 1) * 128, :], w=[f"xt{k}"])
                P.op("act", lambda e, X=X, SS=SS: e.activation(out=sqf[:], in_=X[:], func=AF.Square, accum_out=SS[:, 0:1]),
                     r=[f"xt{k}"], w=["sqf", f"ss{k}"])
                P.op("dve", lambda e, SS=SS: e.tensor_scalar(out=SS[:], in0=SS[:], scalar1=1.0 / D, scalar2=EPS, op0=ALU.mult, op1=ALU.add),
                     r=[f"ss{k}"], w=[f"ss{k}"])
                P.op("act", lambda e, SS=SS: e.activation(out=SS[:], in_=SS[:], func=AF.Sqrt), r=[f"ss{k}"], w=[f"ss{k}"])
                P.op("dve", lambda e, SS=SS: e.reciprocal(out=SS[:], in_=SS[:]), r=[f"ss{k}"], w=[f"ss{k}"])
                P.op("dve", lambda e, X=X, XN=XN, SS=SS: e.tensor_scalar(out=XN[:], in0=X[:], scalar1=SS[:, 0:1], scalar2=None, op0=ALU.mult),
                     r=[f"xt{k}", f"ss{k}"], w=[f"xnb{k}"])
                for c in range(8):
                    P.op("pe", lambda e, PT=PT, XN=XN, c=c: e.transpose(PT[:, c * 128:(c + 1) * 128], XN[:, c * 128:(c + 1) * 128], ident[:]),
                         r=[f"xnb{k}", "ident"], w=[f"pT{k}"])
                for c in range(8):
                    P.op("act", lambda e, HT=HT, PT=PT, c=c: e.activation(out=HT[:, c, :], in_=PT[:, c * 128:(c + 1) * 128], func=AF.Identity,
                                                                     scale=Acol[:, c:c + 1], bias=modc[:, c:c + 1]),
                         r=[f"pT{k}", "Acol", "mod_a"], w=[f"hT{k}_{c}"])

            def mid(t):
                k = t % 2
                HT, Z = hT[k], zsb[k]
                for gi, (c0, cw) in enumerate(groups):
                    pzz = pz[gi % 3]
                    for c in range(8):
                        P.op("pe", lambda e, pzz=pzz, HT=HT, c=c, c0=c0, cw=cw: e.matmul(pzz[:, 0:cw], lhsT=HT[:, c, :], rhs=wb[:, c, c0:c0 + cw],
                                                                                   start=(c == 0), stop=(c == 7)),
                             r=[f"hT{k}_{c}", f"wb{c}"], w=[f"pz{gi % 3}"])
                    if gi % 2 == 0:
                        P.op("act", lambda e, pzz=pzz, Z=Z, c0=c0, cw=cw: e.activation(out=Z[:, c0:c0 + cw], in_=pzz[:, 0:cw], func=AF.Copy),
                             r=[f"pz{gi % 3}"], w=[f"zsb{k}_{gi}"])
                    else:
                        P.op("dve", lambda e, pzz=pzz, Z=Z, c0=c0, cw=cw: e.tensor_copy(out=Z[:, c0:c0 + cw], in_=pzz[:, 0:cw]),
                             r=[f"pz{gi % 3}"], w=[f"zsb{k}_{gi}"])

            def post(t):
                k = t % 2
                Z = zsb[k]
                zkeys = zkeys_of(k)
                for (s0, nh) in NORM_SEGS:
                    V = Z[:, s0:s0 + nh * 64]
                    P.op("pool", lambda e, V=V, nh=nh: e.tensor_tensor(out=sq[:, 0:nh * 64], in0=V, in1=V, op=ALU.mult),
                         r=zkeys, w=["sq"])
                    P.op("dve", lambda e, nh=nh: e.tensor_reduce(out=ss2[:, 0:nh], in_=sq[:, 0:nh * 64].rearrange("p (h d) -> p h d", d=64),
                                                               axis=AX.X, op=ALU.add), r=["sq"], w=["ss2"])
                    P.op("dve", lambda e, nh=nh: e.tensor_scalar(out=ss2[:, 0:nh], in0=ss2[:, 0:nh], scalar1=1.0 / 64, scalar2=EPS,
                                                               op0=ALU.mult, op1=ALU.add), r=["ss2"], w=["ss2"])
                    P.op("act", lambda e, nh=nh: e.activation(out=ss2[:, 0:nh], in_=ss2[:, 0:nh], func=AF.Sqrt), r=["ss2"], w=["ss2"])
                    P.op("dve", lambda e, nh=nh: e.reciprocal(out=ss2[:, 0:nh], in_=ss2[:, 0:nh]), r=["ss2"], w=["ss2"])
                    P.op("dve", lambda e, V=V, nh=nh: e.tensor_tensor(out=V.rearrange("p (h d) -> p h d", d=64),
                                                                    in0=V.rearrange("p (h d) -> p h d", d=64),
                                                                    in1=ss2[:, 0:nh].unsqueeze(2).to_broadcast([128, nh, 64]), op=ALU.mult),
                         r=zkeys + ["ss2"], w=zkeys)
                P.op("act", lambda e, Z=Z: e.activation(out=Z[:, 1280:1304], in_=Z[:, 1280:1304], func=AF.Sigmoid), r=zkeys, w=zkeys)
                P.dma("sp", zo[t * 128:(t + 1) * 128, :], Z[:], r=zkeys)

            front(0)
            for t in range(NT):
                mid(t)
                if t + 1 < NT:
                    front(t + 1)
                post(t)
        except _Stop:
            pass
        import os
        if os.environ.get("TRUNC"):
            N = int(os.environ["TRUNC"])
            for q, I in enumerate(P.ins[:N]):
                pass
            print("TRUNC at", N, "of", len(P.ins), P.ins[N - 1]["e"], P.ins[N - 1]["r"], P.ins[N - 1]["w"])
            P.ins = P.ins[:N]
            P.dma("sp", zo[0:128, 0:16], modc[:], r=["mod_a"])
        print("A", P.emit())
    return nc


def inputs_A(inp, l, core):
    b, r = core // 4, core % 4
    xl = inp["x_cur"][b, r * 2048:(r + 1) * 2048]
    return {
        "x": np.ascontiguousarray(xl),
        "cT": np.ascontiguousarray(inp["c"][b].reshape(8, 128).T),
        "adaw": np.ascontiguousarray(inp["ada_w"][l][:, 0:2048]),
        "adabT": np.ascontiguousarray(inp["ada_b"][l][0:2048].reshape(16, 128).T),
        "lngT": np.ascontiguousarray(inp["ln1_g"][l].reshape(8, 128).T),
        "win": np.ascontiguousarray(inp["w_in"][l]),
        "identd": np.eye(128, dtype=np.float32),
    }

import contextlib
import numpy as np
import concourse.bass as bass

S = 8192
NEGB = -30000.0
GELU_C = 1.5957691216057308


def build_B1(nslots=32):
    nc = bass.Bass("TRN2", target_bir_lowering=False)
    D_ = lambda n, s: nc.dram_tensor(n, s, F32, kind="ExternalInput").ap()
    qnT = D_("qnT", [64, 16384]); qaugc = D_("qaugc", [5, 16384]); gates = D_("gates", [128, 384])
    kcT = D_("kcT", [2, 64, 8224]); ksT = D_("ksT", [2, 64, S]); vsw = D_("vsw", [2, S, 64])
    kaugc = D_("kaugc", [4, S]); kcaugc = D_("kcaugc", [5, 512])
    gq = D_("gq", [64, 1]); gk = D_("gk", [64, 1])
    w1r = D_("w1r", [2, 64, 4096]); posT = D_("posT", [64, 64]); w2r = D_("w2r", [128, 128]); incid = D_("incid", [512, 128])
    enu = D_("enu", [128, S])
    masks = D_("masks", [6, 128, 128])
    cmt = D_("cmt", [32, 128, 512]); addt = D_("addt", [32, 128, 128])
    identd = D_("identd", [128, 128])
    o = nc.dram_tensor("o", [4096, 256], F32, kind="ExternalOutput").ap()
    with contextlib.ExitStack() as st:
        P = Prog(nc, st)
        ident = P.sb([128, 128], BF16, name="ident")
        M4 = P.sb([128, 6, 512], BF16, name="M4")
        Mt = P.sb([128, 6, 128], BF16, name="Mt")
        EN = P.sb([128, S], BF16, name="EN")
        KS = P.sb([68, S], BF16, name="KS"); KW = P.sb([68, S], BF16, name="KW")
        QN = P.sb([69, 16384], BF16, name="QN")
        VS = P.sb([128, 64, 65], BF16, name="VS"); VW = P.sb([128, 64, 65], BF16, name="VW")
        KC = P.sb([69, 512], BF16, name="KC"); VC = P.sb([128, 4, 193], BF16, name="VC")
        G = P.sb([128, 32, 12], F32, name="G")
        gqs = P.sb([64, 1], F32, name="gqs"); gks = P.sb([64, 1], F32, name="gks")
        stg = [P.sb([64, 2048], F32, name=f"stg{k}") for k in range(2)]
        KCT = P.sb([64, 8224], BF16, name="KCT")
        W1b = P.sb([64, 4096], BF16, name="W1b"); posb = P.sb([64, 64], BF16, name="posb"); W2b = P.sb([128, 128], BF16, name="W2b")
        pbias = P.sb([128, 2], F32, name="pbias")
        xh = P.sb([128, 512], F32, name="xh"); x2 = P.sb([128, 512], F32, name="x2"); sgm = P.sb([128, 512], F32, name="sgm")
        GE = P.sb([128, 512], BF16, name="GE")
        sqk = P.sb([128, 64], F32, name="sqk"); ssk = P.sb([128, 1], F32, name="ssk"); kcn = P.sb([128, 64], BF16, name="kcn")
        CMs = [P.sb([128, 512], BF16, name=f"CMs{k}") for k in range(2)]
        ADs = [P.sb([128, 128], F32, name=f"ADs{k}") for k in range(2)]
        PT = [P.sb([128, 512], BF16, name=f"PT{k}") for k in range(4)]
        SELT4 = P.sb([128, 512], BF16, name="SELT4")
        IMP = P.sb([128, 128], F32, name="IMP"); IMP2 = P.sb([128, 128], F32, name="IMP2"); selb = P.sb([128, 128], BF16, name="selb")
        m8a = P.sb([128, 8], F32, name="m8a"); m8b = P.sb([128, 8], F32, name="m8b")
        rd = P.sb([128, 4], F32, name="rd"); cf = P.sb([128, 4], F32, name="cf")
        OACC = [P.sb([128, 4, 64], F32, name=f"OACC{k}") for k in range(2)]
        ps_sc = [P.ps([128, 512], F32, name=f"ps_sc{k}") for k in range(2)]
        ps_o = [P.ps([128, 512], F32, name=f"ps_o{k}") for k in range(4)]
        ps_tr = P.ps([128, 1024], BF16, name="ps_tr")
        ps_m = P.ps([128, 512], F32, name="ps_m")
        P.dma("pool", ident[:], identd[:, :], w=["ident"])
        for m in range(6):
            P.dma("pool", Mt[:, m, :], masks[m, :, :], w=["Mt"])
        for m in range(6):
            for h in range(4):
                P.op("dve", lambda e, m=m, h=h: e.tensor_copy(out=M4[:, m, h * 128:(h + 1) * 128], in_=Mt[:, m, :]), r=["Mt"], w=["M4"])
        for c in range(4):
            cs = slice(c * 2048, (c + 1) * 2048)
            P.dma("pool", EN[:, cs], enu[:, cs], w=["EN"])
            P.dma("pool", KS[64:68, cs], kaugc[:, cs], w=["KSaug"])
            P.dma("pool", KW[64:68, cs], kaugc[:, cs], w=["KWaug"])
        P.dma("pool", KC[64:69, :], kcaugc[:, :], w=["KCaug"])
        P.dma("sp", gqs[:], gq[:, :], w=["gqs"]); P.dma("sp", gks[:], gk[:, :], w=["gks"])
        P.dma("sp", G[:].rearrange("p i c -> p (i c)"), gates[:, :], w=["G"])
        P.dma("pool", posb[:], posT[:, :], w=["posb"]); P.dma("pool", W2b[:], w2r[:, :], w=["W2b"])
        P.op("dve", lambda e: e.memset(VS[:, :, 64:65], 1.0), w=["VSone"])
        P.op("dve", lambda e: e.memset(VW[:, :, 64:65], 1.0), w=["VWone"])
        P.op("dve", lambda e: e.memset(VC[:, :, 64:65], 1.0), w=["VCone"])
        P.op("dve", lambda e: e.memset(GE[:], 0.0), w=["GE"])
        P.dma("pool", VC[:, :, 65:193], incid.rearrange("(nt p) j -> p nt j", p=128), w=["VCinc"])
        for kv, (KT, kk) in enumerate(((KS, "KS"), (KW, "KW"))):
            for c in range(4):
                sg = stg[c % 2]; cs = slice(c * 2048, (c + 1) * 2048)
                P.dma("sp", sg[:], ksT[kv, :, cs], w=[f"stg{c % 2}"])
                P.op("act", lambda e, sg=sg, KT=KT, cs=cs: e.activation(out=KT[0:64, cs], in_=sg[:], func=AF.Identity, scale=gks[:, 0:1]),
                     r=[f"stg{c % 2}", "gks"], w=[kk])
        for kv, (VT, vk) in enumerate(((VS, "VS"), (VW, "VW"))):
            for c in range(4):
                P.dma("pool", VT[:, c * 16:(c + 1) * 16, 0:64], vsw[kv, c * 2048:(c + 1) * 2048, :].rearrange("(kt p) d -> p kt d", p=128), w=[vk])
        for c in range(8):
            sg = stg[c % 2]; cs = slice(c * 2048, (c + 1) * 2048)
            P.dma("sp", sg[:], qnT[:, cs], w=[f"stg{c % 2}"])
            P.op("dve", lambda e, sg=sg, cs=cs: e.tensor_scalar(out=QN[0:64, cs], in0=sg[:], scalar1=gqs[:, 0:1], scalar2=None, op0=ALU.mult),
                 r=[f"stg{c % 2}", "gqs"], w=["QN"])
            P.dma("pool", QN[64:69, cs], qaugc[:, cs], w=["QNaug"])
        KCv = KCT[:].rearrange("p (n s) -> p n s", s=16)
        for kv in range(2):
            for c in range(4):
                P.dma("pool", KCT[:, c * 2056:(c + 1) * 2056], kcT[kv, :, c * 2056:(c + 1) * 2056], w=["KCT"])
            P.dma("pool", W1b[:], w1r[kv, :, :], w=["W1b"])
            for l in range(32):
                rhs = KCv[:, 0:511, l] if l < 16 else KCv[:, 1:512, l - 16]
                P.op("pe", lambda e, l=l, rhs=rhs: e.matmul(ps_m[:, 0:511], lhsT=W1b[:, l * 128:(l + 1) * 128], rhs=rhs, start=(l == 0), stop=(l == 31)),
                     r=["W1b", "KCT"], w=["ps_m"])
            for l in range(32):
                P.op("pe", lambda e, l=l, kv=kv: e.matmul(ps_sc[0][:, 0:1], lhsT=W1b[:, l * 128:(l + 1) * 128], rhs=posb[:, kv * 32 + l:kv * 32 + l + 1],
                                                      start=(l == 0), stop=(l == 31)), r=["W1b", "posb"], w=["ps_sc0"])
            P.op("dve", lambda e, kv=kv: e.tensor_copy(out=pbias[:, kv:kv + 1], in_=ps_sc[0][:, 0:1]), r=["ps_sc0"], w=["pbias"])
            P.op("act", lambda e, kv=kv: e.activation(out=xh[:, 0:511], in_=ps_m[:, 0:511], func=AF.Identity, bias=pbias[:, kv:kv + 1], scale=1.0),
                 r=["ps_m", "pbias"], w=["xh"])
            P.op("dve", lambda e: e.tensor_tensor(out=x2[:, 0:511], in0=xh[:, 0:511], in1=xh[:, 0:511], op=ALU.mult), r=["xh"], w=["x2"])
            P.op("dve", lambda e: e.tensor_scalar(out=x2[:, 0:511], in0=x2[:, 0:511], scalar1=0.044715, scalar2=1.0, op0=ALU.mult, op1=ALU.add), r=["x2"], w=["x2"])
            P.op("dve", lambda e: e.tensor_tensor(out=x2[:, 0:511], in0=x2[:, 0:511], in1=xh[:, 0:511], op=ALU.mult), r=["x2", "xh"], w=["x2"])
            P.op("act", lambda e: e.activation(out=sgm[:, 0:511], in_=x2[:, 0:511], func=AF.Sigmoid, scale=GELU_C), r=["x2"], w=["sgm"])
            P.op("dve", lambda e: e.tensor_tensor(out=GE[:, 0:511], in0=xh[:, 0:511], in1=sgm[:, 0:511], op=ALU.mult), r=["xh", "sgm", "GE"], w=["GE"])
            for nt in range(4):
                P.op("pe", lambda e, nt=nt, kv=kv: e.matmul(ps_sc[1][:, 0:64], lhsT=GE[:, nt * 128:(nt + 1) * 128], rhs=W2b[:, kv * 64:(kv + 1) * 64], start=True, stop=True),
                     r=["GE", "W2b"], w=["ps_sc1"])
                if kv == 0:
                    P.op("act", lambda e: e.activation(out=sqk[:], in_=ps_sc[1][:, 0:64], func=AF.Square, accum_out=ssk[:, 0:1]), r=["ps_sc1"], w=["sqk", "ssk"])
                    P.op("dve", lambda e: e.tensor_scalar(out=ssk[:], in0=ssk[:], scalar1=1.0 / 64, scalar2=1e-6, op0=ALU.mult, op1=ALU.add), r=["ssk"], w=["ssk"])
                    P.op("act", lambda e: e.activation(out=ssk[:], in_=ssk[:], func=AF.Sqrt), r=["ssk"], w=["ssk"])
                    P.op("dve", lambda e: e.reciprocal(out=ssk[:], in_=ssk[:]), r=["ssk"], w=["ssk"])
                    P.op("dve", lambda e: e.tensor_scalar(out=kcn[:], in0=ps_sc[1][:, 0:64], scalar1=ssk[:, 0:1], scalar2=None, op0=ALU.mult),
                         r=["ps_sc1", "ssk"], w=["kcn"])
                    P.op("pe", lambda e: e.transpose(ps_tr[0:64, 0:128], kcn[:, 0:64], ident[:]), r=["kcn", "ident"], w=["ps_tr"])
                    P.op("act", lambda e, nt=nt: e.activation(out=KC[0:64, nt * 128:(nt + 1) * 128], in_=ps_tr[0:64, 0:128], func=AF.Identity, scale=gks[:, 0:1]),
                         r=["ps_tr", "gks"], w=["KC"])
                else:
                    P.op("act", lambda e, nt=nt: e.activation(out=VC[:, nt, 0:64], in_=ps_sc[1][:, 0:64], func=AF.Copy), r=["ps_sc1"], w=["VC"])
        Gv = G[:].rearrange("p i (h b) -> p i h b", b=3)
        u = 0
        scb = [ps_sc[0], ps_sc[1], ps_m]; sckeys = ["ps_sc0", "ps_sc1", "ps_m"]

        def attend(mms_fn, kts, VT, vkeys, ncol, okeys_first):
            nonlocal u
            n = len(kts)
            units = []
            for ki, kt in enumerate(kts):
                units.append((ki, kt, scb[u % 3], sckeys[u % 3], PT[u % 4], f"PT{u % 4}"))
                u += 1

            def scores(U):
                ki, kt, sc, sck, pt, ptk = U
                mms = mms_fn(kt, sc)
                for mi, (o_, l_, r_, rk) in enumerate(mms):
                    P.op("pe", lambda e, o_=o_, l_=l_, r_=r_, mi=mi, nm=len(mms): e.matmul(o_, lhsT=l_, rhs=r_, start=(mi == 0), stop=(mi == nm - 1)), r=rk, w=[sck])

            def exp_pv(U):
                ki, kt, sc, sck, pt, ptk = U
                P.op("act", lambda e, pt=pt, sc=sc: e.activation(out=pt[:], in_=sc[:, 0:512], func=AF.Exp, scale=0.125), r=[sck], w=[ptk])
                for h in range(4):
                    P.op("pe", lambda e, pt=pt, h=h, kt=kt, ki=ki: e.matmul(ps_o[h][:, 0:ncol], lhsT=pt[:, h * 128:(h + 1) * 128], rhs=VT[:, kt, 0:ncol],
                                                                      start=(ki == 0), stop=(ki == n - 1)), r=[ptk] + vkeys, w=[f"ps_o{h}"])
            q = []
            for U in units:
                scores(U)
                q.append(U)
                if len(q) > 2:
                    exp_pv(q.pop(0))
            while q:
                exp_pv(q.pop(0))

        def finish(i, br, OA, oak, first):
            for h in range(4):
                P.op("dve", lambda e, h=h: e.tensor_scalar(out=rd[:, h:h + 1], in0=ps_o[h][:, 64:65], scalar1=1e-30, scalar2=None, op0=ALU.max), r=[f"ps_o{h}"], w=["rd"])
            P.op("dve", lambda e: e.reciprocal(out=rd[:], in_=rd[:]), r=["rd"], w=["rd"])
            P.op("dve", lambda e, i=i, br=br: e.tensor_tensor(out=cf[:], in0=rd[:], in1=Gv[:, i, :, br], op=ALU.mult), r=["rd", "G"], w=["cf"])
            for h in range(4):
                if first:
                    P.op("dve", lambda e, h=h, OA=OA: e.tensor_scalar(out=OA[:, h, :], in0=ps_o[h][:, 0:64], scalar1=cf[:, h:h + 1], scalar2=None, op0=ALU.mult),
                         r=[f"ps_o{h}", "cf"], w=[oak])
                else:
                    P.op("dve", lambda e, h=h, OA=OA: e.scalar_tensor_tensor(out=OA[:, h, :], in0=ps_o[h][:, 0:64], scalar=cf[:, h:h + 1], in1=OA[:, h, :],
                                                                           op0=ALU.mult, op1=ALU.add), r=[f"ps_o{h}", "cf", oak], w=[oak])

        for i in range(nslots):
            cols = slice(i * 512, (i + 1) * 512)
            CM = CMs[i % 2]; cmk = f"CMs{i % 2}"; AD = ADs[i % 2]; adk = f"ADs{i % 2}"
            OA = OACC[i % 2]; oak = f"OACC{i % 2}"
            P.dma("pool", CM[:], cmt[i, :, :], w=[cmk])
            P.dma("sp", AD[:], addt[i, :, :], w=[adk])
            def mm_cmp(nt, sc):
                mms = [(sc[:, 0:512], KC[0:69, nt * 128:(nt + 1) * 128], QN[0:69, cols], ["KC", "KCaug", "QN", "QNaug"])]
                for h in range(4):
                    mms.append((sc[:, h * 128:(h + 1) * 128], ident[:], CM[:, nt * 128:(nt + 1) * 128], ["ident", cmk]))
                return mms
            attend(mm_cmp, list(range(min(3, (256 * i + 224) // 2048) + 1)), VC, ["VC", "VCone", "VCinc"], 193, None)
            finish(i, 0, OA, oak, True)
            P.op("dve", lambda e: e.tensor_scalar(out=IMP[:], in0=ps_o[0][:, 65:193], scalar1=rd[:, 0:1], scalar2=None, op0=ALU.mult), r=["ps_o0", "rd"], w=["IMP"])
            for h in range(1, 4):
                P.op("dve", lambda e, h=h: e.scalar_tensor_tensor(out=IMP[:], in0=ps_o[h][:, 65:193], scalar=rd[:, h:h + 1], in1=IMP[:], op0=ALU.mult, op1=ALU.add),
                     r=[f"ps_o{h}", "rd", "IMP"], w=["IMP"])
            P.op("dve", lambda e, AD=AD: e.tensor_tensor(out=IMP[:], in0=IMP[:], in1=AD[:], op=ALU.add), r=["IMP", adk], w=["IMP"])
            P.op("dve", lambda e: e.max(out=m8a[:], in_=IMP[:]), r=["IMP"], w=["m8a"])
            P.op("dve", lambda e: e.match_replace(out=IMP2[:], in_to_replace=m8a[:], in_values=IMP[:], imm_value=-3.0e38), r=["IMP", "m8a"], w=["IMP2"])
            P.op("dve", lambda e: e.max(out=m8b[:], in_=IMP2[:]), r=["IMP2"], w=["m8b"])
            P.op("dve", lambda e: e.tensor_scalar(out=IMP2[:], in0=IMP[:], scalar1=m8b[:, 7:8], scalar2=None, op0=ALU.is_ge), r=["IMP", "m8b"], w=["IMP2"])
            P.op("dve", lambda e: e.tensor_scalar(out=selb[:], in0=IMP2[:], scalar1=-1.0, scalar2=-NEGB, op0=ALU.add, op1=ALU.mult), r=["IMP2"], w=["selb"])
            P.op("pe", lambda e: e.transpose(ps_tr[:, 0:128], selb[:, :], ident[:]), r=["selb", "ident"], w=["ps_tr"])
            for h in range(4):
                P.op("act", lambda e, h=h: e.activation(out=SELT4[:, h * 128:(h + 1) * 128], in_=ps_tr[:, 0:128], func=AF.Copy), r=["ps_tr"], w=["SELT4"])
            def mm_win(kt, sc):
                mms = [(sc[:, 0:512], KW[0:68, kt * 128:(kt + 1) * 128], QN[0:68, cols], ["KW", "KWaug", "QN", "QNaug"])]
                off = kt - 2 * i
                mi = {-4: 2, -3: 3, 0: 4, 1: 5}.get(off)
                if mi is not None:
                    mms.append((sc[:, 0:512], ident[:], M4[:, mi, :], ["ident", "M4"]))
                return mms
            attend(mm_win, [kt for kt in range(2 * i - 4, 2 * i + 2) if kt >= 0], VW, ["VW", "VWone"], 65, None)
            finish(i, 2, OA, oak, False)
            def mm_sel(kt, sc):
                mms = [(sc[:, 0:512], KS[0:68, kt * 128:(kt + 1) * 128], QN[0:68, cols], ["KS", "KSaug", "QN", "QNaug"]),
                       (sc[:, 0:512], EN[:, kt * 128:(kt + 1) * 128], SELT4[:, :], ["EN", "SELT4"])]
                if kt == 2 * i:
                    mms.append((sc[:, 0:512], ident[:], M4[:, 0, :], ["ident", "M4"]))
                if kt == 2 * i + 1:
                    mms.append((sc[:, 0:512], ident[:], M4[:, 1, :], ["ident", "M4"]))
                return mms
            attend(mm_sel, list(range(2 * i + 2)), VS, ["VS", "VSone"], 65, None)
            finish(i, 1, OA, oak, False)
            P.dma("sp", o[i * 128:(i + 1) * 128, :], OA[:].rearrange("p h d -> p (h d)"), r=[oak])
        print("B1", P.emit())
    return nc


def consts_B1(parity, slopes):
    k = np.arange(128)
    tri = np.where(k[None, :] >= k[:, None], 0.0, NEGB).astype(np.float32)
    tri2 = np.where(k[:, None] > k[None, :], 0.0, NEGB).astype(np.float32)
    opn = np.zeros((128, 128), np.float32); cls = np.full((128, 128), NEGB, np.float32)
    if parity == 0:
        masks = np.stack([tri, cls, tri2, opn, tri, cls])
    else:
        masks = np.stack([opn, tri, cls, tri2, opn, tri])
    n_ = np.arange(128)
    cmt = np.zeros((32, 128, 512), np.float32); addt = np.zeros((32, 128, 128), np.float32)
    j = np.arange(128)
    for i in range(32):
        qt = 2 * i + parity
        t = qt * 128 + np.arange(128)
        for nt in range(4):
            cend = 16 * (128 * nt + n_) + 31
            cmt[i, :, nt * 128:(nt + 1) * 128] = np.where(cend[:, None] <= t[None, :], 0.0, NEGB)
        cur = t // 64
        forced = (j[None, :] == 0) | (j[None, :] == cur[:, None]) | (j[None, :] == cur[:, None] - 1)
        addt[i] = np.where(j[None, :] <= cur[:, None], np.where(forced, 1e4, 0.0), -1e30)
    cmt[:, 127, 384:512] = NEGB
    enu = (np.arange(S)[None, :] // 64 == j[:, None]).astype(np.float32)
    kaug = np.stack([np.ones(S), np.full(S, 128.0), np.arange(S) % 128, (np.arange(S) // 128) * 128.0]).astype(np.float32)
    n512 = np.arange(512)
    kcaug = np.stack([np.ones(512), np.full(512, 128.0), 16.0 * (n512 % 128), 2048.0 * (n512 // 128), np.full(512, 15.5)]).astype(np.float32)
    qa = np.zeros((5, 32, 4, 128), np.float32)
    qp = np.arange(128.0)
    for i in range(32):
        qt = 2 * i + parity
        for h in range(4):
            Sx = 8.0 * slopes[h]
            qa[0, i, h] = -Sx * qp; qa[1, i, h] = -Sx * qt; qa[2:5, i, h] = Sx
    cs = 16 * np.arange(512); ss = 64 * np.arange(128)
    inc = ((cs[:, None] <= ss[None, :] + 63) & (cs[:, None] + 31 >= ss[None, :])).astype(np.float32)
    inc[511] = 0.0
    return dict(masks=masks, cmt=cmt, addt=addt, enu=enu, kaugc=kaug, kcaugc=kcaug, qaugc=qa.reshape(5, 16384), incid=inc,
                identd=np.eye(128, dtype=np.float32))


def inputs_B1(zb, g, parity, gq, gk, pos, w1, w2):
    slopes = 2.0 ** (-8.0 * np.arange(1, 9) / 8)[4 * g:4 * g + 4]
    d = consts_B1(parity, slopes)
    own = (np.arange(4096) // 128 * 2 + parity) * 128 + np.arange(4096) % 128
    q = zb[own][:, 0:512].reshape(32, 128, 8, 64)[:, :, 4 * g:4 * g + 4]
    d["qnT"] = np.ascontiguousarray(q.transpose(3, 0, 2, 1).reshape(64, 16384))
    d["gates"] = np.ascontiguousarray(zb[own][:, 1280 + 12 * g:1280 + 12 * (g + 1)].reshape(32, 128, 12).transpose(1, 0, 2).reshape(128, 384))
    kc = zb[:, 512 + 64 * g:512 + 64 * (g + 1)]; vc = zb[:, 640 + 64 * g:640 + 64 * (g + 1)]
    kcT = np.zeros((2, 64, 8224), np.float32); kcT[0, :, :S] = kc.T; kcT[1, :, :S] = vc.T
    d["kcT"] = kcT
    d["ksT"] = np.ascontiguousarray(np.stack([zb[:, 768 + 64 * g:768 + 64 * (g + 1)].T, zb[:, 1024 + 64 * g:1024 + 64 * (g + 1)].T]))
    d["vsw"] = np.ascontiguousarray(np.stack([zb[:, 896 + 64 * g:896 + 64 * (g + 1)], zb[:, 1152 + 64 * g:1152 + 64 * (g + 1)]]))
    d["gq"] = np.ascontiguousarray(gq.reshape(64, 1)); d["gk"] = np.ascontiguousarray(gk.reshape(64, 1))
    d["w1r"] = np.ascontiguousarray(w1.reshape(2, 32, 64, 128).transpose(0, 2, 1, 3).reshape(2, 64, 4096))
    d["posT"] = np.ascontiguousarray(pos.transpose(2, 0, 1).reshape(64, 64))
    d["w2r"] = np.ascontiguousarray(np.concatenate([w2[0], w2[1]], axis=1))
    return d

import contextlib
import numpy as np
import concourse.bass as bass

S = 8192
NEGB = -30000.0


def build_B2(nslots=32):
    nq = nslots * 128
    nc = bass.Bass("TRN2", target_bir_lowering=False)
    D_ = lambda n, s: nc.dram_tensor(n, s, F32, kind="ExternalInput").ap()
    qmT = D_("qmT", [4, 64, 4096]); kmT = D_("kmT", [4, 64, S]); vm = D_("vm", [4, S, 64])
    qaugc = D_("qaugc", [4, 4, 4096]); kaugc = D_("kaugc", [4, S])
    gq = D_("gq", [64, 1]); gk = D_("gk", [64, 1])
    emoba = D_("emoba", [32, S])
    maska = D_("maska", [128, 128]); maskb = D_("maskb", [128, 128])
    mneg = D_("mneg", [128, 32 * 32]); mvalid = D_("mvalid", [128, 32 * 32]); mcur = D_("mcur", [128, 32 * 32])
    identd = D_("identd", [128, 128])
    o = nc.dram_tensor("o", [4096, 256], F32, kind="ExternalOutput").ap()
    with contextlib.ExitStack() as st:
        P = Prog(nc, st)
        ident = P.sb([128, 128], BF16, name="ident")
        MA = P.sb([128, 128], BF16, name="MA"); MB = P.sb([128, 128], BF16, name="MB")
        EM = P.sb([32, S], BF16, name="EM")
        KA = P.sb([68, S], BF16, name="KA")
        QA = P.sb([68, 4096], BF16, name="QA")
        VA = P.sb([128, 64, 65], BF16, name="VA")
        MSB = P.sb([32, 4096], BF16, name="MSB")
        MNEG = P.sb([128, 1024], F32, name="MNEG"); MVAL = P.sb([128, 1024], F32, name="MVAL"); MCUR = P.sb([128, 1024], F32, name="MCUR")
        gqs = P.sb([64, 1], F32, name="gqs"); gks = P.sb([64, 1], F32, name="gks")
        stg = [P.sb([64, 2048], F32, name=f"stg{k}") for k in range(2)]
        kmf = P.sb([64, 32], F32, name="kmf")
        kmb = P.sb([64, 32], BF16, name="kmb")
        gm = P.sb([128, 32], F32, name="gm"); m8 = P.sb([128, 8], F32, name="m8")
        al = P.sb([128, 32], F32, name="al"); sbb = P.sb([128, 32], BF16, name="sbb")
        PT = [P.sb([128, 512], BF16, name=f"PT{k}") for k in range(4)]
        rden = P.sb([128, 4], F32, name="rden")
        osb = [P.sb([128, 4, 64], F32, name=f"osb{k}") for k in range(2)]
        ps_sc = [P.ps([128, 512], F32, name=f"ps_sc{k}") for k in range(2)]
        ps_o = [P.ps([128, 512], F32, name=f"ps_o{k}") for k in range(4)]
        ps_tr = P.ps([128, 1024], BF16, name="ps_tr")
        ps_g = P.ps([128, 512], F32, name="ps_g")
        P.dma("pool", ident[:], identd[:, :], w=["ident"])
        P.dma("pool", MA[:], maska[:, :], w=["MA"])
        P.dma("pool", MB[:], maskb[:, :], w=["MB"])
        for c in range(4):
            P.dma("pool", EM[:, c * 2048:(c + 1) * 2048], emoba[:, c * 2048:(c + 1) * 2048], w=["EM"])
            P.dma("pool", KA[64:68, c * 2048:(c + 1) * 2048], kaugc[:, c * 2048:(c + 1) * 2048], w=["KAaug"])
        P.dma("sp", MNEG[:], mneg[:, :], w=["MNEG"])
        P.dma("sp", MVAL[:], mvalid[:, :], w=["MVAL"])
        P.dma("sp", MCUR[:], mcur[:, :], w=["MCUR"])
        P.dma("sp", gqs[:], gq[:, :], w=["gqs"])
        P.dma("sp", gks[:], gk[:, :], w=["gks"])
        P.op("dve", lambda e: e.memset(VA[:, :, 64:65], 1.0), w=["VAone"])
        u = 0
        scb = [ps_sc[0], ps_sc[1], ps_g]; sckeys = ["ps_sc0", "ps_sc1", "ps_g"]
        for h in range(4):
            for c in range(4):
                sg = stg[c % 2]
                P.dma("sp", sg[:], kmT[h, :, c * 2048:(c + 1) * 2048], w=[f"stg{c % 2}"])
                P.op("dve", lambda e, sg=sg: e.tensor_scalar(out=sg[:], in0=sg[:], scalar1=gks[:, 0:1], scalar2=None, op0=ALU.mult),
                     r=[f"stg{c % 2}", "gks"], w=[f"stg{c % 2}"])
                P.op("act", lambda e, sg=sg, c=c: e.activation(out=KA[0:64, c * 2048:(c + 1) * 2048], in_=sg[:], func=AF.Copy),
                     r=[f"stg{c % 2}"], w=["KA"])
                P.op("dve", lambda e, sg=sg, c=c: e.tensor_reduce(out=kmf[:, c * 8:(c + 1) * 8], in_=sg[:].rearrange("p (b k) -> p b k", k=256),
                                                                 axis=AX.X, op=ALU.add), r=[f"stg{c % 2}"], w=["kmf"])
            P.op("dve", lambda e: e.tensor_scalar(out=kmb[:], in0=kmf[:], scalar1=1.0 / 256, scalar2=None, op0=ALU.mult), r=["kmf"], w=["kmb"])
            for c in range((nq + 2047) // 2048):
                w_ = min(2048, nq - c * 2048)
                sg = stg[c % 2]
                P.dma("sp", sg[:, 0:w_], qmT[h, :, c * 2048:c * 2048 + w_], w=[f"stg{c % 2}"])
                P.op("dve", lambda e, sg=sg, c=c, w_=w_: e.tensor_scalar(out=QA[0:64, c * 2048:c * 2048 + w_], in0=sg[:, 0:w_], scalar1=gqs[:, 0:1],
                                                                       scalar2=None, op0=ALU.mult), r=[f"stg{c % 2}", "gqs"], w=["QA"])
            P.dma("pool", QA[64:68, 0:nq], qaugc[h, :, 0:nq], w=["QAaug"])
            for c in range(4):
                P.dma("pool", VA[:, c * 16:(c + 1) * 16, 0:64], vm[h, c * 2048:(c + 1) * 2048, :].rearrange("(kt p) d -> p kt d", p=128), w=["VA"])
            for i in range(nslots):
                P.op("pe", lambda e, i=i: e.matmul(ps_g[:, 0:32], lhsT=QA[0:64, i * 128:(i + 1) * 128], rhs=kmb[:, :], start=True, stop=True),
                     r=["QA", "kmb"], w=["ps_g"])
                P.op("dve", lambda e, i=i: e.tensor_tensor(out=gm[:], in0=ps_g[:, 0:32], in1=MNEG[:, i * 32:(i + 1) * 32], op=ALU.add),
                     r=["ps_g", "MNEG"], w=["gm"])
                P.op("dve", lambda e: e.max(out=m8[:], in_=gm[:]), r=["gm"], w=["m8"])
                P.op("dve", lambda e: e.tensor_scalar(out=al[:], in0=gm[:], scalar1=m8[:, 2:3], scalar2=None, op0=ALU.is_ge), r=["gm", "m8"], w=["al"])
                P.op("dve", lambda e, i=i: e.tensor_tensor(out=al[:], in0=al[:], in1=MVAL[:, i * 32:(i + 1) * 32], op=ALU.mult), r=["al", "MVAL"], w=["al"])
                P.op("dve", lambda e, i=i: e.tensor_tensor(out=al[:], in0=al[:], in1=MCUR[:, i * 32:(i + 1) * 32], op=ALU.add), r=["al", "MCUR"], w=["al"])
                P.op("dve", lambda e: e.tensor_scalar(out=sbb[:], in0=al[:], scalar1=-1.0, scalar2=-NEGB, op0=ALU.add, op1=ALU.mult), r=["al"], w=["sbb"])
                P.op("pe", lambda e: e.transpose(ps_tr[0:32, 0:128], sbb[:, 0:32], ident[:]), r=["sbb", "ident"], w=["ps_tr"])
                P.op("act", lambda e, i=i: e.activation(out=MSB[:, i * 128:(i + 1) * 128], in_=ps_tr[0:32, 0:128], func=AF.Copy), r=["ps_tr"], w=["MSB"])
            units = []
            for gi in range(nslots // 4):
                i0 = 4 * gi
                nkt = 2 * (i0 + 3) + 2
                for kt in range(nkt):
                    units.append((gi, i0, kt, nkt, u))
                    u += 1

            def scores(U):
                gi, i0, kt, nkt, uu = U
                cols = slice(i0 * 128, (i0 + 4) * 128)
                sc = scb[uu % 3]; sck = sckeys[uu % 3]
                mms = [(sc[:, 0:512], KA[0:68, kt * 128:(kt + 1) * 128], QA[0:68, cols], ["KA", "KAaug", "QA", "QAaug"]),
                       (sc[:, 0:512], EM[0:32, kt * 128:(kt + 1) * 128], MSB[0:32, cols], ["EM", "MSB"])]
                for j in range(4):
                    if kt == 2 * (i0 + j):
                        mms.append((sc[:, j * 128:(j + 1) * 128], ident[:], MA[:], ["ident", "MA"]))
                    if kt == 2 * (i0 + j) + 1:
                        mms.append((sc[:, j * 128:(j + 1) * 128], ident[:], MB[:], ["ident", "MB"]))
                for mi, (o_, l_, r_, rk) in enumerate(mms):
                    P.op("pe", lambda e, o_=o_, l_=l_, r_=r_, mi=mi, n=len(mms): e.matmul(o_, lhsT=l_, rhs=r_, start=(mi == 0), stop=(mi == n - 1)),
                         r=rk, w=[sck])

            def exp_pv(U):
                gi, i0, kt, nkt, uu = U
                sc = scb[uu % 3]; sck = sckeys[uu % 3]; pt = PT[uu % 4]; ptk = f"PT{uu % 4}"
                P.op("act", lambda e, pt=pt, sc=sc: e.activation(out=pt[:], in_=sc[:, 0:512], func=AF.Exp, scale=0.125), r=[sck], w=[ptk])
                for j in range(4):
                    last = 2 * (i0 + j) + 1
                    if kt <= last:
                        P.op("pe", lambda e, pt=pt, j=j, kt=kt, last=last: e.matmul(ps_o[j][:, 0:65], lhsT=pt[:, j * 128:(j + 1) * 128],
                                                                              rhs=VA[:, kt, 0:65], start=(kt == 0), stop=(kt == last)),
                             r=[ptk, "VA", "VAone"], w=[f"ps_o{j}"])
                if kt == nkt - 1:
                    ob = osb[gi % 2]; obk = f"osb{gi % 2}"
                    for j in range(4):
                        P.op("dve", lambda e, j=j: e.tensor_scalar(out=rden[:, j:j + 1], in0=ps_o[j][:, 64:65], scalar1=1e-30, scalar2=None, op0=ALU.max),
                             r=[f"ps_o{j}"], w=["rden"])
                    P.op("dve", lambda e: e.reciprocal(out=rden[:], in_=rden[:]), r=["rden"], w=["rden"])
                    for j in range(4):
                        P.op("dve", lambda e, j=j, ob=ob: e.tensor_scalar(out=ob[:, j, :], in0=ps_o[j][:, 0:64], scalar1=rden[:, j:j + 1], scalar2=None, op0=ALU.mult),
                             r=[f"ps_o{j}", "rden"], w=[obk])
                    for j in range(4):
                        P.dma("sp", o[(i0 + j) * 128:(i0 + j + 1) * 128, h * 64:(h + 1) * 64], ob[:, j, :], r=[obk])
            q = []
            for U in units:
                scores(U)
                q.append(U)
                if len(q) > 2:
                    exp_pv(q.pop(0))
            while q:
                exp_pv(q.pop(0))
        print("B2", P.emit())
    return nc


def consts_B2(parity):
    k = np.arange(128)
    tri = np.where(k[None, :] >= k[:, None], 0.0, NEGB).astype(np.float32)
    opn = np.zeros((128, 128), np.float32); cls = np.full((128, 128), NEGB, np.float32)
    maska, maskb = (tri, cls) if parity == 0 else (opn, tri)
    j = np.arange(32)
    mneg = np.zeros((128, 32, 32), np.float32); mval = np.zeros((128, 32, 32), np.float32); mcur = np.zeros((128, 32, 32), np.float32)
    for i in range(32):
        cb = i
        mneg[:, i, :] = np.where(j < cb, 0.0, -1e30)[None, :]
        mval[:, i, :] = (j < cb)[None, :]
        mcur[:, i, :] = (j == cb)[None, :]
    emoba = (np.arange(S)[None, :] // 256 == j[:, None]).astype(np.float32)
    kaug = np.stack([np.ones(S), np.full(S, 128.0), np.arange(S) % 128, (np.arange(S) // 128) * 128.0]).astype(np.float32)
    return dict(maska=maska, maskb=maskb, mneg=mneg.reshape(128, 1024), mvalid=mval.reshape(128, 1024), mcur=mcur.reshape(128, 1024),
                emoba=emoba, kaugc=kaug, identd=np.eye(128, dtype=np.float32))


def qaug_rows(slopes, parity, extra=0):
    i = np.arange(4096) // 128
    qp = (np.arange(4096) % 128).astype(np.float64)
    qt = 2 * i + parity
    out = []
    for s in slopes:
        Sx = 8.0 * s
        rows = [-Sx * qp, -Sx * qt, np.full(4096, Sx), np.full(4096, Sx)] + [np.full(4096, Sx)] * extra
        out.append(np.stack(rows))
    return np.stack(out).astype(np.float32)


def alibi(n):
    return 2.0 ** (-8.0 * np.arange(1, n + 1) / n)


def inputs_B2(zb, g, parity, gq, gk):
    own = (np.arange(4096) // 128 * 2 + parity) * 128 + np.arange(4096) % 128
    hs = [4 * g + a for a in range(4)]
    qm = zb[:, 1304:1816].reshape(S, 8, 64); km = zb[:, 1816:2328].reshape(S, 8, 64); vmm = zb[:, 2328:2840].reshape(S, 8, 64)
    d = consts_B2(parity)
    d.update(qmT=np.ascontiguousarray(qm[own][:, hs].transpose(1, 2, 0)),
             kmT=np.ascontiguousarray(km[:, hs].transpose(1, 2, 0)),
             vm=np.ascontiguousarray(vmm[:, hs].transpose(1, 0, 2)),
             qaugc=qaug_rows(alibi(8)[hs], parity),
             gq=np.ascontiguousarray(gq.reshape(64, 1)), gk=np.ascontiguousarray(gk.reshape(64, 1)))
    return d

import contextlib
import numpy as np
import concourse.bass as bass

NTC = 16
NH = 8
NE = 32


def build_C(ne=NE):
    nc = bass.Bass("TRN2", target_bir_lowering=False)
    D_ = lambda n, s: nc.dram_tensor(n, s, F32, kind="ExternalInput").ap()
    x = D_("x", [2048, D]); o = D_("o", [2048, D])
    cT = D_("cT", [128, 8]); adaw = D_("adaw", [D, 4096]); adab = D_("adab", [1, 4096])
    ln2 = D_("ln2", [128, D]); wout = D_("wout", [D, D])
    rw = D_("rw", [D, 32]); rb = D_("rb", [128, 32])
    w1 = D_("w1", [NE, D, 2048]); b1T = D_("b1T", [128, NE * 16]); w2 = D_("w2", [NE, D, D]); b2 = D_("b2", [NE, D])
    identd = D_("identd", [128, 128])
    y = nc.dram_tensor("y", [2048, D], F32, kind="ExternalOutput").ap()
    with contextlib.ExitStack() as st:
        P = Prog(nc, st)
        ident = P.sb([128, 128], BF16, name="ident")
        onesf = P.sb([128, 128], F32, name="onesf")
        condT = P.sb([128, 8], F32, name="condT")
        condrep = P.sb([128, 8, 128], F32, name="condrep")
        adabs = P.sb([1, 256], F32, name="adabs")
        MODB = P.sb([128, 4096], F32, name="MODB")
        A2B = P.sb([128, D], F32, name="A2B")
        rwb = P.sb([128, 8, 32], BF16, name="rwb")
        rbs = P.sb([128, 32], F32, name="rbs")
        b1s = P.sb([128, NE * 16], F32, name="b1s")
        b2b = P.sb([32, D], BF16, name="b2b")
        h2T = P.sb([128, 8, 1024], BF16, name="h2T")
        acc = P.sb([128, NH, D], F32, name="acc")
        GATE = P.sb([128, NH, 32], F32, name="GATE")
        w1ring = [P.sb([128, 8, 512], BF16, name=f"w1r{k}") for k in range(3)]
        w2buf = [P.sb([128, 8, D], BF16, name=f"w2b{k}") for k in range(2)]
        woutb = w2buf[0]
        AT = P.sb([128, 8, 1024], BF16, name="AT")
        rr = 0
        ps = [P.ps([128, 512], F32, name=f"ps{k}") for k in range(6)]
        ps_tr = [P.ps([128, 1024], BF16, name=f"ps_tr{k}") for k in range(2)]
        P.dma("pool", ident[:], identd[:, :], w=["ident"])
        P.op("pool", lambda e: e.memset(onesf[:], 1.0), w=["onesf"])
        P.dma("sp", condT[:], cT[:, :], w=["condT"])
        P.dma("sp", A2B[:], ln2[:, :], w=["A2B"])
        P.dma("sp", rbs[:], rb[:, :], w=["rbs"])
        P.dma("sp", b1s[:], b1T[:, :], w=["b1s"])
        P.dma("pool", b2b[:], b2[:, :], w=["b2b"])
        for c in range(8):
            P.dma("pool", rwb[:, c, :], rw[c * 128:(c + 1) * 128, :], w=["rwb"])
        P.op("act", lambda e: e.activation(out=condT[:], in_=condT[:], func=AF.Silu), r=["condT"], w=["condT"])
        for c in range(8):
            P.op("act", lambda e, c=c: e.activation(out=condrep[:, c, :], in_=onesf[:], func=AF.Identity, scale=condT[:, c:c + 1]),
                 r=["condT", "onesf"], w=["condrep"])
        b1v = b1s[:].rearrange("p (e k) -> p e k", k=16)
        P.op("dve", lambda e: e.tensor_scalar(out=b1v[:, :, 8:16], in0=b1v[:, :, 8:16], scalar1=1.0, scalar2=None, op0=ALU.add), r=["b1s"], w=["b1s"])
        awt = [P.sb([128, 8, 256], F32, name=f"awt{k}") for k in range(2)]
        for n in range(16):
            aw = awt[n % 2]; awk = f"awt{n % 2}"
            for i in range(8):
                P.dma("sp", aw[:, i, :], adaw[i * 128:(i + 1) * 128, n * 256:(n + 1) * 256], w=[awk])
            P.dma("sp", adabs[:], adab[:, n * 256:(n + 1) * 256], w=["adabs"])
            pm = ps[n % 2]; pmk = f"ps{n % 2}"
            for i in range(8):
                P.op("pe", lambda e, pm=pm, aw=aw, i=i: e.matmul(pm[:, 0:256], lhsT=condrep[:, i, :], rhs=aw[:, i, :], start=(i == 0), stop=False),
                     r=["condrep", awk], w=[pmk])
            P.op("pe", lambda e, pm=pm: e.matmul(pm[:, 0:256], lhsT=onesf[0:1, :], rhs=adabs[0:1, :], start=False, stop=True),
                 r=["onesf", "adabs"], w=[pmk])
            P.op("act", lambda e, pm=pm, n=n: e.activation(out=MODB[:, n * 256:(n + 1) * 256], in_=pm[:, 0:256], func=AF.Copy), r=[pmk], w=["MODB"])
        P.op("dve", lambda e: e.scalar_tensor_tensor(out=A2B[:], in0=MODB[:, 2048:3072], scalar=1.0, in1=A2B[:], op0=ALU.add, op1=ALU.mult),
             r=["MODB", "A2B"], w=["A2B"])
        ot = [P.sb([128, D], BF16, name=f"ot{k}") for k in range(2)]
        oT = [P.sb([128, 8, 128], BF16, name=f"oT{k}") for k in range(2)]
        xt = [P.sb([128, D], F32, name=f"xt{k}") for k in range(2)]
        x1 = [P.sb([128, D], F32, name=f"x1{k}") for k in range(2)]
        hb = [P.sb([128, D], BF16, name=f"hb{k}") for k in range(2)]
        sq = P.sb([128, D], F32, name="sq")
        ssv = P.sb([128, 1], F32, name="ssv")
        lg = P.sb([128, 32], F32, name="lg"); m8 = P.sb([128, 8], F32, name="m8"); msk = P.sb([128, 32], F32, name="msk")
        nmx = P.sb([128, 1], F32, name="nmx"); ex = P.sb([128, 32], F32, name="ex"); den = P.sb([128, 1], F32, name="den")
        gb = P.sb([128, 32], BF16, name="gb"); gT = P.sb([32, 128], BF16, name="gT")
        gcl = P.sb([128, 512], F32, name="gcl"); sgl = P.sb([128, 512], F32, name="sgl"); lcl = P.sb([128, 512], F32, name="lcl")
        for half in range(2):
            for c in range(8):
                P.dma("pool", woutb[:, c, :], wout[c * 128:(c + 1) * 128, :], w=["w2b0"])
            for tl in range(NH):
                t = half * NH + tl
                k = t % 2
                P.dma("pool", ot[k][:], o[t * 128:(t + 1) * 128, :], w=[f"ot{k}"])
                P.dma("sp", xt[k][:], x[t * 128:(t + 1) * 128, :], w=[f"xt{k}"])
                for c in range(8):
                    P.op("pe", lambda e, k=k, c=c: e.transpose(ps_tr[k][:, c * 128:(c + 1) * 128], ot[k][:, c * 128:(c + 1) * 128], ident[:]),
                         r=[f"ot{k}", "ident"], w=[f"ps_tr{k}"])
                P.op("act", lambda e, k=k: e.activation(out=oT[k][:].rearrange("p c t -> p (c t)"), in_=ps_tr[k][:, :], func=AF.Copy),
                     r=[f"ps_tr{k}"], w=[f"oT{k}"])
                for n in range(2):
                    pm = ps[2 + n]; pmk = f"ps{2 + n}"
                    for c in range(8):
                        P.op("pe", lambda e, pm=pm, k=k, c=c, n=n: e.matmul(pm[:, 0:512], lhsT=oT[k][:, c, :], rhs=woutb[:, c, n * 512:(n + 1) * 512],
                                                                        start=(c == 0), stop=(c == 7)), r=[f"oT{k}", "w2b0"], w=[pmk])
                    P.op("dve", lambda e, pm=pm, k=k, n=n: e.tensor_tensor(out=x1[k][:, n * 512:(n + 1) * 512], in0=pm[:, 0:512], in1=MODB[:, n * 512:(n + 1) * 512],
                                                                          op=ALU.mult), r=[pmk, "MODB"], w=[f"x1{k}"])
                P.op("dve", lambda e, k=k: e.tensor_tensor(out=x1[k][:], in0=x1[k][:], in1=xt[k][:], op=ALU.add), r=[f"x1{k}", f"xt{k}"], w=[f"x1{k}"])
                P.dma("sp", y[t * 128:(t + 1) * 128, :], x1[k][:], r=[f"x1{k}"], w=[f"y{t}"])
                P.op("act", lambda e, k=k: e.activation(out=sq[:], in_=x1[k][:], func=AF.Square, accum_out=ssv[:, 0:1]), r=[f"x1{k}"], w=["sq", "ssv"])
                P.op("dve", lambda e: e.tensor_scalar(out=ssv[:], in0=ssv[:], scalar1=1.0 / D, scalar2=EPS, op0=ALU.mult, op1=ALU.add), r=["ssv"], w=["ssv"])
                P.op("act", lambda e: e.activation(out=ssv[:], in_=ssv[:], func=AF.Sqrt), r=["ssv"], w=["ssv"])
                P.op("dve", lambda e: e.reciprocal(out=ssv[:], in_=ssv[:]), r=["ssv"], w=["ssv"])
                P.op("dve", lambda e, k=k: e.scalar_tensor_tensor(out=sq[:], in0=x1[k][:], scalar=ssv[:, 0:1], in1=A2B[:], op0=ALU.mult, op1=ALU.mult),
                     r=[f"x1{k}", "ssv", "A2B"], w=["sq"])
                P.op("dve", lambda e, k=k: e.tensor_tensor(out=hb[k][:], in0=sq[:], in1=MODB[:, 1024:2048], op=ALU.add), r=["sq", "MODB"], w=[f"hb{k}"])
                for c in range(8):
                    P.op("pe", lambda e, k=k, c=c: e.transpose(ps_tr[k][:, c * 128:(c + 1) * 128], hb[k][:, c * 128:(c + 1) * 128], ident[:]),
                         r=[f"hb{k}", "ident"], w=[f"ps_tr{k}"])
                P.op("act", lambda e, k=k, t=t, tl=tl: e.activation(out=h2T[:, :, tl * 128:(tl + 1) * 128], in_=ps_tr[k][:, :].rearrange("p (c t) -> p c t", t=128),
                                                          func=AF.Copy), r=[f"ps_tr{k}"], w=["h2T"])
                pm = ps[4]; pmk = "ps4"
                for c in range(8):
                    P.op("pe", lambda e, pm=pm, c=c, t=t, tl=tl: e.matmul(pm[:, 0:32], lhsT=h2T[:, c, tl * 128:(tl + 1) * 128], rhs=rwb[:, c, :], start=(c == 0), stop=(c == 7)),
                         r=["h2T", "rwb"], w=[pmk])
                P.op("dve", lambda e, pm=pm: e.tensor_tensor(out=lg[:], in0=pm[:, 0:32], in1=rbs[:], op=ALU.add), r=[pmk, "rbs"], w=["lg"])
                P.op("dve", lambda e: e.max(out=m8[:], in_=lg[:]), r=["lg"], w=["m8"])
                P.op("dve", lambda e: e.tensor_scalar(out=msk[:], in0=lg[:], scalar1=m8[:, 3:4], scalar2=None, op0=ALU.is_ge), r=["lg", "m8"], w=["msk"])
                P.op("dve", lambda e: e.tensor_scalar(out=nmx[:], in0=m8[:, 0:1], scalar1=-1.0, scalar2=None, op0=ALU.mult), r=["m8"], w=["nmx"])
                P.op("act", lambda e: e.activation(out=ex[:], in_=lg[:], func=AF.Exp, bias=nmx[:, 0:1], scale=1.0), r=["lg", "nmx"], w=["ex"])
                P.op("dve", lambda e: e.tensor_tensor(out=ex[:], in0=ex[:], in1=msk[:], op=ALU.mult), r=["ex", "msk"], w=["ex"])
                P.op("dve", lambda e: e.tensor_reduce(out=den[:], in_=ex[:], axis=AX.X, op=ALU.add), r=["ex"], w=["den"])
                P.op("dve", lambda e: e.reciprocal(out=den[:], in_=den[:]), r=["den"], w=["den"])
                P.op("dve", lambda e, t=t, tl=tl: e.tensor_scalar(out=GATE[:, tl, :], in0=ex[:], scalar1=den[:, 0:1], scalar2=None, op0=ALU.mult), r=["ex", "den"], w=["GATE"])
                P.op("dve", lambda e, t=t, tl=tl: e.tensor_copy(out=gb[:], in_=GATE[:, tl, :]), r=["GATE"], w=["gb"])
                P.op("pe", lambda e, k=k: e.transpose(ps_tr[k][0:32, 0:128], gb[:, 0:32], ident[:]), r=["gb", "ident", "h2T"], w=[f"ps_tr{k}"])
                P.op("act", lambda e, k=k: e.activation(out=gT[:], in_=ps_tr[k][0:32, 0:128], func=AF.Copy), r=[f"ps_tr{k}"], w=["gT"])
                for n in range(2):
                    pm = ps[2 + n]; pmk = f"ps{2 + n}"
                    P.op("pe", lambda e, pm=pm, n=n: e.matmul(pm[:, 0:512], lhsT=gT[:, :], rhs=b2b[:, n * 512:(n + 1) * 512], start=True, stop=True),
                         r=["gT", "b2b"], w=[pmk])
                    P.op("act", lambda e, pm=pm, n=n, t=t, tl=tl: e.activation(out=acc[:, tl, n * 512:(n + 1) * 512], in_=pm[:, 0:512], func=AF.Copy), r=[pmk], w=[f"acc{tl}"])
            u = 0
            for ex_ in range(ne):
                w2t = w2buf[ex_ % 2]; w2k = f"w2b{ex_ % 2}"
                for c in range(8):
                    P.dma("pool", w2t[:, c, :], w2[ex_, c * 128:(c + 1) * 128, :], w=[w2k])
                for kp in range(4):
                    ring = w1ring[rr % 3]; rk = f"w1r{rr % 3}"
                    rr += 1
                    P.dma("pool", ring[:, :, 0:256], w1[ex_, :, kp * 256:(kp + 1) * 256].rearrange("(c p) n -> p c n", p=128), w=[rk])
                    P.dma("pool", ring[:, :, 256:512], w1[ex_, :, 1024 + kp * 256:1024 + (kp + 1) * 256].rearrange("(c p) n -> p c n", p=128), w=[rk])
                    for j in range(2):
                        kk = kp * 2 + j
                        for grp in range(2):
                            tk = slice(grp * 512, (grp + 1) * 512)
                            pg = ps[u % 2]; pgk = f"ps{u % 2}"; pl = ps[2 + u % 2]; plk = f"ps{2 + u % 2}"
                            u += 1
                            for c in range(8):
                                P.op("pe", lambda e, pg=pg, c=c, j=j, tk=tk, ring=ring: e.matmul(pg[:, 0:512], lhsT=ring[:, c, j * 128:(j + 1) * 128], rhs=h2T[:, c, tk],
                                                                                             start=(c == 0), stop=(c == 7)), r=[rk, "h2T"], w=[pgk])
                            for c in range(8):
                                P.op("pe", lambda e, pl=pl, c=c, j=j, tk=tk, ring=ring: e.matmul(pl[:, 0:512], lhsT=ring[:, c, 256 + j * 128:256 + (j + 1) * 128], rhs=h2T[:, c, tk],
                                                                                             start=(c == 0), stop=(c == 7)), r=[rk, "h2T"], w=[plk])
                            bg = b1s[:, ex_ * 16 + kk:ex_ * 16 + kk + 1]; bl = b1s[:, ex_ * 16 + 8 + kk:ex_ * 16 + 8 + kk + 1]
                            P.op("dve", lambda e, pg=pg, bg=bg: e.tensor_scalar(out=gcl[:], in0=pg[:, 0:512], scalar1=bg, scalar2=7.0, op0=ALU.add, op1=ALU.min),
                                 r=[pgk, "b1s"], w=["gcl"])
                            P.op("act", lambda e: e.activation(out=sgl[:], in_=gcl[:], func=AF.Sigmoid, scale=1.702), r=["gcl"], w=["sgl"])
                            P.op("dve", lambda e, pl=pl, bl=bl: e.tensor_scalar(out=lcl[:], in0=pl[:, 0:512], scalar1=bl, scalar2=-6.0, op0=ALU.add, op1=ALU.max),
                                 r=[plk, "b1s"], w=["lcl"])
                            P.op("dve", lambda e: e.scalar_tensor_tensor(out=lcl[:], in0=lcl[:], scalar=8.0, in1=gcl[:], op0=ALU.min, op1=ALU.mult),
                                 r=["lcl", "gcl"], w=["lcl"])
                            P.op("dve", lambda e, kk=kk, tk=tk: e.tensor_tensor(out=AT[:, kk, tk], in0=lcl[:], in1=sgl[:], op=ALU.mult), r=["lcl", "sgl"], w=[f"AT{kk}_{grp}"])
                for tl in range(NH):
                    t = half * NH + tl
                    for n in range(2):
                        py = ps[4 + n]; pyk = f"ps{4 + n}"
                        for kk in range(8):
                            P.op("pe", lambda e, py=py, kk=kk, tl=tl, n=n, w2t=w2t: e.matmul(py[:, 0:512], lhsT=AT[:, kk, tl * 128:(tl + 1) * 128],
                                                                                         rhs=w2t[:, kk, n * 512:(n + 1) * 512], start=(kk == 0), stop=(kk == 7)),
                                 r=[f"AT{kk}_{tl // 4}", w2k], w=[pyk])
                        P.op("dve", lambda e, py=py, t=t, tl=tl, n=n, ex_=ex_: e.scalar_tensor_tensor(out=acc[:, tl, n * 512:(n + 1) * 512], in0=py[:, 0:512],
                                                                                            scalar=GATE[:, tl, ex_:ex_ + 1], in1=acc[:, tl, n * 512:(n + 1) * 512],
                                                                                            op0=ALU.mult, op1=ALU.add), r=[pyk, "GATE", f"acc{tl}"], w=[f"acc{tl}"])
            for tl in range(NH):
                t = half * NH + tl
                k = t % 2
                P.dma("sp", xt[k][:], y[t * 128:(t + 1) * 128, :], r=[f"y{t}"], w=[f"xt{k}"])
                P.op("dve", lambda e, t=t, tl=tl: e.tensor_tensor(out=acc[:, tl, :], in0=acc[:, tl, :], in1=MODB[:, 3072:4096], op=ALU.mult), r=[f"acc{tl}", "MODB"], w=[f"acc{tl}"])
                P.op("dve", lambda e, t=t, tl=tl, k=k: e.tensor_tensor(out=acc[:, tl, :], in0=acc[:, tl, :], in1=xt[k][:], op=ALU.add), r=[f"acc{tl}", f"xt{k}"], w=[f"acc{tl}"])
                P.dma("sp", y[t * 128:(t + 1) * 128, :], acc[:, tl, :], r=[f"acc{tl}"], w=[f"y{t}"])
        print("C", P.emit())
    return nc


def inputs_C(inp, l, core, xcur, ocat):
    b, r = core // 4, core % 4
    sl = slice(r * 2048, (r + 1) * 2048)
    return dict(
        x=np.ascontiguousarray(xcur[b, sl]), o=np.ascontiguousarray(ocat[b, sl]),
        cT=np.ascontiguousarray(inp["c"][b].reshape(8, 128).T),
        adaw=np.ascontiguousarray(inp["ada_w"][l][:, 2048:6144]),
        adab=np.ascontiguousarray(inp["ada_b"][l][2048:6144].reshape(1, 4096)),
        ln2=np.ascontiguousarray(np.broadcast_to(inp["ln2_g"][l][None, :], (128, D))),
        wout=np.ascontiguousarray(inp["w_out"][l]),
        rw=np.ascontiguousarray(inp["router_w"][l]),
        rb=np.ascontiguousarray(np.broadcast_to(inp["router_b"][l][None, :], (128, 32))),
        w1=np.ascontiguousarray(inp["exp_w1"][l]),
        b1T=np.ascontiguousarray(inp["exp_b1"][l].reshape(NE, 16, 128).transpose(2, 0, 1).reshape(128, NE * 16)),
        w2=np.ascontiguousarray(inp["exp_w2"][l]),
        b2=np.ascontiguousarray(inp["exp_b2"][l]),
        identd=np.eye(128, dtype=np.float32),
    )


_CACHE = {}


def _prog(name, fn):
    if name not in _CACHE:
        _CACHE[name] = fn()
    return _CACHE[name]


def kernel(**inputs):
    inp = {k: np.asarray(v) for k, v in inputs.items()}
    x_cur = np.ascontiguousarray(inp["x"], dtype=np.float32)
    cores = list(range(8))
    for l in range(2):
        inp["x_cur"] = x_cur
        resA = run_bass_kernel_spmd(_prog("A", build_A), [inputs_A(inp, l, c) for c in cores], core_ids=cores)
        z = np.stack([np.concatenate([resA.results[b * 4 + r]["z"] for r in range(4)], axis=0) for b in range(2)])
        ocat = np.zeros((2, 8192, 1024), np.float32)
        cfg = [(c // 4, (c // 2) % 2, c % 2) for c in cores]
        maps = [inputs_B1(z[b], g, p, inp["nsa_q_gain"][l], inp["nsa_k_gain"][l], inp["nsa_cmp_pos"][l],
                          inp["nsa_cmp_w1"][l], inp["nsa_cmp_w2"][l]) for (b, g, p) in cfg]
        resB1 = run_bass_kernel_spmd(_prog("B1", build_B1), maps, core_ids=cores)
        maps = [inputs_B2(z[b], g, p, inp["moba_q_gain"][l], inp["moba_k_gain"][l]) for (b, g, p) in cfg]
        resB2 = run_bass_kernel_spmd(_prog("B2", build_B2), maps, core_ids=cores)
        for c, (b, g, p) in enumerate(cfg):
            own = (np.arange(4096) // 128 * 2 + p) * 128 + np.arange(4096) % 128
            ocat[b, own, g * 256:(g + 1) * 256] = resB1.results[c]["o"]
            ocat[b, own, 512 + g * 256:512 + (g + 1) * 256] = resB2.results[c]["o"]
        resC = run_bass_kernel_spmd(_prog("C", build_C), [inputs_C(inp, l, c, x_cur, ocat) for c in cores], core_ids=cores)
        x_cur = np.stack([np.concatenate([resC.results[b * 4 + r]["y"] for r in range(4)], axis=0) for b in range(2)]).astype(np.float32)
    return x_cur
```

```python
from concourse.bass_utils import run_bass_kernel_spmd
D = 1024
EPS = 1e-6

import contextlib
import numpy as np
import concourse.bass as bass
import concourse.mybir as mybir

F32 = mybir.dt.float32
BF16 = mybir.dt.bfloat16
I32 = mybir.dt.int32
U32 = mybir.dt.uint32
AF = mybir.ActivationFunctionType
ALU = mybir.AluOpType
AX = mybir.AxisListType

DMA_K = 6
STRICT_SAME = True


class Prog:
    def __init__(self, nc, stack):
        self.nc = nc
        self.stack = stack
        self.ins = []
        self.eng = {"pe": nc.tensor, "act": nc.scalar, "dve": nc.vector, "pool": nc.gpsimd, "sp": nc.sync}
        self.nt = 0

    def sb(self, shape, dt, name=None):
        self.nt += 1
        return self.stack.enter_context(self.nc.sbuf_tensor(name or f"sb{self.nt}", list(shape), dt))

    def ps(self, shape, dt=F32, name=None):
        self.nt += 1
        return self.stack.enter_context(self.nc.psum_tensor(name or f"ps{self.nt}", list(shape), dt))

    def op(self, eng, fn, r=(), w=()):
        self.ins.append(dict(e=eng, fn=fn, r=tuple(r), w=tuple(w), dma=False))

    def dma(self, q, out, in_, r=(), w=(), **kw):
        def fn(e, out=out, in_=in_, kw=kw):
            return e.dma_start(out=out, in_=in_, **kw)
        self.ins.append(dict(e=q, fn=fn, r=tuple(r), w=tuple(w), dma=True))

    def emit(self):
        nc = self.nc
        ins = self.ins
        n = len(ins)
        last_w = {}
        readers = {}
        deps = [None] * n
        for i, I in enumerate(ins):
            d = set()
            for k in I["r"]:
                d.update(last_w.get(k, ()))
            for k in I["w"]:
                for j in last_w.get(k, ()):
                    if not (I["dma"] and ins[j]["dma"]):
                        d.add(j)
                rd = readers.get(k)
                if rd:
                    for v in rd[0].values():
                        d.add(v)
                    d.update(rd[1])
            d.discard(i)
            deps[i] = d
            for k in I["r"]:
                rd = readers.setdefault(k, ({}, []))
                if I["dma"]:
                    rd[1].append(i)
                else:
                    rd[0][I["e"]] = i
            for k in I["w"]:
                if I["dma"]:
                    last_w[k] = [j for j in last_w.get(k, ()) if ins[j]["dma"]] + [i]
                else:
                    last_w[k] = [i]
                readers[k] = ({}, [])
        signal = [False] * n
        fdeps = [None] * n
        for i, I in enumerate(ins):
            keep = []
            for j in deps[i]:
                J = ins[j]
                if J["dma"]:
                    keep.append(j)
                    continue
                if J["e"] == I["e"]:
                    if I["dma"]:
                        keep.append(j); signal[j] = True
                        continue
                    if STRICT_SAME and I["e"] in ("act", "dve", "pool"):
                        if any(k in J["w"] for k in I["r"]):
                            keep.append(j); signal[j] = True
                    continue
                keep.append(j); signal[j] = True
            fdeps[i] = keep
        engs = ["pe", "act", "dve", "pool", "sp"]
        sems = {e: self.stack.enter_context(nc.semaphore(f"s_{e}")) for e in engs}
        dsems = {e: [self.stack.enter_context(nc.semaphore(f"d_{e}{k}")) for k in range(DMA_K)]
                 for e in ("sp", "act", "pool")}
        cnt = {e: 0 for e in engs}
        dcnt = {e: 0 for e in dsems}
        tag = [None] * n
        for i, I in enumerate(ins):
            if I["dma"]:
                q = I["e"]
                idx = dcnt[q]; dcnt[q] += 1
                tag[i] = ("d", q, idx)
            elif signal[i]:
                cnt[I["e"]] += 1
                tag[i] = ("c", I["e"], cnt[I["e"]])
        waited = {e: {} for e in engs}
        nw = 0
        for i, I in enumerate(ins):
            e = I["e"]
            E = self.eng[e]
            wl = {}
            for j in fdeps[i]:
                t = tag[j]
                if t[0] == "d":
                    s = dsems[t[1]][t[2] % DMA_K]; v = 16 * (t[2] // DMA_K + 1)
                else:
                    s = sems[t[1]]; v = t[2]
                key = id(s)
                if key not in wl or wl[key][1] < v:
                    wl[key] = (s, v)
            if I["dma"]:
                t = tag[i]
                if t[2] >= DMA_K:
                    s = dsems[t[1]][t[2] % DMA_K]; v = 16 * (t[2] // DMA_K)
                    key = id(s)
                    if key not in wl or wl[key][1] < v:
                        wl[key] = (s, v)
            for key, (s, v) in wl.items():
                if waited[e].get(key, 0) >= v:
                    continue
                waited[e][key] = v
                E.wait_ge(s, v)
                nw += 1
            inst = I["fn"](E)
            t = tag[i]
            if t is not None:
                if t[0] == "d":
                    inst.then_inc(dsems[t[1]][t[2] % DMA_K], 16)
                else:
                    inst.then_inc(sems[t[1]], 1)
        E = self.eng["sp"]
        for q, c in dcnt.items():
            for k in range(DMA_K):
                m = (c - k + DMA_K - 1) // DMA_K if c > k else 0
                if m > 0 and waited["sp"].get(id(dsems[q][k]), 0) < 16 * m:
                    E.wait_ge(dsems[q][k], 16 * m)
        self.stats = dict(n=n, waits=nw, cnt=dict(cnt), dcnt=dict(dcnt))
        return self.stats

import contextlib
import numpy as np
import concourse.bass as bass
import concourse.mybir as mybir

INW = 2840
NT = 16
NORM_SEGS = [(0, 8), (768, 2), (1024, 2), (1304, 8), (1816, 8)]


def emit_mod(P, nc, adaw_dram, col0, ncols, condT, adabT, out_cols, tagp):
    nj = ncols // 128
    half = 1024
    pm = P.ps([128, 64], F32, name=f"pm_{tagp}")
    for hh in range(ncols // half):
        aw = P.sb([128, 8, half], F32, name=f"aw_{tagp}{hh}")
        for i in range(8):
            P.dma("sp" if i % 2 == 0 else "pool", aw[:, i, :], adaw_dram[i * 128:(i + 1) * 128, col0 + hh * half: col0 + (hh + 1) * half],
                  w=[f"aw_{tagp}{hh}_{i}"])
        for jj in range(half // 128):
            j = hh * (half // 128) + jj
            for i in range(8):
                P.op("pe", lambda e, aw=aw, i=i, jj=jj, j=j: e.matmul(pm[:, j:j + 1], lhsT=aw[:, i, jj * 128:(jj + 1) * 128],
                                                                rhs=condT[:, i:i + 1], start=(i == 0), stop=(i == 7)),
                     r=[f"aw_{tagp}{hh}_{i}", "condT"], w=[f"pm_{tagp}"])
    P.op("dve", lambda e: e.tensor_tensor(out=out_cols[:, 0:nj], in0=pm[:, 0:nj], in1=adabT, op=ALU.add),
         r=[f"pm_{tagp}", "adab"], w=[f"mod_{tagp}"])


class _Stop(Exception):
    pass


def build_A(stop=99):
    nc = bass.Bass("TRN2", target_bir_lowering=False)
    x = nc.dram_tensor("x", [NT * 128, D], F32, kind="ExternalInput").ap()
    cT = nc.dram_tensor("cT", [128, 8], F32, kind="ExternalInput").ap()
    adaw = nc.dram_tensor("adaw", [D, 2048], F32, kind="ExternalInput").ap()
    adabT = nc.dram_tensor("adabT", [128, 16], F32, kind="ExternalInput").ap()
    lngT = nc.dram_tensor("lngT", [128, 8], F32, kind="ExternalInput").ap()
    win = nc.dram_tensor("win", [D, INW], F32, kind="ExternalInput").ap()
    identd = nc.dram_tensor("identd", [128, 128], F32, kind="ExternalInput").ap()
    zo = nc.dram_tensor("z", [NT * 128, INW], F32, kind="ExternalOutput").ap()
    with contextlib.ExitStack() as st:
        P = Prog(nc, st)
        try:
            ident = P.sb([128, 128], BF16, name="ident")
            identf = P.sb([128, 128], F32, name="identf")
            condT = P.sb([128, 8], F32, name="condT")
            adab = P.sb([128, 16], F32, name="adab")
            lng = P.sb([128, 8], F32, name="lng")
            modc = P.sb([128, 16], F32, name="modc")
            Acol = P.sb([128, 8], F32, name="Acol")
            wb = P.sb([128, 8, INW], BF16, name="wb")
            P.dma("sp", identf[:], identd[:, :], w=["identf"])
            P.op("dve", lambda e: e.tensor_copy(out=ident[:], in_=identf[:]), r=["identf"], w=["ident"])
            P.dma("sp", condT[:], cT[:, :], w=["condT"])
            P.dma("sp", adab[:], adabT[:, :], w=["adab"])
            P.dma("sp", lng[:], lngT[:, :], w=["lng"])
            P.op("act", lambda e: e.activation(out=condT[:], in_=condT[:], func=AF.Silu), r=["condT"], w=["condT"])
            emit_mod(P, nc, adaw, 0, 2048, condT, adab[:, 0:16], modc, "a")
            P.op("dve", lambda e: e.scalar_tensor_tensor(out=Acol[:], in0=modc[:, 8:16], scalar=1.0, in1=lng[:], op0=ALU.add, op1=ALU.mult),
                 r=["mod_a", "lng"], w=["Acol"])
            if stop == 1:
                P.dma("sp", zo[0:128, 0:16], modc[:], r=["mod_a"])
                P.dma("sp", zo[0:128, 16:24], Acol[:], r=["Acol"])
                raise _Stop()
            for i in range(8):
                for cc in range(0, INW, 568):
                    P.dma("pool", wb[:, i, cc:cc + 568], win[i * 128:(i + 1) * 128, cc:cc + 568], w=[f"wb{i}"])
            xt = [P.sb([128, D], F32, name=f"xt{k}") for k in range(2)]
            xnb = [P.sb([128, D], BF16, name=f"xnb{k}") for k in range(2)]
            sq = P.sb([128, D], F32, name="sq")
            ss = [P.sb([128, 1], F32, name=f"ss{k}") for k in range(2)]
            hT = [P.sb([128, 8, 128], BF16, name=f"hT{k}") for k in range(2)]
            pT = [P.ps([128, D], BF16, name=f"pT{k}") for k in range(2)]
            pz = [P.ps([128, 512], F32, name=f"pz{k}") for k in range(3)]
            zsb = [P.sb([128, INW], F32, name=f"zsb{k}") for k in range(2)]
            ss2 = P.sb([128, 8], F32, name="ss2")
            groups = [(c0, min(512, INW - c0)) for c0 in range(0, INW, 512)]
            sqf = P.sb([128, D], F32, name="sqf")
            zkeys_of = lambda k: [f"zsb{k}_{gi}" for gi in range(len(groups))]

            def front(t):
                k = t % 2
                X, XN, SS, HT, PT = xt[k], xnb[k], ss[k], hT[k], pT[k]
                P.dma("sp", X[:], x[t * 128:(t + 1) * 128, :], w=[f"xt{k}"])
                P.op("act", lambda e, X=X, SS=SS: e.activation(out=sqf[:], in_=X[:], func=AF.Square, accum_out=SS[:, 0:1]),
                     r=[f"xt{k}"], w=["sqf", f"ss{k}"])
                P.op("dve", lambda e, SS=SS: e.tensor_scalar(out=SS[:], in0=SS[:], scalar1=1.0 / D, scalar2=EPS, op0=ALU.mult, op1=ALU.add),
                     r=[f"ss{k}"], w=[f"ss{k}"])
                P.op("act", lambda e, SS=SS: e.activation(out=SS[:], in_=SS[:], func=AF.Sqrt), r=[f"ss{k}"], w=[f"ss{k}"])
                P.op("dve", lambda e, SS=SS: e.reciprocal(out=SS[:], in_=SS[:]), r=[f"ss{k}"], w=[f"ss{k}"])
                P.op("dve", lambda e, X=X, XN=XN, SS=SS: e.tensor_scalar(out=XN[:], in0=X[:], scalar1=SS[:, 0:1], scalar2=None, op0=ALU.mult),
                     r=[f"xt{k}", f"ss{k}"], w=[f"xnb{k}"])
                for c in range(8):
                    P.op("pe", lambda e, PT=PT, XN=XN, c=c: e.transpose(PT[:, c * 128:(c + 1) * 128], XN[:, c * 128:(c + 1) * 128], ident[:]),
                         r=[f"xnb{k}", "ident"], w=[f"pT{k}"])
                for c in range(8):
                    P.op("act", lambda e, HT=HT, PT=PT, c=c: e.activation(out=HT[:, c, :], in_=PT[:, c * 128:(c + 1) * 128], func=AF.Identity,
                                                                     scale=Acol[:, c:c + 1], bias=modc[:, c:c + 1]),
                         r=[f"pT{k}", "Acol", "mod_a"], w=[f"hT{k}_{c}"])

            def mid(t):
                k = t % 2
                HT, Z = hT[k], zsb[k]
                for gi, (c0, cw) in enumerate(groups):
                    pzz = pz[gi % 3]
                    for c in range(8):
                        P.op("pe", lambda e, pzz=pzz, HT=HT, c=c, c0=c0, cw=cw: e.matmul(pzz[:, 0:cw], lhsT=HT[:, c, :], rhs=wb[:, c, c0:c0 + cw],
                                                                                   start=(c == 0), stop=(c == 7)),
                             r=[f"hT{k}_{c}", f"wb{c}"], w=[f"pz{gi % 3}"])
                    if gi % 2 == 0:
                        P.op("act", lambda e, pzz=pzz, Z=Z, c0=c0, cw=cw: e.activation(out=Z[:, c0:c0 + cw], in_=pzz[:, 0:cw], func=AF.Copy),
                             r=[f"pz{gi % 3}"], w=[f"zsb{k}_{gi}"])
                    else:
                        P.op("dve", lambda e, pzz=pzz, Z=Z, c0=c0, cw=cw: e.tensor_copy(out=Z[:, c0:c0 + cw], in_=pzz[:, 0:cw]),
                             r=[f"pz{gi % 3}"], w=[f"zsb{k}_{gi}"])

            def post(t):
                k = t % 2
                Z = zsb[k]
                zkeys = zkeys_of(k)
                for (s0, nh) in NORM_SEGS:
                    V = Z[:, s0:s0 + nh * 64]
                    P.op("pool", lambda e, V=V, nh=nh: e.tensor_tensor(out=sq[:, 0:nh * 64], in0=V, in1=V, op=ALU.mult),
                         r=zkeys, w=["sq"])
                    P.op("dve", lambda e, nh=nh: e.tensor_reduce(out=ss2[:, 0:nh], in_=sq[:, 0:nh * 64].rearrange("p (h d) -> p h d", d=64),
                                                               axis=AX.X, op=ALU.add), r=["sq"], w=["ss2"])
                    P.op("dve", lambda e, nh=nh: e.tensor_scalar(out=ss2[:, 0:nh], in0=ss2[:, 0:nh], scalar1=1.0 / 64, scalar2=EPS,
                                                               op0=ALU.mult, op1=ALU.add), r=["ss2"], w=["ss2"])
                    P.op("act", lambda e, nh=nh: e.activation(out=ss2[:, 0:nh], in_=ss2[:, 0:nh], func=AF.Sqrt), r=["ss2"], w=["ss2"])
                    P.op("dve", lambda e, nh=nh: e.reciprocal(out=ss2[:, 0:nh], in_=ss2[:, 0:nh]), r=["ss2"], w=["ss2"])
                    P.op("dve", lambda e, V=V, nh=nh: e.tensor_tensor(out=V.rearrange("p (h d) -> p h d", d=64),
                                                                    in0=V.rearrange("p (h d) -> p h d", d=64),
                                                                    in1=ss2[:, 0:nh].unsqueeze(2).to_broadcast([128, nh, 64]), op=ALU.mult),
                         r=zkeys + ["ss2"], w=zkeys)
                P.op("act", lambda e, Z=Z: e.activation(out=Z[:, 1280:1304], in_=Z[:, 1280:1304], func=AF.Sigmoid), r=zkeys, w=zkeys)
                P.dma("sp", zo[t * 128:(t + 1) * 128, :], Z[:], r=zkeys)

            front(0)
            for t in range(NT):
                mid(t)
                if t + 1 < NT:
                    front(t + 1)
                post(t)
        except _Stop:
            pass
        import os
        if os.environ.get("TRUNC"):
            N = int(os.environ["TRUNC"])
            for q, I in enumerate(P.ins[:N]):
                pass
            print("TRUNC at", N, "of", len(P.ins), P.ins[N - 1]["e"], P.ins[N - 1]["r"], P.ins[N - 1]["w"])
            P.ins = P.ins[:N]
            P.dma("sp", zo[0:128, 0:16], modc[:], r=["mod_a"])
        print("A", P.emit())
    return nc


def inputs_A(inp, l, core):
    b, r = core // 4, core % 4
    xl = inp["x_cur"][b, r * 2048:(r + 1) * 2048]
    return {
        "x": np.ascontiguousarray(xl),
        "cT": np.ascontiguousarray(inp["c"][b].reshape(8, 128).T),
        "adaw": np.ascontiguousarray(inp["ada_w"][l][:, 0:2048]),
        "adabT": np.ascontiguousarray(inp["ada_b"][l][0:2048].reshape(16, 128).T),
        "lngT": np.ascontiguousarray(inp["ln1_g"][l].reshape(8, 128).T),
        "win": np.ascontiguousarray(inp["w_in"][l]),
        "identd": np.eye(128, dtype=np.float32),
    }

import contextlib
import numpy as np
import concourse.bass as bass

S = 8192
NEGB = -30000.0
GELU_C = 1.5957691216057308


def build_B1(nslots=32):
    nc = bass.Bass("TRN2", target_bir_lowering=False)
    D_ = lambda n, s: nc.dram_tensor(n, s, F32, kind="ExternalInput").ap()
    qnT = D_("qnT", [64, 16384]); qaugc = D_("qaugc", [5, 16384]); gates = D_("gates", [128, 384])
    kcT = D_("kcT", [2, 64, 8224]); ksT = D_("ksT", [2, 64, S]); vsw = D_("vsw", [2, S, 64])
    kaugc = D_("kaugc", [4, S]); kcaugc = D_("kcaugc", [5, 512])
    gq = D_("gq", [64, 1]); gk = D_("gk", [64, 1])
    w1r = D_("w1r", [2, 64, 4096]); posT = D_("posT", [64, 64]); w2r = D_("w2r", [128, 128]); incid = D_("incid", [512, 128])
    enu = D_("enu", [128, S])
    masks = D_("masks", [6, 128, 128])
    cmt = D_("cmt", [32, 128, 512]); addt = D_("addt", [32, 128, 128])
    identd = D_("identd", [128, 128])
    o = nc.dram_tensor("o", [4096, 256], F32, kind="ExternalOutput").ap()
    with contextlib.ExitStack() as st:
        P = Prog(nc, st)
        ident = P.sb([128, 128], BF16, name="ident")
        M4 = P.sb([128, 6, 512], BF16, name="M4")
        Mt = P.sb([128, 6, 128], BF16, name="Mt")
        EN = P.sb([128, S], BF16, name="EN")
        KS = P.sb([68, S], BF16, name="KS"); KW = P.sb([68, S], BF16, name="KW")
        QN = P.sb([69, 16384], BF16, name="QN")
        VS = P.sb([128, 64, 65], BF16, name="VS"); VW = P.sb([128, 64, 65], BF16, name="VW")
        KC = P.sb([69, 512], BF16, name="KC"); VC = P.sb([128, 4, 193], BF16, name="VC")
        G = P.sb([128, 32, 12], F32, name="G")
        gqs = P.sb([64, 1], F32, name="gqs"); gks = P.sb([64, 1], F32, name="gks")
        stg = [P.sb([64, 2048], F32, name=f"stg{k}") for k in range(2)]
        KCT = P.sb([64, 8224], BF16, name="KCT")
        W1b = P.sb([64, 4096], BF16, name="W1b"); posb = P.sb([64, 64], BF16, name="posb"); W2b = P.sb([128, 128], BF16, name="W2b")
        pbias = P.sb([128, 2], F32, name="pbias")
        xh = P.sb([128, 512], F32, name="xh"); x2 = P.sb([128, 512], F32, name="x2"); sgm = P.sb([128, 512], F32, name="sgm")
        GE = P.sb([128, 512], BF16, name="GE")
        sqk = P.sb([128, 64], F32, name="sqk"); ssk = P.sb([128, 1], F32, name="ssk"); kcn = P.sb([128, 64], BF16, name="kcn")
        CMs = [P.sb([128, 512], BF16, name=f"CMs{k}") for k in range(2)]
        ADs = [P.sb([128, 128], F32, name=f"ADs{k}") for k in range(2)]
        PT = [P.sb([128, 512], BF16, name=f"PT{k}") for k in range(4)]
        SELT4 = P.sb([128, 512], BF16, name="SELT4")
        IMP = P.sb([128, 128], F32, name="IMP"); IMP2 = P.sb([128, 128], F32, name="IMP2"); selb = P.sb([128, 128], BF16, name="selb")
        m8a = P.sb([128, 8], F32, name="m8a"); m8b = P.sb([128, 8], F32, name="m8b")
        rd = P.sb([128, 4], F32, name="rd"); cf = P.sb([128, 4], F32, name="cf")
        OACC = [P.sb([128, 4, 64], F32, name=f"OACC{k}") for k in range(2)]
        ps_sc = [P.ps([128, 512], F32, name=f"ps_sc{k}") for k in range(2)]
        ps_o = [P.ps([128, 512], F32, name=f"ps_o{k}") for k in range(4)]
        ps_tr = P.ps([128, 1024], BF16, name="ps_tr")
        ps_m = P.ps([128, 512], F32, name="ps_m")
        P.dma("pool", ident[:], identd[:, :], w=["ident"])
        for m in range(6):
            P.dma("pool", Mt[:, m, :], masks[m, :, :], w=["Mt"])
        for m in range(6):
            for h in range(4):
                P.op("dve", lambda e, m=m, h=h: e.tensor_copy(out=M4[:, m, h * 128:(h + 1) * 128], in_=Mt[:, m, :]), r=["Mt"], w=["M4"])
        for c in range(4):
            cs = slice(c * 2048, (c + 1) * 2048)
            P.dma("pool", EN[:, cs], enu[:, cs], w=["EN"])
            P.dma("pool", KS[64:68, cs], kaugc[:, cs], w=["KSaug"])
            P.dma("pool", KW[64:68, cs], kaugc[:, cs], w=["KWaug"])
        P.dma("pool", KC[64:69, :], kcaugc[:, :], w=["KCaug"])
        P.dma("sp", gqs[:], gq[:, :], w=["gqs"]); P.dma("sp", gks[:], gk[:, :], w=["gks"])
        P.dma("sp", G[:].rearrange("p i c -> p (i c)"), gates[:, :], w=["G"])
        P.dma("pool", posb[:], posT[:, :], w=["posb"]); P.dma("pool", W2b[:], w2r[:, :], w=["W2b"])
        P.op("dve", lambda e: e.memset(VS[:, :, 64:65], 1.0), w=["VSone"])
        P.op("dve", lambda e: e.memset(VW[:, :, 64:65], 1.0), w=["VWone"])
        P.op("dve", lambda e: e.memset(VC[:, :, 64:65], 1.0), w=["VCone"])
        P.op("dve", lambda e: e.memset(GE[:], 0.0), w=["GE"])
        P.dma("pool", VC[:, :, 65:193], incid.rearrange("(nt p) j -> p nt j", p=128), w=["VCinc"])
        for kv, (KT, kk) in enumerate(((KS, "KS"), (KW, "KW"))):
            for c in range(4):
                sg = stg[c % 2]; cs = slice(c * 2048, (c + 1) * 2048)
                P.dma("sp", sg[:], ksT[kv, :, cs], w=[f"stg{c % 2}"])
                P.op("act", lambda e, sg=sg, KT=KT, cs=cs: e.activation(out=KT[0:64, cs], in_=sg[:], func=AF.Identity, scale=gks[:, 0:1]),
                     r=[f"stg{c % 2}", "gks"], w=[kk])
        for kv, (VT, vk) in enumerate(((VS, "VS"), (VW, "VW"))):
            for c in range(4):
                P.dma("pool", VT[:, c * 16:(c + 1) * 16, 0:64], vsw[kv, c * 2048:(c + 1) * 2048, :].rearrange("(kt p) d -> p kt d", p=128), w=[vk])
        for c in range(8):
            sg = stg[c % 2]; cs = slice(c * 2048, (c + 1) * 2048)
            P.dma("sp", sg[:], qnT[:, cs], w=[f"stg{c % 2}"])
            P.op("dve", lambda e, sg=sg, cs=cs: e.tensor_scalar(out=QN[0:64, cs], in0=sg[:], scalar1=gqs[:, 0:1], scalar2=None, op0=ALU.mult),
                 r=[f"stg{c % 2}", "gqs"], w=["QN"])
            P.dma("pool", QN[64:69, cs], qaugc[:, cs], w=["QNaug"])
        KCv = KCT[:].rearrange("p (n s) -> p n s", s=16)
        for kv in range(2):
            for c in range(4):
                P.dma("pool", KCT[:, c * 2056:(c + 1) * 2056], kcT[kv, :, c * 2056:(c + 1) * 2056], w=["KCT"])
            P.dma("pool", W1b[:], w1r[kv, :, :], w=["W1b"])
            for l in range(32):
                rhs = KCv[:, 0:511, l] if l < 16 else KCv[:, 1:512, l - 16]
                P.op("pe", lambda e, l=l, rhs=rhs: e.matmul(ps_m[:, 0:511], lhsT=W1b[:, l * 128:(l + 1) * 128], rhs=rhs, start=(l == 0), stop=(l == 31)),
                     r=["W1b", "KCT"], w=["ps_m"])
            for l in range(32):
                P.op("pe", lambda e, l=l, kv=kv: e.matmul(ps_sc[0][:, 0:1], lhsT=W1b[:, l * 128:(l + 1) * 128], rhs=posb[:, kv * 32 + l:kv * 32 + l + 1],
                                                      start=(l == 0), stop=(l == 31)), r=["W1b", "posb"], w=["ps_sc0"])
            P.op("dve", lambda e, kv=kv: e.tensor_copy(out=pbias[:, kv:kv + 1], in_=ps_sc[0][:, 0:1]), r=["ps_sc0"], w=["pbias"])
            P.op("act", lambda e, kv=kv: e.activation(out=xh[:, 0:511], in_=ps_m[:, 0:511], func=AF.Identity, bias=pbias[:, kv:kv + 1], scale=1.0),
                 r=["ps_m", "pbias"], w=["xh"])
            P.op("dve", lambda e: e.tensor_tensor(out=x2[:, 0:511], in0=xh[:, 0:511], in1=xh[:, 0:511], op=ALU.mult), r=["xh"], w=["x2"])
            P.op("dve", lambda e: e.tensor_scalar(out=x2[:, 0:511], in0=x2[:, 0:511], scalar1=0.044715, scalar2=1.0, op0=ALU.mult, op1=ALU.add), r=["x2"], w=["x2"])
            P.op("dve", lambda e: e.tensor_tensor(out=x2[:, 0:511], in0=x2[:, 0:511], in1=xh[:, 0:511], op=ALU.mult), r=["x2", "xh"], w=["x2"])
            P.op("act", lambda e: e.activation(out=sgm[:, 0:511], in_=x2[:, 0:511], func=AF.Sigmoid, scale=GELU_C), r=["x2"], w=["sgm"])
            P.op("dve", lambda e: e.tensor_tensor(out=GE[:, 0:511], in0=xh[:, 0:511], in1=sgm[:, 0:511], op=ALU.mult), r=["xh", "sgm", "GE"], w=["GE"])
            for nt in range(4):
                P.op("pe", lambda e, nt=nt, kv=kv: e.matmul(ps_sc[1][:, 0:64], lhsT=GE[:, nt * 128:(nt + 1) * 128], rhs=W2b[:, kv * 64:(kv + 1) * 64], start=True, stop=True),
                     r=["GE", "W2b"], w=["ps_sc1"])
                if kv == 0:
                    P.op("act", lambda e: e.activation(out=sqk[:], in_=ps_sc[1][:, 0:64], func=AF.Square, accum_out=ssk[:, 0:1]), r=["ps_sc1"], w=["sqk", "ssk"])
                    P.op("dve", lambda e: e.tensor_scalar(out=ssk[:], in0=ssk[:], scalar1=1.0 / 64, scalar2=1e-6, op0=ALU.mult, op1=ALU.add), r=["ssk"], w=["ssk"])
                    P.op("act", lambda e: e.activation(out=ssk[:], in_=ssk[:], func=AF.Sqrt), r=["ssk"], w=["ssk"])
                    P.op("dve", lambda e: e.reciprocal(out=ssk[:], in_=ssk[:]), r=["ssk"], w=["ssk"])
                    P.op("dve", lambda e: e.tensor_scalar(out=kcn[:], in0=ps_sc[1][:, 0:64], scalar1=ssk[:, 0:1], scalar2=None, op0=ALU.mult),
                         r=["ps_sc1", "ssk"], w=["kcn"])
                    P.op("pe", lambda e: e.transpose(ps_tr[0:64, 0:128], kcn[:, 0:64], ident[:]), r=["kcn", "ident"], w=["ps_tr"])
                    P.op("act", lambda e, nt=nt: e.activation(out=KC[0:64, nt * 128:(nt + 1) * 128], in_=ps_tr[0:64, 0:128], func=AF.Identity, scale=gks[:, 0:1]),
                         r=["ps_tr", "gks"], w=["KC"])
                else:
                    P.op("act", lambda e, nt=nt: e.activation(out=VC[:, nt, 0:64], in_=ps_sc[1][:, 0:64], func=AF.Copy), r=["ps_sc1"], w=["VC"])
        Gv = G[:].rearrange("p i (h b) -> p i h b", b=3)
        u = 0
        scb = [ps_sc[0], ps_sc[1], ps_m]; sckeys = ["ps_sc0", "ps_sc1", "ps_m"]

        def attend(mms_fn, kts, VT, vkeys, ncol, okeys_first):
            nonlocal u
            n = len(kts)
            units = []
            for ki, kt in enumerate(kts):
                units.append((ki, kt, scb[u % 3], sckeys[u % 3], PT[u % 4], f"PT{u % 4}"))
                u += 1

            def scores(U):
                ki, kt, sc, sck, pt, ptk = U
                mms = mms_fn(kt, sc)
                for mi, (o_, l_, r_, rk) in enumerate(mms):
                    P.op("pe", lambda e, o_=o_, l_=l_, r_=r_, mi=mi, nm=len(mms): e.matmul(o_, lhsT=l_, rhs=r_, start=(mi == 0), stop=(mi == nm - 1)), r=rk, w=[sck])

            def exp_pv(U):
                ki, kt, sc, sck, pt, ptk = U
                P.op("act", lambda e, pt=pt, sc=sc: e.activation(out=pt[:], in_=sc[:, 0:512], func=AF.Exp, scale=0.125), r=[sck], w=[ptk])
                for h in range(4):
                    P.op("pe", lambda e, pt=pt, h=h, kt=kt, ki=ki: e.matmul(ps_o[h][:, 0:ncol], lhsT=pt[:, h * 128:(h + 1) * 128], rhs=VT[:, kt, 0:ncol],
                                                                      start=(ki == 0), stop=(ki == n - 1)), r=[ptk] + vkeys, w=[f"ps_o{h}"])
            q = []
            for U in units:
                scores(U)
                q.append(U)
                if len(q) > 2:
                    exp_pv(q.pop(0))
            while q:
                exp_pv(q.pop(0))

        def finish(i, br, OA, oak, first):
            for h in range(4):
                P.op("dve", lambda e, h=h: e.tensor_scalar(out=rd[:, h:h + 1], in0=ps_o[h][:, 64:65], scalar1=1e-30, scalar2=None, op0=ALU.max), r=[f"ps_o{h}"], w=["rd"])
            P.op("dve", lambda e: e.reciprocal(out=rd[:], in_=rd[:]), r=["rd"], w=["rd"])
            P.op("dve", lambda e, i=i, br=br: e.tensor_tensor(out=cf[:], in0=rd[:], in1=Gv[:, i, :, br], op=ALU.mult), r=["rd", "G"], w=["cf"])
            for h in range(4):
                if first:
                    P.op("dve", lambda e, h=h, OA=OA: e.tensor_scalar(out=OA[:, h, :], in0=ps_o[h][:, 0:64], scalar1=cf[:, h:h + 1], scalar2=None, op0=ALU.mult),
                         r=[f"ps_o{h}", "cf"], w=[oak])
                else:
                    P.op("dve", lambda e, h=h, OA=OA: e.scalar_tensor_tensor(out=OA[:, h, :], in0=ps_o[h][:, 0:64], scalar=cf[:, h:h + 1], in1=OA[:, h, :],
                                                                           op0=ALU.mult, op1=ALU.add), r=[f"ps_o{h}", "cf", oak], w=[oak])

        for i in range(nslots):
            cols = slice(i * 512, (i + 1) * 512)
            CM = CMs[i % 2]; cmk = f"CMs{i % 2}"; AD = ADs[i % 2]; adk = f"ADs{i % 2}"
            OA = OACC[i % 2]; oak = f"OACC{i % 2}"
            P.dma("pool", CM[:], cmt[i, :, :], w=[cmk])
            P.dma("sp", AD[:], addt[i, :, :], w=[adk])
            def mm_cmp(nt, sc):
                mms = [(sc[:, 0:512], KC[0:69, nt * 128:(nt + 1) * 128], QN[0:69, cols], ["KC", "KCaug", "QN", "QNaug"])]
                for h in range(4):
                    mms.append((sc[:, h * 128:(h + 1) * 128], ident[:], CM[:, nt * 128:(nt + 1) * 128], ["ident", cmk]))
                return mms
            attend(mm_cmp, list(range(min(3, (256 * i + 224) // 2048) + 1)), VC, ["VC", "VCone", "VCinc"], 193, None)
            finish(i, 0, OA, oak, True)
            P.op("dve", lambda e: e.tensor_scalar(out=IMP[:], in0=ps_o[0][:, 65:193], scalar1=rd[:, 0:1], scalar2=None, op0=ALU.mult), r=["ps_o0", "rd"], w=["IMP"])
            for h in range(1, 4):
                P.op("dve", lambda e, h=h: e.scalar_tensor_tensor(out=IMP[:], in0=ps_o[h][:, 65:193], scalar=rd[:, h:h + 1], in1=IMP[:], op0=ALU.mult, op1=ALU.add),
                     r=[f"ps_o{h}", "rd", "IMP"], w=["IMP"])
            P.op("dve", lambda e, AD=AD: e.tensor_tensor(out=IMP[:], in0=IMP[:], in1=AD[:], op=ALU.add), r=["IMP", adk], w=["IMP"])
            P.op("dve", lambda e: e.max(out=m8a[:], in_=IMP[:]), r=["IMP"], w=["m8a"])
            P.op("dve", lambda e: e.match_replace(out=IMP2[:], in_to_replace=m8a[:], in_values=IMP[:], imm_value=-3.0e38), r=["IMP", "m8a"], w=["IMP2"])
            P.op("dve", lambda e: e.max(out=m8b[:], in_=IMP2[:]), r=["IMP2"], w=["m8b"])
            P.op("dve", lambda e: e.tensor_scalar(out=IMP2[:], in0=IMP[:], scalar1=m8b[:, 7:8], scalar2=None, op0=ALU.is_ge), r=["IMP", "m8b"], w=["IMP2"])
            P.op("dve", lambda e: e.tensor_scalar(out=selb[:], in0=IMP2[:], scalar1=-1.0, scalar2=-NEGB, op0=ALU.add, op1=ALU.mult), r=["IMP2"], w=["selb"])
            P.op("pe", lambda e: e.transpose(ps_tr[:, 0:128], selb[:, :], ident[:]), r=["selb", "ident"], w=["ps_tr"])
            for h in range(4):
                P.op("act", lambda e, h=h: e.activation(out=SELT4[:, h * 128:(h + 1) * 128], in_=ps_tr[:, 0:128], func=AF.Copy), r=["ps_tr"], w=["SELT4"])
            def mm_win(kt, sc):
                mms = [(sc[:, 0:512], KW[0:68, kt * 128:(kt + 1) * 128], QN[0:68, cols], ["KW", "KWaug", "QN", "QNaug"])]
                off = kt - 2 * i
                mi = {-4: 2, -3: 3, 0: 4, 1: 5}.get(off)
                if mi is not None:
                    mms.append((sc[:, 0:512], ident[:], M4[:, mi, :], ["ident", "M4"]))
                return mms
            attend(mm_win, [kt for kt in range(2 * i - 4, 2 * i + 2) if kt >= 0], VW, ["VW", "VWone"], 65, None)
            finish(i, 2, OA, oak, False)
            def mm_sel(kt, sc):
                mms = [(sc[:, 0:512], KS[0:68, kt * 128:(kt + 1) * 128], QN[0:68, cols], ["KS", "KSaug", "QN", "QNaug"]),
                       (sc[:, 0:512], EN[:, kt * 128:(kt + 1) * 128], SELT4[:, :], ["EN", "SELT4"])]
                if kt == 2 * i:
                    mms.append((sc[:, 0:512], ident[:], M4[:, 0, :], ["ident", "M4"]))
                if kt == 2 * i + 1:
                    mms.append((sc[:, 0:512], ident[:], M4[:, 1, :], ["ident", "M4"]))
                return mms
            attend(mm_sel, list(range(2 * i + 2)), VS, ["VS", "VSone"], 65, None)
            finish(i, 1, OA, oak, False)
            P.dma("sp", o[i * 128:(i + 1) * 128, :], OA[:].rearrange("p h d -> p (h d)"), r=[oak])
        print("B1", P.emit())
    return nc


def consts_B1(parity, slopes):
    k = np.arange(128)
    tri = np.where(k[None, :] >= k[:, None], 0.0, NEGB).astype(np.float32)
    tri2 = np.where(k[:, None] > k[None, :], 0.0, NEGB).astype(np.float32)
    opn = np.zeros((128, 128), np.float32); cls = np.full((128, 128), NEGB, np.float32)
    if parity == 0:
        masks = np.stack([tri, cls, tri2, opn, tri, cls])
    else:
        masks = np.stack([opn, tri, cls, tri2, opn, tri])
    n_ = np.arange(128)
    cmt = np.zeros((32, 128, 512), np.float32); addt = np.zeros((32, 128, 128), np.float32)
    j = np.arange(128)
    for i in range(32):
        qt = 2 * i + parity
        t = qt * 128 + np.arange(128)
        for nt in range(4):
            cend = 16 * (128 * nt + n_) + 31
            cmt[i, :, nt * 128:(nt + 1) * 128] = np.where(cend[:, None] <= t[None, :], 0.0, NEGB)
        cur = t // 64
        forced = (j[None, :] == 0) | (j[None, :] == cur[:, None]) | (j[None, :] == cur[:, None] - 1)
        addt[i] = np.where(j[None, :] <= cur[:, None], np.where(forced, 1e4, 0.0), -1e30)
    cmt[:, 127, 384:512] = NEGB
    enu = (np.arange(S)[None, :] // 64 == j[:, None]).astype(np.float32)
    kaug = np.stack([np.ones(S), np.full(S, 128.0), np.arange(S) % 128, (np.arange(S) // 128) * 128.0]).astype(np.float32)
    n512 = np.arange(512)
    kcaug = np.stack([np.ones(512), np.full(512, 128.0), 16.0 * (n512 % 128), 2048.0 * (n512 // 128), np.full(512, 15.5)]).astype(np.float32)
    qa = np.zeros((5, 32, 4, 128), np.float32)
    qp = np.arange(128.0)
    for i in range(32):
        qt = 2 * i + parity
        for h in range(4):
            Sx = 8.0 * slopes[h]
            qa[0, i, h] = -Sx * qp; qa[1, i, h] = -Sx * qt; qa[2:5, i, h] = Sx
    cs = 16 * np.arange(512); ss = 64 * np.arange(128)
    inc = ((cs[:, None] <= ss[None, :] + 63) & (cs[:, None] + 31 >= ss[None, :])).astype(np.float32)
    inc[511] = 0.0
    return dict(masks=masks, cmt=cmt, addt=addt, enu=enu, kaugc=kaug, kcaugc=kcaug, qaugc=qa.reshape(5, 16384), incid=inc,
                identd=np.eye(128, dtype=np.float32))


def inputs_B1(zb, g, parity, gq, gk, pos, w1, w2):
    slopes = 2.0 ** (-8.0 * np.arange(1, 9) / 8)[4 * g:4 * g + 4]
    d = consts_B1(parity, slopes)
    own = (np.arange(4096) // 128 * 2 + parity) * 128 + np.arange(4096) % 128
    q = zb[own][:, 0:512].reshape(32, 128, 8, 64)[:, :, 4 * g:4 * g + 4]
    d["qnT"] = np.ascontiguousarray(q.transpose(3, 0, 2, 1).reshape(64, 16384))
    d["gates"] = np.ascontiguousarray(zb[own][:, 1280 + 12 * g:1280 + 12 * (g + 1)].reshape(32, 128, 12).transpose(1, 0, 2).reshape(128, 384))
    kc = zb[:, 512 + 64 * g:512 + 64 * (g + 1)]; vc = zb[:, 640 + 64 * g:640 + 64 * (g + 1)]
    kcT = np.zeros((2, 64, 8224), np.float32); kcT[0, :, :S] = kc.T; kcT[1, :, :S] = vc.T
    d["kcT"] = kcT
    d["ksT"] = np.ascontiguousarray(np.stack([zb[:, 768 + 64 * g:768 + 64 * (g + 1)].T, zb[:, 1024 + 64 * g:1024 + 64 * (g + 1)].T]))
    d["vsw"] = np.ascontiguousarray(np.stack([zb[:, 896 + 64 * g:896 + 64 * (g + 1)], zb[:, 1152 + 64 * g:1152 + 64 * (g + 1)]]))
    d["gq"] = np.ascontiguousarray(gq.reshape(64, 1)); d["gk"] = np.ascontiguousarray(gk.reshape(64, 1))
    d["w1r"] = np.ascontiguousarray(w1.reshape(2, 32, 64, 128).transpose(0, 2, 1, 3).reshape(2, 64, 4096))
    d["posT"] = np.ascontiguousarray(pos.transpose(2, 0, 1).reshape(64, 64))
    d["w2r"] = np.ascontiguousarray(np.concatenate([w2[0], w2[1]], axis=1))
    return d

import contextlib
import numpy as np
import concourse.bass as bass

S = 8192
NEGB = -30000.0


def build_B2(nslots=32):
    nq = nslots * 128
    nc = bass.Bass("TRN2", target_bir_lowering=False)
    D_ = lambda n, s: nc.dram_tensor(n, s, F32, kind="ExternalInput").ap()
    qmT = D_("qmT", [4, 64, 4096]); kmT = D_("kmT", [4, 64, S]); vm = D_("vm", [4, S, 64])
    qaugc = D_("qaugc", [4, 4, 4096]); kaugc = D_("kaugc", [4, S])
    gq = D_("gq", [64, 1]); gk = D_("gk", [64, 1])
    emoba = D_("emoba", [32, S])
    maska = D_("maska", [128, 128]); maskb = D_("maskb", [128, 128])
    mneg = D_("mneg", [128, 32 * 32]); mvalid = D_("mvalid", [128, 32 * 32]); mcur = D_("mcur", [128, 32 * 32])
    identd = D_("identd", [128, 128])
    o = nc.dram_tensor("o", [4096, 256], F32, kind="ExternalOutput").ap()
    with contextlib.ExitStack() as st:
        P = Prog(nc, st)
        ident = P.sb([128, 128], BF16, name="ident")
        MA = P.sb([128, 128], BF16, name="MA"); MB = P.sb([128, 128], BF16, name="MB")
        EM = P.sb([32, S], BF16, name="EM")
        KAs = [P.sb([68, S], BF16, name=f"KA{k}") for k in range(2)]
        QAs = [P.sb([68, 4096], BF16, name=f"QA{k}") for k in range(4)]
        VAs = [P.sb([128, 64, 65], BF16, name=f"VA{k}") for k in range(2)]
        MSBs = [P.sb([32, 4096], BF16, name=f"MSB{k}") for k in range(2)]
        MNEG = P.sb([128, 1024], F32, name="MNEG"); MVAL = P.sb([128, 1024], F32, name="MVAL"); MCUR = P.sb([128, 1024], F32, name="MCUR")
        gqs = P.sb([64, 1], F32, name="gqs"); gks = P.sb([64, 1], F32, name="gks")
        stg = [P.sb([64, 2048], F32, name=f"stg{k}") for k in range(2)]
        kmf = P.sb([64, 32], F32, name="kmf")
        kmbs = [P.sb([64, 32], BF16, name=f"kmb{k}") for k in range(2)]
        gm = P.sb([128, 32], F32, name="gm"); m8 = P.sb([128, 8], F32, name="m8")
        al = P.sb([128, 32], F32, name="al"); sbb = P.sb([128, 32], BF16, name="sbb")
        PT = [P.sb([128, 512], BF16, name=f"PT{k}") for k in range(4)]
        rden = P.sb([128, 4], F32, name="rden")
        osb = [P.sb([128, 4, 64], F32, name=f"osb{k}") for k in range(2)]
        ps_sc = [P.ps([128, 512], F32, name=f"ps_sc{k}") for k in range(2)]
        ps_o = [P.ps([128, 512], F32, name=f"ps_o{k}") for k in range(4)]
        ps_tr = P.ps([128, 1024], BF16, name="ps_tr")
        ps_g = P.ps([128, 512], F32, name="ps_g")
        P.dma("pool", ident[:], identd[:, :], w=["ident"])
        P.dma("pool", MA[:], maska[:, :], w=["MA"])
        P.dma("pool", MB[:], maskb[:, :], w=["MB"])
        for c in range(4):
            P.dma("pool", EM[:, c * 2048:(c + 1) * 2048], emoba[:, c * 2048:(c + 1) * 2048], w=["EM"])
            P.dma("pool", KAs[0][64:68, c * 2048:(c + 1) * 2048], kaugc[:, c * 2048:(c + 1) * 2048], w=["KAaug0"])
            P.dma("pool", KAs[1][64:68, c * 2048:(c + 1) * 2048], kaugc[:, c * 2048:(c + 1) * 2048], w=["KAaug1"])
        P.dma("sp", MNEG[:], mneg[:, :], w=["MNEG"])
        P.dma("sp", MVAL[:], mvalid[:, :], w=["MVAL"])
        P.dma("sp", MCUR[:], mcur[:, :], w=["MCUR"])
        P.dma("sp", gqs[:], gq[:, :], w=["gqs"])
        P.dma("sp", gks[:], gk[:, :], w=["gks"])
        P.op("dve", lambda e: e.memset(VAs[0][:, :, 64:65], 1.0), w=["VAone0"])
        P.op("dve", lambda e: e.memset(VAs[1][:, :, 64:65], 1.0), w=["VAone1"])
        u = 0
        scb = [ps_sc[0], ps_sc[1]]; sckeys = ["ps_sc0", "ps_sc1"]
        NSC = 2

        def prep_sel(h):
            kb = kmbs[h % 2]; kbk = f"kmb{h % 2}"; MS = MSBs[h % 2]; msk = f"MSB{h % 2}"
            QH = QAs[h]; qk = f"QA{h}"; qak = f"QAaug{h}"
            for c in range(4):
                sg = stg[c % 2]
                P.dma("sp", sg[:], kmT[h, :, c * 2048:(c + 1) * 2048], w=[f"stg{c % 2}"])
                P.op("dve", lambda e, sg=sg: e.tensor_scalar(out=sg[:], in0=sg[:], scalar1=gks[:, 0:1], scalar2=None, op0=ALU.mult),
                     r=[f"stg{c % 2}", "gks"], w=[f"stg{c % 2}"])
                P.op("dve", lambda e, sg=sg, c=c: e.tensor_reduce(out=kmf[:, c * 8:(c + 1) * 8], in_=sg[:].rearrange("p (b k) -> p b k", k=256),
                                                                 axis=AX.X, op=ALU.add), r=[f"stg{c % 2}"], w=["kmf"])
            P.op("dve", lambda e, kb=kb: e.tensor_scalar(out=kb[:], in0=kmf[:], scalar1=1.0 / 256, scalar2=None, op0=ALU.mult), r=["kmf"], w=[kbk])
            for c in range((nq + 2047) // 2048):
                w_ = min(2048, nq - c * 2048)
                sg = stg[c % 2]
                P.dma("sp", sg[:, 0:w_], qmT[h, :, c * 2048:c * 2048 + w_], w=[f"stg{c % 2}"])
                P.op("dve", lambda e, sg=sg, c=c, w_=w_, QH=QH: e.tensor_scalar(out=QH[0:64, c * 2048:c * 2048 + w_], in0=sg[:, 0:w_], scalar1=gqs[:, 0:1],
                                                                              scalar2=None, op0=ALU.mult), r=[f"stg{c % 2}", "gqs"], w=[qk])
            P.dma("pool", QH[64:68, 0:nq], qaugc[h, :, 0:nq], w=[qak])
            tasks = []
            for i in range(nslots):
                def task(i=i):
                    P.op("pe", lambda e, i=i: e.matmul(ps_g[:, 0:32], lhsT=QH[0:64, i * 128:(i + 1) * 128], rhs=kb[:, :], start=True, stop=True),
                         r=[qk, kbk], w=["ps_g"])
                    P.op("dve", lambda e, i=i: e.tensor_tensor(out=gm[:], in0=ps_g[:, 0:32], in1=MNEG[:, i * 32:(i + 1) * 32], op=ALU.add),
                         r=["ps_g", "MNEG"], w=["gm"])
                    P.op("dve", lambda e: e.max(out=m8[:], in_=gm[:]), r=["gm"], w=["m8"])
                    P.op("dve", lambda e: e.tensor_scalar(out=al[:], in0=gm[:], scalar1=m8[:, 2:3], scalar2=None, op0=ALU.is_ge), r=["gm", "m8"], w=["al"])
                    P.op("dve", lambda e, i=i: e.tensor_tensor(out=al[:], in0=al[:], in1=MVAL[:, i * 32:(i + 1) * 32], op=ALU.mult), r=["al", "MVAL"], w=["al"])
                    P.op("dve", lambda e, i=i: e.tensor_tensor(out=al[:], in0=al[:], in1=MCUR[:, i * 32:(i + 1) * 32], op=ALU.add), r=["al", "MCUR"], w=["al"])
                    P.op("dve", lambda e: e.tensor_scalar(out=sbb[:], in0=al[:], scalar1=-1.0, scalar2=-NEGB, op0=ALU.add, op1=ALU.mult), r=["al"], w=["sbb"])
                    P.op("pe", lambda e: e.transpose(ps_tr[0:32, 0:128], sbb[:, 0:32], ident[:]), r=["sbb", "ident"], w=["ps_tr"])
                    P.op("dve", lambda e, i=i: e.tensor_copy(out=MS[:, i * 128:(i + 1) * 128], in_=ps_tr[0:32, 0:128]), r=["ps_tr"], w=[msk])
                tasks.append(task)
            return tasks

        def load_kv(h):
            KA = KAs[h % 2]; VA = VAs[h % 2]
            for c in range(4):
                sg = stg[c % 2]
                P.dma("sp", sg[:], kmT[h, :, c * 2048:(c + 1) * 2048], w=[f"stg{c % 2}"])
                P.op("dve", lambda e, sg=sg, c=c, KA=KA: e.tensor_scalar(out=KA[0:64, c * 2048:(c + 1) * 2048], in0=sg[:], scalar1=gks[:, 0:1], scalar2=None, op0=ALU.mult),
                     r=[f"stg{c % 2}", "gks"], w=[f"KA{h % 2}"])
            for c in range(4):
                P.dma("pool", VA[:, c * 16:(c + 1) * 16, 0:64], vm[h, c * 2048:(c + 1) * 2048, :].rearrange("(kt p) d -> p kt d", p=128), w=[f"VA{h % 2}"])

        def attention(h, nxt):
            nonlocal u
            QH = QAs[h]; qk = f"QA{h}"; qak = f"QAaug{h}"; MS = MSBs[h % 2]; msk = f"MSB{h % 2}"
            KA = KAs[h % 2]; VA = VAs[h % 2]; kak = f"KA{h % 2}"; kaak = f"KAaug{h % 2}"; vak = f"VA{h % 2}"; vok = f"VAone{h % 2}"
            units = []
            for gi in range(nslots // 4):
                i0 = 4 * gi
                nkt = 2 * (i0 + 3) + 2
                for kt in range(nkt):
                    units.append((gi, i0, kt, nkt, u))
                    u += 1

            def scores(U):
                gi, i0, kt, nkt, uu = U
                cols = slice(i0 * 128, (i0 + 4) * 128)
                sc = scb[uu % NSC]; sck = sckeys[uu % NSC]
                mms = [(sc[:, 0:512], KA[0:68, kt * 128:(kt + 1) * 128], QH[0:68, cols], [kak, kaak, qk, qak]),
                       (sc[:, 0:512], EM[0:32, kt * 128:(kt + 1) * 128], MS[0:32, cols], ["EM", msk])]
                for j in range(4):
                    if kt == 2 * (i0 + j):
                        mms.append((sc[:, j * 128:(j + 1) * 128], ident[:], MA[:], ["ident", "MA"]))
                    if kt == 2 * (i0 + j) + 1:
                        mms.append((sc[:, j * 128:(j + 1) * 128], ident[:], MB[:], ["ident", "MB"]))
                for mi, (o_, l_, r_, rk) in enumerate(mms):
                    P.op("pe", lambda e, o_=o_, l_=l_, r_=r_, mi=mi, n=len(mms): e.matmul(o_, lhsT=l_, rhs=r_, start=(mi == 0), stop=(mi == n - 1)),
                         r=rk, w=[sck])

            def exp_pv(U):
                gi, i0, kt, nkt, uu = U
                sc = scb[uu % NSC]; sck = sckeys[uu % NSC]; pt = PT[uu % 4]; ptk = f"PT{uu % 4}"
                P.op("act", lambda e, pt=pt, sc=sc: e.activation(out=pt[:], in_=sc[:, 0:512], func=AF.Exp, scale=0.125), r=[sck], w=[ptk])
                for j in range(4):
                    last = 2 * (i0 + j) + 1
                    if kt <= last:
                        P.op("pe", lambda e, pt=pt, j=j, kt=kt, last=last: e.matmul(ps_o[j][:, 0:65], lhsT=pt[:, j * 128:(j + 1) * 128],
                                                                              rhs=VA[:, kt, 0:65], start=(kt == 0), stop=(kt == last)),
                             r=[ptk, vak, vok], w=[f"ps_o{j}"])
                if kt == nkt - 1:
                    ob = osb[gi % 2]; obk = f"osb{gi % 2}"
                    for j in range(4):
                        P.op("dve", lambda e, j=j: e.tensor_scalar(out=rden[:, j:j + 1], in0=ps_o[j][:, 64:65], scalar1=1e-30, scalar2=None, op0=ALU.max),
                             r=[f"ps_o{j}"], w=["rden"])
                    P.op("dve", lambda e: e.reciprocal(out=rden[:], in_=rden[:]), r=["rden"], w=["rden"])
                    for j in range(4):
                        P.op("dve", lambda e, j=j, ob=ob: e.tensor_scalar(out=ob[:, j, :], in0=ps_o[j][:, 0:64], scalar1=rden[:, j:j + 1], scalar2=None, op0=ALU.mult),
                             r=[f"ps_o{j}", "rden"], w=[obk])
                    for j in range(4):
                        P.dma("sp", o[(i0 + j) * 128:(i0 + j + 1) * 128, h * 64:(h + 1) * 64], ob[:, j, :], r=[obk])
            q = []
            every = max(1, len(units) // (len(nxt) + 1)) if nxt else 0
            for n_, U in enumerate(units):
                scores(U)
                q.append(U)
                if len(q) > NSC - 1:
                    exp_pv(q.pop(0))
                if nxt and n_ % every == every - 1:
                    nxt.pop(0)()
            while q:
                exp_pv(q.pop(0))
            while nxt:
                nxt.pop(0)()

        tasks = prep_sel(0)
        for tk_ in tasks:
            tk_()
        load_kv(0)
        for h in range(4):
            if h + 1 < 4:
                load_kv(h + 1)
                nxt = prep_sel(h + 1)
            else:
                nxt = []
            attention(h, nxt)
        print("B2", P.emit())
    return nc


def consts_B2(parity):
    k = np.arange(128)
    tri = np.where(k[None, :] >= k[:, None], 0.0, NEGB).astype(np.float32)
    opn = np.zeros((128, 128), np.float32); cls = np.full((128, 128), NEGB, np.float32)
    maska, maskb = (tri, cls) if parity == 0 else (opn, tri)
    j = np.arange(32)
    mneg = np.zeros((128, 32, 32), np.float32); mval = np.zeros((128, 32, 32), np.float32); mcur = np.zeros((128, 32, 32), np.float32)
    for i in range(32):
        cb = i
        mneg[:, i, :] = np.where(j < cb, 0.0, -1e30)[None, :]
        mval[:, i, :] = (j < cb)[None, :]
        mcur[:, i, :] = (j == cb)[None, :]
    emoba = (np.arange(S)[None, :] // 256 == j[:, None]).astype(np.float32)
    kaug = np.stack([np.ones(S), np.full(S, 128.0), np.arange(S) % 128, (np.arange(S) // 128) * 128.0]).astype(np.float32)
    return dict(maska=maska, maskb=maskb, mneg=mneg.reshape(128, 1024), mvalid=mval.reshape(128, 1024), mcur=mcur.reshape(128, 1024),
                emoba=emoba, kaugc=kaug, identd=np.eye(128, dtype=np.float32))


def qaug_rows(slopes, parity, extra=0):
    i = np.arange(4096) // 128
    qp = (np.arange(4096) % 128).astype(np.float64)
    qt = 2 * i + parity
    out = []
    for s in slopes:
        Sx = 8.0 * s
        rows = [-Sx * qp, -Sx * qt, np.full(4096, Sx), np.full(4096, Sx)] + [np.full(4096, Sx)] * extra
        out.append(np.stack(rows))
    return np.stack(out).astype(np.float32)


def alibi(n):
    return 2.0 ** (-8.0 * np.arange(1, n + 1) / n)


def inputs_B2(zb, g, parity, gq, gk):
    own = (np.arange(4096) // 128 * 2 + parity) * 128 + np.arange(4096) % 128
    hs = [4 * g + a for a in range(4)]
    qm = zb[:, 1304:1816].reshape(S, 8, 64); km = zb[:, 1816:2328].reshape(S, 8, 64); vmm = zb[:, 2328:2840].reshape(S, 8, 64)
    d = consts_B2(parity)
    d.update(qmT=np.ascontiguousarray(qm[own][:, hs].transpose(1, 2, 0)),
             kmT=np.ascontiguousarray(km[:, hs].transpose(1, 2, 0)),
             vm=np.ascontiguousarray(vmm[:, hs].transpose(1, 0, 2)),
             qaugc=qaug_rows(alibi(8)[hs], parity),
             gq=np.ascontiguousarray(gq.reshape(64, 1)), gk=np.ascontiguousarray(gk.reshape(64, 1)))
    return d

import contextlib
import numpy as np
import concourse.bass as bass

NTC = 16
NH = 8
NE = 32


def build_C(ne=NE):
    nc = bass.Bass("TRN2", target_bir_lowering=False)
    D_ = lambda n, s: nc.dram_tensor(n, s, F32, kind="ExternalInput").ap()
    x = D_("x", [2048, D]); o = D_("o", [2048, D])
    cT = D_("cT", [128, 8]); adaw = D_("adaw", [D, 4096]); adab = D_("adab", [1, 4096])
    ln2 = D_("ln2", [128, D]); wout = D_("wout", [D, D])
    rw = D_("rw", [D, 32]); rb = D_("rb", [128, 32])
    w1 = D_("w1", [NE, D, 2048]); b1T = D_("b1T", [128, NE * 16]); w2 = D_("w2", [NE, D, D]); b2 = D_("b2", [NE, D])
    identd = D_("identd", [128, 128])
    y = nc.dram_tensor("y", [2048, D], F32, kind="ExternalOutput").ap()
    with contextlib.ExitStack() as st:
        P = Prog(nc, st)
        ident = P.sb([128, 128], BF16, name="ident")
        onesf = P.sb([128, 128], F32, name="onesf")
        condT = P.sb([128, 8], F32, name="condT")
        condrep = P.sb([128, 8, 128], F32, name="condrep")
        adabs = P.sb([1, 256], F32, name="adabs")
        MODB = P.sb([128, 4096], F32, name="MODB")
        A2B = P.sb([128, D], F32, name="A2B")
        rwb = P.sb([128, 8, 32], BF16, name="rwb")
        rbs = P.sb([128, 32], F32, name="rbs")
        b1s = P.sb([128, NE * 16], F32, name="b1s")
        b2b = P.sb([32, D], BF16, name="b2b")
        h2T = P.sb([128, 8, 1024], BF16, name="h2T")
        acc = P.sb([128, NH, D], F32, name="acc")
        GATE = P.sb([128, NH, 32], F32, name="GATE")
        w1ring = [P.sb([128, 8, 512], BF16, name=f"w1r{k}") for k in range(3)]
        w2buf = [P.sb([128, 8, D], BF16, name=f"w2b{k}") for k in range(2)]
        woutb = w2buf[0]
        AT = P.sb([128, 8, 1024], BF16, name="AT")
        rr = 0
        ps = [P.ps([128, 512], F32, name=f"ps{k}") for k in range(6)]
        ps_tr = [P.ps([128, 1024], BF16, name=f"ps_tr{k}") for k in range(2)]
        P.dma("pool", ident[:], identd[:, :], w=["ident"])
        P.op("pool", lambda e: e.memset(onesf[:], 1.0), w=["onesf"])
        P.dma("sp", condT[:], cT[:, :], w=["condT"])
        P.dma("sp", A2B[:], ln2[:, :], w=["A2B"])
        P.dma("sp", rbs[:], rb[:, :], w=["rbs"])
        P.dma("sp", b1s[:], b1T[:, :], w=["b1s"])
        P.dma("pool", b2b[:], b2[:, :], w=["b2b"])
        for c in range(8):
            P.dma("pool", rwb[:, c, :], rw[c * 128:(c + 1) * 128, :], w=["rwb"])
        P.op("act", lambda e: e.activation(out=condT[:], in_=condT[:], func=AF.Silu), r=["condT"], w=["condT"])
        for c in range(8):
            P.op("act", lambda e, c=c: e.activation(out=condrep[:, c, :], in_=onesf[:], func=AF.Identity, scale=condT[:, c:c + 1]),
                 r=["condT", "onesf"], w=["condrep"])
        b1v = b1s[:].rearrange("p (e k) -> p e k", k=16)
        P.op("dve", lambda e: e.tensor_scalar(out=b1v[:, :, 8:16], in0=b1v[:, :, 8:16], scalar1=1.0, scalar2=None, op0=ALU.add), r=["b1s"], w=["b1s"])
        awt = [P.sb([128, 8, 256], F32, name=f"awt{k}") for k in range(2)]
        for n in range(16):
            aw = awt[n % 2]; awk = f"awt{n % 2}"
            for i in range(8):
                P.dma("sp", aw[:, i, :], adaw[i * 128:(i + 1) * 128, n * 256:(n + 1) * 256], w=[awk])
            P.dma("sp", adabs[:], adab[:, n * 256:(n + 1) * 256], w=["adabs"])
            pm = ps[n % 2]; pmk = f"ps{n % 2}"
            for i in range(8):
                P.op("pe", lambda e, pm=pm, aw=aw, i=i: e.matmul(pm[:, 0:256], lhsT=condrep[:, i, :], rhs=aw[:, i, :], start=(i == 0), stop=False),
                     r=["condrep", awk], w=[pmk])
            P.op("pe", lambda e, pm=pm: e.matmul(pm[:, 0:256], lhsT=onesf[0:1, :], rhs=adabs[0:1, :], start=False, stop=True),
                 r=["onesf", "adabs"], w=[pmk])
            P.op("act", lambda e, pm=pm, n=n: e.activation(out=MODB[:, n * 256:(n + 1) * 256], in_=pm[:, 0:256], func=AF.Copy), r=[pmk], w=["MODB"])
        P.op("dve", lambda e: e.scalar_tensor_tensor(out=A2B[:], in0=MODB[:, 2048:3072], scalar=1.0, in1=A2B[:], op0=ALU.add, op1=ALU.mult),
             r=["MODB", "A2B"], w=["A2B"])
        ot = [P.sb([128, D], BF16, name=f"ot{k}") for k in range(2)]
        oT = [P.sb([128, 8, 128], BF16, name=f"oT{k}") for k in range(2)]
        xt = [P.sb([128, D], F32, name=f"xt{k}") for k in range(2)]
        x1 = [P.sb([128, D], F32, name=f"x1{k}") for k in range(2)]
        hb = [P.sb([128, D], BF16, name=f"hb{k}") for k in range(2)]
        sq = P.sb([128, D], F32, name="sq")
        ssv = P.sb([128, 1], F32, name="ssv")
        lg = P.sb([128, 32], F32, name="lg"); m8 = P.sb([128, 8], F32, name="m8"); msk = P.sb([128, 32], F32, name="msk")
        nmx = P.sb([128, 1], F32, name="nmx"); ex = P.sb([128, 32], F32, name="ex"); den = P.sb([128, 1], F32, name="den")
        gb = P.sb([128, 32], BF16, name="gb"); gT = P.sb([32, 128], BF16, name="gT")
        gcl = P.sb([128, 512], F32, name="gcl"); sgl = P.sb([128, 512], F32, name="sgl"); lcl = P.sb([128, 512], F32, name="lcl")
        for half in range(2):
            for c in range(8):
                P.dma("pool", woutb[:, c, :], wout[c * 128:(c + 1) * 128, :], w=["w2b0"])
            for tl in range(NH):
                t = half * NH + tl
                k = t % 2
                P.dma("pool", ot[k][:], o[t * 128:(t + 1) * 128, :], w=[f"ot{k}"])
                P.dma("sp", xt[k][:], x[t * 128:(t + 1) * 128, :], w=[f"xt{k}"])
                for c in range(8):
                    P.op("pe", lambda e, k=k, c=c: e.transpose(ps_tr[k][:, c * 128:(c + 1) * 128], ot[k][:, c * 128:(c + 1) * 128], ident[:]),
                         r=[f"ot{k}", "ident"], w=[f"ps_tr{k}"])
                P.op("act", lambda e, k=k: e.activation(out=oT[k][:].rearrange("p c t -> p (c t)"), in_=ps_tr[k][:, :], func=AF.Copy),
                     r=[f"ps_tr{k}"], w=[f"oT{k}"])
                for n in range(2):
                    pm = ps[2 + n]; pmk = f"ps{2 + n}"
                    for c in range(8):
                        P.op("pe", lambda e, pm=pm, k=k, c=c, n=n: e.matmul(pm[:, 0:512], lhsT=oT[k][:, c, :], rhs=woutb[:, c, n * 512:(n + 1) * 512],
                                                                        start=(c == 0), stop=(c == 7)), r=[f"oT{k}", "w2b0"], w=[pmk])
                    P.op("dve", lambda e, pm=pm, k=k, n=n: e.tensor_tensor(out=x1[k][:, n * 512:(n + 1) * 512], in0=pm[:, 0:512], in1=MODB[:, n * 512:(n + 1) * 512],
                                                                          op=ALU.mult), r=[pmk, "MODB"], w=[f"x1{k}"])
                P.op("dve", lambda e, k=k: e.tensor_tensor(out=x1[k][:], in0=x1[k][:], in1=xt[k][:], op=ALU.add), r=[f"x1{k}", f"xt{k}"], w=[f"x1{k}"])
                P.dma("sp", y[t * 128:(t + 1) * 128, :], x1[k][:], r=[f"x1{k}"], w=[f"y{t}"])
                P.op("act", lambda e, k=k: e.activation(out=sq[:], in_=x1[k][:], func=AF.Square, accum_out=ssv[:, 0:1]), r=[f"x1{k}"], w=["sq", "ssv"])
                P.op("dve", lambda e: e.tensor_scalar(out=ssv[:], in0=ssv[:], scalar1=1.0 / D, scalar2=EPS, op0=ALU.mult, op1=ALU.add), r=["ssv"], w=["ssv"])
                P.op("act", lambda e: e.activation(out=ssv[:], in_=ssv[:], func=AF.Sqrt), r=["ssv"], w=["ssv"])
                P.op("dve", lambda e: e.reciprocal(out=ssv[:], in_=ssv[:]), r=["ssv"], w=["ssv"])
                P.op("dve", lambda e, k=k: e.scalar_tensor_tensor(out=sq[:], in0=x1[k][:], scalar=ssv[:, 0:1], in1=A2B[:], op0=ALU.mult, op1=ALU.mult),
                     r=[f"x1{k}", "ssv", "A2B"], w=["sq"])
                P.op("dve", lambda e, k=k: e.tensor_tensor(out=hb[k][:], in0=sq[:], in1=MODB[:, 1024:2048], op=ALU.add), r=["sq", "MODB"], w=[f"hb{k}"])
                for c in range(8):
                    P.op("pe", lambda e, k=k, c=c: e.transpose(ps_tr[k][:, c * 128:(c + 1) * 128], hb[k][:, c * 128:(c + 1) * 128], ident[:]),
                         r=[f"hb{k}", "ident"], w=[f"ps_tr{k}"])
                P.op("act", lambda e, k=k, t=t, tl=tl: e.activation(out=h2T[:, :, tl * 128:(tl + 1) * 128], in_=ps_tr[k][:, :].rearrange("p (c t) -> p c t", t=128),
                                                          func=AF.Copy), r=[f"ps_tr{k}"], w=["h2T"])
                pm = ps[4]; pmk = "ps4"
                for c in range(8):
                    P.op("pe", lambda e, pm=pm, c=c, t=t, tl=tl: e.matmul(pm[:, 0:32], lhsT=h2T[:, c, tl * 128:(tl + 1) * 128], rhs=rwb[:, c, :], start=(c == 0), stop=(c == 7)),
                         r=["h2T", "rwb"], w=[pmk])
                P.op("dve", lambda e, pm=pm: e.tensor_tensor(out=lg[:], in0=pm[:, 0:32], in1=rbs[:], op=ALU.add), r=[pmk, "rbs"], w=["lg"])
                P.op("dve", lambda e: e.max(out=m8[:], in_=lg[:]), r=["lg"], w=["m8"])
                P.op("dve", lambda e: e.tensor_scalar(out=msk[:], in0=lg[:], scalar1=m8[:, 3:4], scalar2=None, op0=ALU.is_ge), r=["lg", "m8"], w=["msk"])
                P.op("dve", lambda e: e.tensor_scalar(out=nmx[:], in0=m8[:, 0:1], scalar1=-1.0, scalar2=None, op0=ALU.mult), r=["m8"], w=["nmx"])
                P.op("act", lambda e: e.activation(out=ex[:], in_=lg[:], func=AF.Exp, bias=nmx[:, 0:1], scale=1.0), r=["lg", "nmx"], w=["ex"])
                P.op("dve", lambda e: e.tensor_tensor(out=ex[:], in0=ex[:], in1=msk[:], op=ALU.mult), r=["ex", "msk"], w=["ex"])
                P.op("dve", lambda e: e.tensor_reduce(out=den[:], in_=ex[:], axis=AX.X, op=ALU.add), r=["ex"], w=["den"])
                P.op("dve", lambda e: e.reciprocal(out=den[:], in_=den[:]), r=["den"], w=["den"])
                P.op("dve", lambda e, t=t, tl=tl: e.tensor_scalar(out=GATE[:, tl, :], in0=ex[:], scalar1=den[:, 0:1], scalar2=None, op0=ALU.mult), r=["ex", "den"], w=["GATE"])
                P.op("dve", lambda e, t=t, tl=tl: e.tensor_copy(out=gb[:], in_=GATE[:, tl, :]), r=["GATE"], w=["gb"])
                P.op("pe", lambda e, k=k: e.transpose(ps_tr[k][0:32, 0:128], gb[:, 0:32], ident[:]), r=["gb", "ident", "h2T"], w=[f"ps_tr{k}"])
                P.op("act", lambda e, k=k: e.activation(out=gT[:], in_=ps_tr[k][0:32, 0:128], func=AF.Copy), r=[f"ps_tr{k}"], w=["gT"])
                for n in range(2):
                    pm = ps[2 + n]; pmk = f"ps{2 + n}"
                    P.op("pe", lambda e, pm=pm, n=n: e.matmul(pm[:, 0:512], lhsT=gT[:, :], rhs=b2b[:, n * 512:(n + 1) * 512], start=True, stop=True),
                         r=["gT", "b2b"], w=[pmk])
                    P.op("act", lambda e, pm=pm, n=n, t=t, tl=tl: e.activation(out=acc[:, tl, n * 512:(n + 1) * 512], in_=pm[:, 0:512], func=AF.Copy), r=[pmk], w=[f"acc{tl}"])
            u = 0
            for ex_ in range(ne):
                w2t = w2buf[ex_ % 2]; w2k = f"w2b{ex_ % 2}"
                for c in range(8):
                    P.dma("pool", w2t[:, c, :], w2[ex_, c * 128:(c + 1) * 128, :], w=[w2k])
                for kp in range(4):
                    ring = w1ring[rr % 3]; rk = f"w1r{rr % 3}"
                    rr += 1
                    P.dma("pool", ring[:, :, 0:256], w1[ex_, :, kp * 256:(kp + 1) * 256].rearrange("(c p) n -> p c n", p=128), w=[rk])
                    P.dma("pool", ring[:, :, 256:512], w1[ex_, :, 1024 + kp * 256:1024 + (kp + 1) * 256].rearrange("(c p) n -> p c n", p=128), w=[rk])
                    for j in range(2):
                        kk = kp * 2 + j
                        for grp in range(2):
                            tk = slice(grp * 512, (grp + 1) * 512)
                            pg = ps[u % 2]; pgk = f"ps{u % 2}"; pl = ps[2 + u % 2]; plk = f"ps{2 + u % 2}"
                            u += 1
                            for c in range(8):
                                P.op("pe", lambda e, pg=pg, c=c, j=j, tk=tk, ring=ring: e.matmul(pg[:, 0:512], lhsT=ring[:, c, j * 128:(j + 1) * 128], rhs=h2T[:, c, tk],
                                                                                             start=(c == 0), stop=(c == 7)), r=[rk, "h2T"], w=[pgk])
                            for c in range(8):
                                P.op("pe", lambda e, pl=pl, c=c, j=j, tk=tk, ring=ring: e.matmul(pl[:, 0:512], lhsT=ring[:, c, 256 + j * 128:256 + (j + 1) * 128], rhs=h2T[:, c, tk],
                                                                                             start=(c == 0), stop=(c == 7)), r=[rk, "h2T"], w=[plk])
                            bg = b1s[:, ex_ * 16 + kk:ex_ * 16 + kk + 1]; bl = b1s[:, ex_ * 16 + 8 + kk:ex_ * 16 + 8 + kk + 1]
                            P.op("dve", lambda e, pg=pg, bg=bg: e.tensor_scalar(out=gcl[:], in0=pg[:, 0:512], scalar1=bg, scalar2=7.0, op0=ALU.add, op1=ALU.min),
                                 r=[pgk, "b1s"], w=["gcl"])
                            P.op("act", lambda e: e.activation(out=sgl[:], in_=gcl[:], func=AF.Sigmoid, scale=1.702), r=["gcl"], w=["sgl"])
                            P.op("dve", lambda e, pl=pl, bl=bl: e.tensor_scalar(out=lcl[:], in0=pl[:, 0:512], scalar1=bl, scalar2=-6.0, op0=ALU.add, op1=ALU.max),
                                 r=[plk, "b1s"], w=["lcl"])
                            P.op("dve", lambda e: e.scalar_tensor_tensor(out=lcl[:], in0=lcl[:], scalar=8.0, in1=gcl[:], op0=ALU.min, op1=ALU.mult),
                                 r=["lcl", "gcl"], w=["lcl"])
                            P.op("dve", lambda e, kk=kk, tk=tk: e.tensor_tensor(out=AT[:, kk, tk], in0=lcl[:], in1=sgl[:], op=ALU.mult), r=["lcl", "sgl"], w=[f"AT{kk}_{grp}"])
                for tl in range(NH):
                    t = half * NH + tl
                    for n in range(2):
                        py = ps[4 + n]; pyk = f"ps{4 + n}"
                        for kk in range(8):
                            P.op("pe", lambda e, py=py, kk=kk, tl=tl, n=n, w2t=w2t: e.matmul(py[:, 0:512], lhsT=AT[:, kk, tl * 128:(tl + 1) * 128],
                                                                                         rhs=w2t[:, kk, n * 512:(n + 1) * 512], start=(kk == 0), stop=(kk == 7)),
                                 r=[f"AT{kk}_{tl // 4}", w2k], w=[pyk])
                        P.op("dve", lambda e, py=py, t=t, tl=tl, n=n, ex_=ex_: e.scalar_tensor_tensor(out=acc[:, tl, n * 512:(n + 1) * 512], in0=py[:, 0:512],
                                                                                            scalar=GATE[:, tl, ex_:ex_ + 1], in1=acc[:, tl, n * 512:(n + 1) * 512],
                                                                                            op0=ALU.mult, op1=ALU.add), r=[pyk, "GATE", f"acc{tl}"], w=[f"acc{tl}"])
            for tl in range(NH):
                t = half * NH + tl
                k = t % 2
                P.dma("sp", xt[k][:], y[t * 128:(t + 1) * 128, :], r=[f"y{t}"], w=[f"xt{k}"])
                P.op("dve", lambda e, t=t, tl=tl: e.tensor_tensor(out=acc[:, tl, :], in0=acc[:, tl, :], in1=MODB[:, 3072:4096], op=ALU.mult), r=[f"acc{tl}", "MODB"], w=[f"acc{tl}"])
                P.op("dve", lambda e, t=t, tl=tl, k=k: e.tensor_tensor(out=acc[:, tl, :], in0=acc[:, tl, :], in1=xt[k][:], op=ALU.add), r=[f"acc{tl}", f"xt{k}"], w=[f"acc{tl}"])
                P.dma("sp", y[t * 128:(t + 1) * 128, :], acc[:, tl, :], r=[f"acc{tl}"], w=[f"y{t}"])
        print("C", P.emit())
    return nc


def inputs_C(inp, l, core, xcur, ocat):
    b, r = core // 4, core % 4
    sl = slice(r * 2048, (r + 1) * 2048)
    return dict(
        x=np.ascontiguousarray(xcur[b, sl]), o=np.ascontiguousarray(ocat[b, sl]),
        cT=np.ascontiguousarray(inp["c"][b].reshape(8, 128).T),
        adaw=np.ascontiguousarray(inp["ada_w"][l][:, 2048:6144]),
        adab=np.ascontiguousarray(inp["ada_b"][l][2048:6144].reshape(1, 4096)),
        ln2=np.ascontiguousarray(np.broadcast_to(inp["ln2_g"][l][None, :], (128, D))),
        wout=np.ascontiguousarray(inp["w_out"][l]),
        rw=np.ascontiguousarray(inp["router_w"][l]),
        rb=np.ascontiguousarray(np.broadcast_to(inp["router_b"][l][None, :], (128, 32))),
        w1=np.ascontiguousarray(inp["exp_w1"][l]),
        b1T=np.ascontiguousarray(inp["exp_b1"][l].reshape(NE, 16, 128).transpose(2, 0, 1).reshape(128, NE * 16)),
        w2=np.ascontiguousarray(inp["exp_w2"][l]),
        b2=np.ascontiguousarray(inp["exp_b2"][l]),
        identd=np.eye(128, dtype=np.float32),
    )


_CACHE = {}


def _prog(name, fn):
    if name not in _CACHE:
        _CACHE[name] = fn()
    return _CACHE[name]


def kernel(**inputs):
    inp = {k: np.asarray(v) for k, v in inputs.items()}
    x_cur = np.ascontiguousarray(inp["x"], dtype=np.float32)
    cores = list(range(8))
    for l in range(2):
        inp["x_cur"] = x_cur
        resA = run_bass_kernel_spmd(_prog("A", build_A), [inputs_A(inp, l, c) for c in cores], core_ids=cores)
        z = np.stack([np.concatenate([resA.results[b * 4 + r]["z"] for r in range(4)], axis=0) for b in range(2)])
        ocat = np.zeros((2, 8192, 1024), np.float32)
        cfg = [(c // 4, (c // 2) % 2, c % 2) for c in cores]
        maps = [inputs_B1(z[b], g, p, inp["nsa_q_gain"][l], inp["nsa_k_gain"][l], inp["nsa_cmp_pos"][l],
                          inp["nsa_cmp_w1"][l], inp["nsa_cmp_w2"][l]) for (b, g, p) in cfg]
        resB1 = run_bass_kernel_spmd(_prog("B1", build_B1), maps, core_ids=cores)
        maps = [inputs_B2(z[b], g, p, inp["moba_q_gain"][l], inp["moba_k_gain"][l]) for (b, g, p) in cfg]
        resB2 = run_bass_kernel_spmd(_prog("B2", build_B2), maps, core_ids=cores)
        for c, (b, g, p) in enumerate(cfg):
            own = (np.arange(4096) // 128 * 2 + p) * 128 + np.arange(4096) % 128
            ocat[b, own, g * 256:(g + 1) * 256] = resB1.results[c]["o"]
            ocat[b, own, 512 + g * 256:512 + (g + 1) * 256] = resB2.results[c]["o"]
        resC = run_bass_kernel_spmd(_prog("C", build_C), [inputs_C(inp, l, c, x_cur, ocat) for c in cores], core_ids=cores)
        x_cur = np.stack([np.concatenate([resC.results[b * 4 + r]["y"] for r in range(4)], axis=0) for b in range(2)]).astype(np.float32)
    return x_cur
```
